# Optimizing a Trainium2 kernel written in Bass

```python
import math
import jax, jax.numpy as jnp
from jax import lax
import numpy as np

D_MODEL = 1024
BATCH = 4
SEQ = 8192
DEPTH = 1

CHUNK = 64

D_MIX = D_MODEL
D_POOL = D_MIX // 2
D_SSM = D_MIX - D_POOL
POOL_WINDOWS = (2, 4, 8, 16)
N_POOL_GROUPS = len(POOL_WINDOWS)
POOL_GROUP = D_POOL // N_POOL_GROUPS
SSM_GROUP = 16
N_SSM_GROUPS = D_SSM // SSM_GROUP
SSM_STATE = 64
DT_MIN = 1e-3
DT_MAX = 1e-1

N_EXPERT_GROUPS = 4
EXPERTS_PER_GROUP = 4
N_EXPERTS = N_EXPERT_GROUPS * EXPERTS_PER_GROUP
TOP_K_INNER = 2
D_EXPERT = D_MODEL // 4

EPS = 1e-6

kernel_name = "hymba_pool_s5_hier_moe_block"


def rms_norm(x, g):
    xf = x.astype(jnp.float32)
    y = xf * lax.rsqrt(jnp.mean(xf * xf, axis=-1, keepdims=True) + EPS)
    return (y * g.astype(jnp.float32)).astype(x.dtype)


def pool_mixer(u, pool_w, pool_scale):
    bsz, l, _ = u.shape
    uf = u.astype(jnp.float32)
    cs = jnp.pad(jnp.cumsum(uf, axis=1), ((0, 0), (1, 0), (0, 0)))
    pos = jnp.arange(l)
    outs = []
    for gi, w in enumerate(POOL_WINDOWS):
        c = cs[:, :, gi * POOL_GROUP:(gi + 1) * POOL_GROUP]
        start = jnp.maximum(pos + 1 - w, 0)
        win_sum = c[:, 1:] - c[:, start]
        count = jnp.minimum(pos + 1, w).astype(jnp.float32)
        outs.append(win_sum / count[None, :, None] - uf[:, :, gi * POOL_GROUP:(gi + 1) * POOL_GROUP])
    p = jnp.stack(outs, axis=2)
    p = jnp.einsum('blgc,gcd->blgd', p, pool_w.astype(jnp.float32))
    return (p.reshape(bsz, l, D_POOL) * pool_scale.astype(jnp.float32)).astype(u.dtype)


def _complex_scan_combine(e1, e2):
    a1r, a1i, b1r, b1i = e1
    a2r, a2i, b2r, b2i = e2
    ar = a2r * a1r - a2i * a1i
    ai = a2r * a1i + a2i * a1r
    br = a2r * b1r - a2i * b1i + b2r
    bi = a2r * b1i + a2i * b1r + b2i
    return ar, ai, br, bi


def s5_mixer(u, a_re, a_im, log_step, b_re, b_im, c_re, c_im, d_skip, glu_w, glu_b):
    f32 = jnp.float32
    bsz, l, _ = u.shape
    uf = u.astype(f32).reshape(bsz, l, N_SSM_GROUPS, SSM_GROUP)
    lr = a_re.astype(f32)
    li = a_im.astype(f32)
    step = jnp.exp(log_step.astype(f32))[:, None]
    mag = jnp.exp(lr * step)
    ab_re = mag * jnp.cos(li * step)
    ab_im = mag * jnp.sin(li * step)
    den = lr * lr + li * li
    nr = ab_re - 1.0
    ni = ab_im
    q_re = (nr * lr + ni * li) / den
    q_im = (ni * lr - nr * li) / den
    br = b_re.astype(f32)
    bi = b_im.astype(f32)
    bb_re = q_re[..., None] * br - q_im[..., None] * bi
    bb_im = q_re[..., None] * bi + q_im[..., None] * br
    bu_re = jnp.einsum('blgh,gph->blgp', uf, bb_re)
    bu_im = jnp.einsum('blgh,gph->blgp', uf, bb_im)
    a_seq_re = jnp.broadcast_to(ab_re, (1, l) + ab_re.shape)
    a_seq_im = jnp.broadcast_to(ab_im, (1, l) + ab_im.shape)
    _, _, x_re, x_im = lax.associative_scan(
        _complex_scan_combine, (a_seq_re, a_seq_im, bu_re, bu_im), axis=1)
    y = (jnp.einsum('blgp,ghp->blgh', x_re, c_re.astype(f32))
         - jnp.einsum('blgp,ghp->blgh', x_im, c_im.astype(f32))
         + d_skip.astype(f32).reshape(N_SSM_GROUPS, SSM_GROUP) * uf)
    y = jax.nn.gelu(y.reshape(bsz, l, D_SSM))
    y = y * jax.nn.sigmoid(y @ glu_w.astype(f32) + glu_b.astype(f32))
    return y.astype(u.dtype)


def hier_moe(h, w_coarse, b_coarse, w_fine, b_fine, w_gate, w_up, w_down):
    f32 = jnp.float32
    bsz, l, d = h.shape
    t = h.reshape(-1, d)
    coarse = (t @ w_coarse).astype(f32) + b_coarse.astype(f32)
    p_coarse = jax.nn.softmax(coarse, axis=-1)
    g_idx = jnp.argmax(coarse, axis=-1)
    p_g = jnp.take_along_axis(p_coarse, g_idx[:, None], axis=-1)
    fine = ((t @ w_fine).astype(f32) + b_fine.astype(f32)).reshape(-1, N_EXPERT_GROUPS, EXPERTS_PER_GROUP)
    fine_sel = jnp.take_along_axis(fine, g_idx[:, None, None], axis=1)[:, 0]
    top_v, top_i = lax.top_k(fine_sel, TOP_K_INNER)
    w_sel = jax.nn.softmax(top_v, axis=-1) * p_g
    e_idx = g_idx[:, None] * EXPERTS_PER_GROUP + top_i
    gates = jnp.sum(jax.nn.one_hot(e_idx, N_EXPERTS, dtype=f32) * w_sel[..., None], axis=1)
    out = jnp.zeros(t.shape, f32)
    for e in range(N_EXPERTS):
        a = jax.nn.silu(t @ w_gate[e]) * (t @ w_up[e])
        out = out + gates[:, e:e + 1] * (a @ w_down[e]).astype(f32)
    return out.reshape(bsz, l, d).astype(h.dtype)


def setup_inputs(seed: int = 0) -> dict:
    key = jax.random.key(seed)
    ks = jax.random.split(key, 26)
    f32 = jnp.float32
    nrm = lambda k, shape, s: (jax.random.normal(k, shape, f32) * s).astype(f32)
    x = jax.random.normal(ks[0], (BATCH, SEQ, D_MODEL), f32)
    norm_mix = 1.0 + nrm(ks[1], (DEPTH, D_MODEL), 0.02)
    w_in = nrm(ks[2], (DEPTH, D_MODEL, D_MIX), D_MODEL ** -0.5)
    pool_w = nrm(ks[3], (DEPTH, N_POOL_GROUPS, POOL_GROUP, POOL_GROUP), POOL_GROUP ** -0.5)
    pool_scale = 1.0 + nrm(ks[4], (DEPTH, D_POOL), 0.02)
    n = jnp.arange(SSM_STATE, dtype=f32)
    ssm_a_re = -0.5 + nrm(ks[5], (DEPTH, N_SSM_GROUPS, SSM_STATE), 0.01)
    ssm_a_im = math.pi * n[None, None, :] + nrm(ks[6], (DEPTH, N_SSM_GROUPS, SSM_STATE), 0.01)
    ssm_log_step = jax.random.uniform(ks[7], (DEPTH, N_SSM_GROUPS), f32,
                                      math.log(DT_MIN), math.log(DT_MAX))
    ssm_b_re = nrm(ks[8], (DEPTH, N_SSM_GROUPS, SSM_STATE, SSM_GROUP), (2 * SSM_GROUP) ** -0.5)
    ssm_b_im = nrm(ks[9], (DEPTH, N_SSM_GROUPS, SSM_STATE, SSM_GROUP), (2 * SSM_GROUP) ** -0.5)
    ssm_c_re = nrm(ks[10], (DEPTH, N_SSM_GROUPS, SSM_GROUP, SSM_STATE), (2 * SSM_STATE) ** -0.5)
    ssm_c_im = nrm(ks[11], (DEPTH, N_SSM_GROUPS, SSM_GROUP, SSM_STATE), (2 * SSM_STATE) ** -0.5)
    ssm_d = 1.0 + nrm(ks[12], (DEPTH, D_SSM), 0.1)
    glu_w = nrm(ks[13], (DEPTH, D_SSM, D_SSM), D_SSM ** -0.5)
    glu_b = nrm(ks[14], (DEPTH, D_SSM), 0.02)
    w_out = nrm(ks[15], (DEPTH, D_MIX, D_MODEL), D_MIX ** -0.5)
    norm_ffn = 1.0 + nrm(ks[16], (DEPTH, D_MODEL), 0.02)
    router_coarse_w = nrm(ks[17], (DEPTH, D_MODEL, N_EXPERT_GROUPS), D_MODEL ** -0.5)
    router_coarse_b = nrm(ks[18], (DEPTH, N_EXPERT_GROUPS), 0.01)
    router_fine_w = nrm(ks[19], (DEPTH, D_MODEL, N_EXPERTS), D_MODEL ** -0.5)
    router_fine_b = nrm(ks[20], (DEPTH, N_EXPERTS), 0.01)
    exp_w_gate = nrm(ks[21], (DEPTH, N_EXPERTS, D_MODEL, D_EXPERT), D_MODEL ** -0.5)
    exp_w_up = nrm(ks[22], (DEPTH, N_EXPERTS, D_MODEL, D_EXPERT), D_MODEL ** -0.5)
    exp_w_down = nrm(ks[23], (DEPTH, N_EXPERTS, D_EXPERT, D_MODEL), D_EXPERT ** -0.5)
    norm_final = 1.0 + nrm(ks[24], (D_MODEL,), 0.02)
    return {"x": x, "norm_mix": norm_mix, "w_in": w_in, "pool_w": pool_w, "pool_scale": pool_scale,
            "ssm_a_re": ssm_a_re, "ssm_a_im": ssm_a_im, "ssm_log_step": ssm_log_step,
            "ssm_b_re": ssm_b_re, "ssm_b_im": ssm_b_im, "ssm_c_re": ssm_c_re, "ssm_c_im": ssm_c_im,
            "ssm_d": ssm_d, "glu_w": glu_w, "glu_b": glu_b, "w_out": w_out, "norm_ffn": norm_ffn,
            "router_coarse_w": router_coarse_w, "router_coarse_b": router_coarse_b,
            "router_fine_w": router_fine_w, "router_fine_b": router_fine_b,
            "exp_w_gate": exp_w_gate, "exp_w_up": exp_w_up, "exp_w_down": exp_w_down,
            "norm_final": norm_final}


def reference(x, norm_mix, w_in, pool_w, pool_scale, ssm_a_re, ssm_a_im, ssm_log_step,
              ssm_b_re, ssm_b_im, ssm_c_re, ssm_c_im, ssm_d, glu_w, glu_b, w_out, norm_ffn,
              router_coarse_w, router_coarse_b, router_fine_w, router_fine_b,
              exp_w_gate, exp_w_up, exp_w_down, norm_final):
    for i in range(DEPTH):
        hn = rms_norm(x, norm_mix[i])
        z = hn @ w_in[i]
        y_pool = pool_mixer(z[..., :D_POOL], pool_w[i], pool_scale[i])
        y_ssm = s5_mixer(z[..., D_POOL:], ssm_a_re[i], ssm_a_im[i], ssm_log_step[i],
                         ssm_b_re[i], ssm_b_im[i], ssm_c_re[i], ssm_c_im[i], ssm_d[i],
                         glu_w[i], glu_b[i])
        x = x + jnp.concatenate([y_pool, y_ssm], axis=-1) @ w_out[i]
        x = x + hier_moe(rms_norm(x, norm_ffn[i]), router_coarse_w[i], router_coarse_b[i],
                         router_fine_w[i], router_fine_b[i],
                         exp_w_gate[i], exp_w_up[i], exp_w_down[i])
    return rms_norm(x, norm_final)
```

```python
import math
import numpy as np
from contextlib import ExitStack
import concourse.bass as bass
import concourse.mybir as mybir
from concourse.bass_utils import run_bass_kernel_spmd

F32 = mybir.dt.float32
BF16 = mybir.dt.bfloat16
I32 = mybir.dt.int32
AF = mybir.ActivationFunctionType
ALU = mybir.AluOpType
AX = mybir.AxisListType

NT_PRE = 32
NT_MAIN = 32
SBT = 4
EPS = 1e-6
WINS = (2, 4, 8, 16)
TWO_PI = 2.0 * math.pi


class R:
    def __init__(self, name):
        self.name = name
        self.w = None
        self.rd = {}


class Sched:
    ENG = ("pe", "act", "dve", "pool", "sp")

    def __init__(self, nc, es):
        self.nc = nc
        self.sem = {e: es.enter_context(nc.semaphore("s_" + e)) for e in self.ENG}
        self.cnt = {e: 0 for e in self.ENG}
        self.ops = {e: [] for e in self.ENG}
        self.seen = {e: {} for e in self.ENG}
        self.dma_pool = [es.enter_context(nc.semaphore("d%d" % i)) for i in range(48)]
        self.dma_cnt = {}
        self.dma_of = {}

    def dsem(self, res):
        if res.name not in self.dma_of:
            s = self.dma_pool[len(self.dma_of)]
            self.dma_of[res.name] = s
            self.dma_cnt[s.name] = 0
        return self.dma_of[res.name]

    def op(self, eng, fn, rd=(), wr=(), dma=None):
        need = {}

        def add(tok):
            if tok is None:
                return
            s, v = tok
            if need.get(s.name, (None, -1))[1] < v:
                need[s.name] = (s, v)
        for r in rd:
            add(r.w)
        for r in wr:
            add(r.w)
            for t in r.rd.values():
                add(t)
        waits = []
        for name, (s, v) in need.items():
            if eng == "pe" and s is self.sem["pe"]:
                continue
            if self.seen[eng].get(name, -1) >= v:
                continue
            self.seen[eng][name] = v
            waits.append((s, v))
        if dma is not None:
            s = self.dsem(dma)
            self.dma_cnt[s.name] += 16
            tok = (s, self.dma_cnt[s.name])
            inc = (s, 16)
        else:
            self.cnt[eng] += 1
            tok = (self.sem[eng], self.cnt[eng])
            inc = (self.sem[eng], 1)
        for r in rd:
            old = r.rd.get(tok[0].name)
            if old is None or old[1] < tok[1]:
                r.rd[tok[0].name] = tok
        for r in wr:
            r.w = tok
            r.rd = {}
        self.ops[eng].append((waits, fn, inc))
        return tok

    def final_wait(self, eng, ress):
        waits = []
        for r in ress:
            if r.w is not None:
                waits.append(r.w)
        self.ops[eng].append((waits, None, None))

    def barrier(self):
        toks = [(self.sem[e], self.cnt[e]) for e in self.ENG if self.cnt[e] > 0]
        for name, sm_ in self.dma_of.items():
            toks.append((sm_, self.dma_cnt[sm_.name]))
        for eng in self.ENG:
            waits = []
            for s_, v in toks:
                if s_ is self.sem[eng]:
                    continue
                if self.seen[eng].get(s_.name, -1) >= v:
                    continue
                self.seen[eng][s_.name] = v
                waits.append((s_, v))
            if waits:
                self.ops[eng].append((waits, None, None))

    def replay(self, eng, e):
        for waits, fn, inc in self.ops[eng]:
            for s, v in waits:
                e.wait_ge(s, v)
            if fn is not None:
                fn(e).then_inc(inc[0], inc[1])


def build(debug=False):
    nc = bass.Bass("TRN2", target_bir_lowering=False)

    def din(name, shape, dt=F32):
        return nc.dram_tensor(name, list(shape), dt, kind="ExternalInput").ap()
    xall = din("xall", [(NT_PRE + NT_MAIN) * 128, 1024])
    w_in = din("w_in", [1024, 1024])
    w_out = din("w_out", [1024, 1024])
    glu_w = din("glu_w", [512, 512])
    pool_w = din("pool_w", [4, 128, 128])
    cols = din("cols", [128, 64])
    rows = din("rows", [1, 1024 + 20 + 64])
    sp_s = din("sp_s", [128, 96])
    b1_s = din("b1_s", [128, 32, 16])
    b2_s = din("b2_s", [128, 32, 16])
    c1_s = din("c1_s", [128, 32, 16])
    c2_s = din("c2_s", [128, 32, 16])
    cst = din("cst", [128, 386])
    wr_d = din("wr", [1024, 20])
    wg_d = din("wg", [16, 1024, 256])
    wu_d = din("wu", [16, 1024, 256])
    wd_d = din("wd", [16, 256, 1024])
    out = nc.dram_tensor("out", [NT_MAIN * 128, 1024], F32, kind="ExternalOutput").ap()

    es = ExitStack()
    with es:
        S = Sched(nc, es)

        def sb(name, shape, dt=F32):
            return es.enter_context(nc.sbuf_tensor(name, list(shape), dt))

        def ps(name, shape, dt=F32):
            return es.enter_context(nc.psum_tensor(name, list(shape), dt))

        ident = sb("ident", [128, 128], BF16)
        cstt = sb("cstt", [128, 386])
        colt = sb("colt", [128, 64])
        RB = sb("RB", [128, 84])
        WI = sb("WI", [128, 8, 1024], BF16)
        WO = sb("WO", [128, 8, 1024], BF16)
        GW = sb("GW", [128, 4, 512], BF16)
        PW = sb("PW", [128, 8, 128], BF16)
        WRb = sb("WRb", [128, 8, 20], BF16)
        LBa = sb("LBa", [128, 32, 128], BF16)
        LBb = sb("LBb", [128, 32, 128], BF16)
        LC = sb("LC", [128, 32, 128], BF16)
        Dg = sb("Dg", [128, 4, 128], BF16)
        COS = sb("COS", [128, 32, 128])
        SINM = sb("SINM", [128, 32, 128])
        MAG = sb("MAG", [128, 32])
        CAR = sb("CAR", [128, 32])
        acc = sb("acc", [128, SBT, 1024])
        hn2T = sb("hn2T", [128, 8, SBT * 128], BF16)
        gates = sb("gates", [128, SBT, 16])
        cf = sb("cf", [128, 64])
        sm = sb("sm", [128, 256])
        pzb = sb("pzb", [128, 128], BF16)
        ARENA_W = 21504
        arena = sb("arena", [128, ARENA_W])
        _off = [0]

        def carve(shape, dt=F32):
            n = 1
            for d in shape[1:]:
                n *= d
            nb = n * (4 if dt == F32 else 2)
            nb = (nb + 63) // 64 * 64
            o = _off[0]
            _off[0] += nb
            assert _off[0] <= ARENA_W * 4, ("arena overflow", _off[0])
            v = arena[:, o // 4:(o + nb) // 4]
            if dt != F32:
                v = v.bitcast(dt)
            v = v[:, 0:n]
            if len(shape) == 3:
                v = v.rearrange("p (a b) -> p a b", a=shape[1])
            elif len(shape) == 4:
                v = v.rearrange("p (a b c) -> p a b c", a=shape[1], b=shape[2])
            return v
        xt = [carve([128, 1024]) for _ in range(2)]
        hn = [carve([128, 1024], BF16) for _ in range(2)]
        hnT = [carve([128, 8, 128], BF16) for _ in range(2)]
        yT_off = _off[0]
        yT = [carve([128, 8, 128], BF16) for _ in range(2)]
        yg = [carve([128, 4, 128], BF16) for _ in range(2)]
        ygf = [carve([128, 4, 128]) for _ in range(2)]
        g1 = [carve([128, 128]) for _ in range(2)]
        g2 = [carve([128, 128]) for _ in range(2)]
        Gt = [carve([128, 8, 128]) for _ in range(2)]
        hn2b = [carve([128, 1024], BF16) for _ in range(2)]
        D1 = None
        M1_off = _off[0]
        M1 = [carve([128, 8, 128]) for _ in range(2)]
        M2 = [carve([128, 8, 128]) for _ in range(2)]
        XT = [carve([128, 8, 128]) for _ in range(2)]
        XTb = [carve([128, 8, 128]) for _ in range(2)]
        XA = [carve([128, 8, 128], BF16) for _ in range(2)]
        junk = carve([128, 1024], BF16)
        assert _off[0] >= 48 * 1024
        zT = [carve([128, 8, 144], BF16) for _ in range(2)]
        mixer_end = _off[0]
        _off[0] = 0
        WGs = [carve([128, 8, 256], BF16) for _ in range(2)]
        WUs = [carve([128, 8, 256], BF16) for _ in range(2)]
        WDs = [carve([128, 2, 1024], BF16) for _ in range(2)]
        sgT = [carve([128, 2, 512], BF16) for _ in range(2)]
        hT = [carve([128, 2, 512], BF16) for _ in range(2)]
        outt = [carve([128, 1024]) for _ in range(2)]
        NF = carve([128, 1024])
        junk2 = carve([128, 1024], BF16)
        assert _off[0] <= 48 * 1024
        pp = Gt[0].rearrange("p a b -> p (a b)")[:, 0:512].rearrange("p (a b) -> p a b", a=32)
        pq = XT[0].rearrange("p a b -> p (a b)")[:, 0:512].rearrange("p (a b) -> p a b", a=32)
        pz = acc[:].rearrange("p a b -> p (a b)").rearrange("p (a b) -> p a b", a=32)
        T1 = M1[0]
        T2 = M2[0]

        PB0 = ps("PB0", [128, 1024], BF16)
        PBs = [ps("PB%d" % i, [128, 512]) for i in range(1, 8)]

        res = {}

        def rs(name):
            if name not in res:
                res[name] = R(name)
            return res[name]

        def dma(out_ap, in_ap, rd, wr, slow=False, q="sp"):
            if slow:
                S.op(q, lambda e: e.dma_start(out=out_ap, in_=in_ap, allow_slow_non_contiguous=True), rd=rd, wr=wr, dma=wr[0])
            else:
                S.op(q, lambda e: e.dma_start(out=out_ap, in_=in_ap), rd=rd, wr=wr, dma=wr[0])

        def act(out_ap, in_ap, func, rd, wr, **kw):
            S.op("act", lambda e: e.activation(out=out_ap, in_=in_ap, func=func, **kw), rd=rd, wr=wr)

        def tt(eng, out_ap, a, b, op, rd, wr):
            S.op(eng, lambda e: e.tensor_tensor(out=out_ap, in0=a, in1=b, op=op), rd=rd, wr=wr)

        def tsc(out_ap, a, s1, s2, op0, op1, rd, wr):
            if s2 is None:
                S.op("dve", lambda e: e.tensor_scalar(out=out_ap, in0=a, scalar1=s1, scalar2=None, op0=op0), rd=rd, wr=wr)
            else:
                S.op("dve", lambda e: e.tensor_scalar(out=out_ap, in0=a, scalar1=s1, scalar2=s2, op0=op0, op1=op1), rd=rd, wr=wr)

        def stt(out_ap, a, s, b, op0, op1, rd, wr):
            S.op("dve", lambda e: e.scalar_tensor_tensor(out=out_ap, in0=a, scalar=s, in1=b, op0=op0, op1=op1), rd=rd, wr=wr)

        def cp(eng, out_ap, in_ap, rd, wr):
            S.op(eng, lambda e: e.tensor_copy(out=out_ap, in_=in_ap), rd=rd, wr=wr)

        def mm(out_ap, lhsT, rhs, start, stop, rd, wr):
            S.op("pe", lambda e: e.matmul(out_ap, lhsT=lhsT, rhs=rhs, start=start, stop=stop), rd=rd, wr=wr)

        def tr(out_ap, in_ap, rd, wr):
            S.op("pe", lambda e: e.transpose(out=out_ap, in_=in_ap, identity=ident[:]), rd=rd, wr=wr)

        def red(out_ap, in_ap, op, rd, wr):
            S.op("dve", lambda e: e.tensor_reduce(out=out_ap, in_=in_ap, axis=AX.X, op=op), rd=rd, wr=wr)

        def recip(out_ap, in_ap, rd, wr):
            S.op("dve", lambda e: e.reciprocal(out=out_ap, in_=in_ap), rd=rd, wr=wr)

        def smc(i, n=1):
            return sm[:, i:i + n]

        r_ = rs
        dma(cstt[:], cst[:, :], [], [r_("cstt")])
        dma(colt[:], cols[:, :], [], [r_("colt")])
        dma(RB[:], rows[0:1, 1024:1108].partition_broadcast(128), [], [r_("RB")])
        cp("dve", ident[:], cstt[:, 0:128], [r_("cstt")], [r_("ident")])
        JIDX = cstt[:, 128:256]
        SGN = cstt[:, 256:257]
        c_nm, c_nf, c_ps_, c_d, c_gb = 0, 8, 16, 20, 24

        accv = acc[:].rearrange("p a b -> p (a b)")
        for half in range(2):
            dma(accv[:, 0:4096].rearrange("p (k n) -> p k n", k=4),
                w_in[half * 512:(half + 1) * 512, :].rearrange("(k p) n -> p k n", p=128), [], [r_("acc")])
            for k in range(4):
                kk = half * 4 + k
                tsc(WI[:, kk, :], accv[:, k * 1024:(k + 1) * 1024], colt[:, c_nm + kk:c_nm + kk + 1], None, ALU.mult, None,
                    [r_("acc"), r_("colt")], [r_("WI")])
        for half in range(2):
            dma(accv[:, 0:4096].rearrange("p (k n) -> p k n", k=4),
                w_out[half * 512:(half + 1) * 512, :].rearrange("(k p) n -> p k n", p=128), [r_("WI")], [r_("acc")])
            for k in range(4):
                kk = half * 4 + k
                tsc(WO[:, kk, :], accv[:, k * 1024:(k + 1) * 1024], 1.0 if kk < 4 else 0.25, None, ALU.mult, None, [r_("acc")], [r_("WO")])
        dma(accv[:, 0:2048].rearrange("p (k n) -> p k n", k=4), glu_w[:, :].rearrange("(k p) n -> p k n", p=128), [r_("WO")], [r_("acc")])
        tsc(GW[:].rearrange("p k n -> p (k n)"), accv[:, 0:2048], 0.5, None, ALU.mult, None, [r_("acc")], [r_("GW")])
        dma(accv[:, 0:512].rearrange("p (g n) -> p g n", g=4), pool_w[:, :, :].rearrange("g p n -> p g n"), [r_("GW")], [r_("acc")])
        for gi, w in enumerate(WINS):
            tsc(PW[:, 2 * gi, :], accv[:, gi * 128:(gi + 1) * 128], float(1.0 / w - 1.0), None, ALU.mult, None, [r_("acc")], [r_("PW")])
            tsc(PW[:, 2 * gi + 1, :], accv[:, gi * 128:(gi + 1) * 128], float(1.0 / w), None, ALU.mult, None, [r_("acc")], [r_("PW")])
        dma(accv[:, 0:160].rearrange("p (k n) -> p k n", k=8), wr_d[:, :].rearrange("(k p) n -> p k n", p=128), [r_("PW")], [r_("acc")])
        for k in range(8):
            tsc(WRb[:, k, :], accv[:, k * 20:(k + 1) * 20], colt[:, c_nf + k:c_nf + k + 1], None, ALU.mult, None, [r_("acc"), r_("colt")], [r_("WRb")])

        spt = XTb[0][:, 0, 0:96]
        dma(spt, sp_s[:, :], [], [r_("spt")])
        dma(pp, b1_s[:, :, :], [], [r_("Gt")])
        dma(pq, b2_s[:, :, :], [], [r_("XT")])
        P = XTb[1].rearrange("p a b -> p (a b)")[:, 0:512].rearrange("p (a b) -> p a b", a=16)
        Rp = r_("P")
        LR, LI, LS = spt[:, 0:32], spt[:, 32:64], spt[:, 64:96]
        STEP, ARG, MG, CS, SN, AR, AI, DEN, QR, QI, TA, TB, KI = [P[:, i, :] for i in range(13)]
        KII = sb("KII", [128, 32], I32)

        def exp_to(dst, src, rd):
            act(TA, src, AF.Tanh, rd, [Rp], scale=0.5)
            tsc(TB, TA, -1.0, 1.0, ALU.mult, ALU.add, [Rp], [Rp])
            recip(TB, TB, [Rp], [Rp])
            tsc(TA, TA, 1.0, None, ALU.add, None, [Rp], [Rp])
            tt("dve", dst, TA, TB, ALU.mult, [Rp], [Rp])

        def sin_to(dst, src, shift):
            tsc(TA, src, float(shift), 1.0 / TWO_PI, ALU.add, ALU.mult, [Rp], [Rp])
            cp("dve", KII[:], TA, [Rp], [Rp])
            cp("dve", TB, KII[:], [Rp], [Rp])
            tt("dve", TA, TA, TB, ALU.subtract, [Rp], [Rp])
            act(dst, TA, AF.Sin, [Rp], [Rp], scale=TWO_PI)
        exp_to(STEP, LS, [r_("spt")])
        tt("dve", ARG, LI, STEP, ALU.mult, [Rp, r_("spt")], [Rp])
        tt("dve", MG, LR, STEP, ALU.mult, [Rp, r_("spt")], [Rp])
        cp("dve", P[:, 13, :], MG, [Rp], [Rp])
        exp_to(MG, MG, [Rp])
        cp("dve", MAG[:], MG, [Rp], [r_("MAG")])
        sin_to(SN, ARG, 0.0)
        sin_to(CS, ARG, math.pi / 2)
        tt("dve", AR, MG, CS, ALU.mult, [Rp], [Rp])
        tt("dve", AI, MG, SN, ALU.mult, [Rp], [Rp])
        tt("dve", DEN, LR, LR, ALU.mult, [Rp], [Rp])
        tt("dve", TA, LI, LI, ALU.mult, [Rp], [Rp])
        tt("dve", DEN, DEN, TA, ALU.add, [Rp], [Rp])
        recip(DEN, DEN, [Rp], [Rp])
        tsc(TA, AR, -1.0, None, ALU.add, None, [Rp], [Rp])
        tt("dve", QR, TA, LR, ALU.mult, [Rp], [Rp])
        tt("dve", TB, AI, LI, ALU.mult, [Rp], [Rp])
        tt("dve", QR, QR, TB, ALU.add, [Rp], [Rp])
        tt("dve", QR, QR, DEN, ALU.mult, [Rp], [Rp])
        tt("dve", QI, AI, LR, ALU.mult, [Rp], [Rp])
        tt("dve", TB, TA, LI, ALU.mult, [Rp], [Rp])
        tt("dve", QI, QI, TB, ALU.subtract, [Rp], [Rp])
        tt("dve", QI, QI, DEN, ALU.mult, [Rp], [Rp])
        tsc(QI, QI, SGN, -1.0, ALU.mult, ALU.mult, [Rp, r_("cstt")], [Rp])
        tt("dve", pp, pp, QR.unsqueeze(2).to_broadcast([128, 32, 16]), ALU.mult, [Rp, r_("Gt")], [r_("Gt")])
        tt("dve", pq, pq, QI.unsqueeze(2).to_broadcast([128, 32, 16]), ALU.mult, [Rp, r_("XT")], [r_("XT")])
        tt("dve", pp, pp, pq, ALU.add, [r_("XT")], [r_("Gt")])
        S.op("pool", lambda e: e.memset(pz, 0.0), wr=[r_("acc")])
        for gl in range(8):
            cp("dve", pz[:, gl::8, gl * 16:(gl + 1) * 16], pp[:, gl::8, :], [r_("Gt")], [r_("acc")])
        for g in range(32):
            cp("dve", pzb[:], pz[:, g, :], [r_("acc")], [r_("pzb")])
            tr(PB0[:, 0:128], pzb[:], [r_("pzb"), r_("ident")], [r_("PB0")])
            cp("dve", LBa[:, g, :], PB0[:, 0:128], [r_("PB0")], [r_("LBa")])
        cp("dve", LBb[:, :, 0:64], LBa[:, :, 64:128], [r_("LBa")], [r_("LBb")])
        cp("dve", LBb[:, :, 64:128], LBa[:, :, 0:64], [r_("LBa")], [r_("LBb")])
        dma(pq, c1_s[:, :, :], [r_("Gt")], [r_("XT")])
        tsc(pq, pq, SGN, None, ALU.mult, None, [r_("XT"), r_("cstt")], [r_("XT")])
        S.op("pool", lambda e: e.memset(pz, 0.0), rd=[], wr=[r_("acc")])
        for gl in range(8):
            cp("dve", pz[:, gl::8, gl * 16:(gl + 1) * 16], pq[:, gl::8, :], [r_("XT")], [r_("acc")])
        cp("dve", LC[:].rearrange("p a b -> p (a b)"), pz.rearrange("p a b -> p (a b)"), [r_("acc")], [r_("LC")])
        for t4 in range(4):
            tsc(Dg[:, t4, :], ident[:], colt[:, c_d + t4:c_d + t4 + 1], None, ALU.mult, None, [r_("ident"), r_("colt")], [r_("Dg")])
        SCR = [T1, T2]
        for g in range(32):
            sc = SCR[g % 2]
            rsc = r_("scr%d" % (g % 2))
            tsc(sc[:, 0, :], JIDX, P[:, 1, g:g + 1], 1.0 / TWO_PI, ALU.mult, ALU.mult, [Rp, r_("cstt")], [rsc])
            for ti_, (tab, shift) in enumerate(((SINM, 0.0), (COS, 0.25))):
                rs2 = r_("scr%d_%d" % (g % 2, ti_))
                a, b_, c = 1 + 3 * ti_, 2 + 3 * ti_, 3 + 3 * ti_
                tsc(sc[:, a, :], sc[:, 0, :], float(shift), None, ALU.add, None, [rsc], [rs2])
                cp("dve", sc[:, b_, :].bitcast(I32), sc[:, a, :], [rs2], [rs2])
                cp("dve", sc[:, c, :], sc[:, b_, :].bitcast(I32), [rs2], [rs2])
                tt("dve", sc[:, a, :], sc[:, a, :], sc[:, c, :], ALU.subtract, [rs2], [rs2])
                act(tab[:, g, :], sc[:, a, :], AF.Sin, [rs2], [r_("tab")], scale=TWO_PI)
        tsc(SINM[:].rearrange("p a b -> p (a b)"), SINM[:].rearrange("p a b -> p (a b)"), SGN, None, ALU.mult, None, [r_("tab"), r_("cstt")], [r_("tab")])
        _save = _off[0]
        _off[0] = yT_off
        BBR = carve([128, 32, 16])
        BBIs = carve([128, 32, 16])
        VTa = carve([128, 32, 128], BF16)
        VTb = carve([128, 32, 128], BF16)
        assert _off[0] <= M1_off
        _off[0] = _save
        ztok = [M1[1].rearrange("p a b -> p (a b)").bitcast(BF16)[:, 0:1024][:, i * 512:(i + 1) * 512] for i in range(2)]
        Ytmp = [M2[1].rearrange("p a b -> p (a b)")[:, i * 512:(i + 1) * 512] for i in range(2)]
        CARb = XA[1].rearrange("p a b -> p (a b)").bitcast(F32)[:, 0:32]
        Ssum = XA[1].rearrange("p a b -> p (a b)").bitcast(F32)[:, 32:64]
        Ctmp = XA[1].rearrange("p a b -> p (a b)").bitcast(F32)[:, 64:128]
        A128 = XA[1].rearrange("p a b -> p (a b)").bitcast(F32)[:, 128:192]
        bA = XT[1].rearrange("p a b -> p (a b)")[:, 0:512].rearrange("p (a b) -> p a b", a=32)
        bB = XT[1].rearrange("p a b -> p (a b)")[:, 512:1024].rearrange("p (a b) -> p a b", a=32)
        dma(bA, b2_s[:, :, :], [], [r_("bA")])
        dma(bB, b1_s[:, :, :], [], [r_("bB")])
        tt("dve", bA, bA, QR.unsqueeze(2).to_broadcast([128, 32, 16]), ALU.mult, [Rp, r_("bA")], [r_("bA")])
        tt("dve", bB, bB, QI.unsqueeze(2).to_broadcast([128, 32, 16]), ALU.mult, [Rp, r_("bB")], [r_("bB")])
        tt("dve", bA, bA, bB, ALU.subtract, [r_("bB")], [r_("bA")])
        cp("dve", BBR[0:64], pp[0:64], [r_("Gt")], [r_("BBR")])
        cp("dve", BBR[64:128], bA[64:128], [r_("bA")], [r_("BBR")])
        tsc(BBIs[0:64].rearrange("p a b -> p (a b)"), bA[0:64].rearrange("p a b -> p (a b)"), -1.0, None, ALU.mult, None, [r_("bA")], [r_("BBI")])
        cp("dve", BBIs[64:128], pp[64:128], [r_("Gt")], [r_("BBI")])
        S.barrier()
        SHC = sm[:, 70:71]
        tsc(SHC, SGN, 0.125, 0.125, ALU.mult, ALU.add, [r_("cstt")], [r_("shc")])
        JREV = cstt[:, 258:386]
        pzbs = [XA[0][:, 0, :], XA[0][:, 1, :]]
        for g in range(32):
            sc = SCR[g % 2]
            rsc = r_("vscr%d" % (g % 2))
            rpz = r_("pzbs%d" % (g % 2))
            tsc(sc[:, 0, :], JREV, P[:, 1, g:g + 1], 1.0 / TWO_PI, ALU.mult, ALU.mult, [Rp, r_("cstt"), r_("tab")], [rsc])
            tsc(sc[:, 1, :], sc[:, 0, :], SHC, None, ALU.add, None, [rsc, r_("shc")], [rsc])
            cp("dve", sc[:, 2, :].bitcast(I32), sc[:, 1, :], [rsc], [rsc])
            cp("dve", sc[:, 3, :], sc[:, 2, :].bitcast(I32), [rsc], [rsc])
            tt("dve", sc[:, 1, :], sc[:, 1, :], sc[:, 3, :], ALU.subtract, [rsc], [rsc])
            act(sc[:, 4, :], sc[:, 1, :], AF.Sin, [rsc], [r_("vsb%d" % (g % 2))], scale=TWO_PI)
            act(sc[:, 5, :], JREV, AF.Exp, [r_("cstt"), Rp, rsc], [r_("vsc%d" % (g % 2))], scale=P[:, 13, g:g + 1])
            tt("dve", pzbs[g % 2], sc[:, 4, :], sc[:, 5, :], ALU.mult, [r_("vsb%d" % (g % 2)), r_("vsc%d" % (g % 2))], [rpz])
            tr(PB0[:, (g % 2) * 128:(g % 2 + 1) * 128], pzbs[g % 2], [rpz, r_("ident")], [r_("PB0")])
            cp("dve", VTa[:, g, :], PB0[:, (g % 2) * 128:(g % 2 + 1) * 128], [r_("PB0")], [r_("VTa")])
        cp("dve", VTb[:, :, 0:64], VTa[:, :, 64:128], [r_("VTa")], [r_("VTb")])
        cp("dve", VTb[:, :, 64:128], VTa[:, :, 0:64], [r_("VTa")], [r_("VTb")])
        act(Ctmp[:, 0:32], P[:, 13, :], AF.Exp, [Rp], [r_("ctmp")], scale=128.0)
        tt("dve", A128[:, 0:32], Ctmp[:, 0:32], COS[:, :, 127], ALU.mult, [r_("ctmp"), r_("tab")], [r_("A128")])
        tt("dve", A128[:, 32:64], Ctmp[:, 0:32], SINM[:, :, 127], ALU.mult, [r_("ctmp"), r_("tab")], [r_("A128")])
        tsc(A128[:, 32:64], A128[:, 32:64], -1.0, None, ALU.mult, None, [r_("A128")], [r_("A128")])
        S.op("pool", lambda e: e.memset(CARb, 0.0), wr=[r_("CARb")])
        S.barrier()
        LCs = XTb[0].rearrange("p a b -> p (a b)").bitcast(BF16)[:, 0:2048]
        LCs2 = XTb[1].rearrange("p a b -> p (a b)").bitcast(BF16)[:, 0:2048]
        dma(pq, c2_s[:, :, :], [], [r_("XT")])
        tsc(pq, pq, SGN, -1.0, ALU.mult, ALU.mult, [r_("XT"), r_("cstt")], [r_("XT")])
        S.op("pool", lambda e: e.memset(pz, 0.0), rd=[], wr=[r_("acc")])
        for gl in range(8):
            cp("dve", pz[:, gl::8, gl * 16:(gl + 1) * 16], pq[:, gl::8, :], [r_("XT")], [r_("acc")])
        pzf = pz.rearrange("p a b -> p (a b)")
        cp("dve", LCs, pzf[:, 0:2048], [r_("acc")], [r_("LCs")])
        cp("dve", LCs2, pzf[:, 2048:4096], [r_("acc")], [r_("LCs")])
        LCsv = [LCs.rearrange("p (g c) -> p g c", g=16), LCs2.rearrange("p (g c) -> p g c", g=16)]
        S.op("pool", lambda e: e.memset(CAR[:], 0.0), wr=[r_("CAR")])
        for p_ in range(2):
            S.op("pool", lambda e, p_=p_: e.memset(zT[p_], 0.0), wr=[r_("zT%d" % p_)])
        tsc(sm[:, 64:68], colt[:, c_gb:c_gb + 4], 0.5, None, ALU.mult, None, [r_("colt")], [r_("sm2")])

        def rms(src_ap, dst_bf, srcres, dstres, col, jk):
            S.op("dve", lambda e: e.memset(smc(col), 0.0), wr=[r_("sm%d" % col)])
            act(jk, src_ap, AF.Square, srcres, [r_("junk"), r_("sm%d" % col)], accum_out=smc(col))
            act(smc(col + 1), smc(col), AF.Sqrt, [r_("sm%d" % col)], [r_("sm%d" % col)], scale=1.0 / 1024.0, bias=EPS)
            recip(smc(col + 2), smc(col + 1), [r_("sm%d" % col)], [r_("sm%d" % col)])
            act(dst_bf, src_ap, AF.Copy, srcres + [r_("sm%d" % col)], dstres, scale=smc(col + 2))

        def rms_a(src_ap, srcres, col, jk, jkres):
            S.op("dve", lambda e: e.memset(smc(col), 0.0), wr=[r_("sm%d" % col)])
            act(jk, src_ap, AF.Square, srcres, [jkres, r_("sm%d" % col)], accum_out=smc(col))
            act(smc(col + 1), smc(col), AF.Sqrt, [r_("sm%d" % col)], [r_("sm%d" % col)], scale=1.0 / 1024.0, bias=EPS)

        def rms_b(src_ap, dst_bf, srcres, dstres, col):
            recip(smc(col + 2), smc(col + 1), [r_("sm%d" % col)], [r_("sm%d" % col)])
            act(dst_bf, src_ap, AF.Copy, srcres + [r_("sm%d" % col)], dstres, scale=smc(col + 2))

        def transp8(src_bf, srcres):
            for k in range(8):
                tr(PB0[:, k * 128:(k + 1) * 128], src_bf[:, k * 128:(k + 1) * 128], srcres + [r_("ident")], [r_("PB0")])

        tcnt = [0]

        def ssm_state(tau, full, p):
            g0 = tau * 8
            q = tau % 2
            ba, bb = PBs[2 + q * 2], PBs[3 + q * 2]
            rba, rbb = r_("bank%d" % (2 + q * 2)), r_("bank%d" % (3 + q * 2))
            bav = ba[:].rearrange("p (g t) -> p g t", g=4)
            bbv = bb[:].rearrange("p (g t) -> p g t", g=4)
            rz = r_("zT%d" % p)
            rG = r_("Gt%d" % q)
            for hh in range(2):
                for gl4 in range(4):
                    g = g0 + hh * 4 + gl4
                    mm(bav[:, gl4, :], LBa[:, g, :], zT[p][:, 4 + tau, 16:144], True, True, [r_("LBa"), rz], [rba])
                    mm(bbv[:, gl4, :], LBb[:, g, :], zT[p][:, 4 + tau, 16:144], True, True, [r_("LBb"), rz], [rbb])
                sl = slice(hh * 4, hh * 4 + 4)
                gs = slice(g0 + hh * 4, g0 + hh * 4 + 4)
                rD = r_("D1%d" % hh)
                tt("dve", Gt[q][:, sl, :], bav, COS[:, gs, :], ALU.mult, [rba, r_("tab")], [rG])
                tt("dve", D1[hh][:], bbv, SINM[:, gs, :], ALU.mult, [rbb, r_("tab")], [rD])
                tt("pool", Gt[q][:, sl, :], Gt[q][:, sl, :], D1[hh][:], ALU.add, [rD], [rG])
                yield
            rX = r_("XT%d" % q)
            for gl in range(8):
                g = g0 + gl
                S.op("dve", lambda e, gl=gl, g=g, q=q: e.tensor_tensor_scan(
                    out=XT[q][:, gl, :], data0=MAG[:, g:g + 1].to_broadcast([128, 128]), data1=Gt[q][:, gl, :],
                    initial=CAR[:, g:g + 1], op0=ALU.mult, op1=ALU.add),
                    rd=[rG, r_("MAG"), r_("CAR")], wr=[rX])
            yield
            gs = slice(g0, g0 + 8)
            rXb, rXb2 = r_("XTb%d" % q), r_("XTc%d" % q)
            rM1, rM2 = r_("M1%d" % q), r_("M2%d" % q)
            if full:
                dma(XTb[q][0:64, :, :], XT[q][64:128, :, :], [rX], [rXb])
                dma(XTb[q][64:128, :, :], XT[q][0:64, :, :], [rX], [rXb2])
                tt("dve", M1[q], XT[q], COS[:, gs, :], ALU.mult, [rX, r_("tab")], [rM1])
                tt("pool", M2[q], XTb[q], SINM[:, gs, :], ALU.mult, [rXb, rXb2, r_("tab")], [rM2])
                tt("pool", XA[q], M1[q], M2[q], ALU.subtract, [rM1, rM2], [r_("XA%d" % q)])
                tt("pool", CAR[:, gs], M1[q][:, :, 127], M2[q][:, :, 127], ALU.subtract, [rM1, rM2], [r_("CAR")])
            else:
                dma(XTb[q][0:64, :, 127:128], XT[q][64:128, :, 127:128], [rX], [rXb], slow=True)
                dma(XTb[q][64:128, :, 127:128], XT[q][0:64, :, 127:128], [rX], [rXb2], slow=True)
                tt("pool", M1[q][:, :, 127], XT[q][:, :, 127], COS[:, gs, 127], ALU.mult, [rX, r_("tab")], [rM1])
                tt("pool", M2[q][:, :, 127], XTb[q][:, :, 127], SINM[:, gs, 127], ALU.mult, [rXb, rXb2, r_("tab")], [rM2])
                tt("pool", CAR[:, gs], M1[q][:, :, 127], M2[q][:, :, 127], ALU.subtract, [rM1, rM2], [r_("CAR")])
            yield

        def front(tile_idx, mlist, p):
            rz, rzo = r_("zT%d" % p), r_("zT%d" % (1 - p))
            rx, rh, rhT = r_("xt%d" % p), r_("hn%d" % p), r_("hnT%d" % p)
            dma(xt[p], xall[tile_idx * 128:(tile_idx + 1) * 128, :], [], [rx])
            rms(xt[p], hn[p], [rx], [rh], 0, junk)
            transp8(hn[p], [rh])
            act(hnT[p].rearrange("p k t -> p (k t)"), PB0[:], AF.Copy, [r_("PB0")], [rhT])
            yield
            cp("pool", zT[p][:, 0:4, 0:16], zT[1 - p][:, 0:4, 128:144], [rzo], [rz])
            for m in mlist:
                pb = PBs[0] if m < 4 else PBs[1]
                rpb = r_("bank%d" % (m // 4))
                o = pb[:, (m % 4) * 128:(m % 4 + 1) * 128]
                for k in range(8):
                    mm(o, WI[:, k, m * 128:(m + 1) * 128], hnT[p][:, k, :], k == 0, k == 7, [r_("WI"), rhT], [rpb])
            if 0 in mlist:
                act(zT[p][:, 0:4, 16:144], PBs[0][:].rearrange("p (m t) -> p m t", m=4), AF.Copy, [r_("bank0")], [rz])
            if 4 in mlist:
                act(zT[p][:, 4:8, 16:144], PBs[1][:].rearrange("p (m t) -> p m t", m=4), AF.Copy, [r_("bank1")], [rz])
            yield "F"

        PQ = PBs[6]

        def mixer(tile_idx, lt, first, p):
            rz = r_("zT%d" % p)
            ryT, ryg, rygf = r_("yT%d" % p), r_("yg%d" % p), r_("ygf%d" % p)
            yield from front(tile_idx, list(range(8)), p)
            for gi, w in enumerate(WINS):
                o = PQ[:, gi * 128:(gi + 1) * 128]
                for l in range(w):
                    mm(o, PW[:, 2 * gi + (1 if l > 0 else 0), :], zT[p][:, gi, 16 - l:144 - l], l == 0, (l == w - 1) and not first, [r_("PW"), rz], [r_("bank6")])
                if first:
                    S.op("dve", lambda e, gi=gi: e.tensor_tensor_scan(
                        out=cf[:, 0:16], data0=cstt[:, 257:258].to_broadcast([128, 16]), data1=zT[p][:, gi, 16:32],
                        initial=0.0, op0=ALU.mult, op1=ALU.add), rd=[rz, r_("cstt")], wr=[r_("cf")])
                    tt("dve", cf[:, 16:32], cf[:, 0:16], RB[:, 20 + gi * 16:36 + gi * 16], ALU.mult, [r_("cf"), r_("RB")], [r_("cf2")])
                    cp("dve", pzb[:, 0:16], cf[:, 16:32], [r_("cf2")], [r_("pzb")])
                    mm(o[:, 0:16], PW[:, 2 * gi + 1, :], pzb[:, 0:16], False, True, [r_("PW"), r_("pzb")], [r_("bank6")])
            for gi in range(4):
                act(yT[p][:, gi, :], PQ[:, gi * 128:(gi + 1) * 128], AF.Copy, [r_("bank6")], [ryT], scale=colt[:, c_ps_ + gi:c_ps_ + gi + 1])
            yield
            for tau in range(4):
                q = tau % 2
                yield from ssm_state(tau, True, p)
                o = PBs[0][:, tau * 128:(tau + 1) * 128]
                ry = r_("bank0")
                for gl in range(8):
                    mm(o, LC[:, tau * 8 + gl, :], XA[q][:, gl, :], gl == 0, False, [r_("LC"), r_("XA%d" % q)], [ry])
                mm(o, Dg[:, tau, :], zT[p][:, 4 + tau, 16:144], False, True, [r_("Dg"), rz], [ry])
                rg1, rg2 = r_("g1%d" % q), r_("g2%d" % q)
                act(g1[q], o, AF.Square, [ry], [rg1])
                tsc(g1[q], g1[q], 0.044715, 1.0, ALU.mult, ALU.add, [rg1], [rg1])
                tt("dve", g1[q], g1[q], o, ALU.mult, [rg1, ry], [rg1])
                act(g2[q], g1[q], AF.Tanh, [rg1], [rg2], scale=0.7978845608028654)
                tsc(g2[q], g2[q], 0.5, 0.5, ALU.mult, ALU.add, [rg2], [rg2])
                tt("dve", ygf[p][:, tau, :], g2[q], o, ALU.mult, [rg2, ry], [rygf])
                cp("pool", yg[p][:, tau, :], ygf[p][:, tau, :], [rygf], [ryg])
                yield
            for m in range(4):
                q = m % 2
                rg1 = r_("g1%d" % q)
                o = PBs[1][:, m * 128:(m + 1) * 128]
                for k in range(4):
                    mm(o, GW[:, k, m * 128:(m + 1) * 128], yg[p][:, k, :], k == 0, k == 3, [r_("GW"), ryg], [r_("bank1")])
                act(g1[q], o, AF.Tanh, [r_("bank1"), r_("sm2")], [rg1], scale=0.5, bias=sm[:, 64 + m:65 + m])
                tsc(g1[q], g1[q], 0.5, 0.5, ALU.mult, ALU.add, [rg1], [rg1])
                tt("pool", yT[p][:, 4 + m, :], g1[q], ygf[p][:, m, :], ALU.mult, [rg1, rygf], [ryT])
            yield
            for half in range(2):
                pb = PBs[half]
                rpb = r_("bank%d" % half)
                for k in range(8):
                    mm(pb[:], yT[p][:, k, :], WO[:, k, half * 512:(half + 1) * 512], k == 0, k == 7, [ryT, r_("WO")], [rpb])
                tt("dve", acc[:, lt, half * 512:(half + 1) * 512], pb[:], xt[p][:, half * 512:(half + 1) * 512], ALU.add, [rpb, r_("xt%d" % p)], [r_("acc%d" % lt)])
            yield
            rh = r_("hn%d" % p)
            rms(acc[:, lt, :], hn[p], [r_("acc%d" % lt)], [rh], 4, junk)
            transp8(hn[p], [rh])
            act(hn2T[:, :, lt * 128:(lt + 1) * 128], PB0[:].rearrange("p (k t) -> p k t", k=8), AF.Copy, [r_("PB0")], [r_("hn2T")])
            yield
            o = PQ[:, 0:20]
            for k in range(8):
                mm(o, hn2T[:, k, lt * 128:(lt + 1) * 128], WRb[:, k, :], k == 0, k == 7, [r_("hn2T"), r_("WRb")], [r_("bank6")])
            L_ = sm[:, 100:120]
            rS = r_("smr")
            tt("dve", L_, o, RB[:, 0:20], ALU.add, [r_("bank6"), r_("RB")], [rS])
            cL, fL = sm[:, 100:104], sm[:, 104:120]
            M, GM, CM, TH, NUM, SS, PG = smc(120), smc(121, 4), smc(125, 4), smc(129, 4), smc(133, 4), smc(137), smc(138)
            red(M, cL, ALU.max, [rS], [rS])
            tsc(GM, cL, M, None, ALU.is_equal, None, [rS], [rS])
            tsc(CM, cL, M, None, ALU.subtract, None, [rS], [rS])
            act(TH, CM, AF.Tanh, [rS], [rS], scale=0.5)
            tsc(NUM, TH, 1.0, None, ALU.add, None, [rS], [rS])
            tsc(TH, TH, -1.0, 1.0, ALU.mult, ALU.add, [rS], [rS])
            recip(TH, TH, [rS], [rS])
            tt("dve", NUM, NUM, TH, ALU.mult, [rS], [rS])
            red(SS, NUM, ALU.add, [rS], [rS])
            recip(PG, SS, [rS], [rS])
            FT = sm[:, 140:156]
            tt("dve", FT.rearrange("p (g j) -> p g j", g=4), fL.rearrange("p (g j) -> p g j", g=4),
               GM.unsqueeze(2).to_broadcast([128, 4, 4]), ALU.mult, [rS], [rS])
            FS = smc(156, 4)
            red(FS, FT.rearrange("p (g j) -> p j g", g=4), ALU.add, [rS], [rS])
            M1_, K1, F2, M2_, K2, DD, W1, W2, WJ = smc(160), smc(161, 4), smc(165, 4), smc(169), smc(170, 4), smc(174), smc(175), smc(176), smc(177, 4)
            red(M1_, FS, ALU.max, [rS], [rS])
            tsc(K1, FS, M1_, None, ALU.is_equal, None, [rS], [rS])
            stt(F2, K1, -1e30, FS, ALU.mult, ALU.add, [rS], [rS])
            red(M2_, F2, ALU.max, [rS], [rS])
            tsc(K2, F2, M2_, None, ALU.is_equal, None, [rS], [rS])
            tt("dve", DD, M2_, M1_, ALU.subtract, [rS], [rS])
            act(DD, DD, AF.Tanh, [rS], [rS], scale=0.5)
            tsc(W1, DD, -0.5, 0.5, ALU.mult, ALU.add, [rS], [rS])
            tsc(W2, DD, 0.5, 0.5, ALU.mult, ALU.add, [rS], [rS])
            tsc(WJ, K1, W1, None, ALU.mult, None, [rS], [rS])
            stt(WJ, K2, W2, WJ, ALU.mult, ALU.add, [rS], [rS])
            tsc(WJ, WJ, PG, None, ALU.mult, None, [rS], [rS])
            tt("dve", gates[:, lt, :].rearrange("p (g j) -> p g j", g=4), GM.unsqueeze(2).to_broadcast([128, 4, 4]),
               WJ.unsqueeze(1).to_broadcast([128, 4, 4]), ALU.mult, [rS], [r_("gates")])
            yield

        def router(lt, o, rq0):
            L_ = sm[:, 100:120]
            rS = r_("smr")
            tt("dve", L_, o, RB[:, 0:20], ALU.add, [rq0, r_("RB")], [rS])
            cL, fL = sm[:, 100:104], sm[:, 104:120]
            M, GM, CM, TH, NUM, SS, PG = smc(120), smc(121, 4), smc(125, 4), smc(129, 4), smc(133, 4), smc(137), smc(138)
            red(M, cL, ALU.max, [rS], [rS])
            tsc(GM, cL, M, None, ALU.is_equal, None, [rS], [rS])
            tsc(CM, cL, M, None, ALU.subtract, None, [rS], [rS])
            act(TH, CM, AF.Tanh, [rS], [rS], scale=0.5)
            tsc(NUM, TH, 1.0, None, ALU.add, None, [rS], [rS])
            tsc(TH, TH, -1.0, 1.0, ALU.mult, ALU.add, [rS], [rS])
            recip(TH, TH, [rS], [rS])
            tt("dve", NUM, NUM, TH, ALU.mult, [rS], [rS])
            red(SS, NUM, ALU.add, [rS], [rS])
            recip(PG, SS, [rS], [rS])
            FT = sm[:, 140:156]
            tt("dve", FT.rearrange("p (g j) -> p g j", g=4), fL.rearrange("p (g j) -> p g j", g=4),
               GM.unsqueeze(2).to_broadcast([128, 4, 4]), ALU.mult, [rS], [rS])
            FS = smc(156, 4)
            red(FS, FT.rearrange("p (g j) -> p j g", g=4), ALU.add, [rS], [rS])
            M1_, K1, F2, M2_, K2, DD, W1, W2, WJ = smc(160), smc(161, 4), smc(165, 4), smc(169), smc(170, 4), smc(174), smc(175), smc(176), smc(177, 4)
            red(M1_, FS, ALU.max, [rS], [rS])
            tsc(K1, FS, M1_, None, ALU.is_equal, None, [rS], [rS])
            stt(F2, K1, -1e30, FS, ALU.mult, ALU.add, [rS], [rS])
            red(M2_, F2, ALU.max, [rS], [rS])
            tsc(K2, F2, M2_, None, ALU.is_equal, None, [rS], [rS])
            tt("dve", DD, M2_, M1_, ALU.subtract, [rS], [rS])
            act(DD, DD, AF.Tanh, [rS], [rS], scale=0.5)
            tsc(W1, DD, -0.5, 0.5, ALU.mult, ALU.add, [rS], [rS])
            tsc(W2, DD, 0.5, 0.5, ALU.mult, ALU.add, [rS], [rS])
            tsc(WJ, K1, W1, None, ALU.mult, None, [rS], [rS])
            stt(WJ, K2, W2, WJ, ALU.mult, ALU.add, [rS], [rS])
            tsc(WJ, WJ, PG, None, ALU.mult, None, [rS], [rS])
            tt("dve", gates[:, lt, :].rearrange("p (g j) -> p g j", g=4), GM.unsqueeze(2).to_broadcast([128, 4, 4]),
               WJ.unsqueeze(1).to_broadcast([128, 4, 4]), ALU.mult, [rS], [r_("gates")])

        RRs = [M2[0], M2[1]]

        def build_RR(tau, q):
            gs = slice(tau * 8, tau * 8 + 8)
            act(RRs[q], MAG[:, gs].unsqueeze(2).to_broadcast([128, 8, 128]), AF.Copy, [r_("MAG")], [r_("RR%d" % q)])
            S.op("pool", lambda e: e.memset(RRs[q][:, :, 0:1], 0.0), rd=[], wr=[r_("RR%d" % q)])

        def run_gen(g):
            for _ in g:
                pass

        def g_front(tile_idx, lt, p):
            rz, rzo = r_("zT%d" % p), r_("zT%d" % (1 - p))
            rx, rh, rhT = r_("xt%d" % p), r_("hn%d" % p), r_("hnT%d" % p)
            dma(xt[p], xall[tile_idx * 128:(tile_idx + 1) * 128, :], [], [rx])
            dma(acc[:, lt, :], xall[tile_idx * 128:(tile_idx + 1) * 128, :], [], [r_("acc%d" % lt)])
            rms_a(xt[p], [rx], 0, hn[p], rh)
            yield
            rms_b(xt[p], hn[p], [rx], [rh], 0)
            yield
            transp8(hn[p], [rh])
            act(hnT[p].rearrange("p k t -> p (k t)"), PB0[:], AF.Copy, [r_("PB0")], [rhT])
            yield
            cp("pool", zT[p][:, 0:4, 0:16], zT[1 - p][:, 0:4, 128:144], [rzo], [rz])
            for m in range(8):
                pb = PBs[0] if m < 4 else PBs[1]
                rpb = r_("bank%d" % (m // 4))
                o = pb[:, (m % 4) * 128:(m % 4 + 1) * 128]
                for k in range(8):
                    mm(o, WI[:, k, m * 128:(m + 1) * 128], hnT[p][:, k, :], k == 0, k == 7, [r_("WI"), rhT], [rpb])
                if m == 3:
                    act(zT[p][:, 0:4, 16:144], PBs[0][:].rearrange("p (m t) -> p m t", m=4), AF.Copy, [r_("bank0")], [rz])
            act(zT[p][:, 4:8, 16:144], PBs[1][:].rearrange("p (m t) -> p m t", m=4), AF.Copy, [r_("bank1")], [rz])
            yield

        def st_pool(p, first):
            rz, ryT = r_("zT%d" % p), r_("yT%d" % p)
            rq = r_("bank6")
            for gi, w in enumerate(WINS):
                o = PQ[:, gi * 128:(gi + 1) * 128]
                for l in range(w):
                    mm(o, PW[:, 2 * gi + (1 if l > 0 else 0), :], zT[p][:, gi, 16 - l:144 - l], l == 0, (l == w - 1) and not first, [r_("PW"), rz], [rq])
                if first:
                    S.op("dve", lambda e, gi=gi: e.tensor_tensor_scan(
                        out=cf[:, 0:16], data0=cstt[:, 257:258].to_broadcast([128, 16]), data1=zT[p][:, gi, 16:32],
                        initial=0.0, op0=ALU.mult, op1=ALU.add), rd=[rz, r_("cstt")], wr=[r_("cf")])
                    tt("dve", cf[:, 16:32], cf[:, 0:16], RB[:, 20 + gi * 16:36 + gi * 16], ALU.mult, [r_("cf"), r_("RB")], [r_("cf2")])
                    cp("dve", pzb[:, 0:16], cf[:, 16:32], [r_("cf2")], [r_("pzb")])
                    mm(o[:, 0:16], PW[:, 2 * gi + 1, :], pzb[:, 0:16], False, True, [r_("PW"), r_("pzb")], [rq])
            for gi in range(4):
                act(yT[p][:, gi, :], PQ[:, gi * 128:(gi + 1) * 128], AF.Copy, [rq], [ryT], scale=colt[:, c_ps_ + gi:c_ps_ + gi + 1])

        def st_Dp(p, tau):
            g0 = tau * 8
            rz = r_("zT%d" % p)
            for hh in range(2):
                ba, bb = PBs[2 + hh * 2], PBs[3 + hh * 2]
                rba, rbb = r_("bank%d" % (2 + hh * 2)), r_("bank%d" % (3 + hh * 2))
                bav = ba[:].rearrange("p (g t) -> p g t", g=4)
                bbv = bb[:].rearrange("p (g t) -> p g t", g=4)
                for gl4 in range(4):
                    g = g0 + hh * 4 + gl4
                    mm(bav[:, gl4, :], LBa[:, g, :], zT[p][:, 4 + tau, 16:144], True, True, [r_("LBa"), rz], [rba])
                    mm(bbv[:, gl4, :], LBb[:, g, :], zT[p][:, 4 + tau, 16:144], True, True, [r_("LBb"), rz], [rbb])

        def st_Dd(tau, q):
            g0 = tau * 8
            rG, rM1 = r_("Gt%d" % q), r_("M1t")
            for hh in range(2):
                ba, bb = PBs[2 + hh * 2], PBs[3 + hh * 2]
                rba, rbb = r_("bank%d" % (2 + hh * 2)), r_("bank%d" % (3 + hh * 2))
                bav = ba[:].rearrange("p (g t) -> p g t", g=4)
                bbv = bb[:].rearrange("p (g t) -> p g t", g=4)
                sl = slice(hh * 4, hh * 4 + 4)
                gs = slice(g0 + hh * 4, g0 + hh * 4 + 4)
                tt("dve", Gt[q][:, sl, :], bav, COS[:, gs, :], ALU.mult, [rba, r_("tab")], [rG])
                tt("dve", M1[0][:, sl, :], bbv, SINM[:, gs, :], ALU.mult, [rbb, r_("tab")], [rM1])
                tt("dve", Gt[q][:, sl, :], Gt[q][:, sl, :], M1[0][:, sl, :], ALU.add, [rM1], [rG])

        def st_S(tau, q):
            g0 = tau * 8
            gs = slice(g0, g0 + 8)
            rG, rX = r_("Gt%d" % q), r_("XT%d" % q)
            c8 = sm[:, 80:88]
            tt("dve", c8, MAG[:, gs], CAR[:, gs], ALU.mult, [r_("MAG"), r_("CAR")], [r_("c8")])
            tt("dve", Gt[q][:, :, 0], Gt[q][:, :, 0], c8, ALU.add, [r_("c8")], [rG])
            S.op("dve", lambda e, q=q: e.tensor_tensor_scan(
                out=XT[q].rearrange("p a b -> p (a b)"), data0=RRs[q].rearrange("p a b -> p (a b)"),
                data1=Gt[q].rearrange("p a b -> p (a b)"), initial=0.0, op0=ALU.mult, op1=ALU.add),
                rd=[rG, r_("RR%d" % q)], wr=[rX])
            xs = sm[:, 16 + 8 * q:24 + 8 * q]
            dma(xs[0:64, :], XT[q][64:128, :, 127], [rX], [r_("xs%d" % q)], slow=True)
            dma(xs[64:128, :], XT[q][0:64, :, 127], [rX], [r_("xsb%d" % q)], slow=True)
            build_RR((tau + 2) % 4, q)

        M1b = [XA[0], XA[1]]
        M2b = [M1[1].rearrange("p a b -> p (a b)").bitcast(BF16)[:, i * 1024:(i + 1) * 1024].rearrange("p (a b) -> p a b", a=8) for i in range(2)]

        def st_M(tau, q):
            g0 = tau * 8
            gs = slice(g0, g0 + 8)
            rX = r_("XT%d" % q)
            tt("dve", M1b[q], XT[q], COS[:, gs, :], ALU.mult, [rX, r_("tab")], [r_("XA%d" % q)])
            tt("dve", M2b[q], XT[q], SINM[:, gs, :], ALU.mult, [rX, r_("tab")], [r_("M2b%d" % q)])
            xs = sm[:, 16 + 8 * q:24 + 8 * q]
            t1c = sm[:, 32 + 8 * q:40 + 8 * q]
            tt("dve", t1c, XT[q][:, :, 127], COS[:, gs, 127], ALU.mult, [rX, r_("tab")], [r_("t1c%d" % q)])
            tt("dve", xs, xs, SINM[:, gs, 127], ALU.mult, [r_("xs%d" % q), r_("xsb%d" % q), r_("tab")], [r_("xs%d" % q), r_("xsb%d" % q)])
            tt("dve", CAR[:, gs], t1c, xs, ALU.subtract, [r_("t1c%d" % q), r_("xs%d" % q), r_("xsb%d" % q)], [r_("CAR")])

        def st_Cp(p, tau, q):
            rz = r_("zT%d" % p)
            o = PQ[:, tau * 128:(tau + 1) * 128]
            ry = r_("bank6")
            for gl in range(8):
                g = tau * 8 + gl
                mm(o, LC[:, g, :], M1b[q][:, gl, :], gl == 0, False, [r_("LC"), r_("XA%d" % q)], [ry])
                mm(o, LCsv[g // 16][:, g % 16, :], M2b[q][:, gl, :], False, False, [r_("LCs"), r_("M2b%d" % q)], [ry])
            mm(o, Dg[:, tau, :], zT[p][:, 4 + tau, 16:144], False, True, [r_("Dg"), rz], [ry])
            act(g1[q], o, AF.Square, [ry], [r_("g1%d" % q)])

        def st_Ca(p, tau, q):
            o = PQ[:, tau * 128:(tau + 1) * 128]
            ry = r_("bank6")
            rg1, rg2 = r_("g1%d" % q), r_("g2%d" % q)
            tsc(g1[q], g1[q], 0.044715, 1.0, ALU.mult, ALU.add, [rg1], [rg1])
            tt("dve", g1[q], g1[q], o, ALU.mult, [rg1, ry], [rg1])
            act(g2[q], g1[q], AF.Tanh, [rg1], [rg2], scale=0.7978845608028654)

        def st_Cb(p, tau, q):
            ryg, rygf = r_("yg%d" % p), r_("ygf%d" % p)
            o = PQ[:, tau * 128:(tau + 1) * 128]
            ry = r_("bank6")
            rg2 = r_("g2%d" % q)
            stt(ygf[p][:, tau, :], g2[q], 1.0, o, ALU.add, ALU.mult, [rg2, ry], [rygf])
            act(yg[p][:, tau, :], ygf[p][:, tau, :], AF.Copy, [rygf], [ryg])

        def g_tail(lt, p):
            ryT, ryg, rygf = r_("yT%d" % p), r_("yg%d" % p), r_("ygf%d" % p)
            jf = junk.bitcast(F32)
            glt = [jf[:, i * 128:(i + 1) * 128] for i in range(4)]
            rgl = [r_("glt%d" % i) for i in range(4)]
            rbk = [r_("bank1"), r_("bank0")]
            for m in range(4):
                pbm = PBs[1] if m % 2 == 0 else PBs[0]
                rb = rbk[m % 2]
                o = pbm[:, (m // 2) * 128:(m // 2 + 1) * 128]
                for k in range(4):
                    mm(o, GW[:, k, m * 128:(m + 1) * 128], yg[p][:, k, :], k == 0, k == 3, [r_("GW"), ryg], [rb])
                act(glt[m], o, AF.Tanh, [rb, r_("sm2")], [rgl[m]], scale=0.5, bias=sm[:, 64 + m:65 + m])
            yield
            for m in range(4):
                stt(yT[p][:, 4 + m, :], glt[m], 1.0, ygf[p][:, m, :], ALU.add, ALU.mult, [rgl[m], rygf], [ryT])
            for half in range(2):
                pb = PBs[half]
                rqs = [r_("bank%d" % half)]
                for k in range(8):
                    mm(pb[:], yT[p][:, k, :], WO[:, k, half * 512:(half + 1) * 512], k == 0, k == 7, [ryT, r_("WO")], rqs)
            yield
            for half in range(2):
                pb = PBs[half]
                rqs = [r_("bank%d" % half)]
                tt("dve", acc[:, lt, half * 512:(half + 1) * 512], pb[:], acc[:, lt, half * 512:(half + 1) * 512], ALU.add, rqs + [r_("acc%d" % lt)], [r_("acc%d" % lt)])
            rh = r_("hn2b%d" % p)
            rms_a(acc[:, lt, :], [r_("acc%d" % lt)], 4, hn2b[p], rh)
            yield
            rms_b(acc[:, lt, :], hn2b[p], [r_("acc%d" % lt)], [rh], 4)
            yield
            transp8(hn2b[p], [rh])
            act(hn2T[:, :, lt * 128:(lt + 1) * 128], PB0[:].rearrange("p (k t) -> p k t", k=8), AF.Copy, [r_("PB0")], [r_("hn2T")])
            yield
            o = PQ[:, 0:20]
            rq0 = r_("bank6")
            for k in range(8):
                mm(o, hn2T[:, k, lt * 128:(lt + 1) * 128], WRb[:, k, :], k == 0, k == 7, [r_("hn2T"), r_("WRb")], [rq0])
            router(lt, o, rq0)
            yield

        def mixer_sb(sbi):
            nun = SBT * 4
            par = lambda lt: (sbi * SBT + lt) % 2
            bg = []

            def advance():
                for g in list(bg):
                    try:
                        next(g)
                    except StopIteration:
                        bg.remove(g)
            run_gen(g_front(NT_PRE + sbi * SBT, 0, par(0)))
            build_RR(0, 0)
            build_RR(1, 1)
            st_Dp(par(0), 0)
            if SBT > 1:
                bg.append(g_front(NT_PRE + sbi * SBT + 1, 1, par(1)))
            k = 0
            while k < nun + 4 or bg:
                advance()
                if 0 <= k - 3 < nun:
                    lt3, tau3 = divmod(k - 3, 4)
                    st_Ca(par(lt3), tau3, (k - 3) % 2)
                if 0 <= k - 2 < nun:
                    st_M((k - 2) % 4, (k - 2) % 2)
                if 0 <= k - 3 < nun:
                    st_Cb(par(lt3), tau3, (k - 3) % 2)
                    if tau3 == 3:
                        bg.append(g_tail(lt3, par(lt3)))
                if k < nun:
                    lt, tau = divmod(k, 4)
                    if tau == 2:
                        st_pool(par(lt), sbi == 0 and lt == 0)
                    st_Dd(tau, k % 2)
                    if tau == 3 and 1 <= lt + 1 and lt + 2 < SBT:
                        bg.append(g_front(NT_PRE + sbi * SBT + lt + 2, lt + 2, par(lt + 2)))
                if 0 <= k - 1 < nun:
                    st_S((k - 1) % 4, (k - 1) % 2)
                if 0 <= k - 2 < nun:
                    ltp, taup = divmod(k - 2, 4)
                    st_Cp(par(ltp), taup, (k - 2) % 2)
                if k + 1 < nun:
                    ltn, taun = divmod(k + 1, 4)
                    st_Dp(par(ltn), taun)
                k += 1

        def pipeline(gens):
            gens = list(gens)
            active = []
            while gens or active:
                if gens and len(active) < 2 and (not active or active[-1][1][0]):
                    active.append((gens.pop(0), [False]))
                for it in list(active):
                    g, st = it
                    try:
                        v = next(g)
                        if v == "F":
                            st[0] = True
                    except StopIteration:
                        active.remove(it)

        wgb = nc.dram_tensor("wgb", [16, 128, 2048], BF16).ap()
        wub = nc.dram_tensor("wub", [16, 128, 2048], BF16).ap()
        wdb = nc.dram_tensor("wdb", [16, 128, 2048], BF16).ap()
        accf = acc[:].rearrange("p a b -> p (a b)")
        hn2f = hn2T[:].rearrange("p a b -> p (a b)")
        cast_units = []
        for e in range(16):
            cast_units += [(0, e), (1, e), (2, e)]
        ucnt = [0]

        def cast_unit():
            if not cast_units:
                return
            kind, e = cast_units.pop(0)
            sl = ucnt[0] % 2
            ucnt[0] += 1
            stg = accf[:, sl * 2048:(sl + 1) * 2048]
            tmp = hn2f[:, sl * 2048:(sl + 1) * 2048]
            rs_, rt_ = r_("stg%d" % sl), r_("tmpb%d" % sl)
            if kind == 2:
                dma(stg.rearrange("p (k n) -> p k n", k=2), wd_d[e, :, :].rearrange("(k p) n -> p k n", p=128), [], [rs_], q="pool")
                cp("pool", tmp, stg, [rs_], [rt_])
                dma(wdb[e, :, :], tmp, [rt_], [r_("wscr")], q="pool")
            else:
                src = wg_d if kind == 0 else wu_d
                dst = wgb if kind == 0 else wub
                dma(stg.rearrange("p (k n) -> p k n", k=8), src[e, :, :].rearrange("(k p) n -> p k n", p=128), [], [rs_], q="pool")
                tt("pool", tmp.rearrange("p (k n) -> p k n", k=8), stg.rearrange("p (k n) -> p k n", k=8),
                   colt[:, c_nf:c_nf + 8].unsqueeze(2).to_broadcast([128, 8, 256]), ALU.mult, [rs_, r_("colt")], [rt_])
                dma(dst[e, :, :], tmp, [rt_], [r_("wscr")], q="pool")

        def moe(sbi):
            dma(NF, rows[0:1, 0:1024].partition_broadcast(128), [], [r_("NF")])
            nblk = SBT // 4
            tok = slice(0, SBT * 128)

            def gu(e):
                sl = e % 2
                rwg, rwu, rwd = r_("WG%d" % sl), r_("WU%d" % sl), r_("WD%d" % sl)
                dma(WGs[sl].rearrange("p k n -> p (k n)"), wgb[e, :, :], [r_("wscr")], [rwg])
                dma(WUs[sl].rearrange("p k n -> p (k n)"), wub[e, :, :], [r_("wscr")], [rwu])
                dma(WDs[sl].rearrange("p k n -> p (k n)"), wdb[e, :, :], [r_("wscr")], [rwd])
                for ft in range(2):
                    gp, up = PBs[2 + ft], PBs[4 + ft]
                    rg, ru = r_("bank%d" % (2 + ft)), r_("bank%d" % (4 + ft))
                    rsg, rhT_ = r_("sgT%d%d" % (sl, ft)), r_("hT%d%d" % (sl, ft))
                    for k in range(8):
                        mm(gp[:], WGs[sl][:, k, ft * 128:(ft + 1) * 128], hn2T[:, k, tok], k == 0, k == 7, [rwg, r_("hn2T")], [rg])
                    for k in range(8):
                        mm(up[:], WUs[sl][:, k, ft * 128:(ft + 1) * 128], hn2T[:, k, tok], k == 0, k == 7, [rwu, r_("hn2T")], [ru])
                    act(sgT[sl][:, ft, :], gp[:], AF.Silu, [rg], [rsg])
                    tt("dve", hT[sl][:, ft, :], up[:], sgT[sl][:, ft, :], ALU.mult, [ru, rsg], [rhT_])

            def down(e):
                sl = e % 2
                rwd = r_("WD%d" % sl)
                for lt in range(SBT):
                    for half in range(2):
                        pb = PBs[half]
                        rpb = r_("bank%d" % half)
                        for ft in range(2):
                            mm(pb[:], hT[sl][:, ft, lt * 128:(lt + 1) * 128], WDs[sl][:, ft, half * 512:(half + 1) * 512], ft == 0, ft == 1,
                               [r_("hT%d%d" % (sl, ft)), rwd], [rpb])
                        stt(acc[:, lt, half * 512:(half + 1) * 512], pb[:], gates[:, lt, e:e + 1], acc[:, lt, half * 512:(half + 1) * 512],
                            ALU.mult, ALU.add, [rpb, r_("gates"), r_("acc%d" % lt)], [r_("acc%d" % lt)])
            for e in range(17):
                if e < 16:
                    gu(e)
                if e >= 1:
                    down(e - 1)
            for lt in range(SBT):
                tix = sbi * SBT + lt
                o2 = lt % 2
                ro = r_("outt%d" % o2)
                S.op("dve", lambda e: e.memset(smc(8), 0.0), wr=[r_("sm8")])
                act(junk2, acc[:, lt, :], AF.Square, [r_("acc%d" % lt)], [r_("junk2"), r_("sm8")], accum_out=smc(8))
                act(smc(9), smc(8), AF.Sqrt, [r_("sm8")], [r_("sm8")], scale=1.0 / 1024.0, bias=EPS)
                recip(smc(10), smc(9), [r_("sm8")], [r_("sm8")])
                stt(outt[o2], acc[:, lt, :], smc(10), NF, ALU.mult, ALU.mult, [r_("acc%d" % lt), r_("sm8"), r_("NF")], [ro])
                dma(out[tix * 128:(tix + 1) * 128, :], outt[o2], [ro], [r_("out")])

        S.barrier()
        def pre_tile(t, p):
            last = t == NT_PRE - 1
            yield from front(t, [0, 1, 2, 3] if last else [], p)
            rhT = r_("hnT%d" % p)
            for k in range(8):
                mm(PBs[1][:], hnT[p][:, k, :], WI[:, k, 512:1024], k == 0, k == 7, [r_("WI"), rhT], [r_("bank1")])
            act(ztok[p], PBs[1][:], AF.Copy, [r_("bank1")], [r_("ztok%d" % p)])
            yield
            ya, yb = PBs[2 + 2 * p], PBs[3 + 2 * p]
            rya, ryb = r_("bank%d" % (2 + 2 * p)), r_("bank%d" % (3 + 2 * p))
            for g in range(32):
                mm(ya[:, g * 16:(g + 1) * 16], VTa[:, g, :], ztok[p][:, g * 16:(g + 1) * 16], True, True, [r_("VTa"), r_("ztok%d" % p)], [rya])
            for g in range(32):
                mm(yb[:, g * 16:(g + 1) * 16], VTb[:, g, :], ztok[p][:, g * 16:(g + 1) * 16], True, True, [r_("VTb"), r_("ztok%d" % p)], [ryb])
            yield
            tt("dve", Ytmp[0], ya[:], BBR.rearrange("p a b -> p (a b)"), ALU.mult, [rya, r_("BBR")], [r_("Ytmp0")])
            tt("dve", Ytmp[1], yb[:], BBIs.rearrange("p a b -> p (a b)"), ALU.mult, [ryb, r_("BBI")], [r_("Ytmp1")])
            tt("dve", Ytmp[0], Ytmp[0], Ytmp[1], ALU.add, [r_("Ytmp1")], [r_("Ytmp0")])
            red(Ssum, Ytmp[0].rearrange("p (a b) -> p a b", a=32), ALU.add, [r_("Ytmp0")], [r_("Ssum")])
            tt("dve", Ctmp[:, 0:32], A128[:, 0:32], CAR[:], ALU.mult, [r_("A128"), r_("CAR")], [r_("ctmp")])
            tt("dve", Ctmp[:, 32:64], A128[:, 32:64], CARb, ALU.mult, [r_("A128"), r_("CARb"), r_("CARb2")], [r_("ctmp2")])
            tt("dve", Ctmp[:, 0:32], Ctmp[:, 0:32], Ctmp[:, 32:64], ALU.add, [r_("ctmp2")], [r_("ctmp")])
            tt("dve", CAR[:], Ctmp[:, 0:32], Ssum, ALU.add, [r_("ctmp"), r_("Ssum")], [r_("CAR")])
            dma(CARb[0:64, :], CAR[64:128, :], [r_("CAR")], [r_("CARb")])
            dma(CARb[64:128, :], CAR[0:64, :], [r_("CAR")], [r_("CARb2")])
            cast_unit()
            if t % 2 == 1:
                cast_unit()
            yield
        pipeline([pre_tile(t, t % 2) for t in range(NT_PRE)])
        while cast_units:
            cast_unit()
        S.barrier()
        for sbi in range(NT_MAIN // SBT):
            mixer_sb(sbi)
            S.barrier()
            moe(sbi)
            S.barrier()
        S.final_wait("sp", [r_("out")])

        print('SBUF remaining', nc.sbuf_bytes_remaining, 'mixer_end', mixer_end, 'moe_end', _off[0])
        block = es.enter_context(nc.Block())

        @block.sync
        def _(e):
            S.replay("sp", e)

        @block.tensor
        def _(e):
            S.replay("pe", e)

        @block.scalar
        def _(e):
            S.replay("act", e)

        @block.vector
        def _(e):
            S.replay("dve", e)

        @block.gpsimd
        def _(e):
            S.replay("pool", e)
    return nc


def _col(v, k):
    return np.ascontiguousarray(np.asarray(v, np.float32).reshape(k, 128).T)


def kernel(x, norm_mix, w_in, pool_w, pool_scale, ssm_a_re, ssm_a_im, ssm_log_step,
           ssm_b_re, ssm_b_im, ssm_c_re, ssm_c_im, ssm_d, glu_w, glu_b, w_out, norm_ffn,
           router_coarse_w, router_coarse_b, router_fine_w, router_fine_b,
           exp_w_gate, exp_w_up, exp_w_down, norm_final):
    f = np.float32
    x = np.asarray(x, f)
    cols = np.zeros((128, 64), f)
    cols[:, 0:8] = _col(norm_mix[0], 8)
    cols[:, 8:16] = _col(norm_ffn[0], 8)
    cols[:, 16:20] = _col(pool_scale[0], 4)
    cols[:, 20:24] = _col(ssm_d[0], 4)
    cols[:, 24:28] = _col(glu_b[0], 4)
    are = np.asarray(ssm_a_re[0], f)
    aim = np.asarray(ssm_a_im[0], f)
    ls = np.asarray(ssm_log_step[0], f)
    sp_s = np.zeros((128, 96), f)
    sp_s[:, 0:32] = np.concatenate([are.T, are.T], 0)
    sp_s[:, 32:64] = np.concatenate([aim.T, aim.T], 0)
    sp_s[:, 64:96] = np.broadcast_to(ls[None, :], (128, 32))
    br = np.asarray(ssm_b_re[0], f).transpose(1, 0, 2)
    bi = np.asarray(ssm_b_im[0], f).transpose(1, 0, 2)
    b1 = np.ascontiguousarray(np.concatenate([br, bi], 0))
    b2 = np.ascontiguousarray(np.concatenate([bi, br], 0))
    cr = np.asarray(ssm_c_re[0], f).transpose(2, 0, 1)
    ci = np.asarray(ssm_c_im[0], f).transpose(2, 0, 1)
    c1 = np.ascontiguousarray(np.concatenate([cr, ci], 0))
    c2 = np.ascontiguousarray(np.concatenate([ci, cr], 0))
    cst = np.zeros((128, 386), f)
    cst[:, 258:386] = np.arange(127, -1, -1, dtype=f)[None, :]
    cst[:, 0:128] = np.eye(128, dtype=f)
    cst[:, 128:256] = np.arange(1, 129, dtype=f)[None, :]
    cst[:, 256] = np.where(np.arange(128) < 64, 1.0, -1.0)
    cst[:, 257] = 1.0
    wr = np.ascontiguousarray(np.concatenate([np.asarray(router_coarse_w[0], f), np.asarray(router_fine_w[0], f)], 1))
    rb = np.concatenate([np.asarray(router_coarse_b[0], f), np.asarray(router_fine_b[0], f)])
    in_maps = []
    for c in range(8):
        b, half = c // 2, c % 2
        main = x[b, half * 4096:(half + 1) * 4096]
        pre = x[b, 0:4096] if half == 1 else np.zeros((4096, 1024), f)
        fix = np.zeros((4, 16), f)
        if half == 0:
            for gi, w in enumerate(WINS):
                for t in range(w - 1):
                    fix[gi, t] = w / (t + 1.0) - 1.0
        rows = np.concatenate([np.asarray(norm_final, f), rb, fix.reshape(-1)])[None, :].astype(f)
        in_maps.append({
            "xall": np.ascontiguousarray(np.concatenate([pre, main], 0)),
            "w_in": np.asarray(w_in[0], f), "w_out": np.asarray(w_out[0], f), "glu_w": np.asarray(glu_w[0], f),
            "pool_w": np.asarray(pool_w[0], f), "cols": cols, "rows": rows, "sp_s": sp_s,
            "b1_s": b1, "b2_s": b2, "c1_s": c1, "c2_s": c2, "cst": cst, "wr": wr,
            "wg": np.asarray(exp_w_gate[0], f), "wu": np.asarray(exp_w_up[0], f), "wd": np.asarray(exp_w_down[0], f),
        })
    nc = build()
    res = run_bass_kernel_spmd(nc, in_maps, core_ids=list(range(8)))
    outs = [np.asarray(r["out"], f) for r in res.results]
    full = np.zeros((4, 8192, 1024), f)
    for c in range(8):
        b, half = c // 2, c % 2
        full[b, half * 4096:(half + 1) * 4096] = outs[c]
    return full
```

```python
import math
import numpy as np
from contextlib import ExitStack
import concourse.bass as bass
import concourse.mybir as mybir
from concourse.bass_utils import run_bass_kernel_spmd

F32 = mybir.dt.float32
BF16 = mybir.dt.bfloat16
I32 = mybir.dt.int32
AF = mybir.ActivationFunctionType
ALU = mybir.AluOpType
AX = mybir.AxisListType

NT_PRE = 32
NT_MAIN = 32
SBT = 4
EPS = 1e-6
WINS = (2, 4, 8, 16)
TWO_PI = 2.0 * math.pi


class R:
    def __init__(self, name):
        self.name = name
        self.w = None
        self.rd = {}


class Sched:
    ENG = ("pe", "act", "dve", "pool", "sp")

    def __init__(self, nc, es):
        self.nc = nc
        self.sem = {e: es.enter_context(nc.semaphore("s_" + e)) for e in self.ENG}
        self.cnt = {e: 0 for e in self.ENG}
        self.ops = {e: [] for e in self.ENG}
        self.seen = {e: {} for e in self.ENG}
        self.dma_pool = [es.enter_context(nc.semaphore("d%d" % i)) for i in range(48)]
        self.dma_cnt = {}
        self.dma_of = {}

    def dsem(self, res):
        if res.name not in self.dma_of:
            s = self.dma_pool[len(self.dma_of)]
            self.dma_of[res.name] = s
            self.dma_cnt[s.name] = 0
        return self.dma_of[res.name]

    def op(self, eng, fn, rd=(), wr=(), dma=None):
        need = {}

        def add(tok):
            if tok is None:
                return
            s, v = tok
            if need.get(s.name, (None, -1))[1] < v:
                need[s.name] = (s, v)
        for r in rd:
            add(r.w)
        for r in wr:
            add(r.w)
            for t in r.rd.values():
                add(t)
        waits = []
        for name, (s, v) in need.items():
            if eng == "pe" and s is self.sem["pe"]:
                continue
            if self.seen[eng].get(name, -1) >= v:
                continue
            self.seen[eng][name] = v
            waits.append((s, v))
        if dma is not None:
            s = self.dsem(dma)
            self.dma_cnt[s.name] += 16
            tok = (s, self.dma_cnt[s.name])
            inc = (s, 16)
        else:
            self.cnt[eng] += 1
            tok = (self.sem[eng], self.cnt[eng])
            inc = (self.sem[eng], 1)
        for r in rd:
            old = r.rd.get(tok[0].name)
            if old is None or old[1] < tok[1]:
                r.rd[tok[0].name] = tok
        for r in wr:
            r.w = tok
            r.rd = {}
        self.ops[eng].append((waits, fn, inc))
        return tok

    def final_wait(self, eng, ress):
        waits = []
        for r in ress:
            if r.w is not None:
                waits.append(r.w)
        self.ops[eng].append((waits, None, None))

    def barrier(self):
        toks = [(self.sem[e], self.cnt[e]) for e in self.ENG if self.cnt[e] > 0]
        for name, sm_ in self.dma_of.items():
            toks.append((sm_, self.dma_cnt[sm_.name]))
        for eng in self.ENG:
            waits = []
            for s_, v in toks:
                if s_ is self.sem[eng]:
                    continue
                if self.seen[eng].get(s_.name, -1) >= v:
                    continue
                self.seen[eng][s_.name] = v
                waits.append((s_, v))
            if waits:
                self.ops[eng].append((waits, None, None))

    def replay(self, eng, e):
        for waits, fn, inc in self.ops[eng]:
            for s, v in waits:
                e.wait_ge(s, v)
            if fn is not None:
                fn(e).then_inc(inc[0], inc[1])


def build(debug=False):
    nc = bass.Bass("TRN2", target_bir_lowering=False)

    def din(name, shape, dt=F32):
        return nc.dram_tensor(name, list(shape), dt, kind="ExternalInput").ap()
    xall = din("xall", [(NT_PRE + NT_MAIN) * 128, 1024])
    w_in = din("w_in", [1024, 1024])
    w_out = din("w_out", [1024, 1024])
    glu_w = din("glu_w", [512, 512])
    pool_w = din("pool_w", [4, 128, 128])
    cols = din("cols", [128, 64])
    rows = din("rows", [1, 1024 + 20 + 64])
    sp_s = din("sp_s", [128, 96])
    b1_s = din("b1_s", [128, 32, 16])
    b2_s = din("b2_s", [128, 32, 16])
    c1_s = din("c1_s", [128, 32, 16])
    c2_s = din("c2_s", [128, 32, 16])
    cst = din("cst", [128, 386])
    wr_d = din("wr", [1024, 20])
    wg_d = din("wg", [16, 1024, 256])
    wu_d = din("wu", [16, 1024, 256])
    wd_d = din("wd", [16, 256, 1024])
    out = nc.dram_tensor("out", [NT_MAIN * 128, 1024], F32, kind="ExternalOutput").ap()

    es = ExitStack()
    with es:
        S = Sched(nc, es)

        def sb(name, shape, dt=F32):
            return es.enter_context(nc.sbuf_tensor(name, list(shape), dt))

        def ps(name, shape, dt=F32):
            return es.enter_context(nc.psum_tensor(name, list(shape), dt))

        ident = sb("ident", [128, 128], BF16)
        cstt = sb("cstt", [128, 386])
        colt = sb("colt", [128, 64])
        RB = sb("RB", [128, 84])
        WI = sb("WI", [128, 8, 1024], BF16)
        WO = sb("WO", [128, 8, 1024], BF16)
        GW = sb("GW", [128, 4, 512], BF16)
        PW = sb("PW", [128, 8, 128], BF16)
        WRb = sb("WRb", [128, 8, 20], BF16)
        LBa = sb("LBa", [128, 32, 128], BF16)
        LBb = sb("LBb", [128, 32, 128], BF16)
        LC = sb("LC", [128, 32, 128], BF16)
        Dg = sb("Dg", [128, 4, 128], BF16)
        COS = sb("COS", [128, 32, 128])
        SINM = sb("SINM", [128, 32, 128])
        MAG = sb("MAG", [128, 32])
        CAR = sb("CAR", [128, 32])
        acc = sb("acc", [128, SBT, 1024])
        hn2T = sb("hn2T", [128, 8, SBT * 128], BF16)
        gates = sb("gates", [128, SBT, 16])
        cf = sb("cf", [128, 64])
        sm = sb("sm", [128, 256])
        pzb = sb("pzb", [128, 128], BF16)
        ARENA_W = 21504
        arena = sb("arena", [128, ARENA_W])
        _off = [0]

        def carve(shape, dt=F32):
            n = 1
            for d in shape[1:]:
                n *= d
            nb = n * (4 if dt == F32 else 2)
            nb = (nb + 63) // 64 * 64
            o = _off[0]
            _off[0] += nb
            assert _off[0] <= ARENA_W * 4, ("arena overflow", _off[0])
            v = arena[:, o // 4:(o + nb) // 4]
            if dt != F32:
                v = v.bitcast(dt)
            v = v[:, 0:n]
            if len(shape) == 3:
                v = v.rearrange("p (a b) -> p a b", a=shape[1])
            elif len(shape) == 4:
                v = v.rearrange("p (a b c) -> p a b c", a=shape[1], b=shape[2])
            return v
        xt = [carve([128, 1024]) for _ in range(2)]
        hn = [carve([128, 1024], BF16) for _ in range(2)]
        hnT = [carve([128, 8, 128], BF16) for _ in range(2)]
        yT_off = _off[0]
        yT = [carve([128, 8, 128], BF16) for _ in range(2)]
        yg = [carve([128, 4, 128], BF16) for _ in range(2)]
        ygf = [carve([128, 4, 128]) for _ in range(2)]
        g1 = [carve([128, 128]) for _ in range(2)]
        g2 = [carve([128, 128]) for _ in range(2)]
        Gt = [carve([128, 8, 128]) for _ in range(2)]
        hn2b = [carve([128, 1024], BF16) for _ in range(2)]
        D1 = None
        M1_off = _off[0]
        M1 = [carve([128, 8, 128]) for _ in range(2)]
        M2 = [carve([128, 8, 128]) for _ in range(2)]
        XT = [carve([128, 8, 128]) for _ in range(2)]
        XTb = [carve([128, 8, 128]) for _ in range(2)]
        XA = [carve([128, 8, 128], BF16) for _ in range(2)]
        junk = carve([128, 1024], BF16)
        assert _off[0] >= 48 * 1024
        zT = [carve([128, 8, 144], BF16) for _ in range(2)]
        mixer_end = _off[0]
        _off[0] = 0
        WGs = [carve([128, 8, 256], BF16) for _ in range(2)]
        WUs = [carve([128, 8, 256], BF16) for _ in range(2)]
        WDs = [carve([128, 2, 1024], BF16) for _ in range(2)]
        sgT = [carve([128, 2, 512], BF16) for _ in range(2)]
        hT = [carve([128, 2, 512], BF16) for _ in range(2)]
        outt = [carve([128, 1024]) for _ in range(2)]
        NF = carve([128, 1024])
        junk2 = carve([128, 1024], BF16)
        assert _off[0] <= 48 * 1024
        pp = Gt[0].rearrange("p a b -> p (a b)")[:, 0:512].rearrange("p (a b) -> p a b", a=32)
        pq = XT[0].rearrange("p a b -> p (a b)")[:, 0:512].rearrange("p (a b) -> p a b", a=32)
        pz = acc[:].rearrange("p a b -> p (a b)").rearrange("p (a b) -> p a b", a=32)
        T1 = M1[0]
        T2 = M2[0]

        PB0 = ps("PB0", [128, 1024], BF16)
        PBs = [ps("PB%d" % i, [128, 512]) for i in range(1, 8)]

        res = {}

        def rs(name):
            if name not in res:
                res[name] = R(name)
            return res[name]

        def dma(out_ap, in_ap, rd, wr, slow=False, q="sp"):
            if slow:
                S.op(q, lambda e: e.dma_start(out=out_ap, in_=in_ap, allow_slow_non_contiguous=True), rd=rd, wr=wr, dma=wr[0])
            else:
                S.op(q, lambda e: e.dma_start(out=out_ap, in_=in_ap), rd=rd, wr=wr, dma=wr[0])

        def act(out_ap, in_ap, func, rd, wr, **kw):
            S.op("act", lambda e: e.activation(out=out_ap, in_=in_ap, func=func, **kw), rd=rd, wr=wr)

        def tt(eng, out_ap, a, b, op, rd, wr):
            S.op(eng, lambda e: e.tensor_tensor(out=out_ap, in0=a, in1=b, op=op), rd=rd, wr=wr)

        def tsc(out_ap, a, s1, s2, op0, op1, rd, wr):
            if s2 is None:
                S.op("dve", lambda e: e.tensor_scalar(out=out_ap, in0=a, scalar1=s1, scalar2=None, op0=op0), rd=rd, wr=wr)
            else:
                S.op("dve", lambda e: e.tensor_scalar(out=out_ap, in0=a, scalar1=s1, scalar2=s2, op0=op0, op1=op1), rd=rd, wr=wr)

        def stt(out_ap, a, s, b, op0, op1, rd, wr):
            S.op("dve", lambda e: e.scalar_tensor_tensor(out=out_ap, in0=a, scalar=s, in1=b, op0=op0, op1=op1), rd=rd, wr=wr)

        def cp(eng, out_ap, in_ap, rd, wr):
            S.op(eng, lambda e: e.tensor_copy(out=out_ap, in_=in_ap), rd=rd, wr=wr)

        def mm(out_ap, lhsT, rhs, start, stop, rd, wr):
            S.op("pe", lambda e: e.matmul(out_ap, lhsT=lhsT, rhs=rhs, start=start, stop=stop), rd=rd, wr=wr)

        def tr(out_ap, in_ap, rd, wr):
            S.op("pe", lambda e: e.transpose(out=out_ap, in_=in_ap, identity=ident[:]), rd=rd, wr=wr)

        def red(out_ap, in_ap, op, rd, wr):
            S.op("dve", lambda e: e.tensor_reduce(out=out_ap, in_=in_ap, axis=AX.X, op=op), rd=rd, wr=wr)

        def recip(out_ap, in_ap, rd, wr):
            S.op("dve", lambda e: e.reciprocal(out=out_ap, in_=in_ap), rd=rd, wr=wr)

        def smc(i, n=1):
            return sm[:, i:i + n]

        r_ = rs
        dma(cstt[:], cst[:, :], [], [r_("cstt")])
        dma(colt[:], cols[:, :], [], [r_("colt")])
        dma(RB[:], rows[0:1, 1024:1108].partition_broadcast(128), [], [r_("RB")])
        cp("dve", ident[:], cstt[:, 0:128], [r_("cstt")], [r_("ident")])
        JIDX = cstt[:, 128:256]
        SGN = cstt[:, 256:257]
        c_nm, c_nf, c_ps_, c_d, c_gb = 0, 8, 16, 20, 24

        accv = acc[:].rearrange("p a b -> p (a b)")
        for half in range(2):
            dma(accv[:, 0:4096].rearrange("p (k n) -> p k n", k=4),
                w_in[half * 512:(half + 1) * 512, :].rearrange("(k p) n -> p k n", p=128), [], [r_("acc")])
            for k in range(4):
                kk = half * 4 + k
                tsc(WI[:, kk, :], accv[:, k * 1024:(k + 1) * 1024], colt[:, c_nm + kk:c_nm + kk + 1], None, ALU.mult, None,
                    [r_("acc"), r_("colt")], [r_("WI")])
        for half in range(2):
            dma(accv[:, 0:4096].rearrange("p (k n) -> p k n", k=4),
                w_out[half * 512:(half + 1) * 512, :].rearrange("(k p) n -> p k n", p=128), [r_("WI")], [r_("acc")])
            for k in range(4):
                kk = half * 4 + k
                tsc(WO[:, kk, :], accv[:, k * 1024:(k + 1) * 1024], 1.0 if kk < 4 else 0.25, None, ALU.mult, None, [r_("acc")], [r_("WO")])
        dma(accv[:, 0:2048].rearrange("p (k n) -> p k n", k=4), glu_w[:, :].rearrange("(k p) n -> p k n", p=128), [r_("WO")], [r_("acc")])
        tsc(GW[:].rearrange("p k n -> p (k n)"), accv[:, 0:2048], 0.5, None, ALU.mult, None, [r_("acc")], [r_("GW")])
        dma(accv[:, 0:512].rearrange("p (g n) -> p g n", g=4), pool_w[:, :, :].rearrange("g p n -> p g n"), [r_("GW")], [r_("acc")])
        for gi, w in enumerate(WINS):
            tsc(PW[:, 2 * gi, :], accv[:, gi * 128:(gi + 1) * 128], float(1.0 / w - 1.0), None, ALU.mult, None, [r_("acc")], [r_("PW")])
            tsc(PW[:, 2 * gi + 1, :], accv[:, gi * 128:(gi + 1) * 128], float(1.0 / w), None, ALU.mult, None, [r_("acc")], [r_("PW")])
        dma(accv[:, 0:160].rearrange("p (k n) -> p k n", k=8), wr_d[:, :].rearrange("(k p) n -> p k n", p=128), [r_("PW")], [r_("acc")])
        for k in range(8):
            tsc(WRb[:, k, :], accv[:, k * 20:(k + 1) * 20], colt[:, c_nf + k:c_nf + k + 1], None, ALU.mult, None, [r_("acc"), r_("colt")], [r_("WRb")])

        spt = XTb[0][:, 0, 0:96]
        dma(spt, sp_s[:, :], [], [r_("spt")])
        dma(pp, b1_s[:, :, :], [], [r_("Gt")])
        dma(pq, b2_s[:, :, :], [], [r_("XT")])
        P = XTb[1].rearrange("p a b -> p (a b)")[:, 0:512].rearrange("p (a b) -> p a b", a=16)
        Rp = r_("P")
        LR, LI, LS = spt[:, 0:32], spt[:, 32:64], spt[:, 64:96]
        STEP, ARG, MG, CS, SN, AR, AI, DEN, QR, QI, TA, TB, KI = [P[:, i, :] for i in range(13)]
        KII = sb("KII", [128, 32], I32)

        def exp_to(dst, src, rd):
            act(TA, src, AF.Tanh, rd, [Rp], scale=0.5)
            tsc(TB, TA, -1.0, 1.0, ALU.mult, ALU.add, [Rp], [Rp])
            recip(TB, TB, [Rp], [Rp])
            tsc(TA, TA, 1.0, None, ALU.add, None, [Rp], [Rp])
            tt("dve", dst, TA, TB, ALU.mult, [Rp], [Rp])

        def sin_to(dst, src, shift):
            tsc(TA, src, float(shift), 1.0 / TWO_PI, ALU.add, ALU.mult, [Rp], [Rp])
            cp("dve", KII[:], TA, [Rp], [Rp])
            cp("dve", TB, KII[:], [Rp], [Rp])
            tt("dve", TA, TA, TB, ALU.subtract, [Rp], [Rp])
            act(dst, TA, AF.Sin, [Rp], [Rp], scale=TWO_PI)
        exp_to(STEP, LS, [r_("spt")])
        tt("dve", ARG, LI, STEP, ALU.mult, [Rp, r_("spt")], [Rp])
        tt("dve", MG, LR, STEP, ALU.mult, [Rp, r_("spt")], [Rp])
        cp("dve", P[:, 13, :], MG, [Rp], [Rp])
        exp_to(MG, MG, [Rp])
        cp("dve", MAG[:], MG, [Rp], [r_("MAG")])
        sin_to(SN, ARG, 0.0)
        sin_to(CS, ARG, math.pi / 2)
        tt("dve", AR, MG, CS, ALU.mult, [Rp], [Rp])
        tt("dve", AI, MG, SN, ALU.mult, [Rp], [Rp])
        tt("dve", DEN, LR, LR, ALU.mult, [Rp], [Rp])
        tt("dve", TA, LI, LI, ALU.mult, [Rp], [Rp])
        tt("dve", DEN, DEN, TA, ALU.add, [Rp], [Rp])
        recip(DEN, DEN, [Rp], [Rp])
        tsc(TA, AR, -1.0, None, ALU.add, None, [Rp], [Rp])
        tt("dve", QR, TA, LR, ALU.mult, [Rp], [Rp])
        tt("dve", TB, AI, LI, ALU.mult, [Rp], [Rp])
        tt("dve", QR, QR, TB, ALU.add, [Rp], [Rp])
        tt("dve", QR, QR, DEN, ALU.mult, [Rp], [Rp])
        tt("dve", QI, AI, LR, ALU.mult, [Rp], [Rp])
        tt("dve", TB, TA, LI, ALU.mult, [Rp], [Rp])
        tt("dve", QI, QI, TB, ALU.subtract, [Rp], [Rp])
        tt("dve", QI, QI, DEN, ALU.mult, [Rp], [Rp])
        tsc(QI, QI, SGN, -1.0, ALU.mult, ALU.mult, [Rp, r_("cstt")], [Rp])
        tt("dve", pp, pp, QR.unsqueeze(2).to_broadcast([128, 32, 16]), ALU.mult, [Rp, r_("Gt")], [r_("Gt")])
        tt("dve", pq, pq, QI.unsqueeze(2).to_broadcast([128, 32, 16]), ALU.mult, [Rp, r_("XT")], [r_("XT")])
        tt("dve", pp, pp, pq, ALU.add, [r_("XT")], [r_("Gt")])
        S.op("pool", lambda e: e.memset(pz, 0.0), wr=[r_("acc")])
        for gl in range(8):
            cp("dve", pz[:, gl::8, gl * 16:(gl + 1) * 16], pp[:, gl::8, :], [r_("Gt")], [r_("acc")])
        for g in range(32):
            cp("dve", pzb[:], pz[:, g, :], [r_("acc")], [r_("pzb")])
            tr(PB0[:, 0:128], pzb[:], [r_("pzb"), r_("ident")], [r_("PB0")])
            cp("dve", LBa[:, g, :], PB0[:, 0:128], [r_("PB0")], [r_("LBa")])
        cp("dve", LBb[:, :, 0:64], LBa[:, :, 64:128], [r_("LBa")], [r_("LBb")])
        cp("dve", LBb[:, :, 64:128], LBa[:, :, 0:64], [r_("LBa")], [r_("LBb")])
        dma(pq, c1_s[:, :, :], [r_("Gt")], [r_("XT")])
        tsc(pq, pq, SGN, None, ALU.mult, None, [r_("XT"), r_("cstt")], [r_("XT")])
        S.op("pool", lambda e: e.memset(pz, 0.0), rd=[], wr=[r_("acc")])
        for gl in range(8):
            cp("dve", pz[:, gl::8, gl * 16:(gl + 1) * 16], pq[:, gl::8, :], [r_("XT")], [r_("acc")])
        cp("dve", LC[:].rearrange("p a b -> p (a b)"), pz.rearrange("p a b -> p (a b)"), [r_("acc")], [r_("LC")])
        for t4 in range(4):
            tsc(Dg[:, t4, :], ident[:], colt[:, c_d + t4:c_d + t4 + 1], None, ALU.mult, None, [r_("ident"), r_("colt")], [r_("Dg")])
        SCR = [T1, T2]
        for g in range(32):
            sc = SCR[g % 2]
            rsc = r_("scr%d" % (g % 2))
            tsc(sc[:, 0, :], JIDX, P[:, 1, g:g + 1], 1.0 / TWO_PI, ALU.mult, ALU.mult, [Rp, r_("cstt")], [rsc])
            for ti_, (tab, shift) in enumerate(((SINM, 0.0), (COS, 0.25))):
                rs2 = r_("scr%d_%d" % (g % 2, ti_))
                a, b_, c = 1 + 3 * ti_, 2 + 3 * ti_, 3 + 3 * ti_
                tsc(sc[:, a, :], sc[:, 0, :], float(shift), None, ALU.add, None, [rsc], [rs2])
                cp("dve", sc[:, b_, :].bitcast(I32), sc[:, a, :], [rs2], [rs2])
                cp("dve", sc[:, c, :], sc[:, b_, :].bitcast(I32), [rs2], [rs2])
                tt("dve", sc[:, a, :], sc[:, a, :], sc[:, c, :], ALU.subtract, [rs2], [rs2])
                act(tab[:, g, :], sc[:, a, :], AF.Sin, [rs2], [r_("tab")], scale=TWO_PI)
        tsc(SINM[:].rearrange("p a b -> p (a b)"), SINM[:].rearrange("p a b -> p (a b)"), SGN, None, ALU.mult, None, [r_("tab"), r_("cstt")], [r_("tab")])
        _save = _off[0]
        _off[0] = yT_off
        BBR = carve([128, 32, 16])
        BBIs = carve([128, 32, 16])
        VTa = carve([128, 32, 128], BF16)
        VTb = carve([128, 32, 128], BF16)
        assert _off[0] <= M1_off
        _off[0] = _save
        ztok = [M1[1].rearrange("p a b -> p (a b)").bitcast(BF16)[:, 0:1024][:, i * 512:(i + 1) * 512] for i in range(2)]
        Ytmp = [M2[1].rearrange("p a b -> p (a b)")[:, i * 512:(i + 1) * 512] for i in range(2)]
        CARb = XA[1].rearrange("p a b -> p (a b)").bitcast(F32)[:, 0:32]
        Ssum = XA[1].rearrange("p a b -> p (a b)").bitcast(F32)[:, 32:64]
        Ctmp = XA[1].rearrange("p a b -> p (a b)").bitcast(F32)[:, 64:128]
        A128 = XA[1].rearrange("p a b -> p (a b)").bitcast(F32)[:, 128:192]
        bA = XT[1].rearrange("p a b -> p (a b)")[:, 0:512].rearrange("p (a b) -> p a b", a=32)
        bB = XT[1].rearrange("p a b -> p (a b)")[:, 512:1024].rearrange("p (a b) -> p a b", a=32)
        dma(bA, b2_s[:, :, :], [], [r_("bA")])
        dma(bB, b1_s[:, :, :], [], [r_("bB")])
        tt("dve", bA, bA, QR.unsqueeze(2).to_broadcast([128, 32, 16]), ALU.mult, [Rp, r_("bA")], [r_("bA")])
        tt("dve", bB, bB, QI.unsqueeze(2).to_broadcast([128, 32, 16]), ALU.mult, [Rp, r_("bB")], [r_("bB")])
        tt("dve", bA, bA, bB, ALU.subtract, [r_("bB")], [r_("bA")])
        cp("dve", BBR[0:64], pp[0:64], [r_("Gt")], [r_("BBR")])
        cp("dve", BBR[64:128], bA[64:128], [r_("bA")], [r_("BBR")])
        tsc(BBIs[0:64].rearrange("p a b -> p (a b)"), bA[0:64].rearrange("p a b -> p (a b)"), -1.0, None, ALU.mult, None, [r_("bA")], [r_("BBI")])
        cp("dve", BBIs[64:128], pp[64:128], [r_("Gt")], [r_("BBI")])
        S.barrier()
        SHC = sm[:, 70:71]
        tsc(SHC, SGN, 0.125, 0.125, ALU.mult, ALU.add, [r_("cstt")], [r_("shc")])
        JREV = cstt[:, 258:386]
        pzbs = [XA[0][:, 0, :], XA[0][:, 1, :]]
        for g in range(32):
            sc = SCR[g % 2]
            rsc = r_("vscr%d" % (g % 2))
            rpz = r_("pzbs%d" % (g % 2))
            tsc(sc[:, 0, :], JREV, P[:, 1, g:g + 1], 1.0 / TWO_PI, ALU.mult, ALU.mult, [Rp, r_("cstt"), r_("tab")], [rsc])
            tsc(sc[:, 1, :], sc[:, 0, :], SHC, None, ALU.add, None, [rsc, r_("shc")], [rsc])
            cp("dve", sc[:, 2, :].bitcast(I32), sc[:, 1, :], [rsc], [rsc])
            cp("dve", sc[:, 3, :], sc[:, 2, :].bitcast(I32), [rsc], [rsc])
            tt("dve", sc[:, 1, :], sc[:, 1, :], sc[:, 3, :], ALU.subtract, [rsc], [rsc])
            act(sc[:, 4, :], sc[:, 1, :], AF.Sin, [rsc], [r_("vsb%d" % (g % 2))], scale=TWO_PI)
            act(sc[:, 5, :], JREV, AF.Exp, [r_("cstt"), Rp, rsc], [r_("vsc%d" % (g % 2))], scale=P[:, 13, g:g + 1])
            tt("dve", pzbs[g % 2], sc[:, 4, :], sc[:, 5, :], ALU.mult, [r_("vsb%d" % (g % 2)), r_("vsc%d" % (g % 2))], [rpz])
            tr(PB0[:, (g % 2) * 128:(g % 2 + 1) * 128], pzbs[g % 2], [rpz, r_("ident")], [r_("PB0")])
            cp("dve", VTa[:, g, :], PB0[:, (g % 2) * 128:(g % 2 + 1) * 128], [r_("PB0")], [r_("VTa")])
        cp("dve", VTb[:, :, 0:64], VTa[:, :, 64:128], [r_("VTa")], [r_("VTb")])
        cp("dve", VTb[:, :, 64:128], VTa[:, :, 0:64], [r_("VTa")], [r_("VTb")])
        act(Ctmp[:, 0:32], P[:, 13, :], AF.Exp, [Rp], [r_("ctmp")], scale=128.0)
        tt("dve", A128[:, 0:32], Ctmp[:, 0:32], COS[:, :, 127], ALU.mult, [r_("ctmp"), r_("tab")], [r_("A128")])
        tt("dve", A128[:, 32:64], Ctmp[:, 0:32], SINM[:, :, 127], ALU.mult, [r_("ctmp"), r_("tab")], [r_("A128")])
        tsc(A128[:, 32:64], A128[:, 32:64], -1.0, None, ALU.mult, None, [r_("A128")], [r_("A128")])
        S.op("pool", lambda e: e.memset(CARb, 0.0), wr=[r_("CARb")])
        S.barrier()
        LCs = XTb[0].rearrange("p a b -> p (a b)").bitcast(BF16)[:, 0:2048]
        LCs2 = XTb[1].rearrange("p a b -> p (a b)").bitcast(BF16)[:, 0:2048]
        dma(pq, c2_s[:, :, :], [], [r_("XT")])
        tsc(pq, pq, SGN, -1.0, ALU.mult, ALU.mult, [r_("XT"), r_("cstt")], [r_("XT")])
        S.op("pool", lambda e: e.memset(pz, 0.0), rd=[], wr=[r_("acc")])
        for gl in range(8):
            cp("dve", pz[:, gl::8, gl * 16:(gl + 1) * 16], pq[:, gl::8, :], [r_("XT")], [r_("acc")])
        pzf = pz.rearrange("p a b -> p (a b)")
        cp("dve", LCs, pzf[:, 0:2048], [r_("acc")], [r_("LCs")])
        cp("dve", LCs2, pzf[:, 2048:4096], [r_("acc")], [r_("LCs")])
        LCsv = [LCs.rearrange("p (g c) -> p g c", g=16), LCs2.rearrange("p (g c) -> p g c", g=16)]
        S.op("pool", lambda e: e.memset(CAR[:], 0.0), wr=[r_("CAR")])
        for p_ in range(2):
            S.op("pool", lambda e, p_=p_: e.memset(zT[p_], 0.0), wr=[r_("zT%d" % p_)])
        tsc(sm[:, 64:68], colt[:, c_gb:c_gb + 4], 0.5, None, ALU.mult, None, [r_("colt")], [r_("sm2")])

        def rms(src_ap, dst_bf, srcres, dstres, col, jk):
            S.op("dve", lambda e: e.memset(smc(col), 0.0), wr=[r_("sm%d" % col)])
            act(jk, src_ap, AF.Square, srcres, [r_("junk"), r_("sm%d" % col)], accum_out=smc(col))
            act(smc(col + 1), smc(col), AF.Sqrt, [r_("sm%d" % col)], [r_("sm%d" % col)], scale=1.0 / 1024.0, bias=EPS)
            recip(smc(col + 2), smc(col + 1), [r_("sm%d" % col)], [r_("sm%d" % col)])
            act(dst_bf, src_ap, AF.Copy, srcres + [r_("sm%d" % col)], dstres, scale=smc(col + 2))

        def rms_a(src_ap, srcres, col, jk, jkres):
            S.op("dve", lambda e: e.memset(smc(col), 0.0), wr=[r_("sm%d" % col)])
            act(jk, src_ap, AF.Square, srcres, [jkres, r_("sm%d" % col)], accum_out=smc(col))
            act(smc(col + 1), smc(col), AF.Sqrt, [r_("sm%d" % col)], [r_("sm%d" % col)], scale=1.0 / 1024.0, bias=EPS)

        def rms_b(src_ap, dst_bf, srcres, dstres, col):
            recip(smc(col + 2), smc(col + 1), [r_("sm%d" % col)], [r_("sm%d" % col)])
            act(dst_bf, src_ap, AF.Copy, srcres + [r_("sm%d" % col)], dstres, scale=smc(col + 2))

        def transp8(src_bf, srcres):
            for k in range(8):
                tr(PB0[:, k * 128:(k + 1) * 128], src_bf[:, k * 128:(k + 1) * 128], srcres + [r_("ident")], [r_("PB0")])

        tcnt = [0]

        def ssm_state(tau, full, p):
            g0 = tau * 8
            q = tau % 2
            ba, bb = PBs[2 + q * 2], PBs[3 + q * 2]
            rba, rbb = r_("bank%d" % (2 + q * 2)), r_("bank%d" % (3 + q * 2))
            bav = ba[:].rearrange("p (g t) -> p g t", g=4)
            bbv = bb[:].rearrange("p (g t) -> p g t", g=4)
            rz = r_("zT%d" % p)
            rG = r_("Gt%d" % q)
            for hh in range(2):
                for gl4 in range(4):
                    g = g0 + hh * 4 + gl4
                    mm(bav[:, gl4, :], LBa[:, g, :], zT[p][:, 4 + tau, 16:144], True, True, [r_("LBa"), rz], [rba])
                    mm(bbv[:, gl4, :], LBb[:, g, :], zT[p][:, 4 + tau, 16:144], True, True, [r_("LBb"), rz], [rbb])
                sl = slice(hh * 4, hh * 4 + 4)
                gs = slice(g0 + hh * 4, g0 + hh * 4 + 4)
                rD = r_("D1%d" % hh)
                tt("dve", Gt[q][:, sl, :], bav, COS[:, gs, :], ALU.mult, [rba, r_("tab")], [rG])
                tt("dve", D1[hh][:], bbv, SINM[:, gs, :], ALU.mult, [rbb, r_("tab")], [rD])
                tt("pool", Gt[q][:, sl, :], Gt[q][:, sl, :], D1[hh][:], ALU.add, [rD], [rG])
                yield
            rX = r_("XT%d" % q)
            for gl in range(8):
                g = g0 + gl
                S.op("dve", lambda e, gl=gl, g=g, q=q: e.tensor_tensor_scan(
                    out=XT[q][:, gl, :], data0=MAG[:, g:g + 1].to_broadcast([128, 128]), data1=Gt[q][:, gl, :],
                    initial=CAR[:, g:g + 1], op0=ALU.mult, op1=ALU.add),
                    rd=[rG, r_("MAG"), r_("CAR")], wr=[rX])
            yield
            gs = slice(g0, g0 + 8)
            rXb, rXb2 = r_("XTb%d" % q), r_("XTc%d" % q)
            rM1, rM2 = r_("M1%d" % q), r_("M2%d" % q)
            if full:
                dma(XTb[q][0:64, :, :], XT[q][64:128, :, :], [rX], [rXb])
                dma(XTb[q][64:128, :, :], XT[q][0:64, :, :], [rX], [rXb2])
                tt("dve", M1[q], XT[q], COS[:, gs, :], ALU.mult, [rX, r_("tab")], [rM1])
                tt("pool", M2[q], XTb[q], SINM[:, gs, :], ALU.mult, [rXb, rXb2, r_("tab")], [rM2])
                tt("pool", XA[q], M1[q], M2[q], ALU.subtract, [rM1, rM2], [r_("XA%d" % q)])
                tt("pool", CAR[:, gs], M1[q][:, :, 127], M2[q][:, :, 127], ALU.subtract, [rM1, rM2], [r_("CAR")])
            else:
                dma(XTb[q][0:64, :, 127:128], XT[q][64:128, :, 127:128], [rX], [rXb], slow=True)
                dma(XTb[q][64:128, :, 127:128], XT[q][0:64, :, 127:128], [rX], [rXb2], slow=True)
                tt("pool", M1[q][:, :, 127], XT[q][:, :, 127], COS[:, gs, 127], ALU.mult, [rX, r_("tab")], [rM1])
                tt("pool", M2[q][:, :, 127], XTb[q][:, :, 127], SINM[:, gs, 127], ALU.mult, [rXb, rXb2, r_("tab")], [rM2])
                tt("pool", CAR[:, gs], M1[q][:, :, 127], M2[q][:, :, 127], ALU.subtract, [rM1, rM2], [r_("CAR")])
            yield

        def front(tile_idx, mlist, p):
            rz, rzo = r_("zT%d" % p), r_("zT%d" % (1 - p))
            rx, rh, rhT = r_("xt%d" % p), r_("hn%d" % p), r_("hnT%d" % p)
            dma(xt[p], xall[tile_idx * 128:(tile_idx + 1) * 128, :], [], [rx])
            rms(xt[p], hn[p], [rx], [rh], 0, junk)
            transp8(hn[p], [rh])
            act(hnT[p].rearrange("p k t -> p (k t)"), PB0[:], AF.Copy, [r_("PB0")], [rhT])
            yield
            cp("pool", zT[p][:, 0:4, 0:16], zT[1 - p][:, 0:4, 128:144], [rzo], [rz])
            for m in mlist:
                pb = PBs[0] if m < 4 else PBs[1]
                rpb = r_("bank%d" % (m // 4))
                o = pb[:, (m % 4) * 128:(m % 4 + 1) * 128]
                for k in range(8):
                    mm(o, WI[:, k, m * 128:(m + 1) * 128], hnT[p][:, k, :], k == 0, k == 7, [r_("WI"), rhT], [rpb])
            if 0 in mlist:
                act(zT[p][:, 0:4, 16:144], PBs[0][:].rearrange("p (m t) -> p m t", m=4), AF.Copy, [r_("bank0")], [rz])
            if 4 in mlist:
                act(zT[p][:, 4:8, 16:144], PBs[1][:].rearrange("p (m t) -> p m t", m=4), AF.Copy, [r_("bank1")], [rz])
            yield "F"

        PQ = PBs[6]

        def mixer(tile_idx, lt, first, p):
            rz = r_("zT%d" % p)
            ryT, ryg, rygf = r_("yT%d" % p), r_("yg%d" % p), r_("ygf%d" % p)
            yield from front(tile_idx, list(range(8)), p)
            for gi, w in enumerate(WINS):
                o = PQ[:, gi * 128:(gi + 1) * 128]
                for l in range(w):
                    mm(o, PW[:, 2 * gi + (1 if l > 0 else 0), :], zT[p][:, gi, 16 - l:144 - l], l == 0, (l == w - 1) and not first, [r_("PW"), rz], [r_("bank6")])
                if first:
                    S.op("dve", lambda e, gi=gi: e.tensor_tensor_scan(
                        out=cf[:, 0:16], data0=cstt[:, 257:258].to_broadcast([128, 16]), data1=zT[p][:, gi, 16:32],
                        initial=0.0, op0=ALU.mult, op1=ALU.add), rd=[rz, r_("cstt")], wr=[r_("cf")])
                    tt("dve", cf[:, 16:32], cf[:, 0:16], RB[:, 20 + gi * 16:36 + gi * 16], ALU.mult, [r_("cf"), r_("RB")], [r_("cf2")])
                    cp("dve", pzb[:, 0:16], cf[:, 16:32], [r_("cf2")], [r_("pzb")])
                    mm(o[:, 0:16], PW[:, 2 * gi + 1, :], pzb[:, 0:16], False, True, [r_("PW"), r_("pzb")], [r_("bank6")])
            for gi in range(4):
                act(yT[p][:, gi, :], PQ[:, gi * 128:(gi + 1) * 128], AF.Copy, [r_("bank6")], [ryT], scale=colt[:, c_ps_ + gi:c_ps_ + gi + 1])
            yield
            for tau in range(4):
                q = tau % 2
                yield from ssm_state(tau, True, p)
                o = PBs[0][:, tau * 128:(tau + 1) * 128]
                ry = r_("bank0")
                for gl in range(8):
                    mm(o, LC[:, tau * 8 + gl, :], XA[q][:, gl, :], gl == 0, False, [r_("LC"), r_("XA%d" % q)], [ry])
                mm(o, Dg[:, tau, :], zT[p][:, 4 + tau, 16:144], False, True, [r_("Dg"), rz], [ry])
                rg1, rg2 = r_("g1%d" % q), r_("g2%d" % q)
                act(g1[q], o, AF.Square, [ry], [rg1])
                tsc(g1[q], g1[q], 0.044715, 1.0, ALU.mult, ALU.add, [rg1], [rg1])
                tt("dve", g1[q], g1[q], o, ALU.mult, [rg1, ry], [rg1])
                act(g2[q], g1[q], AF.Tanh, [rg1], [rg2], scale=0.7978845608028654)
                tsc(g2[q], g2[q], 0.5, 0.5, ALU.mult, ALU.add, [rg2], [rg2])
                tt("dve", ygf[p][:, tau, :], g2[q], o, ALU.mult, [rg2, ry], [rygf])
                cp("pool", yg[p][:, tau, :], ygf[p][:, tau, :], [rygf], [ryg])
                yield
            for m in range(4):
                q = m % 2
                rg1 = r_("g1%d" % q)
                o = PBs[1][:, m * 128:(m + 1) * 128]
                for k in range(4):
                    mm(o, GW[:, k, m * 128:(m + 1) * 128], yg[p][:, k, :], k == 0, k == 3, [r_("GW"), ryg], [r_("bank1")])
                act(g1[q], o, AF.Tanh, [r_("bank1"), r_("sm2")], [rg1], scale=0.5, bias=sm[:, 64 + m:65 + m])
                tsc(g1[q], g1[q], 0.5, 0.5, ALU.mult, ALU.add, [rg1], [rg1])
                tt("pool", yT[p][:, 4 + m, :], g1[q], ygf[p][:, m, :], ALU.mult, [rg1, rygf], [ryT])
            yield
            for half in range(2):
                pb = PBs[half]
                rpb = r_("bank%d" % half)
                for k in range(8):
                    mm(pb[:], yT[p][:, k, :], WO[:, k, half * 512:(half + 1) * 512], k == 0, k == 7, [ryT, r_("WO")], [rpb])
                tt("dve", acc[:, lt, half * 512:(half + 1) * 512], pb[:], xt[p][:, half * 512:(half + 1) * 512], ALU.add, [rpb, r_("xt%d" % p)], [r_("acc%d" % lt)])
            yield
            rh = r_("hn%d" % p)
            rms(acc[:, lt, :], hn[p], [r_("acc%d" % lt)], [rh], 4, junk)
            transp8(hn[p], [rh])
            act(hn2T[:, :, lt * 128:(lt + 1) * 128], PB0[:].rearrange("p (k t) -> p k t", k=8), AF.Copy, [r_("PB0")], [r_("hn2T")])
            yield
            o = PQ[:, 0:20]
            for k in range(8):
                mm(o, hn2T[:, k, lt * 128:(lt + 1) * 128], WRb[:, k, :], k == 0, k == 7, [r_("hn2T"), r_("WRb")], [r_("bank6")])
            L_ = sm[:, 100:120]
            rS = r_("smr")
            tt("dve", L_, o, RB[:, 0:20], ALU.add, [r_("bank6"), r_("RB")], [rS])
            cL, fL = sm[:, 100:104], sm[:, 104:120]
            M, GM, CM, TH, NUM, SS, PG = smc(120), smc(121, 4), smc(125, 4), smc(129, 4), smc(133, 4), smc(137), smc(138)
            red(M, cL, ALU.max, [rS], [rS])
            tsc(GM, cL, M, None, ALU.is_equal, None, [rS], [rS])
            tsc(CM, cL, M, None, ALU.subtract, None, [rS], [rS])
            act(TH, CM, AF.Tanh, [rS], [rS], scale=0.5)
            tsc(NUM, TH, 1.0, None, ALU.add, None, [rS], [rS])
            tsc(TH, TH, -1.0, 1.0, ALU.mult, ALU.add, [rS], [rS])
            recip(TH, TH, [rS], [rS])
            tt("dve", NUM, NUM, TH, ALU.mult, [rS], [rS])
            red(SS, NUM, ALU.add, [rS], [rS])
            recip(PG, SS, [rS], [rS])
            FT = sm[:, 140:156]
            tt("dve", FT.rearrange("p (g j) -> p g j", g=4), fL.rearrange("p (g j) -> p g j", g=4),
               GM.unsqueeze(2).to_broadcast([128, 4, 4]), ALU.mult, [rS], [rS])
            FS = smc(156, 4)
            red(FS, FT.rearrange("p (g j) -> p j g", g=4), ALU.add, [rS], [rS])
            M1_, K1, F2, M2_, K2, DD, W1, W2, WJ = smc(160), smc(161, 4), smc(165, 4), smc(169), smc(170, 4), smc(174), smc(175), smc(176), smc(177, 4)
            red(M1_, FS, ALU.max, [rS], [rS])
            tsc(K1, FS, M1_, None, ALU.is_equal, None, [rS], [rS])
            stt(F2, K1, -1e30, FS, ALU.mult, ALU.add, [rS], [rS])
            red(M2_, F2, ALU.max, [rS], [rS])
            tsc(K2, F2, M2_, None, ALU.is_equal, None, [rS], [rS])
            tt("dve", DD, M2_, M1_, ALU.subtract, [rS], [rS])
            act(DD, DD, AF.Tanh, [rS], [rS], scale=0.5)
            tsc(W1, DD, -0.5, 0.5, ALU.mult, ALU.add, [rS], [rS])
            tsc(W2, DD, 0.5, 0.5, ALU.mult, ALU.add, [rS], [rS])
            tsc(WJ, K1, W1, None, ALU.mult, None, [rS], [rS])
            stt(WJ, K2, W2, WJ, ALU.mult, ALU.add, [rS], [rS])
            tsc(WJ, WJ, PG, None, ALU.mult, None, [rS], [rS])
            tt("dve", gates[:, lt, :].rearrange("p (g j) -> p g j", g=4), GM.unsqueeze(2).to_broadcast([128, 4, 4]),
               WJ.unsqueeze(1).to_broadcast([128, 4, 4]), ALU.mult, [rS], [r_("gates")])
            yield

        def router(lt, o, rq0):
            L_ = sm[:, 100:120]
            rS = r_("smr")
            tt("dve", L_, o, RB[:, 0:20], ALU.add, [rq0, r_("RB")], [rS])
            cL, fL = sm[:, 100:104], sm[:, 104:120]
            M, GM, CM, TH, NUM, SS, PG = smc(120), smc(121, 4), smc(125, 4), smc(129, 4), smc(133, 4), smc(137), smc(138)
            red(M, cL, ALU.max, [rS], [rS])
            tsc(GM, cL, M, None, ALU.is_equal, None, [rS], [rS])
            tsc(CM, cL, M, None, ALU.subtract, None, [rS], [rS])
            act(TH, CM, AF.Tanh, [rS], [rS], scale=0.5)
            tsc(NUM, TH, 1.0, None, ALU.add, None, [rS], [rS])
            tsc(TH, TH, -1.0, 1.0, ALU.mult, ALU.add, [rS], [rS])
            recip(TH, TH, [rS], [rS])
            tt("dve", NUM, NUM, TH, ALU.mult, [rS], [rS])
            red(SS, NUM, ALU.add, [rS], [rS])
            recip(PG, SS, [rS], [rS])
            FT = sm[:, 140:156]
            tt("dve", FT.rearrange("p (g j) -> p g j", g=4), fL.rearrange("p (g j) -> p g j", g=4),
               GM.unsqueeze(2).to_broadcast([128, 4, 4]), ALU.mult, [rS], [rS])
            FS = smc(156, 4)
            red(FS, FT.rearrange("p (g j) -> p j g", g=4), ALU.add, [rS], [rS])
            M1_, K1, F2, M2_, K2, DD, W1, W2, WJ = smc(160), smc(161, 4), smc(165, 4), smc(169), smc(170, 4), smc(174), smc(175), smc(176), smc(177, 4)
            red(M1_, FS, ALU.max, [rS], [rS])
            tsc(K1, FS, M1_, None, ALU.is_equal, None, [rS], [rS])
            stt(F2, K1, -1e30, FS, ALU.mult, ALU.add, [rS], [rS])
            red(M2_, F2, ALU.max, [rS], [rS])
            tsc(K2, F2, M2_, None, ALU.is_equal, None, [rS], [rS])
            tt("dve", DD, M2_, M1_, ALU.subtract, [rS], [rS])
            act(DD, DD, AF.Tanh, [rS], [rS], scale=0.5)
            tsc(W1, DD, -0.5, 0.5, ALU.mult, ALU.add, [rS], [rS])
            tsc(W2, DD, 0.5, 0.5, ALU.mult, ALU.add, [rS], [rS])
            tsc(WJ, K1, W1, None, ALU.mult, None, [rS], [rS])
            stt(WJ, K2, W2, WJ, ALU.mult, ALU.add, [rS], [rS])
            tsc(WJ, WJ, PG, None, ALU.mult, None, [rS], [rS])
            tt("dve", gates[:, lt, :].rearrange("p (g j) -> p g j", g=4), GM.unsqueeze(2).to_broadcast([128, 4, 4]),
               WJ.unsqueeze(1).to_broadcast([128, 4, 4]), ALU.mult, [rS], [r_("gates")])

        RRs = [M2[0], M2[1]]

        def build_RR(tau, q):
            gs = slice(tau * 8, tau * 8 + 8)
            act(RRs[q], MAG[:, gs].unsqueeze(2).to_broadcast([128, 8, 128]), AF.Copy, [r_("MAG")], [r_("RR%d" % q)])
            S.op("pool", lambda e: e.memset(RRs[q][:, :, 0:1], 0.0), rd=[], wr=[r_("RR%d" % q)])

        def run_gen(g):
            for _ in g:
                pass

        def g_front(tile_idx, lt, p):
            rz, rzo = r_("zT%d" % p), r_("zT%d" % (1 - p))
            rx, rh, rhT = r_("xt%d" % p), r_("hn%d" % p), r_("hnT%d" % p)
            dma(xt[p], xall[tile_idx * 128:(tile_idx + 1) * 128, :], [], [rx])
            dma(acc[:, lt, :], xall[tile_idx * 128:(tile_idx + 1) * 128, :], [], [r_("acc%d" % lt)])
            rms_a(xt[p], [rx], 0, hn[p], rh)
            yield
            rms_b(xt[p], hn[p], [rx], [rh], 0)
            yield
            transp8(hn[p], [rh])
            act(hnT[p].rearrange("p k t -> p (k t)"), PB0[:], AF.Copy, [r_("PB0")], [rhT])
            yield
            cp("pool", zT[p][:, 0:4, 0:16], zT[1 - p][:, 0:4, 128:144], [rzo], [rz])
            for m in range(8):
                pb = PBs[0] if m < 4 else PBs[1]
                rpb = r_("bank%d" % (m // 4))
                o = pb[:, (m % 4) * 128:(m % 4 + 1) * 128]
                for k in range(8):
                    mm(o, WI[:, k, m * 128:(m + 1) * 128], hnT[p][:, k, :], k == 0, k == 7, [r_("WI"), rhT], [rpb])
                if m == 3:
                    act(zT[p][:, 0:4, 16:144], PBs[0][:].rearrange("p (m t) -> p m t", m=4), AF.Copy, [r_("bank0")], [rz])
            act(zT[p][:, 4:8, 16:144], PBs[1][:].rearrange("p (m t) -> p m t", m=4), AF.Copy, [r_("bank1")], [rz])
            yield

        def st_pool(p, first):
            rz, ryT = r_("zT%d" % p), r_("yT%d" % p)
            rq = r_("bank6")
            for gi, w in enumerate(WINS):
                o = PQ[:, gi * 128:(gi + 1) * 128]
                for l in range(w):
                    mm(o, PW[:, 2 * gi + (1 if l > 0 else 0), :], zT[p][:, gi, 16 - l:144 - l], l == 0, (l == w - 1) and not first, [r_("PW"), rz], [rq])
                if first:
                    S.op("dve", lambda e, gi=gi: e.tensor_tensor_scan(
                        out=cf[:, 0:16], data0=cstt[:, 257:258].to_broadcast([128, 16]), data1=zT[p][:, gi, 16:32],
                        initial=0.0, op0=ALU.mult, op1=ALU.add), rd=[rz, r_("cstt")], wr=[r_("cf")])
                    tt("dve", cf[:, 16:32], cf[:, 0:16], RB[:, 20 + gi * 16:36 + gi * 16], ALU.mult, [r_("cf"), r_("RB")], [r_("cf2")])
                    cp("dve", pzb[:, 0:16], cf[:, 16:32], [r_("cf2")], [r_("pzb")])
                    mm(o[:, 0:16], PW[:, 2 * gi + 1, :], pzb[:, 0:16], False, True, [r_("PW"), r_("pzb")], [rq])
            for gi in range(4):
                act(yT[p][:, gi, :], PQ[:, gi * 128:(gi + 1) * 128], AF.Copy, [rq], [ryT], scale=colt[:, c_ps_ + gi:c_ps_ + gi + 1])

        def st_Dp(p, tau):
            g0 = tau * 8
            rz = r_("zT%d" % p)
            for hh in range(2):
                ba, bb = PBs[2 + hh * 2], PBs[3 + hh * 2]
                rba, rbb = r_("bank%d" % (2 + hh * 2)), r_("bank%d" % (3 + hh * 2))
                bav = ba[:].rearrange("p (g t) -> p g t", g=4)
                bbv = bb[:].rearrange("p (g t) -> p g t", g=4)
                for gl4 in range(4):
                    g = g0 + hh * 4 + gl4
                    mm(bav[:, gl4, :], LBa[:, g, :], zT[p][:, 4 + tau, 16:144], True, True, [r_("LBa"), rz], [rba])
                    mm(bbv[:, gl4, :], LBb[:, g, :], zT[p][:, 4 + tau, 16:144], True, True, [r_("LBb"), rz], [rbb])

        def st_Dd(tau, q):
            g0 = tau * 8
            rG, rM1 = r_("Gt%d" % q), r_("M1t")
            for hh in range(2):
                ba, bb = PBs[2 + hh * 2], PBs[3 + hh * 2]
                rba, rbb = r_("bank%d" % (2 + hh * 2)), r_("bank%d" % (3 + hh * 2))
                bav = ba[:].rearrange("p (g t) -> p g t", g=4)
                bbv = bb[:].rearrange("p (g t) -> p g t", g=4)
                sl = slice(hh * 4, hh * 4 + 4)
                gs = slice(g0 + hh * 4, g0 + hh * 4 + 4)
                tt("dve", Gt[q][:, sl, :], bav, COS[:, gs, :], ALU.mult, [rba, r_("tab")], [rG])
                tt("dve", M1[0][:, sl, :], bbv, SINM[:, gs, :], ALU.mult, [rbb, r_("tab")], [rM1])
                tt("dve", Gt[q][:, sl, :], Gt[q][:, sl, :], M1[0][:, sl, :], ALU.add, [rM1], [rG])

        def st_S(tau, q):
            g0 = tau * 8
            gs = slice(g0, g0 + 8)
            rG, rX = r_("Gt%d" % q), r_("XT%d" % q)
            c8 = sm[:, 80:88]
            tt("dve", c8, MAG[:, gs], CAR[:, gs], ALU.mult, [r_("MAG"), r_("CAR")], [r_("c8")])
            tt("dve", Gt[q][:, :, 0], Gt[q][:, :, 0], c8, ALU.add, [r_("c8")], [rG])
            S.op("dve", lambda e, q=q: e.tensor_tensor_scan(
                out=XT[q].rearrange("p a b -> p (a b)"), data0=RRs[q].rearrange("p a b -> p (a b)"),
                data1=Gt[q].rearrange("p a b -> p (a b)"), initial=0.0, op0=ALU.mult, op1=ALU.add),
                rd=[rG, r_("RR%d" % q)], wr=[rX])
            xs = sm[:, 16 + 8 * q:24 + 8 * q]
            dma(xs[0:64, :], XT[q][64:128, :, 127], [rX], [r_("xs%d" % q)], slow=True)
            dma(xs[64:128, :], XT[q][0:64, :, 127], [rX], [r_("xsb%d" % q)], slow=True)
            build_RR((tau + 2) % 4, q)

        M1b = [XA[0], XA[1]]
        M2b = [M1[1].rearrange("p a b -> p (a b)").bitcast(BF16)[:, i * 1024:(i + 1) * 1024].rearrange("p (a b) -> p a b", a=8) for i in range(2)]

        def st_M(tau, q):
            g0 = tau * 8
            gs = slice(g0, g0 + 8)
            rX = r_("XT%d" % q)
            tt("dve", M1b[q], XT[q], COS[:, gs, :], ALU.mult, [rX, r_("tab")], [r_("XA%d" % q)])
            tt("dve", M2b[q], XT[q], SINM[:, gs, :], ALU.mult, [rX, r_("tab")], [r_("M2b%d" % q)])
            xs = sm[:, 16 + 8 * q:24 + 8 * q]
            t1c = sm[:, 32 + 8 * q:40 + 8 * q]
            tt("dve", t1c, XT[q][:, :, 127], COS[:, gs, 127], ALU.mult, [rX, r_("tab")], [r_("t1c%d" % q)])
            tt("dve", xs, xs, SINM[:, gs, 127], ALU.mult, [r_("xs%d" % q), r_("xsb%d" % q), r_("tab")], [r_("xs%d" % q), r_("xsb%d" % q)])
            tt("dve", CAR[:, gs], t1c, xs, ALU.subtract, [r_("t1c%d" % q), r_("xs%d" % q), r_("xsb%d" % q)], [r_("CAR")])

        def st_Cp(p, tau, q):
            rz = r_("zT%d" % p)
            o = PQ[:, tau * 128:(tau + 1) * 128]
            ry = r_("bank6")
            for gl in range(8):
                g = tau * 8 + gl
                mm(o, LC[:, g, :], M1b[q][:, gl, :], gl == 0, False, [r_("LC"), r_("XA%d" % q)], [ry])
                mm(o, LCsv[g // 16][:, g % 16, :], M2b[q][:, gl, :], False, False, [r_("LCs"), r_("M2b%d" % q)], [ry])
            mm(o, Dg[:, tau, :], zT[p][:, 4 + tau, 16:144], False, True, [r_("Dg"), rz], [ry])
            act(g1[q], o, AF.Square, [ry], [r_("g1%d" % q)], scale=0.21145921592590347)

        def st_Ca(p, tau, q):
            o = PQ[:, tau * 128:(tau + 1) * 128]
            ry = r_("bank6")
            rg1, rg2 = r_("g1%d" % q), r_("g2%d" % q)
            stt(g1[q], g1[q], 1.0, o, ALU.add, ALU.mult, [rg1, ry], [rg1])
            act(g2[q], g1[q], AF.Tanh, [rg1], [rg2], scale=0.7978845608028654)

        def st_Cb(p, tau, q):
            ryg, rygf = r_("yg%d" % p), r_("ygf%d" % p)
            o = PQ[:, tau * 128:(tau + 1) * 128]
            ry = r_("bank6")
            rg2 = r_("g2%d" % q)
            stt(ygf[p][:, tau, :], g2[q], 1.0, o, ALU.add, ALU.mult, [rg2, ry], [rygf])
            act(yg[p][:, tau, :], ygf[p][:, tau, :], AF.Copy, [rygf], [ryg])

        def g_tail(lt, p):
            ryT, ryg, rygf = r_("yT%d" % p), r_("yg%d" % p), r_("ygf%d" % p)
            jf = junk.bitcast(F32)
            glt = [jf[:, i * 128:(i + 1) * 128] for i in range(4)]
            rgl = [r_("glt%d" % i) for i in range(4)]
            rbk = [r_("bank1"), r_("bank0")]
            for m in range(4):
                pbm = PBs[1] if m % 2 == 0 else PBs[0]
                rb = rbk[m % 2]
                o = pbm[:, (m // 2) * 128:(m // 2 + 1) * 128]
                for k in range(4):
                    mm(o, GW[:, k, m * 128:(m + 1) * 128], yg[p][:, k, :], k == 0, k == 3, [r_("GW"), ryg], [rb])
                act(glt[m], o, AF.Tanh, [rb, r_("sm2")], [rgl[m]], scale=0.5, bias=sm[:, 64 + m:65 + m])
            yield
            for m in range(4):
                stt(yT[p][:, 4 + m, :], glt[m], 1.0, ygf[p][:, m, :], ALU.add, ALU.mult, [rgl[m], rygf], [ryT])
            for half in range(2):
                pb = PBs[half]
                rqs = [r_("bank%d" % half)]
                for k in range(8):
                    mm(pb[:], yT[p][:, k, :], WO[:, k, half * 512:(half + 1) * 512], k == 0, k == 7, [ryT, r_("WO")], rqs)
            yield
            for half in range(2):
                pb = PBs[half]
                rqs = [r_("bank%d" % half)]
                tt("dve", acc[:, lt, half * 512:(half + 1) * 512], pb[:], acc[:, lt, half * 512:(half + 1) * 512], ALU.add, rqs + [r_("acc%d" % lt)], [r_("acc%d" % lt)])
            rh = r_("hn2b%d" % p)
            rms_a(acc[:, lt, :], [r_("acc%d" % lt)], 4, hn2b[p], rh)
            yield
            rms_b(acc[:, lt, :], hn2b[p], [r_("acc%d" % lt)], [rh], 4)
            yield
            transp8(hn2b[p], [rh])
            act(hn2T[:, :, lt * 128:(lt + 1) * 128], PB0[:].rearrange("p (k t) -> p k t", k=8), AF.Copy, [r_("PB0")], [r_("hn2T")])
            yield
            o = PQ[:, 0:20]
            rq0 = r_("bank6")
            for k in range(8):
                mm(o, hn2T[:, k, lt * 128:(lt + 1) * 128], WRb[:, k, :], k == 0, k == 7, [r_("hn2T"), r_("WRb")], [rq0])
            router(lt, o, rq0)
            yield

        def mixer_sb(sbi):
            nun = SBT * 4
            par = lambda lt: (sbi * SBT + lt) % 2
            bg = []

            def advance():
                for g in list(bg):
                    try:
                        next(g)
                    except StopIteration:
                        bg.remove(g)
            run_gen(g_front(NT_PRE + sbi * SBT, 0, par(0)))
            build_RR(0, 0)
            build_RR(1, 1)
            st_Dp(par(0), 0)
            if SBT > 1:
                bg.append(g_front(NT_PRE + sbi * SBT + 1, 1, par(1)))
            k = 0
            while k < nun + 4 or bg:
                advance()
                if 0 <= k - 4 < nun:
                    lt3, tau3 = divmod(k - 4, 4)
                    st_Ca(par(lt3), tau3, (k - 4) % 2)
                if 0 <= k - 3 < nun:
                    st_M((k - 3) % 4, (k - 3) % 2)
                if 0 <= k - 4 < nun:
                    st_Cb(par(lt3), tau3, (k - 4) % 2)
                    if tau3 == 3:
                        bg.append(g_tail(lt3, par(lt3)))
                if k < nun:
                    lt, tau = divmod(k, 4)
                    if tau == 2:
                        st_pool(par(lt), sbi == 0 and lt == 0)
                    st_Dd(tau, k % 2)
                    if tau == 3 and 1 <= lt + 1 and lt + 2 < SBT:
                        bg.append(g_front(NT_PRE + sbi * SBT + lt + 2, lt + 2, par(lt + 2)))
                if 0 <= k - 1 < nun:
                    st_S((k - 1) % 4, (k - 1) % 2)
                if 0 <= k - 3 < nun:
                    ltp, taup = divmod(k - 3, 4)
                    st_Cp(par(ltp), taup, (k - 3) % 2)
                if k + 1 < nun:
                    ltn, taun = divmod(k + 1, 4)
                    st_Dp(par(ltn), taun)
                k += 1

        def pipeline(gens):
            gens = list(gens)
            active = []
            while gens or active:
                if gens and len(active) < 2 and (not active or active[-1][1][0]):
                    active.append((gens.pop(0), [False]))
                for it in list(active):
                    g, st = it
                    try:
                        v = next(g)
                        if v == "F":
                            st[0] = True
                    except StopIteration:
                        active.remove(it)

        wgb = nc.dram_tensor("wgb", [16, 128, 2048], BF16).ap()
        wub = nc.dram_tensor("wub", [16, 128, 2048], BF16).ap()
        wdb = nc.dram_tensor("wdb", [16, 128, 2048], BF16).ap()
        accf = acc[:].rearrange("p a b -> p (a b)")
        hn2f = hn2T[:].rearrange("p a b -> p (a b)")
        cast_units = []
        for e in range(16):
            cast_units += [(0, e), (1, e), (2, e)]
        ucnt = [0]

        def cast_unit():
            if not cast_units:
                return
            kind, e = cast_units.pop(0)
            sl = ucnt[0] % 2
            ucnt[0] += 1
            stg = accf[:, sl * 2048:(sl + 1) * 2048]
            tmp = hn2f[:, sl * 2048:(sl + 1) * 2048]
            rs_, rt_ = r_("stg%d" % sl), r_("tmpb%d" % sl)
            if kind == 2:
                dma(stg.rearrange("p (k n) -> p k n", k=2), wd_d[e, :, :].rearrange("(k p) n -> p k n", p=128), [], [rs_], q="pool")
                cp("pool", tmp, stg, [rs_], [rt_])
                dma(wdb[e, :, :], tmp, [rt_], [r_("wscr")], q="pool")
            else:
                src = wg_d if kind == 0 else wu_d
                dst = wgb if kind == 0 else wub
                dma(stg.rearrange("p (k n) -> p k n", k=8), src[e, :, :].rearrange("(k p) n -> p k n", p=128), [], [rs_], q="pool")
                tt("pool", tmp.rearrange("p (k n) -> p k n", k=8), stg.rearrange("p (k n) -> p k n", k=8),
                   colt[:, c_nf:c_nf + 8].unsqueeze(2).to_broadcast([128, 8, 256]), ALU.mult, [rs_, r_("colt")], [rt_])
                dma(dst[e, :, :], tmp, [rt_], [r_("wscr")], q="pool")

        def moe(sbi):
            dma(NF, rows[0:1, 0:1024].partition_broadcast(128), [], [r_("NF")])
            nblk = SBT // 4
            tok = slice(0, SBT * 128)

            def gu(e):
                sl = e % 2
                rwg, rwu, rwd = r_("WG%d" % sl), r_("WU%d" % sl), r_("WD%d" % sl)
                dma(WGs[sl].rearrange("p k n -> p (k n)"), wgb[e, :, :], [r_("wscr")], [rwg])
                dma(WUs[sl].rearrange("p k n -> p (k n)"), wub[e, :, :], [r_("wscr")], [rwu])
                dma(WDs[sl].rearrange("p k n -> p (k n)"), wdb[e, :, :], [r_("wscr")], [rwd])
                for ft in range(2):
                    gp, up = PBs[2 + ft], PBs[4 + ft]
                    rg, ru = r_("bank%d" % (2 + ft)), r_("bank%d" % (4 + ft))
                    rsg, rhT_ = r_("sgT%d%d" % (sl, ft)), r_("hT%d%d" % (sl, ft))
                    for k in range(8):
                        mm(gp[:], WGs[sl][:, k, ft * 128:(ft + 1) * 128], hn2T[:, k, tok], k == 0, k == 7, [rwg, r_("hn2T")], [rg])
                    for k in range(8):
                        mm(up[:], WUs[sl][:, k, ft * 128:(ft + 1) * 128], hn2T[:, k, tok], k == 0, k == 7, [rwu, r_("hn2T")], [ru])
                    act(sgT[sl][:, ft, :], gp[:], AF.Silu, [rg], [rsg])
                    tt("dve", hT[sl][:, ft, :], up[:], sgT[sl][:, ft, :], ALU.mult, [ru, rsg], [rhT_])

            def down(e):
                sl = e % 2
                rwd = r_("WD%d" % sl)
                for lt in range(SBT):
                    for half in range(2):
                        pb = PBs[half]
                        rpb = r_("bank%d" % half)
                        for ft in range(2):
                            mm(pb[:], hT[sl][:, ft, lt * 128:(lt + 1) * 128], WDs[sl][:, ft, half * 512:(half + 1) * 512], ft == 0, ft == 1,
                               [r_("hT%d%d" % (sl, ft)), rwd], [rpb])
                        stt(acc[:, lt, half * 512:(half + 1) * 512], pb[:], gates[:, lt, e:e + 1], acc[:, lt, half * 512:(half + 1) * 512],
                            ALU.mult, ALU.add, [rpb, r_("gates"), r_("acc%d" % lt)], [r_("acc%d" % lt)])
            for e in range(17):
                if e < 16:
                    gu(e)
                if e >= 1:
                    down(e - 1)
            for lt in range(SBT):
                tix = sbi * SBT + lt
                o2 = lt % 2
                ro = r_("outt%d" % o2)
                S.op("dve", lambda e: e.memset(smc(8), 0.0), wr=[r_("sm8")])
                act(junk2, acc[:, lt, :], AF.Square, [r_("acc%d" % lt)], [r_("junk2"), r_("sm8")], accum_out=smc(8))
                act(smc(9), smc(8), AF.Sqrt, [r_("sm8")], [r_("sm8")], scale=1.0 / 1024.0, bias=EPS)
                recip(smc(10), smc(9), [r_("sm8")], [r_("sm8")])
                stt(outt[o2], acc[:, lt, :], smc(10), NF, ALU.mult, ALU.mult, [r_("acc%d" % lt), r_("sm8"), r_("NF")], [ro])
                dma(out[tix * 128:(tix + 1) * 128, :], outt[o2], [ro], [r_("out")])

        S.barrier()
        def pre_tile(t, p):
            last = t == NT_PRE - 1
            yield from front(t, [0, 1, 2, 3] if last else [], p)
            rhT = r_("hnT%d" % p)
            for k in range(8):
                mm(PBs[1][:], hnT[p][:, k, :], WI[:, k, 512:1024], k == 0, k == 7, [r_("WI"), rhT], [r_("bank1")])
            act(ztok[p], PBs[1][:], AF.Copy, [r_("bank1")], [r_("ztok%d" % p)])
            yield
            ya, yb = PBs[2 + 2 * p], PBs[3 + 2 * p]
            rya, ryb = r_("bank%d" % (2 + 2 * p)), r_("bank%d" % (3 + 2 * p))
            for g in range(32):
                mm(ya[:, g * 16:(g + 1) * 16], VTa[:, g, :], ztok[p][:, g * 16:(g + 1) * 16], True, True, [r_("VTa"), r_("ztok%d" % p)], [rya])
            for g in range(32):
                mm(yb[:, g * 16:(g + 1) * 16], VTb[:, g, :], ztok[p][:, g * 16:(g + 1) * 16], True, True, [r_("VTb"), r_("ztok%d" % p)], [ryb])
            yield
            tt("dve", Ytmp[0], ya[:], BBR.rearrange("p a b -> p (a b)"), ALU.mult, [rya, r_("BBR")], [r_("Ytmp0")])
            tt("dve", Ytmp[1], yb[:], BBIs.rearrange("p a b -> p (a b)"), ALU.mult, [ryb, r_("BBI")], [r_("Ytmp1")])
            tt("dve", Ytmp[0], Ytmp[0], Ytmp[1], ALU.add, [r_("Ytmp1")], [r_("Ytmp0")])
            red(Ssum, Ytmp[0].rearrange("p (a b) -> p a b", a=32), ALU.add, [r_("Ytmp0")], [r_("Ssum")])
            tt("dve", Ctmp[:, 0:32], A128[:, 0:32], CAR[:], ALU.mult, [r_("A128"), r_("CAR")], [r_("ctmp")])
            tt("dve", Ctmp[:, 32:64], A128[:, 32:64], CARb, ALU.mult, [r_("A128"), r_("CARb"), r_("CARb2")], [r_("ctmp2")])
            tt("dve", Ctmp[:, 0:32], Ctmp[:, 0:32], Ctmp[:, 32:64], ALU.add, [r_("ctmp2")], [r_("ctmp")])
            tt("dve", CAR[:], Ctmp[:, 0:32], Ssum, ALU.add, [r_("ctmp"), r_("Ssum")], [r_("CAR")])
            dma(CARb[0:64, :], CAR[64:128, :], [r_("CAR")], [r_("CARb")])
            dma(CARb[64:128, :], CAR[0:64, :], [r_("CAR")], [r_("CARb2")])
            cast_unit()
            if t % 2 == 1:
                cast_unit()
            yield
        pipeline([pre_tile(t, t % 2) for t in range(NT_PRE)])
        while cast_units:
            cast_unit()
        S.barrier()
        for sbi in range(NT_MAIN // SBT):
            mixer_sb(sbi)
            S.barrier()
            moe(sbi)
            S.barrier()
        S.final_wait("sp", [r_("out")])

        print('SBUF remaining', nc.sbuf_bytes_remaining, 'mixer_end', mixer_end, 'moe_end', _off[0])
        block = es.enter_context(nc.Block())

        @block.sync
        def _(e):
            S.replay("sp", e)

        @block.tensor
        def _(e):
            S.replay("pe", e)

        @block.scalar
        def _(e):
            S.replay("act", e)

        @block.vector
        def _(e):
            S.replay("dve", e)

        @block.gpsimd
        def _(e):
            S.replay("pool", e)
    return nc


def _col(v, k):
    return np.ascontiguousarray(np.asarray(v, np.float32).reshape(k, 128).T)


def kernel(x, norm_mix, w_in, pool_w, pool_scale, ssm_a_re, ssm_a_im, ssm_log_step,
           ssm_b_re, ssm_b_im, ssm_c_re, ssm_c_im, ssm_d, glu_w, glu_b, w_out, norm_ffn,
           router_coarse_w, router_coarse_b, router_fine_w, router_fine_b,
           exp_w_gate, exp_w_up, exp_w_down, norm_final):
    f = np.float32
    x = np.asarray(x, f)
    cols = np.zeros((128, 64), f)
    cols[:, 0:8] = _col(norm_mix[0], 8)
    cols[:, 8:16] = _col(norm_ffn[0], 8)
    cols[:, 16:20] = _col(pool_scale[0], 4)
    cols[:, 20:24] = _col(ssm_d[0], 4)
    cols[:, 24:28] = _col(glu_b[0], 4)
    are = np.asarray(ssm_a_re[0], f)
    aim = np.asarray(ssm_a_im[0], f)
    ls = np.asarray(ssm_log_step[0], f)
    sp_s = np.zeros((128, 96), f)
    sp_s[:, 0:32] = np.concatenate([are.T, are.T], 0)
    sp_s[:, 32:64] = np.concatenate([aim.T, aim.T], 0)
    sp_s[:, 64:96] = np.broadcast_to(ls[None, :], (128, 32))
    br = np.asarray(ssm_b_re[0], f).transpose(1, 0, 2)
    bi = np.asarray(ssm_b_im[0], f).transpose(1, 0, 2)
    b1 = np.ascontiguousarray(np.concatenate([br, bi], 0))
    b2 = np.ascontiguousarray(np.concatenate([bi, br], 0))
    cr = np.asarray(ssm_c_re[0], f).transpose(2, 0, 1)
    ci = np.asarray(ssm_c_im[0], f).transpose(2, 0, 1)
    c1 = np.ascontiguousarray(np.concatenate([cr, ci], 0))
    c2 = np.ascontiguousarray(np.concatenate([ci, cr], 0))
    cst = np.zeros((128, 386), f)
    cst[:, 258:386] = np.arange(127, -1, -1, dtype=f)[None, :]
    cst[:, 0:128] = np.eye(128, dtype=f)
    cst[:, 128:256] = np.arange(1, 129, dtype=f)[None, :]
    cst[:, 256] = np.where(np.arange(128) < 64, 1.0, -1.0)
    cst[:, 257] = 1.0
    wr = np.ascontiguousarray(np.concatenate([np.asarray(router_coarse_w[0], f), np.asarray(router_fine_w[0], f)], 1))
    rb = np.concatenate([np.asarray(router_coarse_b[0], f), np.asarray(router_fine_b[0], f)])
    in_maps = []
    for c in range(8):
        b, half = c // 2, c % 2
        main = x[b, half * 4096:(half + 1) * 4096]
        pre = x[b, 0:4096] if half == 1 else np.zeros((4096, 1024), f)
        fix = np.zeros((4, 16), f)
        if half == 0:
            for gi, w in enumerate(WINS):
                for t in range(w - 1):
                    fix[gi, t] = w / (t + 1.0) - 1.0
        rows = np.concatenate([np.asarray(norm_final, f), rb, fix.reshape(-1)])[None, :].astype(f)
        in_maps.append({
            "xall": np.ascontiguousarray(np.concatenate([pre, main], 0)),
            "w_in": np.asarray(w_in[0], f), "w_out": np.asarray(w_out[0], f), "glu_w": np.asarray(glu_w[0], f),
            "pool_w": np.asarray(pool_w[0], f), "cols": cols, "rows": rows, "sp_s": sp_s,
            "b1_s": b1, "b2_s": b2, "c1_s": c1, "c2_s": c2, "cst": cst, "wr": wr,
            "wg": np.asarray(exp_w_gate[0], f), "wu": np.asarray(exp_w_up[0], f), "wd": np.asarray(exp_w_down[0], f),
        })
    nc = build()
    res = run_bass_kernel_spmd(nc, in_maps, core_ids=list(range(8)))
    outs = [np.asarray(r["out"], f) for r in res.results]
    full = np.zeros((4, 8192, 1024), f)
    for c in range(8):
        b, half = c // 2, c % 2
        full[b, half * 4096:(half + 1) * 4096] = outs[c]
    return full
```

```python
import math
import numpy as np
from contextlib import ExitStack
import concourse.bass as bass
import concourse.mybir as mybir
from concourse.bass_utils import run_bass_kernel_spmd

F32 = mybir.dt.float32
BF16 = mybir.dt.bfloat16
I32 = mybir.dt.int32
AF = mybir.ActivationFunctionType
ALU = mybir.AluOpType
AX = mybir.AxisListType

NT_PRE = 32
NT_MAIN = 32
SBT = 4
EPS = 1e-6
WINS = (2, 4, 8, 16)
TWO_PI = 2.0 * math.pi


class R:
    def __init__(self, name):
        self.name = name
        self.w = None
        self.rd = {}


class Sched:
    ENG = ("pe", "act", "dve", "pool", "sp")

    def __init__(self, nc, es):
        self.nc = nc
        self.sem = {e: es.enter_context(nc.semaphore("s_" + e)) for e in self.ENG}
        self.cnt = {e: 0 for e in self.ENG}
        self.ops = {e: [] for e in self.ENG}
        self.seen = {e: {} for e in self.ENG}
        self.dma_pool = [es.enter_context(nc.semaphore("d%d" % i)) for i in range(48)]
        self.dma_cnt = {}
        self.dma_of = {}

    def dsem(self, res):
        if res.name not in self.dma_of:
            s = self.dma_pool[len(self.dma_of)]
            self.dma_of[res.name] = s
            self.dma_cnt[s.name] = 0
        return self.dma_of[res.name]

    def op(self, eng, fn, rd=(), wr=(), dma=None):
        need = {}

        def add(tok):
            if tok is None:
                return
            s, v = tok
            if need.get(s.name, (None, -1))[1] < v:
                need[s.name] = (s, v)
        for r in rd:
            add(r.w)
        for r in wr:
            add(r.w)
            for t in r.rd.values():
                add(t)
        waits = []
        for name, (s, v) in need.items():
            if eng == "pe" and s is self.sem["pe"]:
                continue
            if self.seen[eng].get(name, -1) >= v:
                continue
            self.seen[eng][name] = v
            waits.append((s, v))
        if dma is not None:
            s = self.dsem(dma)
            self.dma_cnt[s.name] += 16
            tok = (s, self.dma_cnt[s.name])
            inc = (s, 16)
        else:
            self.cnt[eng] += 1
            tok = (self.sem[eng], self.cnt[eng])
            inc = (self.sem[eng], 1)
        for r in rd:
            old = r.rd.get(tok[0].name)
            if old is None or old[1] < tok[1]:
                r.rd[tok[0].name] = tok
        for r in wr:
            r.w = tok
            r.rd = {}
        self.ops[eng].append((waits, fn, inc))
        return tok

    def final_wait(self, eng, ress):
        waits = []
        for r in ress:
            if r.w is not None:
                waits.append(r.w)
        self.ops[eng].append((waits, None, None))

    def barrier(self):
        toks = [(self.sem[e], self.cnt[e]) for e in self.ENG if self.cnt[e] > 0]
        for name, sm_ in self.dma_of.items():
            toks.append((sm_, self.dma_cnt[sm_.name]))
        for eng in self.ENG:
            waits = []
            for s_, v in toks:
                if s_ is self.sem[eng]:
                    continue
                if self.seen[eng].get(s_.name, -1) >= v:
                    continue
                self.seen[eng][s_.name] = v
                waits.append((s_, v))
            if waits:
                self.ops[eng].append((waits, None, None))

    def replay(self, eng, e):
        for waits, fn, inc in self.ops[eng]:
            for s, v in waits:
                e.wait_ge(s, v)
            if fn is not None:
                fn(e).then_inc(inc[0], inc[1])


def build(debug=False):
    nc = bass.Bass("TRN2", target_bir_lowering=False)

    def din(name, shape, dt=F32):
        return nc.dram_tensor(name, list(shape), dt, kind="ExternalInput").ap()
    xall = din("xall", [(NT_PRE + NT_MAIN) * 128, 1024])
    w_in = din("w_in", [1024, 1024])
    w_out = din("w_out", [1024, 1024])
    glu_w = din("glu_w", [512, 512])
    pool_w = din("pool_w", [4, 128, 128])
    cols = din("cols", [128, 64])
    rows = din("rows", [1, 1024 + 20 + 64])
    sp_s = din("sp_s", [128, 96])
    b1_s = din("b1_s", [128, 32, 16])
    b2_s = din("b2_s", [128, 32, 16])
    c1_s = din("c1_s", [128, 32, 16])
    c2_s = din("c2_s", [128, 32, 16])
    cst = din("cst", [128, 386])
    wr_d = din("wr", [1024, 20])
    wg_d = din("wg", [16, 1024, 256])
    wu_d = din("wu", [16, 1024, 256])
    wd_d = din("wd", [16, 256, 1024])
    out = nc.dram_tensor("out", [NT_MAIN * 128, 1024], F32, kind="ExternalOutput").ap()

    es = ExitStack()
    with es:
        S = Sched(nc, es)

        def sb(name, shape, dt=F32):
            return es.enter_context(nc.sbuf_tensor(name, list(shape), dt))

        def ps(name, shape, dt=F32):
            return es.enter_context(nc.psum_tensor(name, list(shape), dt))

        ident = sb("ident", [128, 128], BF16)
        cstt = sb("cstt", [128, 386])
        colt = sb("colt", [128, 64])
        RB = sb("RB", [128, 84])
        WI = sb("WI", [128, 8, 1024], BF16)
        WO = sb("WO", [128, 8, 1024], BF16)
        GW = sb("GW", [128, 4, 512], BF16)
        PW = sb("PW", [128, 8, 128], BF16)
        WRb = sb("WRb", [128, 8, 20], BF16)
        LBa = sb("LBa", [128, 32, 128], BF16)
        LBb = sb("LBb", [128, 32, 128], BF16)
        LC = sb("LC", [128, 32, 128], BF16)
        Dg = sb("Dg", [128, 4, 128], BF16)
        COS = sb("COS", [128, 32, 128])
        SINM = sb("SINM", [128, 32, 128])
        MAG = sb("MAG", [128, 32])
        CAR = sb("CAR", [128, 32])
        acc = sb("acc", [128, SBT, 1024])
        hn2T = sb("hn2T", [128, 8, SBT * 128], BF16)
        gates = sb("gates", [128, SBT, 16])
        cf = sb("cf", [128, 64])
        sm = sb("sm", [128, 256])
        pzb = sb("pzb", [128, 128], BF16)
        ARENA_W = 21504
        arena = sb("arena", [128, ARENA_W])
        _off = [0]

        def carve(shape, dt=F32):
            n = 1
            for d in shape[1:]:
                n *= d
            nb = n * (4 if dt == F32 else 2)
            nb = (nb + 63) // 64 * 64
            o = _off[0]
            _off[0] += nb
            assert _off[0] <= ARENA_W * 4, ("arena overflow", _off[0])
            v = arena[:, o // 4:(o + nb) // 4]
            if dt != F32:
                v = v.bitcast(dt)
            v = v[:, 0:n]
            if len(shape) == 3:
                v = v.rearrange("p (a b) -> p a b", a=shape[1])
            elif len(shape) == 4:
                v = v.rearrange("p (a b c) -> p a b c", a=shape[1], b=shape[2])
            return v
        xt = [carve([128, 1024]) for _ in range(2)]
        hn = [carve([128, 1024], BF16) for _ in range(2)]
        hnT = [carve([128, 8, 128], BF16) for _ in range(2)]
        yT_off = _off[0]
        yT = [carve([128, 8, 128], BF16) for _ in range(2)]
        yg = [carve([128, 4, 128], BF16) for _ in range(2)]
        ygf = [carve([128, 4, 128]) for _ in range(2)]
        g1 = [carve([128, 128]) for _ in range(2)]
        g2 = [carve([128, 128]) for _ in range(2)]
        Gt = [carve([128, 8, 128]) for _ in range(2)]
        hn2b = [carve([128, 1024], BF16) for _ in range(2)]
        D1 = None
        M1_off = _off[0]
        M1 = [carve([128, 8, 128]) for _ in range(2)]
        M2 = [carve([128, 8, 128]) for _ in range(2)]
        XT = [carve([128, 8, 128]) for _ in range(2)]
        XTb = [carve([128, 8, 128]) for _ in range(2)]
        XA = [carve([128, 8, 128], BF16) for _ in range(2)]
        junk = carve([128, 1024], BF16)
        assert _off[0] >= 48 * 1024
        zT = [carve([128, 8, 144], BF16) for _ in range(2)]
        mixer_end = _off[0]
        _off[0] = 0
        WGs = [carve([128, 8, 256], BF16) for _ in range(2)]
        WUs = [carve([128, 8, 256], BF16) for _ in range(2)]
        WDs = [carve([128, 2, 1024], BF16) for _ in range(2)]
        sgT = [carve([128, 2, 512], BF16) for _ in range(2)]
        hT = [carve([128, 2, 512], BF16) for _ in range(2)]
        outt = [carve([128, 1024]) for _ in range(2)]
        NF = carve([128, 1024])
        junk2 = carve([128, 1024], BF16)
        assert _off[0] <= 48 * 1024
        pp = Gt[0].rearrange("p a b -> p (a b)")[:, 0:512].rearrange("p (a b) -> p a b", a=32)
        pq = XT[0].rearrange("p a b -> p (a b)")[:, 0:512].rearrange("p (a b) -> p a b", a=32)
        pz = acc[:].rearrange("p a b -> p (a b)").rearrange("p (a b) -> p a b", a=32)
        T1 = M1[0]
        T2 = M2[0]

        PB0 = ps("PB0", [128, 1024], BF16)
        PBs = [ps("PB%d" % i, [128, 512]) for i in range(1, 8)]

        res = {}

        def rs(name):
            if name not in res:
                res[name] = R(name)
            return res[name]

        def dma(out_ap, in_ap, rd, wr, slow=False, q="sp"):
            if slow:
                S.op(q, lambda e: e.dma_start(out=out_ap, in_=in_ap, allow_slow_non_contiguous=True), rd=rd, wr=wr, dma=wr[0])
            else:
                S.op(q, lambda e: e.dma_start(out=out_ap, in_=in_ap), rd=rd, wr=wr, dma=wr[0])

        def act(out_ap, in_ap, func, rd, wr, **kw):
            S.op("act", lambda e: e.activation(out=out_ap, in_=in_ap, func=func, **kw), rd=rd, wr=wr)

        def tt(eng, out_ap, a, b, op, rd, wr):
            S.op(eng, lambda e: e.tensor_tensor(out=out_ap, in0=a, in1=b, op=op), rd=rd, wr=wr)

        def tsc(out_ap, a, s1, s2, op0, op1, rd, wr):
            if s2 is None:
                S.op("dve", lambda e: e.tensor_scalar(out=out_ap, in0=a, scalar1=s1, scalar2=None, op0=op0), rd=rd, wr=wr)
            else:
                S.op("dve", lambda e: e.tensor_scalar(out=out_ap, in0=a, scalar1=s1, scalar2=s2, op0=op0, op1=op1), rd=rd, wr=wr)

        def stt(out_ap, a, s, b, op0, op1, rd, wr):
            S.op("dve", lambda e: e.scalar_tensor_tensor(out=out_ap, in0=a, scalar=s, in1=b, op0=op0, op1=op1), rd=rd, wr=wr)

        def cp(eng, out_ap, in_ap, rd, wr):
            S.op(eng, lambda e: e.tensor_copy(out=out_ap, in_=in_ap), rd=rd, wr=wr)

        def mm(out_ap, lhsT, rhs, start, stop, rd, wr):
            S.op("pe", lambda e: e.matmul(out_ap, lhsT=lhsT, rhs=rhs, start=start, stop=stop), rd=rd, wr=wr)

        def tr(out_ap, in_ap, rd, wr):
            S.op("pe", lambda e: e.transpose(out=out_ap, in_=in_ap, identity=ident[:]), rd=rd, wr=wr)

        def red(out_ap, in_ap, op, rd, wr):
            S.op("dve", lambda e: e.tensor_reduce(out=out_ap, in_=in_ap, axis=AX.X, op=op), rd=rd, wr=wr)

        def recip(out_ap, in_ap, rd, wr):
            S.op("dve", lambda e: e.reciprocal(out=out_ap, in_=in_ap), rd=rd, wr=wr)

        def smc(i, n=1):
            return sm[:, i:i + n]

        r_ = rs
        dma(cstt[:], cst[:, :], [], [r_("cstt")])
        dma(colt[:], cols[:, :], [], [r_("colt")])
        dma(RB[:], rows[0:1, 1024:1108].partition_broadcast(128), [], [r_("RB")])
        cp("dve", ident[:], cstt[:, 0:128], [r_("cstt")], [r_("ident")])
        JIDX = cstt[:, 128:256]
        SGN = cstt[:, 256:257]
        c_nm, c_nf, c_ps_, c_d, c_gb = 0, 8, 16, 20, 24

        accv = acc[:].rearrange("p a b -> p (a b)")
        for half in range(2):
            dma(accv[:, 0:4096].rearrange("p (k n) -> p k n", k=4),
                w_in[half * 512:(half + 1) * 512, :].rearrange("(k p) n -> p k n", p=128), [], [r_("acc")])
            for k in range(4):
                kk = half * 4 + k
                tsc(WI[:, kk, :], accv[:, k * 1024:(k + 1) * 1024], colt[:, c_nm + kk:c_nm + kk + 1], None, ALU.mult, None,
                    [r_("acc"), r_("colt")], [r_("WI")])
        for half in range(2):
            dma(accv[:, 0:4096].rearrange("p (k n) -> p k n", k=4),
                w_out[half * 512:(half + 1) * 512, :].rearrange("(k p) n -> p k n", p=128), [r_("WI")], [r_("acc")])
            for k in range(4):
                kk = half * 4 + k
                tsc(WO[:, kk, :], accv[:, k * 1024:(k + 1) * 1024], 1.0 if kk < 4 else 0.25, None, ALU.mult, None, [r_("acc")], [r_("WO")])
        dma(accv[:, 0:2048].rearrange("p (k n) -> p k n", k=4), glu_w[:, :].rearrange("(k p) n -> p k n", p=128), [r_("WO")], [r_("acc")])
        tsc(GW[:].rearrange("p k n -> p (k n)"), accv[:, 0:2048], 0.5, None, ALU.mult, None, [r_("acc")], [r_("GW")])
        dma(accv[:, 0:512].rearrange("p (g n) -> p g n", g=4), pool_w[:, :, :].rearrange("g p n -> p g n"), [r_("GW")], [r_("acc")])
        for gi, w in enumerate(WINS):
            tsc(PW[:, 2 * gi, :], accv[:, gi * 128:(gi + 1) * 128], float(1.0 / w - 1.0), None, ALU.mult, None, [r_("acc")], [r_("PW")])
            tsc(PW[:, 2 * gi + 1, :], accv[:, gi * 128:(gi + 1) * 128], float(1.0 / w), None, ALU.mult, None, [r_("acc")], [r_("PW")])
        dma(accv[:, 0:160].rearrange("p (k n) -> p k n", k=8), wr_d[:, :].rearrange("(k p) n -> p k n", p=128), [r_("PW")], [r_("acc")])
        for k in range(8):
            tsc(WRb[:, k, :], accv[:, k * 20:(k + 1) * 20], colt[:, c_nf + k:c_nf + k + 1], None, ALU.mult, None, [r_("acc"), r_("colt")], [r_("WRb")])

        spt = XTb[0][:, 0, 0:96]
        dma(spt, sp_s[:, :], [], [r_("spt")])
        dma(pp, b1_s[:, :, :], [], [r_("Gt")])
        dma(pq, b2_s[:, :, :], [], [r_("XT")])
        P = XTb[1].rearrange("p a b -> p (a b)")[:, 0:512].rearrange("p (a b) -> p a b", a=16)
        Rp = r_("P")
        LR, LI, LS = spt[:, 0:32], spt[:, 32:64], spt[:, 64:96]
        STEP, ARG, MG, CS, SN, AR, AI, DEN, QR, QI, TA, TB, KI = [P[:, i, :] for i in range(13)]
        KII = sb("KII", [128, 32], I32)

        def exp_to(dst, src, rd):
            act(TA, src, AF.Tanh, rd, [Rp], scale=0.5)
            tsc(TB, TA, -1.0, 1.0, ALU.mult, ALU.add, [Rp], [Rp])
            recip(TB, TB, [Rp], [Rp])
            tsc(TA, TA, 1.0, None, ALU.add, None, [Rp], [Rp])
            tt("dve", dst, TA, TB, ALU.mult, [Rp], [Rp])

        def sin_to(dst, src, shift):
            tsc(TA, src, float(shift), 1.0 / TWO_PI, ALU.add, ALU.mult, [Rp], [Rp])
            cp("dve", KII[:], TA, [Rp], [Rp])
            cp("dve", TB, KII[:], [Rp], [Rp])
            tt("dve", TA, TA, TB, ALU.subtract, [Rp], [Rp])
            act(dst, TA, AF.Sin, [Rp], [Rp], scale=TWO_PI)
        exp_to(STEP, LS, [r_("spt")])
        tt("dve", ARG, LI, STEP, ALU.mult, [Rp, r_("spt")], [Rp])
        tt("dve", MG, LR, STEP, ALU.mult, [Rp, r_("spt")], [Rp])
        cp("dve", P[:, 13, :], MG, [Rp], [Rp])
        exp_to(MG, MG, [Rp])
        cp("dve", MAG[:], MG, [Rp], [r_("MAG")])
        sin_to(SN, ARG, 0.0)
        sin_to(CS, ARG, math.pi / 2)
        tt("dve", AR, MG, CS, ALU.mult, [Rp], [Rp])
        tt("dve", AI, MG, SN, ALU.mult, [Rp], [Rp])
        tt("dve", DEN, LR, LR, ALU.mult, [Rp], [Rp])
        tt("dve", TA, LI, LI, ALU.mult, [Rp], [Rp])
        tt("dve", DEN, DEN, TA, ALU.add, [Rp], [Rp])
        recip(DEN, DEN, [Rp], [Rp])
        tsc(TA, AR, -1.0, None, ALU.add, None, [Rp], [Rp])
        tt("dve", QR, TA, LR, ALU.mult, [Rp], [Rp])
        tt("dve", TB, AI, LI, ALU.mult, [Rp], [Rp])
        tt("dve", QR, QR, TB, ALU.add, [Rp], [Rp])
        tt("dve", QR, QR, DEN, ALU.mult, [Rp], [Rp])
        tt("dve", QI, AI, LR, ALU.mult, [Rp], [Rp])
        tt("dve", TB, TA, LI, ALU.mult, [Rp], [Rp])
        tt("dve", QI, QI, TB, ALU.subtract, [Rp], [Rp])
        tt("dve", QI, QI, DEN, ALU.mult, [Rp], [Rp])
        tsc(QI, QI, SGN, -1.0, ALU.mult, ALU.mult, [Rp, r_("cstt")], [Rp])
        tt("dve", pp, pp, QR.unsqueeze(2).to_broadcast([128, 32, 16]), ALU.mult, [Rp, r_("Gt")], [r_("Gt")])
        tt("dve", pq, pq, QI.unsqueeze(2).to_broadcast([128, 32, 16]), ALU.mult, [Rp, r_("XT")], [r_("XT")])
        tt("dve", pp, pp, pq, ALU.add, [r_("XT")], [r_("Gt")])
        S.op("pool", lambda e: e.memset(pz, 0.0), wr=[r_("acc")])
        for gl in range(8):
            cp("dve", pz[:, gl::8, gl * 16:(gl + 1) * 16], pp[:, gl::8, :], [r_("Gt")], [r_("acc")])
        for g in range(32):
            cp("dve", pzb[:], pz[:, g, :], [r_("acc")], [r_("pzb")])
            tr(PB0[:, 0:128], pzb[:], [r_("pzb"), r_("ident")], [r_("PB0")])
            cp("dve", LBa[:, g, :], PB0[:, 0:128], [r_("PB0")], [r_("LBa")])
        cp("dve", LBb[:, :, 0:64], LBa[:, :, 64:128], [r_("LBa")], [r_("LBb")])
        cp("dve", LBb[:, :, 64:128], LBa[:, :, 0:64], [r_("LBa")], [r_("LBb")])
        dma(pq, c1_s[:, :, :], [r_("Gt")], [r_("XT")])
        tsc(pq, pq, SGN, None, ALU.mult, None, [r_("XT"), r_("cstt")], [r_("XT")])
        S.op("pool", lambda e: e.memset(pz, 0.0), rd=[], wr=[r_("acc")])
        for gl in range(8):
            cp("dve", pz[:, gl::8, gl * 16:(gl + 1) * 16], pq[:, gl::8, :], [r_("XT")], [r_("acc")])
        cp("dve", LC[:].rearrange("p a b -> p (a b)"), pz.rearrange("p a b -> p (a b)"), [r_("acc")], [r_("LC")])
        for t4 in range(4):
            tsc(Dg[:, t4, :], ident[:], colt[:, c_d + t4:c_d + t4 + 1], None, ALU.mult, None, [r_("ident"), r_("colt")], [r_("Dg")])
        SCR = [T1, T2]
        for g in range(32):
            sc = SCR[g % 2]
            rsc = r_("scr%d" % (g % 2))
            tsc(sc[:, 0, :], JIDX, P[:, 1, g:g + 1], 1.0 / TWO_PI, ALU.mult, ALU.mult, [Rp, r_("cstt")], [rsc])
            for ti_, (tab, shift) in enumerate(((SINM, 0.0), (COS, 0.25))):
                rs2 = r_("scr%d_%d" % (g % 2, ti_))
                a, b_, c = 1 + 3 * ti_, 2 + 3 * ti_, 3 + 3 * ti_
                tsc(sc[:, a, :], sc[:, 0, :], float(shift), None, ALU.add, None, [rsc], [rs2])
                cp("dve", sc[:, b_, :].bitcast(I32), sc[:, a, :], [rs2], [rs2])
                cp("dve", sc[:, c, :], sc[:, b_, :].bitcast(I32), [rs2], [rs2])
                tt("dve", sc[:, a, :], sc[:, a, :], sc[:, c, :], ALU.subtract, [rs2], [rs2])
                act(tab[:, g, :], sc[:, a, :], AF.Sin, [rs2], [r_("tab")], scale=TWO_PI)
        tsc(SINM[:].rearrange("p a b -> p (a b)"), SINM[:].rearrange("p a b -> p (a b)"), SGN, None, ALU.mult, None, [r_("tab"), r_("cstt")], [r_("tab")])
        _save = _off[0]
        _off[0] = yT_off
        BBR = carve([128, 32, 16])
        BBIs = carve([128, 32, 16])
        VTa = carve([128, 32, 128], BF16)
        VTb = carve([128, 32, 128], BF16)
        assert _off[0] <= M1_off
        _off[0] = _save
        ztok = [M1[1].rearrange("p a b -> p (a b)").bitcast(BF16)[:, 0:1024][:, i * 512:(i + 1) * 512] for i in range(2)]
        Ytmp = [M2[1].rearrange("p a b -> p (a b)")[:, i * 512:(i + 1) * 512] for i in range(2)]
        CARb = XA[1].rearrange("p a b -> p (a b)").bitcast(F32)[:, 0:32]
        Ssum = XA[1].rearrange("p a b -> p (a b)").bitcast(F32)[:, 32:64]
        Ctmp = XA[1].rearrange("p a b -> p (a b)").bitcast(F32)[:, 64:128]
        A128 = XA[1].rearrange("p a b -> p (a b)").bitcast(F32)[:, 128:192]
        bA = XT[1].rearrange("p a b -> p (a b)")[:, 0:512].rearrange("p (a b) -> p a b", a=32)
        bB = XT[1].rearrange("p a b -> p (a b)")[:, 512:1024].rearrange("p (a b) -> p a b", a=32)
        dma(bA, b2_s[:, :, :], [], [r_("bA")])
        dma(bB, b1_s[:, :, :], [], [r_("bB")])
        tt("dve", bA, bA, QR.unsqueeze(2).to_broadcast([128, 32, 16]), ALU.mult, [Rp, r_("bA")], [r_("bA")])
        tt("dve", bB, bB, QI.unsqueeze(2).to_broadcast([128, 32, 16]), ALU.mult, [Rp, r_("bB")], [r_("bB")])
        tt("dve", bA, bA, bB, ALU.subtract, [r_("bB")], [r_("bA")])
        cp("dve", BBR[0:64], pp[0:64], [r_("Gt")], [r_("BBR")])
        cp("dve", BBR[64:128], bA[64:128], [r_("bA")], [r_("BBR")])
        tsc(BBIs[0:64].rearrange("p a b -> p (a b)"), bA[0:64].rearrange("p a b -> p (a b)"), -1.0, None, ALU.mult, None, [r_("bA")], [r_("BBI")])
        cp("dve", BBIs[64:128], pp[64:128], [r_("Gt")], [r_("BBI")])
        S.barrier()
        SHC = sm[:, 70:71]
        tsc(SHC, SGN, 0.125, 0.125, ALU.mult, ALU.add, [r_("cstt")], [r_("shc")])
        JREV = cstt[:, 258:386]
        pzbs = [XA[0][:, 0, :], XA[0][:, 1, :]]
        for g in range(32):
            sc = SCR[g % 2]
            rsc = r_("vscr%d" % (g % 2))
            rpz = r_("pzbs%d" % (g % 2))
            tsc(sc[:, 0, :], JREV, P[:, 1, g:g + 1], 1.0 / TWO_PI, ALU.mult, ALU.mult, [Rp, r_("cstt"), r_("tab")], [rsc])
            tsc(sc[:, 1, :], sc[:, 0, :], SHC, None, ALU.add, None, [rsc, r_("shc")], [rsc])
            cp("dve", sc[:, 2, :].bitcast(I32), sc[:, 1, :], [rsc], [rsc])
            cp("dve", sc[:, 3, :], sc[:, 2, :].bitcast(I32), [rsc], [rsc])
            tt("dve", sc[:, 1, :], sc[:, 1, :], sc[:, 3, :], ALU.subtract, [rsc], [rsc])
            act(sc[:, 4, :], sc[:, 1, :], AF.Sin, [rsc], [r_("vsb%d" % (g % 2))], scale=TWO_PI)
            act(sc[:, 5, :], JREV, AF.Exp, [r_("cstt"), Rp, rsc], [r_("vsc%d" % (g % 2))], scale=P[:, 13, g:g + 1])
            tt("dve", pzbs[g % 2], sc[:, 4, :], sc[:, 5, :], ALU.mult, [r_("vsb%d" % (g % 2)), r_("vsc%d" % (g % 2))], [rpz])
            tr(PB0[:, (g % 2) * 128:(g % 2 + 1) * 128], pzbs[g % 2], [rpz, r_("ident")], [r_("PB0")])
            cp("dve", VTa[:, g, :], PB0[:, (g % 2) * 128:(g % 2 + 1) * 128], [r_("PB0")], [r_("VTa")])
        cp("dve", VTb[:, :, 0:64], VTa[:, :, 64:128], [r_("VTa")], [r_("VTb")])
        cp("dve", VTb[:, :, 64:128], VTa[:, :, 0:64], [r_("VTa")], [r_("VTb")])
        act(Ctmp[:, 0:32], P[:, 13, :], AF.Exp, [Rp], [r_("ctmp")], scale=128.0)
        tt("dve", A128[:, 0:32], Ctmp[:, 0:32], COS[:, :, 127], ALU.mult, [r_("ctmp"), r_("tab")], [r_("A128")])
        tt("dve", A128[:, 32:64], Ctmp[:, 0:32], SINM[:, :, 127], ALU.mult, [r_("ctmp"), r_("tab")], [r_("A128")])
        tsc(A128[:, 32:64], A128[:, 32:64], -1.0, None, ALU.mult, None, [r_("A128")], [r_("A128")])
        S.op("pool", lambda e: e.memset(CARb, 0.0), wr=[r_("CARb")])
        S.barrier()
        LCs = XTb[0].rearrange("p a b -> p (a b)").bitcast(BF16)[:, 0:2048]
        LCs2 = XTb[1].rearrange("p a b -> p (a b)").bitcast(BF16)[:, 0:2048]
        dma(pq, c2_s[:, :, :], [], [r_("XT")])
        tsc(pq, pq, SGN, -1.0, ALU.mult, ALU.mult, [r_("XT"), r_("cstt")], [r_("XT")])
        S.op("pool", lambda e: e.memset(pz, 0.0), rd=[], wr=[r_("acc")])
        for gl in range(8):
            cp("dve", pz[:, gl::8, gl * 16:(gl + 1) * 16], pq[:, gl::8, :], [r_("XT")], [r_("acc")])
        pzf = pz.rearrange("p a b -> p (a b)")
        cp("dve", LCs, pzf[:, 0:2048], [r_("acc")], [r_("LCs")])
        cp("dve", LCs2, pzf[:, 2048:4096], [r_("acc")], [r_("LCs")])
        LCsv = [LCs.rearrange("p (g c) -> p g c", g=16), LCs2.rearrange("p (g c) -> p g c", g=16)]
        S.op("pool", lambda e: e.memset(CAR[:], 0.0), wr=[r_("CAR")])
        for p_ in range(2):
            S.op("pool", lambda e, p_=p_: e.memset(zT[p_], 0.0), wr=[r_("zT%d" % p_)])
        tsc(sm[:, 64:68], colt[:, c_gb:c_gb + 4], 0.5, None, ALU.mult, None, [r_("colt")], [r_("sm2")])

        def rms(src_ap, dst_bf, srcres, dstres, col, jk):
            S.op("dve", lambda e: e.memset(smc(col), 0.0), wr=[r_("sm%d" % col)])
            act(jk, src_ap, AF.Square, srcres, [r_("junk"), r_("sm%d" % col)], accum_out=smc(col))
            act(smc(col + 1), smc(col), AF.Sqrt, [r_("sm%d" % col)], [r_("sm%d" % col)], scale=1.0 / 1024.0, bias=EPS)
            recip(smc(col + 2), smc(col + 1), [r_("sm%d" % col)], [r_("sm%d" % col)])
            act(dst_bf, src_ap, AF.Copy, srcres + [r_("sm%d" % col)], dstres, scale=smc(col + 2))

        def rms_a(src_ap, srcres, col, jk, jkres):
            S.op("pool", lambda e: e.memset(smc(col), 0.0), wr=[r_("sm%d" % col)])
            act(jk, src_ap, AF.Square, srcres, [jkres, r_("sm%d" % col)], accum_out=smc(col))
            act(smc(col + 1), smc(col), AF.Sqrt, [r_("sm%d" % col)], [r_("sm%d" % col)], scale=1.0 / 1024.0, bias=EPS)

        def rms_b(src_ap, dst_bf, srcres, dstres, col):
            recip(smc(col + 2), smc(col + 1), [r_("sm%d" % col)], [r_("sm%d" % col)])
            act(dst_bf, src_ap, AF.Copy, srcres + [r_("sm%d" % col)], dstres, scale=smc(col + 2))

        def transp8(src_bf, srcres):
            for k in range(8):
                tr(PB0[:, k * 128:(k + 1) * 128], src_bf[:, k * 128:(k + 1) * 128], srcres + [r_("ident")], [r_("PB0")])

        tcnt = [0]

        def ssm_state(tau, full, p):
            g0 = tau * 8
            q = tau % 2
            ba, bb = PBs[2 + q * 2], PBs[3 + q * 2]
            rba, rbb = r_("bank%d" % (2 + q * 2)), r_("bank%d" % (3 + q * 2))
            bav = ba[:].rearrange("p (g t) -> p g t", g=4)
            bbv = bb[:].rearrange("p (g t) -> p g t", g=4)
            rz = r_("zT%d" % p)
            rG = r_("Gt%d" % q)
            for hh in range(2):
                for gl4 in range(4):
                    g = g0 + hh * 4 + gl4
                    mm(bav[:, gl4, :], LBa[:, g, :], zT[p][:, 4 + tau, 16:144], True, True, [r_("LBa"), rz], [rba])
                    mm(bbv[:, gl4, :], LBb[:, g, :], zT[p][:, 4 + tau, 16:144], True, True, [r_("LBb"), rz], [rbb])
                sl = slice(hh * 4, hh * 4 + 4)
                gs = slice(g0 + hh * 4, g0 + hh * 4 + 4)
                rD = r_("D1%d" % hh)
                tt("dve", Gt[q][:, sl, :], bav, COS[:, gs, :], ALU.mult, [rba, r_("tab")], [rG])
                tt("dve", D1[hh][:], bbv, SINM[:, gs, :], ALU.mult, [rbb, r_("tab")], [rD])
                tt("pool", Gt[q][:, sl, :], Gt[q][:, sl, :], D1[hh][:], ALU.add, [rD], [rG])
                yield
            rX = r_("XT%d" % q)
            for gl in range(8):
                g = g0 + gl
                S.op("dve", lambda e, gl=gl, g=g, q=q: e.tensor_tensor_scan(
                    out=XT[q][:, gl, :], data0=MAG[:, g:g + 1].to_broadcast([128, 128]), data1=Gt[q][:, gl, :],
                    initial=CAR[:, g:g + 1], op0=ALU.mult, op1=ALU.add),
                    rd=[rG, r_("MAG"), r_("CAR")], wr=[rX])
            yield
            gs = slice(g0, g0 + 8)
            rXb, rXb2 = r_("XTb%d" % q), r_("XTc%d" % q)
            rM1, rM2 = r_("M1%d" % q), r_("M2%d" % q)
            if full:
                dma(XTb[q][0:64, :, :], XT[q][64:128, :, :], [rX], [rXb])
                dma(XTb[q][64:128, :, :], XT[q][0:64, :, :], [rX], [rXb2])
                tt("dve", M1[q], XT[q], COS[:, gs, :], ALU.mult, [rX, r_("tab")], [rM1])
                tt("pool", M2[q], XTb[q], SINM[:, gs, :], ALU.mult, [rXb, rXb2, r_("tab")], [rM2])
                tt("pool", XA[q], M1[q], M2[q], ALU.subtract, [rM1, rM2], [r_("XA%d" % q)])
                tt("pool", CAR[:, gs], M1[q][:, :, 127], M2[q][:, :, 127], ALU.subtract, [rM1, rM2], [r_("CAR")])
            else:
                dma(XTb[q][0:64, :, 127:128], XT[q][64:128, :, 127:128], [rX], [rXb], slow=True)
                dma(XTb[q][64:128, :, 127:128], XT[q][0:64, :, 127:128], [rX], [rXb2], slow=True)
                tt("pool", M1[q][:, :, 127], XT[q][:, :, 127], COS[:, gs, 127], ALU.mult, [rX, r_("tab")], [rM1])
                tt("pool", M2[q][:, :, 127], XTb[q][:, :, 127], SINM[:, gs, 127], ALU.mult, [rXb, rXb2, r_("tab")], [rM2])
                tt("pool", CAR[:, gs], M1[q][:, :, 127], M2[q][:, :, 127], ALU.subtract, [rM1, rM2], [r_("CAR")])
            yield

        def front(tile_idx, mlist, p):
            rz, rzo = r_("zT%d" % p), r_("zT%d" % (1 - p))
            rx, rh, rhT = r_("xt%d" % p), r_("hn%d" % p), r_("hnT%d" % p)
            dma(xt[p], xall[tile_idx * 128:(tile_idx + 1) * 128, :], [], [rx])
            rms(xt[p], hn[p], [rx], [rh], 0, junk)
            transp8(hn[p], [rh])
            act(hnT[p].rearrange("p k t -> p (k t)"), PB0[:], AF.Copy, [r_("PB0")], [rhT])
            yield
            cp("pool", zT[p][:, 0:4, 0:16], zT[1 - p][:, 0:4, 128:144], [rzo], [rz])
            for m in mlist:
                pb = PBs[0] if m < 4 else PBs[1]
                rpb = r_("bank%d" % (m // 4))
                o = pb[:, (m % 4) * 128:(m % 4 + 1) * 128]
                for k in range(8):
                    mm(o, WI[:, k, m * 128:(m + 1) * 128], hnT[p][:, k, :], k == 0, k == 7, [r_("WI"), rhT], [rpb])
            if 0 in mlist:
                act(zT[p][:, 0:4, 16:144], PBs[0][:].rearrange("p (m t) -> p m t", m=4), AF.Copy, [r_("bank0")], [rz])
            if 4 in mlist:
                act(zT[p][:, 4:8, 16:144], PBs[1][:].rearrange("p (m t) -> p m t", m=4), AF.Copy, [r_("bank1")], [rz])
            yield "F"

        PQ = PBs[6]

        def mixer(tile_idx, lt, first, p):
            rz = r_("zT%d" % p)
            ryT, ryg, rygf = r_("yT%d" % p), r_("yg%d" % p), r_("ygf%d" % p)
            yield from front(tile_idx, list(range(8)), p)
            for gi, w in enumerate(WINS):
                o = PQ[:, gi * 128:(gi + 1) * 128]
                for l in range(w):
                    mm(o, PW[:, 2 * gi + (1 if l > 0 else 0), :], zT[p][:, gi, 16 - l:144 - l], l == 0, (l == w - 1) and not first, [r_("PW"), rz], [r_("bank6")])
                if first:
                    S.op("dve", lambda e, gi=gi: e.tensor_tensor_scan(
                        out=cf[:, 0:16], data0=cstt[:, 257:258].to_broadcast([128, 16]), data1=zT[p][:, gi, 16:32],
                        initial=0.0, op0=ALU.mult, op1=ALU.add), rd=[rz, r_("cstt")], wr=[r_("cf")])
                    tt("dve", cf[:, 16:32], cf[:, 0:16], RB[:, 20 + gi * 16:36 + gi * 16], ALU.mult, [r_("cf"), r_("RB")], [r_("cf2")])
                    cp("dve", pzb[:, 0:16], cf[:, 16:32], [r_("cf2")], [r_("pzb")])
                    mm(o[:, 0:16], PW[:, 2 * gi + 1, :], pzb[:, 0:16], False, True, [r_("PW"), r_("pzb")], [r_("bank6")])
            for gi in range(4):
                act(yT[p][:, gi, :], PQ[:, gi * 128:(gi + 1) * 128], AF.Copy, [r_("bank6")], [ryT], scale=colt[:, c_ps_ + gi:c_ps_ + gi + 1])
            yield
            for tau in range(4):
                q = tau % 2
                yield from ssm_state(tau, True, p)
                o = PBs[0][:, tau * 128:(tau + 1) * 128]
                ry = r_("bank0")
                for gl in range(8):
                    mm(o, LC[:, tau * 8 + gl, :], XA[q][:, gl, :], gl == 0, False, [r_("LC"), r_("XA%d" % q)], [ry])
                mm(o, Dg[:, tau, :], zT[p][:, 4 + tau, 16:144], False, True, [r_("Dg"), rz], [ry])
                rg1, rg2 = r_("g1%d" % q), r_("g2%d" % q)
                act(g1[q], o, AF.Square, [ry], [rg1])
                tsc(g1[q], g1[q], 0.044715, 1.0, ALU.mult, ALU.add, [rg1], [rg1])
                tt("dve", g1[q], g1[q], o, ALU.mult, [rg1, ry], [rg1])
                act(g2[q], g1[q], AF.Tanh, [rg1], [rg2], scale=0.7978845608028654)
                tsc(g2[q], g2[q], 0.5, 0.5, ALU.mult, ALU.add, [rg2], [rg2])
                tt("dve", ygf[p][:, tau, :], g2[q], o, ALU.mult, [rg2, ry], [rygf])
                cp("pool", yg[p][:, tau, :], ygf[p][:, tau, :], [rygf], [ryg])
                yield
            for m in range(4):
                q = m % 2
                rg1 = r_("g1%d" % q)
                o = PBs[1][:, m * 128:(m + 1) * 128]
                for k in range(4):
                    mm(o, GW[:, k, m * 128:(m + 1) * 128], yg[p][:, k, :], k == 0, k == 3, [r_("GW"), ryg], [r_("bank1")])
                act(g1[q], o, AF.Tanh, [r_("bank1"), r_("sm2")], [rg1], scale=0.5, bias=sm[:, 64 + m:65 + m])
                tsc(g1[q], g1[q], 0.5, 0.5, ALU.mult, ALU.add, [rg1], [rg1])
                tt("pool", yT[p][:, 4 + m, :], g1[q], ygf[p][:, m, :], ALU.mult, [rg1, rygf], [ryT])
            yield
            for half in range(2):
                pb = PBs[half]
                rpb = r_("bank%d" % half)
                for k in range(8):
                    mm(pb[:], yT[p][:, k, :], WO[:, k, half * 512:(half + 1) * 512], k == 0, k == 7, [ryT, r_("WO")], [rpb])
                tt("dve", acc[:, lt, half * 512:(half + 1) * 512], pb[:], xt[p][:, half * 512:(half + 1) * 512], ALU.add, [rpb, r_("xt%d" % p)], [r_("acc%d" % lt)])
            yield
            rh = r_("hn%d" % p)
            rms(acc[:, lt, :], hn[p], [r_("acc%d" % lt)], [rh], 4, junk)
            transp8(hn[p], [rh])
            act(hn2T[:, :, lt * 128:(lt + 1) * 128], PB0[:].rearrange("p (k t) -> p k t", k=8), AF.Copy, [r_("PB0")], [r_("hn2T")])
            yield
            o = PQ[:, 0:20]
            for k in range(8):
                mm(o, hn2T[:, k, lt * 128:(lt + 1) * 128], WRb[:, k, :], k == 0, k == 7, [r_("hn2T"), r_("WRb")], [r_("bank6")])
            L_ = sm[:, 100:120]
            rS = r_("smr")
            tt("dve", L_, o, RB[:, 0:20], ALU.add, [r_("bank6"), r_("RB")], [rS])
            cL, fL = sm[:, 100:104], sm[:, 104:120]
            M, GM, CM, TH, NUM, SS, PG = smc(120), smc(121, 4), smc(125, 4), smc(129, 4), smc(133, 4), smc(137), smc(138)
            red(M, cL, ALU.max, [rS], [rS])
            tsc(GM, cL, M, None, ALU.is_equal, None, [rS], [rS])
            tsc(CM, cL, M, None, ALU.subtract, None, [rS], [rS])
            act(TH, CM, AF.Tanh, [rS], [rS], scale=0.5)
            tsc(NUM, TH, 1.0, None, ALU.add, None, [rS], [rS])
            tsc(TH, TH, -1.0, 1.0, ALU.mult, ALU.add, [rS], [rS])
            recip(TH, TH, [rS], [rS])
            tt("dve", NUM, NUM, TH, ALU.mult, [rS], [rS])
            red(SS, NUM, ALU.add, [rS], [rS])
            recip(PG, SS, [rS], [rS])
            FT = sm[:, 140:156]
            tt("dve", FT.rearrange("p (g j) -> p g j", g=4), fL.rearrange("p (g j) -> p g j", g=4),
               GM.unsqueeze(2).to_broadcast([128, 4, 4]), ALU.mult, [rS], [rS])
            FS = smc(156, 4)
            red(FS, FT.rearrange("p (g j) -> p j g", g=4), ALU.add, [rS], [rS])
            M1_, K1, F2, M2_, K2, DD, W1, W2, WJ = smc(160), smc(161, 4), smc(165, 4), smc(169), smc(170, 4), smc(174), smc(175), smc(176), smc(177, 4)
            red(M1_, FS, ALU.max, [rS], [rS])
            tsc(K1, FS, M1_, None, ALU.is_equal, None, [rS], [rS])
            stt(F2, K1, -1e30, FS, ALU.mult, ALU.add, [rS], [rS])
            red(M2_, F2, ALU.max, [rS], [rS])
            tsc(K2, F2, M2_, None, ALU.is_equal, None, [rS], [rS])
            tt("dve", DD, M2_, M1_, ALU.subtract, [rS], [rS])
            act(DD, DD, AF.Tanh, [rS], [rS], scale=0.5)
            tsc(W1, DD, -0.5, 0.5, ALU.mult, ALU.add, [rS], [rS])
            tsc(W2, DD, 0.5, 0.5, ALU.mult, ALU.add, [rS], [rS])
            tsc(WJ, K1, W1, None, ALU.mult, None, [rS], [rS])
            stt(WJ, K2, W2, WJ, ALU.mult, ALU.add, [rS], [rS])
            tsc(WJ, WJ, PG, None, ALU.mult, None, [rS], [rS])
            tt("dve", gates[:, lt, :].rearrange("p (g j) -> p g j", g=4), GM.unsqueeze(2).to_broadcast([128, 4, 4]),
               WJ.unsqueeze(1).to_broadcast([128, 4, 4]), ALU.mult, [rS], [r_("gates")])
            yield

        def router(lt, o, rq0):
            L_ = sm[:, 100:120]
            rS = r_("smr")
            tt("dve", L_, o, RB[:, 0:20], ALU.add, [rq0, r_("RB")], [rS])
            cL, fL = sm[:, 100:104], sm[:, 104:120]
            M, GM, CM, TH, NUM, SS, PG = smc(120), smc(121, 4), smc(125, 4), smc(129, 4), smc(133, 4), smc(137), smc(138)
            red(M, cL, ALU.max, [rS], [rS])
            tsc(GM, cL, M, None, ALU.is_equal, None, [rS], [rS])
            tsc(CM, cL, M, None, ALU.subtract, None, [rS], [rS])
            act(TH, CM, AF.Tanh, [rS], [rS], scale=0.5)
            tsc(NUM, TH, 1.0, None, ALU.add, None, [rS], [rS])
            tsc(TH, TH, -1.0, 1.0, ALU.mult, ALU.add, [rS], [rS])
            recip(TH, TH, [rS], [rS])
            tt("dve", NUM, NUM, TH, ALU.mult, [rS], [rS])
            red(SS, NUM, ALU.add, [rS], [rS])
            recip(PG, SS, [rS], [rS])
            FT = sm[:, 140:156]
            tt("dve", FT.rearrange("p (g j) -> p g j", g=4), fL.rearrange("p (g j) -> p g j", g=4),
               GM.unsqueeze(2).to_broadcast([128, 4, 4]), ALU.mult, [rS], [rS])
            FS = smc(156, 4)
            red(FS, FT.rearrange("p (g j) -> p j g", g=4), ALU.add, [rS], [rS])
            M1_, K1, F2, M2_, K2, DD, W1, W2, WJ = smc(160), smc(161, 4), smc(165, 4), smc(169), smc(170, 4), smc(174), smc(175), smc(176), smc(177, 4)
            red(M1_, FS, ALU.max, [rS], [rS])
            tsc(K1, FS, M1_, None, ALU.is_equal, None, [rS], [rS])
            stt(F2, K1, -1e30, FS, ALU.mult, ALU.add, [rS], [rS])
            red(M2_, F2, ALU.max, [rS], [rS])
            tsc(K2, F2, M2_, None, ALU.is_equal, None, [rS], [rS])
            tt("dve", DD, M2_, M1_, ALU.subtract, [rS], [rS])
            act(DD, DD, AF.Tanh, [rS], [rS], scale=0.5)
            tsc(W1, DD, -0.5, 0.5, ALU.mult, ALU.add, [rS], [rS])
            tsc(W2, DD, 0.5, 0.5, ALU.mult, ALU.add, [rS], [rS])
            tsc(WJ, K1, W1, None, ALU.mult, None, [rS], [rS])
            stt(WJ, K2, W2, WJ, ALU.mult, ALU.add, [rS], [rS])
            tsc(WJ, WJ, PG, None, ALU.mult, None, [rS], [rS])
            tt("dve", gates[:, lt, :].rearrange("p (g j) -> p g j", g=4), GM.unsqueeze(2).to_broadcast([128, 4, 4]),
               WJ.unsqueeze(1).to_broadcast([128, 4, 4]), ALU.mult, [rS], [r_("gates")])

        RRs = [M2[0], M2[1]]

        def build_RR(tau, q):
            gs = slice(tau * 8, tau * 8 + 8)
            act(RRs[q], MAG[:, gs].unsqueeze(2).to_broadcast([128, 8, 128]), AF.Copy, [r_("MAG")], [r_("RR%d" % q)])
            S.op("pool", lambda e: e.memset(RRs[q][:, :, 0:1], 0.0), rd=[], wr=[r_("RR%d" % q)])

        def run_gen(g):
            for _ in g:
                pass

        def g_front(tile_idx, lt, p):
            rz, rzo = r_("zT%d" % p), r_("zT%d" % (1 - p))
            rx, rh, rhT = r_("xt%d" % p), r_("hn%d" % p), r_("hnT%d" % p)
            dma(xt[p], xall[tile_idx * 128:(tile_idx + 1) * 128, :], [], [rx])
            dma(acc[:, lt, :], xall[tile_idx * 128:(tile_idx + 1) * 128, :], [], [r_("acc%d" % lt)])
            rms_a(xt[p], [rx], 0, hn[p], rh)
            yield
            rms_b(xt[p], hn[p], [rx], [rh], 0)
            yield
            transp8(hn[p], [rh])
            act(hnT[p].rearrange("p k t -> p (k t)"), PB0[:], AF.Copy, [r_("PB0")], [rhT])
            yield
            cp("pool", zT[p][:, 0:4, 0:16], zT[1 - p][:, 0:4, 128:144], [rzo], [rz])
            for m in range(8):
                pb = PBs[0] if m < 4 else PBs[1]
                rpb = r_("bank%d" % (m // 4))
                o = pb[:, (m % 4) * 128:(m % 4 + 1) * 128]
                for k in range(8):
                    mm(o, WI[:, k, m * 128:(m + 1) * 128], hnT[p][:, k, :], k == 0, k == 7, [r_("WI"), rhT], [rpb])
                if m == 3:
                    act(zT[p][:, 0:4, 16:144], PBs[0][:].rearrange("p (m t) -> p m t", m=4), AF.Copy, [r_("bank0")], [rz])
            act(zT[p][:, 4:8, 16:144], PBs[1][:].rearrange("p (m t) -> p m t", m=4), AF.Copy, [r_("bank1")], [rz])
            yield

        def st_pool(p, first):
            rz, ryT = r_("zT%d" % p), r_("yT%d" % p)
            rq = r_("bank6")
            for gi, w in enumerate(WINS):
                o = PQ[:, gi * 128:(gi + 1) * 128]
                for l in range(w):
                    mm(o, PW[:, 2 * gi + (1 if l > 0 else 0), :], zT[p][:, gi, 16 - l:144 - l], l == 0, (l == w - 1) and not first, [r_("PW"), rz], [rq])
                if first:
                    S.op("dve", lambda e, gi=gi: e.tensor_tensor_scan(
                        out=cf[:, 0:16], data0=cstt[:, 257:258].to_broadcast([128, 16]), data1=zT[p][:, gi, 16:32],
                        initial=0.0, op0=ALU.mult, op1=ALU.add), rd=[rz, r_("cstt")], wr=[r_("cf")])
                    tt("dve", cf[:, 16:32], cf[:, 0:16], RB[:, 20 + gi * 16:36 + gi * 16], ALU.mult, [r_("cf"), r_("RB")], [r_("cf2")])
                    cp("dve", pzb[:, 0:16], cf[:, 16:32], [r_("cf2")], [r_("pzb")])
                    mm(o[:, 0:16], PW[:, 2 * gi + 1, :], pzb[:, 0:16], False, True, [r_("PW"), r_("pzb")], [rq])
            for gi in range(4):
                act(yT[p][:, gi, :], PQ[:, gi * 128:(gi + 1) * 128], AF.Copy, [rq], [ryT], scale=colt[:, c_ps_ + gi:c_ps_ + gi + 1])

        def st_Dp(p, tau):
            g0 = tau * 8
            rz = r_("zT%d" % p)
            for hh in range(2):
                ba, bb = PBs[2 + hh * 2], PBs[3 + hh * 2]
                rba, rbb = r_("bank%d" % (2 + hh * 2)), r_("bank%d" % (3 + hh * 2))
                bav = ba[:].rearrange("p (g t) -> p g t", g=4)
                bbv = bb[:].rearrange("p (g t) -> p g t", g=4)
                for gl4 in range(4):
                    g = g0 + hh * 4 + gl4
                    mm(bav[:, gl4, :], LBa[:, g, :], zT[p][:, 4 + tau, 16:144], True, True, [r_("LBa"), rz], [rba])
                    mm(bbv[:, gl4, :], LBb[:, g, :], zT[p][:, 4 + tau, 16:144], True, True, [r_("LBb"), rz], [rbb])

        def st_Dd(tau, q):
            g0 = tau * 8
            rG, rM1 = r_("Gt%d" % q), r_("M1t")
            for hh in range(2):
                ba, bb = PBs[2 + hh * 2], PBs[3 + hh * 2]
                rba, rbb = r_("bank%d" % (2 + hh * 2)), r_("bank%d" % (3 + hh * 2))
                bav = ba[:].rearrange("p (g t) -> p g t", g=4)
                bbv = bb[:].rearrange("p (g t) -> p g t", g=4)
                sl = slice(hh * 4, hh * 4 + 4)
                gs = slice(g0 + hh * 4, g0 + hh * 4 + 4)
                tt("dve", Gt[q][:, sl, :], bav, COS[:, gs, :], ALU.mult, [rba, r_("tab")], [rG])
                tt("dve", M1[0][:, sl, :], bbv, SINM[:, gs, :], ALU.mult, [rbb, r_("tab")], [rM1])
                tt("dve", Gt[q][:, sl, :], Gt[q][:, sl, :], M1[0][:, sl, :], ALU.add, [rM1], [rG])

        def st_S(tau, q):
            g0 = tau * 8
            gs = slice(g0, g0 + 8)
            rG, rX = r_("Gt%d" % q), r_("XT%d" % q)
            c8 = sm[:, 80:88]
            tt("dve", c8, MAG[:, gs], CAR[:, gs], ALU.mult, [r_("MAG"), r_("CAR")], [r_("c8")])
            tt("dve", Gt[q][:, :, 0], Gt[q][:, :, 0], c8, ALU.add, [r_("c8")], [rG])
            S.op("dve", lambda e, q=q: e.tensor_tensor_scan(
                out=XT[q].rearrange("p a b -> p (a b)"), data0=RRs[q].rearrange("p a b -> p (a b)"),
                data1=Gt[q].rearrange("p a b -> p (a b)"), initial=0.0, op0=ALU.mult, op1=ALU.add),
                rd=[rG, r_("RR%d" % q)], wr=[rX])
            xs = sm[:, 16 + 8 * q:24 + 8 * q]
            dma(xs[0:64, :], XT[q][64:128, :, 127], [rX], [r_("xs%d" % q)], slow=True)
            dma(xs[64:128, :], XT[q][0:64, :, 127], [rX], [r_("xsb%d" % q)], slow=True)
            build_RR((tau + 2) % 4, q)

        M1b = [XA[0], XA[1]]
        M2b = [M1[1].rearrange("p a b -> p (a b)").bitcast(BF16)[:, i * 1024:(i + 1) * 1024].rearrange("p (a b) -> p a b", a=8) for i in range(2)]

        def st_M(tau, q):
            g0 = tau * 8
            gs = slice(g0, g0 + 8)
            rX = r_("XT%d" % q)
            tt("dve", M1b[q], XT[q], COS[:, gs, :], ALU.mult, [rX, r_("tab")], [r_("XA%d" % q)])
            tt("dve", M2b[q], XT[q], SINM[:, gs, :], ALU.mult, [rX, r_("tab")], [r_("M2b%d" % q)])
            xs = sm[:, 16 + 8 * q:24 + 8 * q]
            t1c = sm[:, 32 + 8 * q:40 + 8 * q]
            tt("dve", t1c, XT[q][:, :, 127], COS[:, gs, 127], ALU.mult, [rX, r_("tab")], [r_("t1c%d" % q)])
            tt("dve", xs, xs, SINM[:, gs, 127], ALU.mult, [r_("xs%d" % q), r_("xsb%d" % q), r_("tab")], [r_("xs%d" % q), r_("xsb%d" % q)])
            tt("dve", CAR[:, gs], t1c, xs, ALU.subtract, [r_("t1c%d" % q), r_("xs%d" % q), r_("xsb%d" % q)], [r_("CAR")])

        def st_Cp(p, tau, q):
            rz = r_("zT%d" % p)
            o = PQ[:, tau * 128:(tau + 1) * 128]
            ry = r_("bank6")
            for gl in range(8):
                g = tau * 8 + gl
                mm(o, LC[:, g, :], M1b[q][:, gl, :], gl == 0, False, [r_("LC"), r_("XA%d" % q)], [ry])
                mm(o, LCsv[g // 16][:, g % 16, :], M2b[q][:, gl, :], False, False, [r_("LCs"), r_("M2b%d" % q)], [ry])
            mm(o, Dg[:, tau, :], zT[p][:, 4 + tau, 16:144], False, True, [r_("Dg"), rz], [ry])
            act(g1[q], o, AF.Square, [ry], [r_("g1%d" % q)], scale=0.21145921592590347)

        def st_Ca(p, tau, q):
            o = PQ[:, tau * 128:(tau + 1) * 128]
            ry = r_("bank6")
            rg1, rg2 = r_("g1%d" % q), r_("g2%d" % q)
            stt(g1[q], g1[q], 1.0, o, ALU.add, ALU.mult, [rg1, ry], [rg1])
            act(g2[q], g1[q], AF.Tanh, [rg1], [rg2], scale=0.7978845608028654)

        def st_Cb(p, tau, q):
            ryg, rygf = r_("yg%d" % p), r_("ygf%d" % p)
            o = PQ[:, tau * 128:(tau + 1) * 128]
            ry = r_("bank6")
            rg2 = r_("g2%d" % q)
            stt(ygf[p][:, tau, :], g2[q], 1.0, o, ALU.add, ALU.mult, [rg2, ry], [rygf])
            act(yg[p][:, tau, :], ygf[p][:, tau, :], AF.Copy, [rygf], [ryg])

        def g_tail(lt, p):
            ryT, ryg, rygf = r_("yT%d" % p), r_("yg%d" % p), r_("ygf%d" % p)
            jf = junk.bitcast(F32)
            glt = [jf[:, i * 128:(i + 1) * 128] for i in range(4)]
            rgl = [r_("glt%d" % i) for i in range(4)]
            rbk = [r_("bank1"), r_("bank0")]
            for m in range(4):
                pbm = PBs[1] if m % 2 == 0 else PBs[0]
                rb = rbk[m % 2]
                o = pbm[:, (m // 2) * 128:(m // 2 + 1) * 128]
                for k in range(4):
                    mm(o, GW[:, k, m * 128:(m + 1) * 128], yg[p][:, k, :], k == 0, k == 3, [r_("GW"), ryg], [rb])
                act(glt[m], o, AF.Tanh, [rb, r_("sm2")], [rgl[m]], scale=0.5, bias=sm[:, 64 + m:65 + m])
            yield
            for m in range(4):
                stt(yT[p][:, 4 + m, :], glt[m], 1.0, ygf[p][:, m, :], ALU.add, ALU.mult, [rgl[m], rygf], [ryT])
            for half in range(2):
                pb = PBs[half]
                rqs = [r_("bank%d" % half)]
                for k in range(8):
                    mm(pb[:], yT[p][:, k, :], WO[:, k, half * 512:(half + 1) * 512], k == 0, k == 7, [ryT, r_("WO")], rqs)
            yield
            for half in range(2):
                pb = PBs[half]
                rqs = [r_("bank%d" % half)]
                tt("dve", acc[:, lt, half * 512:(half + 1) * 512], pb[:], acc[:, lt, half * 512:(half + 1) * 512], ALU.add, rqs + [r_("acc%d" % lt)], [r_("acc%d" % lt)])
            rh = r_("hn2b%d" % p)
            rms_a(acc[:, lt, :], [r_("acc%d" % lt)], 4, hn2b[p], rh)
            yield
            rms_b(acc[:, lt, :], hn2b[p], [r_("acc%d" % lt)], [rh], 4)
            yield
            transp8(hn2b[p], [rh])
            act(hn2T[:, :, lt * 128:(lt + 1) * 128], PB0[:].rearrange("p (k t) -> p k t", k=8), AF.Copy, [r_("PB0")], [r_("hn2T")])
            yield
            o = PQ[:, 0:20]
            rq0 = r_("bank6")
            for k in range(8):
                mm(o, hn2T[:, k, lt * 128:(lt + 1) * 128], WRb[:, k, :], k == 0, k == 7, [r_("hn2T"), r_("WRb")], [rq0])
            router(lt, o, rq0)
            yield

        def mixer_sb(sbi):
            nun = SBT * 4
            par = lambda lt: (sbi * SBT + lt) % 2
            bg = []

            def advance():
                for g in list(bg):
                    try:
                        next(g)
                    except StopIteration:
                        bg.remove(g)
            run_gen(g_front(NT_PRE + sbi * SBT, 0, par(0)))
            build_RR(0, 0)
            build_RR(1, 1)
            st_Dp(par(0), 0)
            if SBT > 1:
                bg.append(g_front(NT_PRE + sbi * SBT + 1, 1, par(1)))
            k = 0
            while k < nun + 4 or bg:
                advance()
                if 0 <= k - 4 < nun:
                    lt3, tau3 = divmod(k - 4, 4)
                    st_Ca(par(lt3), tau3, (k - 4) % 2)
                if 0 <= k - 3 < nun:
                    st_M((k - 3) % 4, (k - 3) % 2)
                if 0 <= k - 4 < nun:
                    st_Cb(par(lt3), tau3, (k - 4) % 2)
                    if tau3 == 3:
                        bg.append(g_tail(lt3, par(lt3)))
                if k < nun:
                    lt, tau = divmod(k, 4)
                    if tau == 2:
                        st_pool(par(lt), sbi == 0 and lt == 0)
                    st_Dd(tau, k % 2)
                    if tau == 3 and 1 <= lt + 1 and lt + 2 < SBT:
                        bg.append(g_front(NT_PRE + sbi * SBT + lt + 2, lt + 2, par(lt + 2)))
                if 0 <= k - 1 < nun:
                    st_S((k - 1) % 4, (k - 1) % 2)
                if 0 <= k - 3 < nun:
                    ltp, taup = divmod(k - 3, 4)
                    st_Cp(par(ltp), taup, (k - 3) % 2)
                if k + 1 < nun:
                    ltn, taun = divmod(k + 1, 4)
                    st_Dp(par(ltn), taun)
                k += 1

        def pipeline(gens):
            gens = list(gens)
            active = []
            while gens or active:
                if gens and len(active) < 2 and (not active or active[-1][1][0]):
                    active.append((gens.pop(0), [False]))
                for it in list(active):
                    g, st = it
                    try:
                        v = next(g)
                        if v == "F":
                            st[0] = True
                    except StopIteration:
                        active.remove(it)

        wgb = nc.dram_tensor("wgb", [16, 128, 2048], BF16).ap()
        wub = nc.dram_tensor("wub", [16, 128, 2048], BF16).ap()
        wdb = nc.dram_tensor("wdb", [16, 128, 2048], BF16).ap()
        accf = acc[:].rearrange("p a b -> p (a b)")
        hn2f = hn2T[:].rearrange("p a b -> p (a b)")
        cast_units = []
        for e in range(16):
            cast_units += [(0, e), (1, e), (2, e)]
        ucnt = [0]

        def cast_unit():
            if not cast_units:
                return
            kind, e = cast_units.pop(0)
            sl = ucnt[0] % 2
            ucnt[0] += 1
            stg = accf[:, sl * 2048:(sl + 1) * 2048]
            tmp = hn2f[:, sl * 2048:(sl + 1) * 2048]
            rs_, rt_ = r_("stg%d" % sl), r_("tmpb%d" % sl)
            if kind == 2:
                dma(stg.rearrange("p (k n) -> p k n", k=2), wd_d[e, :, :].rearrange("(k p) n -> p k n", p=128), [], [rs_], q="pool")
                cp("pool", tmp, stg, [rs_], [rt_])
                dma(wdb[e, :, :], tmp, [rt_], [r_("wscr")], q="pool")
            else:
                src = wg_d if kind == 0 else wu_d
                dst = wgb if kind == 0 else wub
                dma(stg.rearrange("p (k n) -> p k n", k=8), src[e, :, :].rearrange("(k p) n -> p k n", p=128), [], [rs_], q="pool")
                tt("pool", tmp.rearrange("p (k n) -> p k n", k=8), stg.rearrange("p (k n) -> p k n", k=8),
                   colt[:, c_nf:c_nf + 8].unsqueeze(2).to_broadcast([128, 8, 256]), ALU.mult, [rs_, r_("colt")], [rt_])
                dma(dst[e, :, :], tmp, [rt_], [r_("wscr")], q="pool")

        def moe(sbi):
            dma(NF, rows[0:1, 0:1024].partition_broadcast(128), [], [r_("NF")])
            nblk = SBT // 4
            tok = slice(0, SBT * 128)

            def gu(e):
                sl = e % 2
                rwg, rwu, rwd = r_("WG%d" % sl), r_("WU%d" % sl), r_("WD%d" % sl)
                dma(WGs[sl].rearrange("p k n -> p (k n)"), wgb[e, :, :], [r_("wscr")], [rwg])
                dma(WUs[sl].rearrange("p k n -> p (k n)"), wub[e, :, :], [r_("wscr")], [rwu])
                dma(WDs[sl].rearrange("p k n -> p (k n)"), wdb[e, :, :], [r_("wscr")], [rwd])
                for ft in range(2):
                    gp, up = PBs[2 + ft], PBs[4 + ft]
                    rg, ru = r_("bank%d" % (2 + ft)), r_("bank%d" % (4 + ft))
                    rsg, rhT_ = r_("sgT%d%d" % (sl, ft)), r_("hT%d%d" % (sl, ft))
                    for k in range(8):
                        mm(gp[:], WGs[sl][:, k, ft * 128:(ft + 1) * 128], hn2T[:, k, tok], k == 0, k == 7, [rwg, r_("hn2T")], [rg])
                    for k in range(8):
                        mm(up[:], WUs[sl][:, k, ft * 128:(ft + 1) * 128], hn2T[:, k, tok], k == 0, k == 7, [rwu, r_("hn2T")], [ru])
                    act(sgT[sl][:, ft, :], gp[:], AF.Silu, [rg], [rsg])
                    tt("dve", hT[sl][:, ft, :], up[:], sgT[sl][:, ft, :], ALU.mult, [ru, rsg], [rhT_])

            def down(e):
                sl = e % 2
                rwd = r_("WD%d" % sl)
                for lt in range(SBT):
                    for half in range(2):
                        pb = PBs[half]
                        rpb = r_("bank%d" % half)
                        for ft in range(2):
                            mm(pb[:], hT[sl][:, ft, lt * 128:(lt + 1) * 128], WDs[sl][:, ft, half * 512:(half + 1) * 512], ft == 0, ft == 1,
                               [r_("hT%d%d" % (sl, ft)), rwd], [rpb])
                        stt(acc[:, lt, half * 512:(half + 1) * 512], pb[:], gates[:, lt, e:e + 1], acc[:, lt, half * 512:(half + 1) * 512],
                            ALU.mult, ALU.add, [rpb, r_("gates"), r_("acc%d" % lt)], [r_("acc%d" % lt)])
            for e in range(17):
                if e < 16:
                    gu(e)
                if e >= 1:
                    down(e - 1)
            for lt in range(SBT):
                tix = sbi * SBT + lt
                o2 = lt % 2
                ro = r_("outt%d" % o2)
                S.op("dve", lambda e: e.memset(smc(8), 0.0), wr=[r_("sm8")])
                act(junk2, acc[:, lt, :], AF.Square, [r_("acc%d" % lt)], [r_("junk2"), r_("sm8")], accum_out=smc(8))
                act(smc(9), smc(8), AF.Sqrt, [r_("sm8")], [r_("sm8")], scale=1.0 / 1024.0, bias=EPS)
                recip(smc(10), smc(9), [r_("sm8")], [r_("sm8")])
                stt(outt[o2], acc[:, lt, :], smc(10), NF, ALU.mult, ALU.mult, [r_("acc%d" % lt), r_("sm8"), r_("NF")], [ro])
                dma(out[tix * 128:(tix + 1) * 128, :], outt[o2], [ro], [r_("out")])

        S.barrier()
        def pre_tile(t, p):
            last = t == NT_PRE - 1
            yield from front(t, [0, 1, 2, 3] if last else [], p)
            rhT = r_("hnT%d" % p)
            for k in range(8):
                mm(PBs[1][:], hnT[p][:, k, :], WI[:, k, 512:1024], k == 0, k == 7, [r_("WI"), rhT], [r_("bank1")])
            act(ztok[p], PBs[1][:], AF.Copy, [r_("bank1")], [r_("ztok%d" % p)])
            yield
            ya, yb = PBs[2 + 2 * p], PBs[3 + 2 * p]
            rya, ryb = r_("bank%d" % (2 + 2 * p)), r_("bank%d" % (3 + 2 * p))
            for g in range(32):
                mm(ya[:, g * 16:(g + 1) * 16], VTa[:, g, :], ztok[p][:, g * 16:(g + 1) * 16], True, True, [r_("VTa"), r_("ztok%d" % p)], [rya])
            for g in range(32):
                mm(yb[:, g * 16:(g + 1) * 16], VTb[:, g, :], ztok[p][:, g * 16:(g + 1) * 16], True, True, [r_("VTb"), r_("ztok%d" % p)], [ryb])
            yield
            tt("dve", Ytmp[0], ya[:], BBR.rearrange("p a b -> p (a b)"), ALU.mult, [rya, r_("BBR")], [r_("Ytmp0")])
            tt("dve", Ytmp[1], yb[:], BBIs.rearrange("p a b -> p (a b)"), ALU.mult, [ryb, r_("BBI")], [r_("Ytmp1")])
            tt("dve", Ytmp[0], Ytmp[0], Ytmp[1], ALU.add, [r_("Ytmp1")], [r_("Ytmp0")])
            red(Ssum, Ytmp[0].rearrange("p (a b) -> p a b", a=32), ALU.add, [r_("Ytmp0")], [r_("Ssum")])
            tt("dve", Ctmp[:, 0:32], A128[:, 0:32], CAR[:], ALU.mult, [r_("A128"), r_("CAR")], [r_("ctmp")])
            tt("dve", Ctmp[:, 32:64], A128[:, 32:64], CARb, ALU.mult, [r_("A128"), r_("CARb"), r_("CARb2")], [r_("ctmp2")])
            tt("dve", Ctmp[:, 0:32], Ctmp[:, 0:32], Ctmp[:, 32:64], ALU.add, [r_("ctmp2")], [r_("ctmp")])
            tt("dve", CAR[:], Ctmp[:, 0:32], Ssum, ALU.add, [r_("ctmp"), r_("Ssum")], [r_("CAR")])
            dma(CARb[0:64, :], CAR[64:128, :], [r_("CAR")], [r_("CARb")])
            dma(CARb[64:128, :], CAR[0:64, :], [r_("CAR")], [r_("CARb2")])
            cast_unit()
            if t % 2 == 1:
                cast_unit()
            yield
        pipeline([pre_tile(t, t % 2) for t in range(NT_PRE)])
        while cast_units:
            cast_unit()
        S.barrier()
        for sbi in range(NT_MAIN // SBT):
            mixer_sb(sbi)
            S.barrier()
            moe(sbi)
            S.barrier()
        S.final_wait("sp", [r_("out")])

        print('SBUF remaining', nc.sbuf_bytes_remaining, 'mixer_end', mixer_end, 'moe_end', _off[0])
        block = es.enter_context(nc.Block())

        @block.sync
        def _(e):
            S.replay("sp", e)

        @block.tensor
        def _(e):
            S.replay("pe", e)

        @block.scalar
        def _(e):
            S.replay("act", e)

        @block.vector
        def _(e):
            S.replay("dve", e)

        @block.gpsimd
        def _(e):
            S.replay("pool", e)
    return nc


def _col(v, k):
    return np.ascontiguousarray(np.asarray(v, np.float32).reshape(k, 128).T)


def kernel(x, norm_mix, w_in, pool_w, pool_scale, ssm_a_re, ssm_a_im, ssm_log_step,
           ssm_b_re, ssm_b_im, ssm_c_re, ssm_c_im, ssm_d, glu_w, glu_b, w_out, norm_ffn,
           router_coarse_w, router_coarse_b, router_fine_w, router_fine_b,
           exp_w_gate, exp_w_up, exp_w_down, norm_final):
    f = np.float32
    x = np.asarray(x, f)
    cols = np.zeros((128, 64), f)
    cols[:, 0:8] = _col(norm_mix[0], 8)
    cols[:, 8:16] = _col(norm_ffn[0], 8)
    cols[:, 16:20] = _col(pool_scale[0], 4)
    cols[:, 20:24] = _col(ssm_d[0], 4)
    cols[:, 24:28] = _col(glu_b[0], 4)
    are = np.asarray(ssm_a_re[0], f)
    aim = np.asarray(ssm_a_im[0], f)
    ls = np.asarray(ssm_log_step[0], f)
    sp_s = np.zeros((128, 96), f)
    sp_s[:, 0:32] = np.concatenate([are.T, are.T], 0)
    sp_s[:, 32:64] = np.concatenate([aim.T, aim.T], 0)
    sp_s[:, 64:96] = np.broadcast_to(ls[None, :], (128, 32))
    br = np.asarray(ssm_b_re[0], f).transpose(1, 0, 2)
    bi = np.asarray(ssm_b_im[0], f).transpose(1, 0, 2)
    b1 = np.ascontiguousarray(np.concatenate([br, bi], 0))
    b2 = np.ascontiguousarray(np.concatenate([bi, br], 0))
    cr = np.asarray(ssm_c_re[0], f).transpose(2, 0, 1)
    ci = np.asarray(ssm_c_im[0], f).transpose(2, 0, 1)
    c1 = np.ascontiguousarray(np.concatenate([cr, ci], 0))
    c2 = np.ascontiguousarray(np.concatenate([ci, cr], 0))
    cst = np.zeros((128, 386), f)
    cst[:, 258:386] = np.arange(127, -1, -1, dtype=f)[None, :]
    cst[:, 0:128] = np.eye(128, dtype=f)
    cst[:, 128:256] = np.arange(1, 129, dtype=f)[None, :]
    cst[:, 256] = np.where(np.arange(128) < 64, 1.0, -1.0)
    cst[:, 257] = 1.0
    wr = np.ascontiguousarray(np.concatenate([np.asarray(router_coarse_w[0], f), np.asarray(router_fine_w[0], f)], 1))
    rb = np.concatenate([np.asarray(router_coarse_b[0], f), np.asarray(router_fine_b[0], f)])
    in_maps = []
    for c in range(8):
        b, half = c // 2, c % 2
        main = x[b, half * 4096:(half + 1) * 4096]
        pre = x[b, 0:4096] if half == 1 else np.zeros((4096, 1024), f)
        fix = np.zeros((4, 16), f)
        if half == 0:
            for gi, w in enumerate(WINS):
                for t in range(w - 1):
                    fix[gi, t] = w / (t + 1.0) - 1.0
        rows = np.concatenate([np.asarray(norm_final, f), rb, fix.reshape(-1)])[None, :].astype(f)
        in_maps.append({
            "xall": np.ascontiguousarray(np.concatenate([pre, main], 0)),
            "w_in": np.asarray(w_in[0], f), "w_out": np.asarray(w_out[0], f), "glu_w": np.asarray(glu_w[0], f),
            "pool_w": np.asarray(pool_w[0], f), "cols": cols, "rows": rows, "sp_s": sp_s,
            "b1_s": b1, "b2_s": b2, "c1_s": c1, "c2_s": c2, "cst": cst, "wr": wr,
            "wg": np.asarray(exp_w_gate[0], f), "wu": np.asarray(exp_w_up[0], f), "wd": np.asarray(exp_w_down[0], f),
        })
    nc = build()
    res = run_bass_kernel_spmd(nc, in_maps, core_ids=list(range(8)))
    outs = [np.asarray(r["out"], f) for r in res.results]
    full = np.zeros((4, 8192, 1024), f)
    for c in range(8):
        b, half = c // 2, c % 2
        full[b, half * 4096:(half + 1) * 4096] = outs[c]
    return full
```

```python
import math
import numpy as np
from contextlib import ExitStack
import concourse.bass as bass
import concourse.mybir as mybir
from concourse.bass_utils import run_bass_kernel_spmd

F32 = mybir.dt.float32
BF16 = mybir.dt.bfloat16
I32 = mybir.dt.int32
AF = mybir.ActivationFunctionType
ALU = mybir.AluOpType
AX = mybir.AxisListType

NT_PRE = 32
NT_MAIN = 32
SBT = 4
EPS = 1e-6
WINS = (2, 4, 8, 16)
TWO_PI = 2.0 * math.pi


class R:
    def __init__(self, name):
        self.name = name
        self.w = None
        self.rd = {}


class Sched:
    ENG = ("pe", "act", "dve", "pool", "sp")

    def __init__(self, nc, es):
        self.nc = nc
        self.sem = {e: es.enter_context(nc.semaphore("s_" + e)) for e in self.ENG}
        self.cnt = {e: 0 for e in self.ENG}
        self.ops = {e: [] for e in self.ENG}
        self.seen = {e: {} for e in self.ENG}
        self.dma_pool = [es.enter_context(nc.semaphore("d%d" % i)) for i in range(48)]
        self.dma_cnt = {}
        self.dma_of = {}

    def dsem(self, res):
        if res.name not in self.dma_of:
            s = self.dma_pool[len(self.dma_of)]
            self.dma_of[res.name] = s
            self.dma_cnt[s.name] = 0
        return self.dma_of[res.name]

    def op(self, eng, fn, rd=(), wr=(), dma=None):
        need = {}

        def add(tok):
            if tok is None:
                return
            s, v = tok
            if need.get(s.name, (None, -1))[1] < v:
                need[s.name] = (s, v)
        for r in rd:
            add(r.w)
        for r in wr:
            add(r.w)
            for t in r.rd.values():
                add(t)
        waits = []
        for name, (s, v) in need.items():
            if eng == "pe" and s is self.sem["pe"]:
                continue
            if self.seen[eng].get(name, -1) >= v:
                continue
            self.seen[eng][name] = v
            waits.append((s, v))
        if dma is not None:
            s = self.dsem(dma)
            self.dma_cnt[s.name] += 16
            tok = (s, self.dma_cnt[s.name])
            inc = (s, 16)
        else:
            self.cnt[eng] += 1
            tok = (self.sem[eng], self.cnt[eng])
            inc = (self.sem[eng], 1)
        for r in rd:
            old = r.rd.get(tok[0].name)
            if old is None or old[1] < tok[1]:
                r.rd[tok[0].name] = tok
        for r in wr:
            r.w = tok
            r.rd = {}
        self.ops[eng].append((waits, fn, inc))
        return tok

    def final_wait(self, eng, ress):
        waits = []
        for r in ress:
            if r.w is not None:
                waits.append(r.w)
        self.ops[eng].append((waits, None, None))

    def barrier(self):
        toks = [(self.sem[e], self.cnt[e]) for e in self.ENG if self.cnt[e] > 0]
        for name, sm_ in self.dma_of.items():
            toks.append((sm_, self.dma_cnt[sm_.name]))
        for eng in self.ENG:
            waits = []
            for s_, v in toks:
                if s_ is self.sem[eng]:
                    continue
                if self.seen[eng].get(s_.name, -1) >= v:
                    continue
                self.seen[eng][s_.name] = v
                waits.append((s_, v))
            if waits:
                self.ops[eng].append((waits, None, None))

    def replay(self, eng, e):
        for waits, fn, inc in self.ops[eng]:
            for s, v in waits:
                e.wait_ge(s, v)
            if fn is not None:
                fn(e).then_inc(inc[0], inc[1])


def build(debug=False):
    nc = bass.Bass("TRN2", target_bir_lowering=False)

    def din(name, shape, dt=F32):
        return nc.dram_tensor(name, list(shape), dt, kind="ExternalInput").ap()
    xall = din("xall", [(NT_PRE + NT_MAIN) * 128, 1024])
    w_in = din("w_in", [1024, 1024])
    w_out = din("w_out", [1024, 1024])
    glu_w = din("glu_w", [512, 512])
    pool_w = din("pool_w", [4, 128, 128])
    cols = din("cols", [128, 64])
    rows = din("rows", [1, 1024 + 20 + 64])
    sp_s = din("sp_s", [128, 96])
    b1_s = din("b1_s", [128, 32, 16])
    b2_s = din("b2_s", [128, 32, 16])
    c1_s = din("c1_s", [128, 32, 16])
    c2_s = din("c2_s", [128, 32, 16])
    cst = din("cst", [128, 386])
    wr_d = din("wr", [1024, 20])
    wg_d = din("wg", [16, 1024, 256])
    wu_d = din("wu", [16, 1024, 256])
    wd_d = din("wd", [16, 256, 1024])
    out = nc.dram_tensor("out", [NT_MAIN * 128, 1024], F32, kind="ExternalOutput").ap()

    es = ExitStack()
    with es:
        S = Sched(nc, es)

        def sb(name, shape, dt=F32):
            return es.enter_context(nc.sbuf_tensor(name, list(shape), dt))

        def ps(name, shape, dt=F32):
            return es.enter_context(nc.psum_tensor(name, list(shape), dt))

        ident = sb("ident", [128, 128], BF16)
        cstt = sb("cstt", [128, 386])
        colt = sb("colt", [128, 64])
        RB = sb("RB", [128, 84])
        WI = sb("WI", [128, 8, 1024], BF16)
        WO = sb("WO", [128, 8, 1024], BF16)
        GW = sb("GW", [128, 4, 512], BF16)
        PW = sb("PW", [128, 8, 128], BF16)
        WRb = sb("WRb", [128, 8, 20], BF16)
        LBa = sb("LBa", [128, 32, 128], BF16)
        LBb = sb("LBb", [128, 32, 128], BF16)
        LC = sb("LC", [128, 32, 128], BF16)
        Dg = sb("Dg", [128, 4, 128], BF16)
        COS = sb("COS", [128, 32, 128])
        SINM = sb("SINM", [128, 32, 128])
        MAG = sb("MAG", [128, 32])
        CAR = sb("CAR", [128, 32])
        acc = sb("acc", [128, SBT, 1024])
        hn2T = sb("hn2T", [128, 8, SBT * 128], BF16)
        gates = sb("gates", [128, SBT, 16])
        cf = sb("cf", [128, 64])
        sm = sb("sm", [128, 256])
        pzb = sb("pzb", [128, 128], BF16)
        ARENA_W = 21504
        arena = sb("arena", [128, ARENA_W])
        _off = [0]

        def carve(shape, dt=F32):
            n = 1
            for d in shape[1:]:
                n *= d
            nb = n * (4 if dt == F32 else 2)
            nb = (nb + 63) // 64 * 64
            o = _off[0]
            _off[0] += nb
            assert _off[0] <= ARENA_W * 4, ("arena overflow", _off[0])
            v = arena[:, o // 4:(o + nb) // 4]
            if dt != F32:
                v = v.bitcast(dt)
            v = v[:, 0:n]
            if len(shape) == 3:
                v = v.rearrange("p (a b) -> p a b", a=shape[1])
            elif len(shape) == 4:
                v = v.rearrange("p (a b c) -> p a b c", a=shape[1], b=shape[2])
            return v
        xt = [carve([128, 1024]) for _ in range(2)]
        hn = [carve([128, 1024], BF16) for _ in range(2)]
        hnT = [carve([128, 8, 128], BF16) for _ in range(2)]
        yT_off = _off[0]
        yT = [carve([128, 8, 128], BF16) for _ in range(2)]
        yg = [carve([128, 4, 128], BF16) for _ in range(2)]
        ygf = [carve([128, 4, 128]) for _ in range(2)]
        g1 = [carve([128, 128]) for _ in range(2)]
        g2 = [carve([128, 128]) for _ in range(2)]
        Gt = [carve([128, 8, 128]) for _ in range(2)]
        hn2b = [carve([128, 1024], BF16) for _ in range(2)]
        D1 = None
        M1_off = _off[0]
        M1 = [carve([128, 8, 128]) for _ in range(2)]
        M2 = [carve([128, 8, 128]) for _ in range(2)]
        XT = [carve([128, 8, 128]) for _ in range(2)]
        XTb = [carve([128, 8, 128]) for _ in range(2)]
        XA = [carve([128, 8, 128], BF16) for _ in range(2)]
        junk = carve([128, 1024], BF16)
        assert _off[0] >= 48 * 1024
        zT = [carve([128, 8, 144], BF16) for _ in range(2)]
        mixer_end = _off[0]
        _off[0] = 0
        WGs = [carve([128, 8, 256], BF16) for _ in range(2)]
        WUs = [carve([128, 8, 256], BF16) for _ in range(2)]
        WDs = [carve([128, 2, 1024], BF16) for _ in range(2)]
        sgT = [carve([128, 2, 512], BF16) for _ in range(2)]
        hT = [carve([128, 2, 512], BF16) for _ in range(2)]
        outt = [carve([128, 1024]) for _ in range(2)]
        NF = carve([128, 1024])
        junk2 = carve([128, 1024], BF16)
        assert _off[0] <= 48 * 1024
        pp = Gt[0].rearrange("p a b -> p (a b)")[:, 0:512].rearrange("p (a b) -> p a b", a=32)
        pq = XT[0].rearrange("p a b -> p (a b)")[:, 0:512].rearrange("p (a b) -> p a b", a=32)
        pz = acc[:].rearrange("p a b -> p (a b)").rearrange("p (a b) -> p a b", a=32)
        T1 = M1[0]
        T2 = M2[0]

        PB0 = ps("PB0", [128, 1024], BF16)
        PBs = [ps("PB%d" % i, [128, 512]) for i in range(1, 8)]

        res = {}

        def rs(name):
            if name not in res:
                res[name] = R(name)
            return res[name]

        def dma(out_ap, in_ap, rd, wr, slow=False, q="sp"):
            if slow:
                S.op(q, lambda e: e.dma_start(out=out_ap, in_=in_ap, allow_slow_non_contiguous=True), rd=rd, wr=wr, dma=wr[0])
            else:
                S.op(q, lambda e: e.dma_start(out=out_ap, in_=in_ap), rd=rd, wr=wr, dma=wr[0])

        def act(out_ap, in_ap, func, rd, wr, **kw):
            S.op("act", lambda e: e.activation(out=out_ap, in_=in_ap, func=func, **kw), rd=rd, wr=wr)

        def tt(eng, out_ap, a, b, op, rd, wr):
            S.op(eng, lambda e: e.tensor_tensor(out=out_ap, in0=a, in1=b, op=op), rd=rd, wr=wr)

        def tsc(out_ap, a, s1, s2, op0, op1, rd, wr):
            if s2 is None:
                S.op("dve", lambda e: e.tensor_scalar(out=out_ap, in0=a, scalar1=s1, scalar2=None, op0=op0), rd=rd, wr=wr)
            else:
                S.op("dve", lambda e: e.tensor_scalar(out=out_ap, in0=a, scalar1=s1, scalar2=s2, op0=op0, op1=op1), rd=rd, wr=wr)

        def stt(out_ap, a, s, b, op0, op1, rd, wr):
            S.op("dve", lambda e: e.scalar_tensor_tensor(out=out_ap, in0=a, scalar=s, in1=b, op0=op0, op1=op1), rd=rd, wr=wr)

        def cp(eng, out_ap, in_ap, rd, wr):
            S.op(eng, lambda e: e.tensor_copy(out=out_ap, in_=in_ap), rd=rd, wr=wr)

        def mm(out_ap, lhsT, rhs, start, stop, rd, wr):
            S.op("pe", lambda e: e.matmul(out_ap, lhsT=lhsT, rhs=rhs, start=start, stop=stop), rd=rd, wr=wr)

        def tr(out_ap, in_ap, rd, wr):
            S.op("pe", lambda e: e.transpose(out=out_ap, in_=in_ap, identity=ident[:]), rd=rd, wr=wr)

        def red(out_ap, in_ap, op, rd, wr):
            S.op("dve", lambda e: e.tensor_reduce(out=out_ap, in_=in_ap, axis=AX.X, op=op), rd=rd, wr=wr)

        def recip(out_ap, in_ap, rd, wr):
            S.op("dve", lambda e: e.reciprocal(out=out_ap, in_=in_ap), rd=rd, wr=wr)

        def smc(i, n=1):
            return sm[:, i:i + n]

        r_ = rs
        dma(cstt[:], cst[:, :], [], [r_("cstt")])
        dma(colt[:], cols[:, :], [], [r_("colt")])
        dma(RB[:], rows[0:1, 1024:1108].partition_broadcast(128), [], [r_("RB")])
        cp("dve", ident[:], cstt[:, 0:128], [r_("cstt")], [r_("ident")])
        JIDX = cstt[:, 128:256]
        SGN = cstt[:, 256:257]
        c_nm, c_nf, c_ps_, c_d, c_gb = 0, 8, 16, 20, 24

        accv = acc[:].rearrange("p a b -> p (a b)")
        for half in range(2):
            dma(accv[:, 0:4096].rearrange("p (k n) -> p k n", k=4),
                w_in[half * 512:(half + 1) * 512, :].rearrange("(k p) n -> p k n", p=128), [], [r_("acc")])
            for k in range(4):
                kk = half * 4 + k
                tsc(WI[:, kk, :], accv[:, k * 1024:(k + 1) * 1024], colt[:, c_nm + kk:c_nm + kk + 1], None, ALU.mult, None,
                    [r_("acc"), r_("colt")], [r_("WI")])
        for half in range(2):
            dma(accv[:, 0:4096].rearrange("p (k n) -> p k n", k=4),
                w_out[half * 512:(half + 1) * 512, :].rearrange("(k p) n -> p k n", p=128), [r_("WI")], [r_("acc")])
            for k in range(4):
                kk = half * 4 + k
                tsc(WO[:, kk, :], accv[:, k * 1024:(k + 1) * 1024], 1.0 if kk < 4 else 0.25, None, ALU.mult, None, [r_("acc")], [r_("WO")])
        dma(accv[:, 0:2048].rearrange("p (k n) -> p k n", k=4), glu_w[:, :].rearrange("(k p) n -> p k n", p=128), [r_("WO")], [r_("acc")])
        tsc(GW[:].rearrange("p k n -> p (k n)"), accv[:, 0:2048], 0.5, None, ALU.mult, None, [r_("acc")], [r_("GW")])
        dma(accv[:, 0:512].rearrange("p (g n) -> p g n", g=4), pool_w[:, :, :].rearrange("g p n -> p g n"), [r_("GW")], [r_("acc")])
        for gi, w in enumerate(WINS):
            tsc(PW[:, 2 * gi, :], accv[:, gi * 128:(gi + 1) * 128], float(1.0 / w - 1.0), None, ALU.mult, None, [r_("acc")], [r_("PW")])
            tsc(PW[:, 2 * gi + 1, :], accv[:, gi * 128:(gi + 1) * 128], float(1.0 / w), None, ALU.mult, None, [r_("acc")], [r_("PW")])
        dma(accv[:, 0:160].rearrange("p (k n) -> p k n", k=8), wr_d[:, :].rearrange("(k p) n -> p k n", p=128), [r_("PW")], [r_("acc")])
        for k in range(8):
            tsc(WRb[:, k, :], accv[:, k * 20:(k + 1) * 20], colt[:, c_nf + k:c_nf + k + 1], None, ALU.mult, None, [r_("acc"), r_("colt")], [r_("WRb")])

        spt = XTb[0][:, 0, 0:96]
        dma(spt, sp_s[:, :], [], [r_("spt")])
        dma(pp, b1_s[:, :, :], [], [r_("Gt")])
        dma(pq, b2_s[:, :, :], [], [r_("XT")])
        P = XTb[1].rearrange("p a b -> p (a b)")[:, 0:512].rearrange("p (a b) -> p a b", a=16)
        Rp = r_("P")
        LR, LI, LS = spt[:, 0:32], spt[:, 32:64], spt[:, 64:96]
        STEP, ARG, MG, CS, SN, AR, AI, DEN, QR, QI, TA, TB, KI = [P[:, i, :] for i in range(13)]
        KII = sb("KII", [128, 32], I32)

        def exp_to(dst, src, rd):
            act(TA, src, AF.Tanh, rd, [Rp], scale=0.5)
            tsc(TB, TA, -1.0, 1.0, ALU.mult, ALU.add, [Rp], [Rp])
            recip(TB, TB, [Rp], [Rp])
            tsc(TA, TA, 1.0, None, ALU.add, None, [Rp], [Rp])
            tt("dve", dst, TA, TB, ALU.mult, [Rp], [Rp])

        def sin_to(dst, src, shift):
            tsc(TA, src, float(shift), 1.0 / TWO_PI, ALU.add, ALU.mult, [Rp], [Rp])
            cp("dve", KII[:], TA, [Rp], [Rp])
            cp("dve", TB, KII[:], [Rp], [Rp])
            tt("dve", TA, TA, TB, ALU.subtract, [Rp], [Rp])
            act(dst, TA, AF.Sin, [Rp], [Rp], scale=TWO_PI)
        exp_to(STEP, LS, [r_("spt")])
        tt("dve", ARG, LI, STEP, ALU.mult, [Rp, r_("spt")], [Rp])
        tt("dve", MG, LR, STEP, ALU.mult, [Rp, r_("spt")], [Rp])
        cp("dve", P[:, 13, :], MG, [Rp], [Rp])
        exp_to(MG, MG, [Rp])
        cp("dve", MAG[:], MG, [Rp], [r_("MAG")])
        sin_to(SN, ARG, 0.0)
        sin_to(CS, ARG, math.pi / 2)
        tt("dve", AR, MG, CS, ALU.mult, [Rp], [Rp])
        tt("dve", AI, MG, SN, ALU.mult, [Rp], [Rp])
        tt("dve", DEN, LR, LR, ALU.mult, [Rp], [Rp])
        tt("dve", TA, LI, LI, ALU.mult, [Rp], [Rp])
        tt("dve", DEN, DEN, TA, ALU.add, [Rp], [Rp])
        recip(DEN, DEN, [Rp], [Rp])
        tsc(TA, AR, -1.0, None, ALU.add, None, [Rp], [Rp])
        tt("dve", QR, TA, LR, ALU.mult, [Rp], [Rp])
        tt("dve", TB, AI, LI, ALU.mult, [Rp], [Rp])
        tt("dve", QR, QR, TB, ALU.add, [Rp], [Rp])
        tt("dve", QR, QR, DEN, ALU.mult, [Rp], [Rp])
        tt("dve", QI, AI, LR, ALU.mult, [Rp], [Rp])
        tt("dve", TB, TA, LI, ALU.mult, [Rp], [Rp])
        tt("dve", QI, QI, TB, ALU.subtract, [Rp], [Rp])
        tt("dve", QI, QI, DEN, ALU.mult, [Rp], [Rp])
        tsc(QI, QI, SGN, -1.0, ALU.mult, ALU.mult, [Rp, r_("cstt")], [Rp])
        tt("dve", pp, pp, QR.unsqueeze(2).to_broadcast([128, 32, 16]), ALU.mult, [Rp, r_("Gt")], [r_("Gt")])
        tt("dve", pq, pq, QI.unsqueeze(2).to_broadcast([128, 32, 16]), ALU.mult, [Rp, r_("XT")], [r_("XT")])
        tt("dve", pp, pp, pq, ALU.add, [r_("XT")], [r_("Gt")])
        S.op("pool", lambda e: e.memset(pz, 0.0), wr=[r_("acc")])
        for gl in range(8):
            cp("dve", pz[:, gl::8, gl * 16:(gl + 1) * 16], pp[:, gl::8, :], [r_("Gt")], [r_("acc")])
        for g in range(32):
            cp("dve", pzb[:], pz[:, g, :], [r_("acc")], [r_("pzb")])
            tr(PB0[:, 0:128], pzb[:], [r_("pzb"), r_("ident")], [r_("PB0")])
            cp("dve", LBa[:, g, :], PB0[:, 0:128], [r_("PB0")], [r_("LBa")])
        cp("dve", LBb[:, :, 0:64], LBa[:, :, 64:128], [r_("LBa")], [r_("LBb")])
        cp("dve", LBb[:, :, 64:128], LBa[:, :, 0:64], [r_("LBa")], [r_("LBb")])
        dma(pq, c1_s[:, :, :], [r_("Gt")], [r_("XT")])
        tsc(pq, pq, SGN, None, ALU.mult, None, [r_("XT"), r_("cstt")], [r_("XT")])
        S.op("pool", lambda e: e.memset(pz, 0.0), rd=[], wr=[r_("acc")])
        for gl in range(8):
            cp("dve", pz[:, gl::8, gl * 16:(gl + 1) * 16], pq[:, gl::8, :], [r_("XT")], [r_("acc")])
        cp("dve", LC[:].rearrange("p a b -> p (a b)"), pz.rearrange("p a b -> p (a b)"), [r_("acc")], [r_("LC")])
        for t4 in range(4):
            tsc(Dg[:, t4, :], ident[:], colt[:, c_d + t4:c_d + t4 + 1], None, ALU.mult, None, [r_("ident"), r_("colt")], [r_("Dg")])
        SCR = [T1, T2]
        for g in range(32):
            sc = SCR[g % 2]
            rsc = r_("scr%d" % (g % 2))
            tsc(sc[:, 0, :], JIDX, P[:, 1, g:g + 1], 1.0 / TWO_PI, ALU.mult, ALU.mult, [Rp, r_("cstt")], [rsc])
            for ti_, (tab, shift) in enumerate(((SINM, 0.0), (COS, 0.25))):
                rs2 = r_("scr%d_%d" % (g % 2, ti_))
                a, b_, c = 1 + 3 * ti_, 2 + 3 * ti_, 3 + 3 * ti_
                tsc(sc[:, a, :], sc[:, 0, :], float(shift), None, ALU.add, None, [rsc], [rs2])
                cp("dve", sc[:, b_, :].bitcast(I32), sc[:, a, :], [rs2], [rs2])
                cp("dve", sc[:, c, :], sc[:, b_, :].bitcast(I32), [rs2], [rs2])
                tt("dve", sc[:, a, :], sc[:, a, :], sc[:, c, :], ALU.subtract, [rs2], [rs2])
                act(tab[:, g, :], sc[:, a, :], AF.Sin, [rs2], [r_("tab")], scale=TWO_PI)
        tsc(SINM[:].rearrange("p a b -> p (a b)"), SINM[:].rearrange("p a b -> p (a b)"), SGN, None, ALU.mult, None, [r_("tab"), r_("cstt")], [r_("tab")])
        _save = _off[0]
        _off[0] = yT_off
        BBR = carve([128, 32, 16])
        BBIs = carve([128, 32, 16])
        VTa = carve([128, 32, 128], BF16)
        VTb = carve([128, 32, 128], BF16)
        assert _off[0] <= M1_off
        _off[0] = _save
        ztok = [M1[1].rearrange("p a b -> p (a b)").bitcast(BF16)[:, 0:1024][:, i * 512:(i + 1) * 512] for i in range(2)]
        Ytmp = [M2[1].rearrange("p a b -> p (a b)")[:, i * 512:(i + 1) * 512] for i in range(2)]
        CARb = XA[1].rearrange("p a b -> p (a b)").bitcast(F32)[:, 0:32]
        Ssum = XA[1].rearrange("p a b -> p (a b)").bitcast(F32)[:, 32:64]
        Ctmp = XA[1].rearrange("p a b -> p (a b)").bitcast(F32)[:, 64:128]
        A128 = XA[1].rearrange("p a b -> p (a b)").bitcast(F32)[:, 128:192]
        bA = XT[1].rearrange("p a b -> p (a b)")[:, 0:512].rearrange("p (a b) -> p a b", a=32)
        bB = XT[1].rearrange("p a b -> p (a b)")[:, 512:1024].rearrange("p (a b) -> p a b", a=32)
        dma(bA, b2_s[:, :, :], [], [r_("bA")])
        dma(bB, b1_s[:, :, :], [], [r_("bB")])
        tt("dve", bA, bA, QR.unsqueeze(2).to_broadcast([128, 32, 16]), ALU.mult, [Rp, r_("bA")], [r_("bA")])
        tt("dve", bB, bB, QI.unsqueeze(2).to_broadcast([128, 32, 16]), ALU.mult, [Rp, r_("bB")], [r_("bB")])
        tt("dve", bA, bA, bB, ALU.subtract, [r_("bB")], [r_("bA")])
        cp("dve", BBR[0:64], pp[0:64], [r_("Gt")], [r_("BBR")])
        cp("dve", BBR[64:128], bA[64:128], [r_("bA")], [r_("BBR")])
        tsc(BBIs[0:64].rearrange("p a b -> p (a b)"), bA[0:64].rearrange("p a b -> p (a b)"), -1.0, None, ALU.mult, None, [r_("bA")], [r_("BBI")])
        cp("dve", BBIs[64:128], pp[64:128], [r_("Gt")], [r_("BBI")])
        S.barrier()
        SHC = sm[:, 70:71]
        tsc(SHC, SGN, 0.125, 0.125, ALU.mult, ALU.add, [r_("cstt")], [r_("shc")])
        JREV = cstt[:, 258:386]
        pzbs = [XA[0][:, 0, :], XA[0][:, 1, :]]
        for g in range(32):
            act(VTb[:, g, :], JREV, AF.Exp, [r_("cstt"), Rp], [r_("VTb")], scale=P[:, 13, g:g + 1])
        for g in range(32):
            sc = SCR[g % 2]
            rsc = r_("vscr%d" % (g % 2))
            rpz = r_("pzbs%d" % (g % 2))
            tsc(sc[:, 0, :], JREV, P[:, 1, g:g + 1], 1.0 / TWO_PI, ALU.mult, ALU.mult, [Rp, r_("cstt"), r_("tab")], [rsc])
            tsc(sc[:, 1, :], sc[:, 0, :], SHC, None, ALU.add, None, [rsc, r_("shc")], [rsc])
            cp("dve", sc[:, 2, :].bitcast(I32), sc[:, 1, :], [rsc], [rsc])
            cp("dve", sc[:, 3, :], sc[:, 2, :].bitcast(I32), [rsc], [rsc])
            tt("dve", sc[:, 1, :], sc[:, 1, :], sc[:, 3, :], ALU.subtract, [rsc], [rsc])
            act(sc[:, 4, :], sc[:, 1, :], AF.Sin, [rsc], [r_("vsb%d" % (g % 2))], scale=TWO_PI)
            tt("dve", pzbs[g % 2], sc[:, 4, :], VTb[:, g, :], ALU.mult, [r_("vsb%d" % (g % 2)), r_("VTb")], [rpz])
            tr(PB0[:, (g % 2) * 128:(g % 2 + 1) * 128], pzbs[g % 2], [rpz, r_("ident")], [r_("PB0")])
            cp("dve", VTa[:, g, :], PB0[:, (g % 2) * 128:(g % 2 + 1) * 128], [r_("PB0")], [r_("VTa")])
        cp("dve", VTb[:, :, 0:64], VTa[:, :, 64:128], [r_("VTa")], [r_("VTb")])
        cp("dve", VTb[:, :, 64:128], VTa[:, :, 0:64], [r_("VTa")], [r_("VTb")])
        act(Ctmp[:, 0:32], P[:, 13, :], AF.Exp, [Rp], [r_("ctmp")], scale=128.0)
        tt("dve", A128[:, 0:32], Ctmp[:, 0:32], COS[:, :, 127], ALU.mult, [r_("ctmp"), r_("tab")], [r_("A128")])
        tt("dve", A128[:, 32:64], Ctmp[:, 0:32], SINM[:, :, 127], ALU.mult, [r_("ctmp"), r_("tab")], [r_("A128")])
        tsc(A128[:, 32:64], A128[:, 32:64], -1.0, None, ALU.mult, None, [r_("A128")], [r_("A128")])
        S.op("pool", lambda e: e.memset(CARb, 0.0), wr=[r_("CARb")])
        S.barrier()
        LCs = XTb[0].rearrange("p a b -> p (a b)").bitcast(BF16)[:, 0:2048]
        LCs2 = XTb[1].rearrange("p a b -> p (a b)").bitcast(BF16)[:, 0:2048]
        dma(pq, c2_s[:, :, :], [], [r_("XT")])
        tsc(pq, pq, SGN, -1.0, ALU.mult, ALU.mult, [r_("XT"), r_("cstt")], [r_("XT")])
        S.op("pool", lambda e: e.memset(pz, 0.0), rd=[], wr=[r_("acc")])
        for gl in range(8):
            cp("dve", pz[:, gl::8, gl * 16:(gl + 1) * 16], pq[:, gl::8, :], [r_("XT")], [r_("acc")])
        pzf = pz.rearrange("p a b -> p (a b)")
        cp("dve", LCs, pzf[:, 0:2048], [r_("acc")], [r_("LCs")])
        cp("dve", LCs2, pzf[:, 2048:4096], [r_("acc")], [r_("LCs")])
        LCsv = [LCs.rearrange("p (g c) -> p g c", g=16), LCs2.rearrange("p (g c) -> p g c", g=16)]
        S.op("pool", lambda e: e.memset(CAR[:], 0.0), wr=[r_("CAR")])
        for p_ in range(2):
            S.op("pool", lambda e, p_=p_: e.memset(zT[p_], 0.0), wr=[r_("zT%d" % p_)])
        tsc(sm[:, 64:68], colt[:, c_gb:c_gb + 4], 0.5, None, ALU.mult, None, [r_("colt")], [r_("sm2")])

        def rms(src_ap, dst_bf, srcres, dstres, col, jk):
            S.op("dve", lambda e: e.memset(smc(col), 0.0), wr=[r_("sm%d" % col)])
            act(jk, src_ap, AF.Square, srcres, [r_("junk"), r_("sm%d" % col)], accum_out=smc(col))
            act(smc(col + 1), smc(col), AF.Sqrt, [r_("sm%d" % col)], [r_("sm%d" % col)], scale=1.0 / 1024.0, bias=EPS)
            recip(smc(col + 2), smc(col + 1), [r_("sm%d" % col)], [r_("sm%d" % col)])
            act(dst_bf, src_ap, AF.Copy, srcres + [r_("sm%d" % col)], dstres, scale=smc(col + 2))

        def rms_a(src_ap, srcres, col, jk, jkres):
            S.op("dve", lambda e: e.memset(smc(col), 0.0), wr=[r_("sm%d" % col)])
            act(jk, src_ap, AF.Square, srcres, [jkres, r_("sm%d" % col)], accum_out=smc(col))
            act(smc(col + 1), smc(col), AF.Sqrt, [r_("sm%d" % col)], [r_("sm%d" % col)], scale=1.0 / 1024.0, bias=EPS)

        def rms_b(src_ap, dst_bf, srcres, dstres, col):
            recip(smc(col + 2), smc(col + 1), [r_("sm%d" % col)], [r_("sm%d" % col)])
            act(dst_bf, src_ap, AF.Copy, srcres + [r_("sm%d" % col)], dstres, scale=smc(col + 2))

        def transp8(src_bf, srcres):
            for k in range(8):
                tr(PB0[:, k * 128:(k + 1) * 128], src_bf[:, k * 128:(k + 1) * 128], srcres + [r_("ident")], [r_("PB0")])

        tcnt = [0]

        def ssm_state(tau, full, p):
            g0 = tau * 8
            q = tau % 2
            ba, bb = PBs[2 + q * 2], PBs[3 + q * 2]
            rba, rbb = r_("bank%d" % (2 + q * 2)), r_("bank%d" % (3 + q * 2))
            bav = ba[:].rearrange("p (g t) -> p g t", g=4)
            bbv = bb[:].rearrange("p (g t) -> p g t", g=4)
            rz = r_("zT%d" % p)
            rG = r_("Gt%d" % q)
            for hh in range(2):
                for gl4 in range(4):
                    g = g0 + hh * 4 + gl4
                    mm(bav[:, gl4, :], LBa[:, g, :], zT[p][:, 4 + tau, 16:144], True, True, [r_("LBa"), rz], [rba])
                    mm(bbv[:, gl4, :], LBb[:, g, :], zT[p][:, 4 + tau, 16:144], True, True, [r_("LBb"), rz], [rbb])
                sl = slice(hh * 4, hh * 4 + 4)
                gs = slice(g0 + hh * 4, g0 + hh * 4 + 4)
                rD = r_("D1%d" % hh)
                tt("dve", Gt[q][:, sl, :], bav, COS[:, gs, :], ALU.mult, [rba, r_("tab")], [rG])
                tt("dve", D1[hh][:], bbv, SINM[:, gs, :], ALU.mult, [rbb, r_("tab")], [rD])
                tt("pool", Gt[q][:, sl, :], Gt[q][:, sl, :], D1[hh][:], ALU.add, [rD], [rG])
                yield
            rX = r_("XT%d" % q)
            for gl in range(8):
                g = g0 + gl
                S.op("dve", lambda e, gl=gl, g=g, q=q: e.tensor_tensor_scan(
                    out=XT[q][:, gl, :], data0=MAG[:, g:g + 1].to_broadcast([128, 128]), data1=Gt[q][:, gl, :],
                    initial=CAR[:, g:g + 1], op0=ALU.mult, op1=ALU.add),
                    rd=[rG, r_("MAG"), r_("CAR")], wr=[rX])
            yield
            gs = slice(g0, g0 + 8)
            rXb, rXb2 = r_("XTb%d" % q), r_("XTc%d" % q)
            rM1, rM2 = r_("M1%d" % q), r_("M2%d" % q)
            if full:
                dma(XTb[q][0:64, :, :], XT[q][64:128, :, :], [rX], [rXb])
                dma(XTb[q][64:128, :, :], XT[q][0:64, :, :], [rX], [rXb2])
                tt("dve", M1[q], XT[q], COS[:, gs, :], ALU.mult, [rX, r_("tab")], [rM1])
                tt("pool", M2[q], XTb[q], SINM[:, gs, :], ALU.mult, [rXb, rXb2, r_("tab")], [rM2])
                tt("pool", XA[q], M1[q], M2[q], ALU.subtract, [rM1, rM2], [r_("XA%d" % q)])
                tt("pool", CAR[:, gs], M1[q][:, :, 127], M2[q][:, :, 127], ALU.subtract, [rM1, rM2], [r_("CAR")])
            else:
                dma(XTb[q][0:64, :, 127:128], XT[q][64:128, :, 127:128], [rX], [rXb], slow=True)
                dma(XTb[q][64:128, :, 127:128], XT[q][0:64, :, 127:128], [rX], [rXb2], slow=True)
                tt("pool", M1[q][:, :, 127], XT[q][:, :, 127], COS[:, gs, 127], ALU.mult, [rX, r_("tab")], [rM1])
                tt("pool", M2[q][:, :, 127], XTb[q][:, :, 127], SINM[:, gs, 127], ALU.mult, [rXb, rXb2, r_("tab")], [rM2])
                tt("pool", CAR[:, gs], M1[q][:, :, 127], M2[q][:, :, 127], ALU.subtract, [rM1, rM2], [r_("CAR")])
            yield

        def front(tile_idx, mlist, p):
            rz, rzo = r_("zT%d" % p), r_("zT%d" % (1 - p))
            rx, rh, rhT = r_("xt%d" % p), r_("hn%d" % p), r_("hnT%d" % p)
            dma(xt[p], xall[tile_idx * 128:(tile_idx + 1) * 128, :], [], [rx])
            rms(xt[p], hn[p], [rx], [rh], 0, junk)
            transp8(hn[p], [rh])
            act(hnT[p].rearrange("p k t -> p (k t)"), PB0[:], AF.Copy, [r_("PB0")], [rhT])
            yield
            cp("pool", zT[p][:, 0:4, 0:16], zT[1 - p][:, 0:4, 128:144], [rzo], [rz])
            for m in mlist:
                pb = PBs[0] if m < 4 else PBs[1]
                rpb = r_("bank%d" % (m // 4))
                o = pb[:, (m % 4) * 128:(m % 4 + 1) * 128]
                for k in range(8):
                    mm(o, WI[:, k, m * 128:(m + 1) * 128], hnT[p][:, k, :], k == 0, k == 7, [r_("WI"), rhT], [rpb])
            if 0 in mlist:
                act(zT[p][:, 0:4, 16:144], PBs[0][:].rearrange("p (m t) -> p m t", m=4), AF.Copy, [r_("bank0")], [rz])
            if 4 in mlist:
                act(zT[p][:, 4:8, 16:144], PBs[1][:].rearrange("p (m t) -> p m t", m=4), AF.Copy, [r_("bank1")], [rz])
            yield "F"

        PQ = PBs[6]

        def mixer(tile_idx, lt, first, p):
            rz = r_("zT%d" % p)
            ryT, ryg, rygf = r_("yT%d" % p), r_("yg%d" % p), r_("ygf%d" % p)
            yield from front(tile_idx, list(range(8)), p)
            for gi, w in enumerate(WINS):
                o = PQ[:, gi * 128:(gi + 1) * 128]
                for l in range(w):
                    mm(o, PW[:, 2 * gi + (1 if l > 0 else 0), :], zT[p][:, gi, 16 - l:144 - l], l == 0, (l == w - 1) and not first, [r_("PW"), rz], [r_("bank6")])
                if first:
                    S.op("dve", lambda e, gi=gi: e.tensor_tensor_scan(
                        out=cf[:, 0:16], data0=cstt[:, 257:258].to_broadcast([128, 16]), data1=zT[p][:, gi, 16:32],
                        initial=0.0, op0=ALU.mult, op1=ALU.add), rd=[rz, r_("cstt")], wr=[r_("cf")])
                    tt("dve", cf[:, 16:32], cf[:, 0:16], RB[:, 20 + gi * 16:36 + gi * 16], ALU.mult, [r_("cf"), r_("RB")], [r_("cf2")])
                    cp("dve", pzb[:, 0:16], cf[:, 16:32], [r_("cf2")], [r_("pzb")])
                    mm(o[:, 0:16], PW[:, 2 * gi + 1, :], pzb[:, 0:16], False, True, [r_("PW"), r_("pzb")], [r_("bank6")])
            for gi in range(4):
                act(yT[p][:, gi, :], PQ[:, gi * 128:(gi + 1) * 128], AF.Copy, [r_("bank6")], [ryT], scale=colt[:, c_ps_ + gi:c_ps_ + gi + 1])
            yield
            for tau in range(4):
                q = tau % 2
                yield from ssm_state(tau, True, p)
                o = PBs[0][:, tau * 128:(tau + 1) * 128]
                ry = r_("bank0")
                for gl in range(8):
                    mm(o, LC[:, tau * 8 + gl, :], XA[q][:, gl, :], gl == 0, False, [r_("LC"), r_("XA%d" % q)], [ry])
                mm(o, Dg[:, tau, :], zT[p][:, 4 + tau, 16:144], False, True, [r_("Dg"), rz], [ry])
                rg1, rg2 = r_("g1%d" % q), r_("g2%d" % q)
                act(g1[q], o, AF.Square, [ry], [rg1])
                tsc(g1[q], g1[q], 0.044715, 1.0, ALU.mult, ALU.add, [rg1], [rg1])
                tt("dve", g1[q], g1[q], o, ALU.mult, [rg1, ry], [rg1])
                act(g2[q], g1[q], AF.Tanh, [rg1], [rg2], scale=0.7978845608028654)
                tsc(g2[q], g2[q], 0.5, 0.5, ALU.mult, ALU.add, [rg2], [rg2])
                tt("dve", ygf[p][:, tau, :], g2[q], o, ALU.mult, [rg2, ry], [rygf])
                cp("pool", yg[p][:, tau, :], ygf[p][:, tau, :], [rygf], [ryg])
                yield
            for m in range(4):
                q = m % 2
                rg1 = r_("g1%d" % q)
                o = PBs[1][:, m * 128:(m + 1) * 128]
                for k in range(4):
                    mm(o, GW[:, k, m * 128:(m + 1) * 128], yg[p][:, k, :], k == 0, k == 3, [r_("GW"), ryg], [r_("bank1")])
                act(g1[q], o, AF.Tanh, [r_("bank1"), r_("sm2")], [rg1], scale=0.5, bias=sm[:, 64 + m:65 + m])
                tsc(g1[q], g1[q], 0.5, 0.5, ALU.mult, ALU.add, [rg1], [rg1])
                tt("pool", yT[p][:, 4 + m, :], g1[q], ygf[p][:, m, :], ALU.mult, [rg1, rygf], [ryT])
            yield
            for half in range(2):
                pb = PBs[half]
                rpb = r_("bank%d" % half)
                for k in range(8):
                    mm(pb[:], yT[p][:, k, :], WO[:, k, half * 512:(half + 1) * 512], k == 0, k == 7, [ryT, r_("WO")], [rpb])
                tt("dve", acc[:, lt, half * 512:(half + 1) * 512], pb[:], xt[p][:, half * 512:(half + 1) * 512], ALU.add, [rpb, r_("xt%d" % p)], [r_("acc%d" % lt)])
            yield
            rh = r_("hn%d" % p)
            rms(acc[:, lt, :], hn[p], [r_("acc%d" % lt)], [rh], 4, junk)
            transp8(hn[p], [rh])
            act(hn2T[:, :, lt * 128:(lt + 1) * 128], PB0[:].rearrange("p (k t) -> p k t", k=8), AF.Copy, [r_("PB0")], [r_("hn2T")])
            yield
            o = PQ[:, 0:20]
            for k in range(8):
                mm(o, hn2T[:, k, lt * 128:(lt + 1) * 128], WRb[:, k, :], k == 0, k == 7, [r_("hn2T"), r_("WRb")], [r_("bank6")])
            L_ = sm[:, 100:120]
            rS = r_("smr")
            tt("dve", L_, o, RB[:, 0:20], ALU.add, [r_("bank6"), r_("RB")], [rS])
            cL, fL = sm[:, 100:104], sm[:, 104:120]
            M, GM, CM, TH, NUM, SS, PG = smc(120), smc(121, 4), smc(125, 4), smc(129, 4), smc(133, 4), smc(137), smc(138)
            red(M, cL, ALU.max, [rS], [rS])
            tsc(GM, cL, M, None, ALU.is_equal, None, [rS], [rS])
            tsc(CM, cL, M, None, ALU.subtract, None, [rS], [rS])
            act(TH, CM, AF.Tanh, [rS], [rS], scale=0.5)
            tsc(NUM, TH, 1.0, None, ALU.add, None, [rS], [rS])
            tsc(TH, TH, -1.0, 1.0, ALU.mult, ALU.add, [rS], [rS])
            recip(TH, TH, [rS], [rS])
            tt("dve", NUM, NUM, TH, ALU.mult, [rS], [rS])
            red(SS, NUM, ALU.add, [rS], [rS])
            recip(PG, SS, [rS], [rS])
            FT = sm[:, 140:156]
            tt("dve", FT.rearrange("p (g j) -> p g j", g=4), fL.rearrange("p (g j) -> p g j", g=4),
               GM.unsqueeze(2).to_broadcast([128, 4, 4]), ALU.mult, [rS], [rS])
            FS = smc(156, 4)
            red(FS, FT.rearrange("p (g j) -> p j g", g=4), ALU.add, [rS], [rS])
            M1_, K1, F2, M2_, K2, DD, W1, W2, WJ = smc(160), smc(161, 4), smc(165, 4), smc(169), smc(170, 4), smc(174), smc(175), smc(176), smc(177, 4)
            red(M1_, FS, ALU.max, [rS], [rS])
            tsc(K1, FS, M1_, None, ALU.is_equal, None, [rS], [rS])
            stt(F2, K1, -1e30, FS, ALU.mult, ALU.add, [rS], [rS])
            red(M2_, F2, ALU.max, [rS], [rS])
            tsc(K2, F2, M2_, None, ALU.is_equal, None, [rS], [rS])
            tt("dve", DD, M2_, M1_, ALU.subtract, [rS], [rS])
            act(DD, DD, AF.Tanh, [rS], [rS], scale=0.5)
            tsc(W1, DD, -0.5, 0.5, ALU.mult, ALU.add, [rS], [rS])
            tsc(W2, DD, 0.5, 0.5, ALU.mult, ALU.add, [rS], [rS])
            tsc(WJ, K1, W1, None, ALU.mult, None, [rS], [rS])
            stt(WJ, K2, W2, WJ, ALU.mult, ALU.add, [rS], [rS])
            tsc(WJ, WJ, PG, None, ALU.mult, None, [rS], [rS])
            tt("dve", gates[:, lt, :].rearrange("p (g j) -> p g j", g=4), GM.unsqueeze(2).to_broadcast([128, 4, 4]),
               WJ.unsqueeze(1).to_broadcast([128, 4, 4]), ALU.mult, [rS], [r_("gates")])
            yield

        def router(lt, o, rq0):
            L_ = sm[:, 100:120]
            rS = r_("smr")
            tt("dve", L_, o, RB[:, 0:20], ALU.add, [rq0, r_("RB")], [rS])
            cL, fL = sm[:, 100:104], sm[:, 104:120]
            M, GM, CM, TH, NUM, SS, PG = smc(120), smc(121, 4), smc(125, 4), smc(129, 4), smc(133, 4), smc(137), smc(138)
            red(M, cL, ALU.max, [rS], [rS])
            tsc(GM, cL, M, None, ALU.is_equal, None, [rS], [rS])
            tsc(CM, cL, M, None, ALU.subtract, None, [rS], [rS])
            act(TH, CM, AF.Tanh, [rS], [rS], scale=0.5)
            tsc(NUM, TH, 1.0, None, ALU.add, None, [rS], [rS])
            tsc(TH, TH, -1.0, 1.0, ALU.mult, ALU.add, [rS], [rS])
            recip(TH, TH, [rS], [rS])
            tt("dve", NUM, NUM, TH, ALU.mult, [rS], [rS])
            red(SS, NUM, ALU.add, [rS], [rS])
            recip(PG, SS, [rS], [rS])
            FT = sm[:, 140:156]
            tt("dve", FT.rearrange("p (g j) -> p g j", g=4), fL.rearrange("p (g j) -> p g j", g=4),
               GM.unsqueeze(2).to_broadcast([128, 4, 4]), ALU.mult, [rS], [rS])
            FS = smc(156, 4)
            red(FS, FT.rearrange("p (g j) -> p j g", g=4), ALU.add, [rS], [rS])
            M1_, K1, F2, M2_, K2, DD, W1, W2, WJ = smc(160), smc(161, 4), smc(165, 4), smc(169), smc(170, 4), smc(174), smc(175), smc(176), smc(177, 4)
            red(M1_, FS, ALU.max, [rS], [rS])
            tsc(K1, FS, M1_, None, ALU.is_equal, None, [rS], [rS])
            stt(F2, K1, -1e30, FS, ALU.mult, ALU.add, [rS], [rS])
            red(M2_, F2, ALU.max, [rS], [rS])
            tsc(K2, F2, M2_, None, ALU.is_equal, None, [rS], [rS])
            tt("dve", DD, M2_, M1_, ALU.subtract, [rS], [rS])
            act(DD, DD, AF.Tanh, [rS], [rS], scale=0.5)
            tsc(W1, DD, -0.5, 0.5, ALU.mult, ALU.add, [rS], [rS])
            tsc(W2, DD, 0.5, 0.5, ALU.mult, ALU.add, [rS], [rS])
            tsc(WJ, K1, W1, None, ALU.mult, None, [rS], [rS])
            stt(WJ, K2, W2, WJ, ALU.mult, ALU.add, [rS], [rS])
            tsc(WJ, WJ, PG, None, ALU.mult, None, [rS], [rS])
            tt("dve", gates[:, lt, :].rearrange("p (g j) -> p g j", g=4), GM.unsqueeze(2).to_broadcast([128, 4, 4]),
               WJ.unsqueeze(1).to_broadcast([128, 4, 4]), ALU.mult, [rS], [r_("gates")])

        RRs = [M2[0], M2[1]]

        def build_RR(tau, q):
            gs = slice(tau * 8, tau * 8 + 8)
            act(RRs[q], MAG[:, gs].unsqueeze(2).to_broadcast([128, 8, 128]), AF.Copy, [r_("MAG")], [r_("RR%d" % q)])
            S.op("pool", lambda e: e.memset(RRs[q][:, :, 0:1], 0.0), rd=[], wr=[r_("RR%d" % q)])

        def run_gen(g):
            for _ in g:
                pass

        def g_front(tile_idx, lt, p):
            rz, rzo = r_("zT%d" % p), r_("zT%d" % (1 - p))
            rx, rh, rhT = r_("xt%d" % p), r_("hn%d" % p), r_("hnT%d" % p)
            dma(xt[p], xall[tile_idx * 128:(tile_idx + 1) * 128, :], [], [rx])
            dma(acc[:, lt, :], xall[tile_idx * 128:(tile_idx + 1) * 128, :], [], [r_("acc%d" % lt)])
            rms_a(xt[p], [rx], 0, hn[p], rh)
            yield
            rms_b(xt[p], hn[p], [rx], [rh], 0)
            yield
            transp8(hn[p], [rh])
            act(hnT[p].rearrange("p k t -> p (k t)"), PB0[:], AF.Copy, [r_("PB0")], [rhT])
            yield
            cp("pool", zT[p][:, 0:4, 0:16], zT[1 - p][:, 0:4, 128:144], [rzo], [rz])
            for m in range(8):
                pb = PBs[0] if m < 4 else PBs[1]
                rpb = r_("bank%d" % (m // 4))
                o = pb[:, (m % 4) * 128:(m % 4 + 1) * 128]
                for k in range(8):
                    mm(o, WI[:, k, m * 128:(m + 1) * 128], hnT[p][:, k, :], k == 0, k == 7, [r_("WI"), rhT], [rpb])
                if m == 3:
                    act(zT[p][:, 0:4, 16:144], PBs[0][:].rearrange("p (m t) -> p m t", m=4), AF.Copy, [r_("bank0")], [rz])
            act(zT[p][:, 4:8, 16:144], PBs[1][:].rearrange("p (m t) -> p m t", m=4), AF.Copy, [r_("bank1")], [rz])
            yield

        def st_pool(p, first):
            rz, ryT = r_("zT%d" % p), r_("yT%d" % p)
            rq = r_("bank6")
            for gi, w in enumerate(WINS):
                o = PQ[:, gi * 128:(gi + 1) * 128]
                for l in range(w):
                    mm(o, PW[:, 2 * gi + (1 if l > 0 else 0), :], zT[p][:, gi, 16 - l:144 - l], l == 0, (l == w - 1) and not first, [r_("PW"), rz], [rq])
                if first:
                    S.op("dve", lambda e, gi=gi: e.tensor_tensor_scan(
                        out=cf[:, 0:16], data0=cstt[:, 257:258].to_broadcast([128, 16]), data1=zT[p][:, gi, 16:32],
                        initial=0.0, op0=ALU.mult, op1=ALU.add), rd=[rz, r_("cstt")], wr=[r_("cf")])
                    tt("dve", cf[:, 16:32], cf[:, 0:16], RB[:, 20 + gi * 16:36 + gi * 16], ALU.mult, [r_("cf"), r_("RB")], [r_("cf2")])
                    cp("dve", pzb[:, 0:16], cf[:, 16:32], [r_("cf2")], [r_("pzb")])
                    mm(o[:, 0:16], PW[:, 2 * gi + 1, :], pzb[:, 0:16], False, True, [r_("PW"), r_("pzb")], [rq])
            for gi in range(4):
                act(yT[p][:, gi, :], PQ[:, gi * 128:(gi + 1) * 128], AF.Copy, [rq], [ryT], scale=colt[:, c_ps_ + gi:c_ps_ + gi + 1])

        def st_Dp(p, tau):
            g0 = tau * 8
            rz = r_("zT%d" % p)
            for hh in range(2):
                ba, bb = PBs[2 + hh * 2], PBs[3 + hh * 2]
                rba, rbb = r_("bank%d" % (2 + hh * 2)), r_("bank%d" % (3 + hh * 2))
                bav = ba[:].rearrange("p (g t) -> p g t", g=4)
                bbv = bb[:].rearrange("p (g t) -> p g t", g=4)
                for gl4 in range(4):
                    g = g0 + hh * 4 + gl4
                    mm(bav[:, gl4, :], LBa[:, g, :], zT[p][:, 4 + tau, 16:144], True, True, [r_("LBa"), rz], [rba])
                    mm(bbv[:, gl4, :], LBb[:, g, :], zT[p][:, 4 + tau, 16:144], True, True, [r_("LBb"), rz], [rbb])

        def st_Dd(tau, q):
            g0 = tau * 8
            rG, rM1 = r_("Gt%d" % q), r_("M1t")
            for hh in range(2):
                ba, bb = PBs[2 + hh * 2], PBs[3 + hh * 2]
                rba, rbb = r_("bank%d" % (2 + hh * 2)), r_("bank%d" % (3 + hh * 2))
                bav = ba[:].rearrange("p (g t) -> p g t", g=4)
                bbv = bb[:].rearrange("p (g t) -> p g t", g=4)
                sl = slice(hh * 4, hh * 4 + 4)
                gs = slice(g0 + hh * 4, g0 + hh * 4 + 4)
                tt("dve", Gt[q][:, sl, :], bav, COS[:, gs, :], ALU.mult, [rba, r_("tab")], [rG])
                tt("dve", M1[0][:, sl, :], bbv, SINM[:, gs, :], ALU.mult, [rbb, r_("tab")], [rM1])
                tt("dve", Gt[q][:, sl, :], Gt[q][:, sl, :], M1[0][:, sl, :], ALU.add, [rM1], [rG])

        def st_S(tau, q):
            g0 = tau * 8
            gs = slice(g0, g0 + 8)
            rG, rX = r_("Gt%d" % q), r_("XT%d" % q)
            c8 = sm[:, 80:88]
            tt("dve", c8, MAG[:, gs], CAR[:, gs], ALU.mult, [r_("MAG"), r_("CAR")], [r_("c8")])
            tt("dve", Gt[q][:, :, 0], Gt[q][:, :, 0], c8, ALU.add, [r_("c8")], [rG])
            S.op("dve", lambda e, q=q: e.tensor_tensor_scan(
                out=XT[q].rearrange("p a b -> p (a b)"), data0=RRs[q].rearrange("p a b -> p (a b)"),
                data1=Gt[q].rearrange("p a b -> p (a b)"), initial=0.0, op0=ALU.mult, op1=ALU.add),
                rd=[rG, r_("RR%d" % q)], wr=[rX])
            xs = sm[:, 16 + 8 * q:24 + 8 * q]
            dma(xs[0:64, :], XT[q][64:128, :, 127], [rX], [r_("xs%d" % q)], slow=True)
            dma(xs[64:128, :], XT[q][0:64, :, 127], [rX], [r_("xsb%d" % q)], slow=True)
            build_RR((tau + 2) % 4, q)

        M1b = [XA[0], XA[1]]
        M2b = [M1[1].rearrange("p a b -> p (a b)").bitcast(BF16)[:, i * 1024:(i + 1) * 1024].rearrange("p (a b) -> p a b", a=8) for i in range(2)]

        def st_M(tau, q):
            g0 = tau * 8
            gs = slice(g0, g0 + 8)
            rX = r_("XT%d" % q)
            tt("dve", M1b[q], XT[q], COS[:, gs, :], ALU.mult, [rX, r_("tab")], [r_("XA%d" % q)])
            tt("dve", M2b[q], XT[q], SINM[:, gs, :], ALU.mult, [rX, r_("tab")], [r_("M2b%d" % q)])
            xs = sm[:, 16 + 8 * q:24 + 8 * q]
            t1c = sm[:, 32 + 8 * q:40 + 8 * q]
            tt("dve", t1c, XT[q][:, :, 127], COS[:, gs, 127], ALU.mult, [rX, r_("tab")], [r_("t1c%d" % q)])
            tt("dve", xs, xs, SINM[:, gs, 127], ALU.mult, [r_("xs%d" % q), r_("xsb%d" % q), r_("tab")], [r_("xs%d" % q), r_("xsb%d" % q)])
            tt("dve", CAR[:, gs], t1c, xs, ALU.subtract, [r_("t1c%d" % q), r_("xs%d" % q), r_("xsb%d" % q)], [r_("CAR")])

        def st_Cp(p, tau, q):
            rz = r_("zT%d" % p)
            o = PQ[:, tau * 128:(tau + 1) * 128]
            ry = r_("bank6")
            for gl in range(8):
                g = tau * 8 + gl
                mm(o, LC[:, g, :], M1b[q][:, gl, :], gl == 0, False, [r_("LC"), r_("XA%d" % q)], [ry])
                mm(o, LCsv[g // 16][:, g % 16, :], M2b[q][:, gl, :], False, False, [r_("LCs"), r_("M2b%d" % q)], [ry])
            mm(o, Dg[:, tau, :], zT[p][:, 4 + tau, 16:144], False, True, [r_("Dg"), rz], [ry])
            act(g1[q], o, AF.Square, [ry], [r_("g1%d" % q)], scale=0.21145921592590347)

        def st_Ca(p, tau, q):
            o = PQ[:, tau * 128:(tau + 1) * 128]
            ry = r_("bank6")
            rg1, rg2 = r_("g1%d" % q), r_("g2%d" % q)
            stt(g1[q], g1[q], 1.0, o, ALU.add, ALU.mult, [rg1, ry], [rg1])
            act(g2[q], g1[q], AF.Tanh, [rg1], [rg2], scale=0.7978845608028654)

        def st_Cb(p, tau, q):
            ryg, rygf = r_("yg%d" % p), r_("ygf%d" % p)
            o = PQ[:, tau * 128:(tau + 1) * 128]
            ry = r_("bank6")
            rg2 = r_("g2%d" % q)
            stt(ygf[p][:, tau, :], g2[q], 1.0, o, ALU.add, ALU.mult, [rg2, ry], [rygf])
            act(yg[p][:, tau, :], ygf[p][:, tau, :], AF.Copy, [rygf], [ryg])

        def g_tail(lt, p):
            ryT, ryg, rygf = r_("yT%d" % p), r_("yg%d" % p), r_("ygf%d" % p)
            jf = junk.bitcast(F32)
            glt = [jf[:, i * 128:(i + 1) * 128] for i in range(4)]
            rgl = [r_("glt%d" % i) for i in range(4)]
            rbk = [r_("bank1"), r_("bank0")]
            for m in range(4):
                pbm = PBs[1] if m % 2 == 0 else PBs[0]
                rb = rbk[m % 2]
                o = pbm[:, (m // 2) * 128:(m // 2 + 1) * 128]
                for k in range(4):
                    mm(o, GW[:, k, m * 128:(m + 1) * 128], yg[p][:, k, :], k == 0, k == 3, [r_("GW"), ryg], [rb])
                act(glt[m], o, AF.Tanh, [rb, r_("sm2")], [rgl[m]], scale=0.5, bias=sm[:, 64 + m:65 + m])
            yield
            for m in range(4):
                stt(yT[p][:, 4 + m, :], glt[m], 1.0, ygf[p][:, m, :], ALU.add, ALU.mult, [rgl[m], rygf], [ryT])
            for half in range(2):
                pb = PBs[half]
                rqs = [r_("bank%d" % half)]
                for k in range(8):
                    mm(pb[:], yT[p][:, k, :], WO[:, k, half * 512:(half + 1) * 512], k == 0, k == 7, [ryT, r_("WO")], rqs)
            yield
            for half in range(2):
                pb = PBs[half]
                rqs = [r_("bank%d" % half)]
                tt("dve", acc[:, lt, half * 512:(half + 1) * 512], pb[:], acc[:, lt, half * 512:(half + 1) * 512], ALU.add, rqs + [r_("acc%d" % lt)], [r_("acc%d" % lt)])
            rh = r_("hn2b%d" % p)
            rms_a(acc[:, lt, :], [r_("acc%d" % lt)], 4, hn2b[p], rh)
            yield
            rms_b(acc[:, lt, :], hn2b[p], [r_("acc%d" % lt)], [rh], 4)
            yield
            transp8(hn2b[p], [rh])
            act(hn2T[:, :, lt * 128:(lt + 1) * 128], PB0[:].rearrange("p (k t) -> p k t", k=8), AF.Copy, [r_("PB0")], [r_("hn2T")])
            yield
            o = PQ[:, 0:20]
            rq0 = r_("bank6")
            for k in range(8):
                mm(o, hn2T[:, k, lt * 128:(lt + 1) * 128], WRb[:, k, :], k == 0, k == 7, [r_("hn2T"), r_("WRb")], [rq0])
            router(lt, o, rq0)
            yield

        def mixer_sb(sbi):
            nun = SBT * 4
            par = lambda lt: (sbi * SBT + lt) % 2
            bg = []

            def advance():
                for g in list(bg):
                    try:
                        next(g)
                    except StopIteration:
                        bg.remove(g)
            run_gen(g_front(NT_PRE + sbi * SBT, 0, par(0)))
            build_RR(0, 0)
            build_RR(1, 1)
            st_Dp(par(0), 0)
            if SBT > 1:
                bg.append(g_front(NT_PRE + sbi * SBT + 1, 1, par(1)))
            k = 0
            while k < nun + 4 or bg:
                advance()
                if 0 <= k - 4 < nun:
                    lt3, tau3 = divmod(k - 4, 4)
                    st_Ca(par(lt3), tau3, (k - 4) % 2)
                if 0 <= k - 3 < nun:
                    st_M((k - 3) % 4, (k - 3) % 2)
                if 0 <= k - 4 < nun:
                    st_Cb(par(lt3), tau3, (k - 4) % 2)
                    if tau3 == 3:
                        bg.append(g_tail(lt3, par(lt3)))
                if k < nun:
                    lt, tau = divmod(k, 4)
                    if tau == 2:
                        st_pool(par(lt), sbi == 0 and lt == 0)
                    st_Dd(tau, k % 2)
                    if tau == 3 and 1 <= lt + 1 and lt + 2 < SBT:
                        bg.append(g_front(NT_PRE + sbi * SBT + lt + 2, lt + 2, par(lt + 2)))
                if 0 <= k - 1 < nun:
                    st_S((k - 1) % 4, (k - 1) % 2)
                if 0 <= k - 3 < nun:
                    ltp, taup = divmod(k - 3, 4)
                    st_Cp(par(ltp), taup, (k - 3) % 2)
                if k + 1 < nun:
                    ltn, taun = divmod(k + 1, 4)
                    st_Dp(par(ltn), taun)
                k += 1

        def pipeline(gens):
            gens = list(gens)
            active = []
            while gens or active:
                if gens and len(active) < 2 and (not active or active[-1][1][0]):
                    active.append((gens.pop(0), [False]))
                for it in list(active):
                    g, st = it
                    try:
                        v = next(g)
                        if v == "F":
                            st[0] = True
                    except StopIteration:
                        active.remove(it)

        wgb = nc.dram_tensor("wgb", [16, 128, 2048], BF16).ap()
        wub = nc.dram_tensor("wub", [16, 128, 2048], BF16).ap()
        wdb = nc.dram_tensor("wdb", [16, 128, 2048], BF16).ap()
        accf = acc[:].rearrange("p a b -> p (a b)")
        hn2f = hn2T[:].rearrange("p a b -> p (a b)")
        cast_units = []
        for e in range(16):
            cast_units += [(0, e), (1, e), (2, e)]
        ucnt = [0]

        def cast_unit():
            if not cast_units:
                return
            kind, e = cast_units.pop(0)
            sl = ucnt[0] % 2
            ucnt[0] += 1
            stg = accf[:, sl * 2048:(sl + 1) * 2048]
            tmp = hn2f[:, sl * 2048:(sl + 1) * 2048]
            rs_, rt_ = r_("stg%d" % sl), r_("tmpb%d" % sl)
            if kind == 2:
                dma(stg.rearrange("p (k n) -> p k n", k=2), wd_d[e, :, :].rearrange("(k p) n -> p k n", p=128), [], [rs_], q="pool")
                cp("pool", tmp, stg, [rs_], [rt_])
                dma(wdb[e, :, :], tmp, [rt_], [r_("wscr")], q="pool")
            else:
                src = wg_d if kind == 0 else wu_d
                dst = wgb if kind == 0 else wub
                dma(stg.rearrange("p (k n) -> p k n", k=8), src[e, :, :].rearrange("(k p) n -> p k n", p=128), [], [rs_], q="pool")
                tt("pool", tmp.rearrange("p (k n) -> p k n", k=8), stg.rearrange("p (k n) -> p k n", k=8),
                   colt[:, c_nf:c_nf + 8].unsqueeze(2).to_broadcast([128, 8, 256]), ALU.mult, [rs_, r_("colt")], [rt_])
                dma(dst[e, :, :], tmp, [rt_], [r_("wscr")], q="pool")

        def moe(sbi):
            dma(NF, rows[0:1, 0:1024].partition_broadcast(128), [], [r_("NF")])
            nblk = SBT // 4
            tok = slice(0, SBT * 128)

            def gu(e):
                sl = e % 2
                rwg, rwu, rwd = r_("WG%d" % sl), r_("WU%d" % sl), r_("WD%d" % sl)
                dma(WGs[sl].rearrange("p k n -> p (k n)"), wgb[e, :, :], [r_("wscr")], [rwg])
                dma(WUs[sl].rearrange("p k n -> p (k n)"), wub[e, :, :], [r_("wscr")], [rwu])
                dma(WDs[sl].rearrange("p k n -> p (k n)"), wdb[e, :, :], [r_("wscr")], [rwd])
                for ft in range(2):
                    gp, up = PBs[2 + ft], PBs[4 + ft]
                    rg, ru = r_("bank%d" % (2 + ft)), r_("bank%d" % (4 + ft))
                    rsg, rhT_ = r_("sgT%d%d" % (sl, ft)), r_("hT%d%d" % (sl, ft))
                    for k in range(8):
                        mm(gp[:], WGs[sl][:, k, ft * 128:(ft + 1) * 128], hn2T[:, k, tok], k == 0, k == 7, [rwg, r_("hn2T")], [rg])
                    for k in range(8):
                        mm(up[:], WUs[sl][:, k, ft * 128:(ft + 1) * 128], hn2T[:, k, tok], k == 0, k == 7, [rwu, r_("hn2T")], [ru])
                    act(sgT[sl][:, ft, :], gp[:], AF.Silu, [rg], [rsg])
                    tt("dve", hT[sl][:, ft, :], up[:], sgT[sl][:, ft, :], ALU.mult, [ru, rsg], [rhT_])

            def down(e):
                sl = e % 2
                rwd = r_("WD%d" % sl)
                for lt in range(SBT):
                    for half in range(2):
                        pb = PBs[half]
                        rpb = r_("bank%d" % half)
                        for ft in range(2):
                            mm(pb[:], hT[sl][:, ft, lt * 128:(lt + 1) * 128], WDs[sl][:, ft, half * 512:(half + 1) * 512], ft == 0, ft == 1,
                               [r_("hT%d%d" % (sl, ft)), rwd], [rpb])
                        stt(acc[:, lt, half * 512:(half + 1) * 512], pb[:], gates[:, lt, e:e + 1], acc[:, lt, half * 512:(half + 1) * 512],
                            ALU.mult, ALU.add, [rpb, r_("gates"), r_("acc%d" % lt)], [r_("acc%d" % lt)])
            for e in range(17):
                if e < 16:
                    gu(e)
                if e >= 1:
                    down(e - 1)
            for lt in range(SBT):
                tix = sbi * SBT + lt
                o2 = lt % 2
                ro = r_("outt%d" % o2)
                S.op("dve", lambda e: e.memset(smc(8), 0.0), wr=[r_("sm8")])
                act(junk2, acc[:, lt, :], AF.Square, [r_("acc%d" % lt)], [r_("junk2"), r_("sm8")], accum_out=smc(8))
                act(smc(9), smc(8), AF.Sqrt, [r_("sm8")], [r_("sm8")], scale=1.0 / 1024.0, bias=EPS)
                recip(smc(10), smc(9), [r_("sm8")], [r_("sm8")])
                stt(outt[o2], acc[:, lt, :], smc(10), NF, ALU.mult, ALU.mult, [r_("acc%d" % lt), r_("sm8"), r_("NF")], [ro])
                dma(out[tix * 128:(tix + 1) * 128, :], outt[o2], [ro], [r_("out")])

        S.barrier()
        def pre_tile(t, p):
            last = t == NT_PRE - 1
            yield from front(t, [0, 1, 2, 3] if last else [], p)
            rhT = r_("hnT%d" % p)
            for k in range(8):
                mm(PBs[1][:], hnT[p][:, k, :], WI[:, k, 512:1024], k == 0, k == 7, [r_("WI"), rhT], [r_("bank1")])
            act(ztok[p], PBs[1][:], AF.Copy, [r_("bank1")], [r_("ztok%d" % p)])
            yield
            ya, yb = PBs[2 + 2 * p], PBs[3 + 2 * p]
            rya, ryb = r_("bank%d" % (2 + 2 * p)), r_("bank%d" % (3 + 2 * p))
            for g in range(32):
                mm(ya[:, g * 16:(g + 1) * 16], VTa[:, g, :], ztok[p][:, g * 16:(g + 1) * 16], True, True, [r_("VTa"), r_("ztok%d" % p)], [rya])
            for g in range(32):
                mm(yb[:, g * 16:(g + 1) * 16], VTb[:, g, :], ztok[p][:, g * 16:(g + 1) * 16], True, True, [r_("VTb"), r_("ztok%d" % p)], [ryb])
            yield
            tt("dve", Ytmp[0], ya[:], BBR.rearrange("p a b -> p (a b)"), ALU.mult, [rya, r_("BBR")], [r_("Ytmp0")])
            tt("dve", Ytmp[1], yb[:], BBIs.rearrange("p a b -> p (a b)"), ALU.mult, [ryb, r_("BBI")], [r_("Ytmp1")])
            tt("dve", Ytmp[0], Ytmp[0], Ytmp[1], ALU.add, [r_("Ytmp1")], [r_("Ytmp0")])
            red(Ssum, Ytmp[0].rearrange("p (a b) -> p a b", a=32), ALU.add, [r_("Ytmp0")], [r_("Ssum")])
            tt("dve", Ctmp[:, 0:32], A128[:, 0:32], CAR[:], ALU.mult, [r_("A128"), r_("CAR")], [r_("ctmp")])
            tt("dve", Ctmp[:, 32:64], A128[:, 32:64], CARb, ALU.mult, [r_("A128"), r_("CARb"), r_("CARb2")], [r_("ctmp2")])
            tt("dve", Ctmp[:, 0:32], Ctmp[:, 0:32], Ctmp[:, 32:64], ALU.add, [r_("ctmp2")], [r_("ctmp")])
            tt("dve", CAR[:], Ctmp[:, 0:32], Ssum, ALU.add, [r_("ctmp"), r_("Ssum")], [r_("CAR")])
            dma(CARb[0:64, :], CAR[64:128, :], [r_("CAR")], [r_("CARb")])
            dma(CARb[64:128, :], CAR[0:64, :], [r_("CAR")], [r_("CARb2")])
            cast_unit()
            if t % 2 == 1:
                cast_unit()
            yield
        pipeline([pre_tile(t, t % 2) for t in range(NT_PRE)])
        while cast_units:
            cast_unit()
        S.barrier()
        for sbi in range(NT_MAIN // SBT):
            mixer_sb(sbi)
            S.barrier()
            moe(sbi)
            S.barrier()
        S.final_wait("sp", [r_("out")])

        print('SBUF remaining', nc.sbuf_bytes_remaining, 'mixer_end', mixer_end, 'moe_end', _off[0])
        block = es.enter_context(nc.Block())

        @block.sync
        def _(e):
            S.replay("sp", e)

        @block.tensor
        def _(e):
            S.replay("pe", e)

        @block.scalar
        def _(e):
            S.replay("act", e)

        @block.vector
        def _(e):
            S.replay("dve", e)

        @block.gpsimd
        def _(e):
            S.replay("pool", e)
    return nc


def _col(v, k):
    return np.ascontiguousarray(np.asarray(v, np.float32).reshape(k, 128).T)


def kernel(x, norm_mix, w_in, pool_w, pool_scale, ssm_a_re, ssm_a_im, ssm_log_step,
           ssm_b_re, ssm_b_im, ssm_c_re, ssm_c_im, ssm_d, glu_w, glu_b, w_out, norm_ffn,
           router_coarse_w, router_coarse_b, router_fine_w, router_fine_b,
           exp_w_gate, exp_w_up, exp_w_down, norm_final):
    f = np.float32
    x = np.asarray(x, f)
    cols = np.zeros((128, 64), f)
    cols[:, 0:8] = _col(norm_mix[0], 8)
    cols[:, 8:16] = _col(norm_ffn[0], 8)
    cols[:, 16:20] = _col(pool_scale[0], 4)
    cols[:, 20:24] = _col(ssm_d[0], 4)
    cols[:, 24:28] = _col(glu_b[0], 4)
    are = np.asarray(ssm_a_re[0], f)
    aim = np.asarray(ssm_a_im[0], f)
    ls = np.asarray(ssm_log_step[0], f)
    sp_s = np.zeros((128, 96), f)
    sp_s[:, 0:32] = np.concatenate([are.T, are.T], 0)
    sp_s[:, 32:64] = np.concatenate([aim.T, aim.T], 0)
    sp_s[:, 64:96] = np.broadcast_to(ls[None, :], (128, 32))
    br = np.asarray(ssm_b_re[0], f).transpose(1, 0, 2)
    bi = np.asarray(ssm_b_im[0], f).transpose(1, 0, 2)
    b1 = np.ascontiguousarray(np.concatenate([br, bi], 0))
    b2 = np.ascontiguousarray(np.concatenate([bi, br], 0))
    cr = np.asarray(ssm_c_re[0], f).transpose(2, 0, 1)
    ci = np.asarray(ssm_c_im[0], f).transpose(2, 0, 1)
    c1 = np.ascontiguousarray(np.concatenate([cr, ci], 0))
    c2 = np.ascontiguousarray(np.concatenate([ci, cr], 0))
    cst = np.zeros((128, 386), f)
    cst[:, 258:386] = np.arange(127, -1, -1, dtype=f)[None, :]
    cst[:, 0:128] = np.eye(128, dtype=f)
    cst[:, 128:256] = np.arange(1, 129, dtype=f)[None, :]
    cst[:, 256] = np.where(np.arange(128) < 64, 1.0, -1.0)
    cst[:, 257] = 1.0
    wr = np.ascontiguousarray(np.concatenate([np.asarray(router_coarse_w[0], f), np.asarray(router_fine_w[0], f)], 1))
    rb = np.concatenate([np.asarray(router_coarse_b[0], f), np.asarray(router_fine_b[0], f)])
    in_maps = []
    for c in range(8):
        b, half = c // 2, c % 2
        main = x[b, half * 4096:(half + 1) * 4096]
        pre = x[b, 0:4096] if half == 1 else np.zeros((4096, 1024), f)
        fix = np.zeros((4, 16), f)
        if half == 0:
            for gi, w in enumerate(WINS):
                for t in range(w - 1):
                    fix[gi, t] = w / (t + 1.0) - 1.0
        rows = np.concatenate([np.asarray(norm_final, f), rb, fix.reshape(-1)])[None, :].astype(f)
        in_maps.append({
            "xall": np.ascontiguousarray(np.concatenate([pre, main], 0)),
            "w_in": np.asarray(w_in[0], f), "w_out": np.asarray(w_out[0], f), "glu_w": np.asarray(glu_w[0], f),
            "pool_w": np.asarray(pool_w[0], f), "cols": cols, "rows": rows, "sp_s": sp_s,
            "b1_s": b1, "b2_s": b2, "c1_s": c1, "c2_s": c2, "cst": cst, "wr": wr,
            "wg": np.asarray(exp_w_gate[0], f), "wu": np.asarray(exp_w_up[0], f), "wd": np.asarray(exp_w_down[0], f),
        })
    nc = build()
    res = run_bass_kernel_spmd(nc, in_maps, core_ids=list(range(8)))
    outs = [np.asarray(r["out"], f) for r in res.results]
    full = np.zeros((4, 8192, 1024), f)
    for c in range(8):
        b, half = c // 2, c % 2
        full[b, half * 4096:(half + 1) * 4096] = outs[c]
    return full
```

```python
import math
import numpy as np
from contextlib import ExitStack
import concourse.bass as bass
import concourse.mybir as mybir
from concourse.bass_utils import run_bass_kernel_spmd

F32 = mybir.dt.float32
BF16 = mybir.dt.bfloat16
I32 = mybir.dt.int32
AF = mybir.ActivationFunctionType
ALU = mybir.AluOpType
AX = mybir.AxisListType

NT_PRE = 32
NT_MAIN = 32
SBT = 4
EPS = 1e-6
WINS = (2, 4, 8, 16)
TWO_PI = 2.0 * math.pi


class R:
    def __init__(self, name):
        self.name = name
        self.w = None
        self.rd = {}


class Sched:
    ENG = ("pe", "act", "dve", "pool", "sp")

    def __init__(self, nc, es):
        self.nc = nc
        self.sem = {e: es.enter_context(nc.semaphore("s_" + e)) for e in self.ENG}
        self.cnt = {e: 0 for e in self.ENG}
        self.ops = {e: [] for e in self.ENG}
        self.seen = {e: {} for e in self.ENG}
        self.dma_pool = [es.enter_context(nc.semaphore("d%d" % i)) for i in range(48)]
        self.dma_cnt = {}
        self.dma_of = {}

    def dsem(self, res):
        if res.name not in self.dma_of:
            s = self.dma_pool[len(self.dma_of)]
            self.dma_of[res.name] = s
            self.dma_cnt[s.name] = 0
        return self.dma_of[res.name]

    def op(self, eng, fn, rd=(), wr=(), dma=None):
        need = {}

        def add(tok):
            if tok is None:
                return
            s, v = tok
            if need.get(s.name, (None, -1))[1] < v:
                need[s.name] = (s, v)
        for r in rd:
            add(r.w)
        for r in wr:
            add(r.w)
            for t in r.rd.values():
                add(t)
        waits = []
        for name, (s, v) in need.items():
            if eng == "pe" and s is self.sem["pe"]:
                continue
            if self.seen[eng].get(name, -1) >= v:
                continue
            self.seen[eng][name] = v
            waits.append((s, v))
        if dma is not None:
            s = self.dsem(dma)
            self.dma_cnt[s.name] += 16
            tok = (s, self.dma_cnt[s.name])
            inc = (s, 16)
        else:
            self.cnt[eng] += 1
            tok = (self.sem[eng], self.cnt[eng])
            inc = (self.sem[eng], 1)
        for r in rd:
            old = r.rd.get(tok[0].name)
            if old is None or old[1] < tok[1]:
                r.rd[tok[0].name] = tok
        for r in wr:
            r.w = tok
            r.rd = {}
        self.ops[eng].append((waits, fn, inc))
        return tok

    def final_wait(self, eng, ress):
        waits = []
        for r in ress:
            if r.w is not None:
                waits.append(r.w)
        self.ops[eng].append((waits, None, None))

    def barrier(self):
        toks = [(self.sem[e], self.cnt[e]) for e in self.ENG if self.cnt[e] > 0]
        for name, sm_ in self.dma_of.items():
            toks.append((sm_, self.dma_cnt[sm_.name]))
        for eng in self.ENG:
            waits = []
            for s_, v in toks:
                if s_ is self.sem[eng]:
                    continue
                if self.seen[eng].get(s_.name, -1) >= v:
                    continue
                self.seen[eng][s_.name] = v
                waits.append((s_, v))
            if waits:
                self.ops[eng].append((waits, None, None))

    def replay(self, eng, e):
        for waits, fn, inc in self.ops[eng]:
            for s, v in waits:
                e.wait_ge(s, v)
            if fn is not None:
                fn(e).then_inc(inc[0], inc[1])


def build(debug=False):
    nc = bass.Bass("TRN2", target_bir_lowering=False)

    def din(name, shape, dt=F32):
        return nc.dram_tensor(name, list(shape), dt, kind="ExternalInput").ap()
    xall = din("xall", [(NT_PRE + NT_MAIN) * 128, 1024])
    w_in = din("w_in", [1024, 1024])
    w_out = din("w_out", [1024, 1024])
    glu_w = din("glu_w", [512, 512])
    pool_w = din("pool_w", [4, 128, 128])
    cols = din("cols", [128, 64])
    rows = din("rows", [1, 1024 + 20 + 64])
    sp_s = din("sp_s", [128, 96])
    b1_s = din("b1_s", [128, 32, 16])
    b2_s = din("b2_s", [128, 32, 16])
    c1_s = din("c1_s", [128, 32, 16])
    c2_s = din("c2_s", [128, 32, 16])
    cst = din("cst", [128, 386])
    wr_d = din("wr", [1024, 20])
    wg_d = din("wg", [16, 1024, 256])
    wu_d = din("wu", [16, 1024, 256])
    wd_d = din("wd", [16, 256, 1024])
    out = nc.dram_tensor("out", [NT_MAIN * 128, 1024], F32, kind="ExternalOutput").ap()

    es = ExitStack()
    with es:
        S = Sched(nc, es)

        def sb(name, shape, dt=F32):
            return es.enter_context(nc.sbuf_tensor(name, list(shape), dt))

        def ps(name, shape, dt=F32):
            return es.enter_context(nc.psum_tensor(name, list(shape), dt))

        ident = sb("ident", [128, 128], BF16)
        cstt = sb("cstt", [128, 386])
        colt = sb("colt", [128, 64])
        RB = sb("RB", [128, 84])
        WI = sb("WI", [128, 8, 1024], BF16)
        WO = sb("WO", [128, 8, 1024], BF16)
        GW = sb("GW", [128, 4, 512], BF16)
        PW = sb("PW", [128, 8, 128], BF16)
        WRb = sb("WRb", [128, 8, 20], BF16)
        LBa = sb("LBa", [128, 32, 128], BF16)
        LBb = sb("LBb", [128, 32, 128], BF16)
        LC = sb("LC", [128, 32, 128], BF16)
        Dg = sb("Dg", [128, 4, 128], BF16)
        COS = sb("COS", [128, 32, 128])
        SINM = sb("SINM", [128, 32, 128])
        MAG = sb("MAG", [128, 32])
        CAR = sb("CAR", [128, 32])
        acc = sb("acc", [128, SBT, 1024])
        hn2T = sb("hn2T", [128, 8, SBT * 128], BF16)
        gates = sb("gates", [128, SBT, 16])
        cf = sb("cf", [128, 64])
        sm = sb("sm", [128, 256])
        pzb = sb("pzb", [128, 128], BF16)
        ARENA_W = 21504
        arena = sb("arena", [128, ARENA_W])
        _off = [0]

        def carve(shape, dt=F32):
            n = 1
            for d in shape[1:]:
                n *= d
            nb = n * (4 if dt == F32 else 2)
            nb = (nb + 63) // 64 * 64
            o = _off[0]
            _off[0] += nb
            assert _off[0] <= ARENA_W * 4, ("arena overflow", _off[0])
            v = arena[:, o // 4:(o + nb) // 4]
            if dt != F32:
                v = v.bitcast(dt)
            v = v[:, 0:n]
            if len(shape) == 3:
                v = v.rearrange("p (a b) -> p a b", a=shape[1])
            elif len(shape) == 4:
                v = v.rearrange("p (a b c) -> p a b c", a=shape[1], b=shape[2])
            return v
        xt = [carve([128, 1024]) for _ in range(2)]
        hn = [carve([128, 1024], BF16) for _ in range(2)]
        hnT = [carve([128, 8, 128], BF16) for _ in range(2)]
        yT_off = _off[0]
        yT = [carve([128, 8, 128], BF16) for _ in range(2)]
        yg = [carve([128, 4, 128], BF16) for _ in range(2)]
        ygf = [carve([128, 4, 128]) for _ in range(2)]
        g1 = [carve([128, 128]) for _ in range(2)]
        g2 = [carve([128, 128]) for _ in range(2)]
        Gt = [carve([128, 8, 128]) for _ in range(2)]
        hn2b = [carve([128, 1024], BF16) for _ in range(2)]
        D1 = None
        M1_off = _off[0]
        M1 = [carve([128, 8, 128]) for _ in range(2)]
        M2 = [carve([128, 8, 128]) for _ in range(2)]
        XT = [carve([128, 8, 128]) for _ in range(2)]
        XTb = [carve([128, 8, 128]) for _ in range(2)]
        XA = [carve([128, 8, 128], BF16) for _ in range(2)]
        junk = carve([128, 1024], BF16)
        assert _off[0] >= 48 * 1024
        zT = [carve([128, 8, 144], BF16) for _ in range(2)]
        mixer_end = _off[0]
        _off[0] = 0
        WGs = [carve([128, 8, 256], BF16) for _ in range(2)]
        WUs = [carve([128, 8, 256], BF16) for _ in range(2)]
        WDs = [carve([128, 2, 1024], BF16) for _ in range(2)]
        sgT = [carve([128, 2, 512], BF16) for _ in range(2)]
        hT = [carve([128, 2, 512], BF16) for _ in range(2)]
        outt = [carve([128, 1024]) for _ in range(2)]
        NF = carve([128, 1024])
        junk2 = carve([128, 1024], BF16)
        assert _off[0] <= 48 * 1024
        pp = Gt[0].rearrange("p a b -> p (a b)")[:, 0:512].rearrange("p (a b) -> p a b", a=32)
        pq = XT[0].rearrange("p a b -> p (a b)")[:, 0:512].rearrange("p (a b) -> p a b", a=32)
        pz = acc[:].rearrange("p a b -> p (a b)").rearrange("p (a b) -> p a b", a=32)
        T1 = M1[0]
        T2 = M2[0]

        PB0 = ps("PB0", [128, 1024], BF16)
        PBs = [ps("PB%d" % i, [128, 512]) for i in range(1, 8)]

        res = {}

        def rs(name):
            if name not in res:
                res[name] = R(name)
            return res[name]

        def dma(out_ap, in_ap, rd, wr, slow=False, q="sp"):
            if slow:
                S.op(q, lambda e: e.dma_start(out=out_ap, in_=in_ap, allow_slow_non_contiguous=True), rd=rd, wr=wr, dma=wr[0])
            else:
                S.op(q, lambda e: e.dma_start(out=out_ap, in_=in_ap), rd=rd, wr=wr, dma=wr[0])

        def act(out_ap, in_ap, func, rd, wr, **kw):
            S.op("act", lambda e: e.activation(out=out_ap, in_=in_ap, func=func, **kw), rd=rd, wr=wr)

        def tt(eng, out_ap, a, b, op, rd, wr):
            S.op(eng, lambda e: e.tensor_tensor(out=out_ap, in0=a, in1=b, op=op), rd=rd, wr=wr)

        def tsc(out_ap, a, s1, s2, op0, op1, rd, wr):
            if s2 is None:
                S.op("dve", lambda e: e.tensor_scalar(out=out_ap, in0=a, scalar1=s1, scalar2=None, op0=op0), rd=rd, wr=wr)
            else:
                S.op("dve", lambda e: e.tensor_scalar(out=out_ap, in0=a, scalar1=s1, scalar2=s2, op0=op0, op1=op1), rd=rd, wr=wr)

        def stt(out_ap, a, s, b, op0, op1, rd, wr):
            S.op("dve", lambda e: e.scalar_tensor_tensor(out=out_ap, in0=a, scalar=s, in1=b, op0=op0, op1=op1), rd=rd, wr=wr)

        def cp(eng, out_ap, in_ap, rd, wr):
            S.op(eng, lambda e: e.tensor_copy(out=out_ap, in_=in_ap), rd=rd, wr=wr)

        def mm(out_ap, lhsT, rhs, start, stop, rd, wr):
            S.op("pe", lambda e: e.matmul(out_ap, lhsT=lhsT, rhs=rhs, start=start, stop=stop), rd=rd, wr=wr)

        def tr(out_ap, in_ap, rd, wr):
            S.op("pe", lambda e: e.transpose(out=out_ap, in_=in_ap, identity=ident[:]), rd=rd, wr=wr)

        def red(out_ap, in_ap, op, rd, wr):
            S.op("dve", lambda e: e.tensor_reduce(out=out_ap, in_=in_ap, axis=AX.X, op=op), rd=rd, wr=wr)

        def recip(out_ap, in_ap, rd, wr):
            S.op("dve", lambda e: e.reciprocal(out=out_ap, in_=in_ap), rd=rd, wr=wr)

        def smc(i, n=1):
            return sm[:, i:i + n]

        r_ = rs
        dma(cstt[:], cst[:, :], [], [r_("cstt")])
        dma(colt[:], cols[:, :], [], [r_("colt")])
        dma(RB[:], rows[0:1, 1024:1108].partition_broadcast(128), [], [r_("RB")])
        cp("dve", ident[:], cstt[:, 0:128], [r_("cstt")], [r_("ident")])
        JIDX = cstt[:, 128:256]
        SGN = cstt[:, 256:257]
        c_nm, c_nf, c_ps_, c_d, c_gb = 0, 8, 16, 20, 24

        accv = acc[:].rearrange("p a b -> p (a b)")
        for half in range(2):
            dma(accv[:, 0:4096].rearrange("p (k n) -> p k n", k=4),
                w_in[half * 512:(half + 1) * 512, :].rearrange("(k p) n -> p k n", p=128), [], [r_("acc")])
            for k in range(4):
                kk = half * 4 + k
                tsc(WI[:, kk, :], accv[:, k * 1024:(k + 1) * 1024], colt[:, c_nm + kk:c_nm + kk + 1], None, ALU.mult, None,
                    [r_("acc"), r_("colt")], [r_("WI")])
        for half in range(2):
            dma(accv[:, 0:4096].rearrange("p (k n) -> p k n", k=4),
                w_out[half * 512:(half + 1) * 512, :].rearrange("(k p) n -> p k n", p=128), [r_("WI")], [r_("acc")])
            for k in range(4):
                kk = half * 4 + k
                tsc(WO[:, kk, :], accv[:, k * 1024:(k + 1) * 1024], 1.0 if kk < 4 else 0.25, None, ALU.mult, None, [r_("acc")], [r_("WO")])
        dma(accv[:, 0:2048].rearrange("p (k n) -> p k n", k=4), glu_w[:, :].rearrange("(k p) n -> p k n", p=128), [r_("WO")], [r_("acc")])
        tsc(GW[:].rearrange("p k n -> p (k n)"), accv[:, 0:2048], 0.5, None, ALU.mult, None, [r_("acc")], [r_("GW")])
        dma(accv[:, 0:512].rearrange("p (g n) -> p g n", g=4), pool_w[:, :, :].rearrange("g p n -> p g n"), [r_("GW")], [r_("acc")])
        for gi, w in enumerate(WINS):
            tsc(PW[:, 2 * gi, :], accv[:, gi * 128:(gi + 1) * 128], float(1.0 / w - 1.0), None, ALU.mult, None, [r_("acc")], [r_("PW")])
            tsc(PW[:, 2 * gi + 1, :], accv[:, gi * 128:(gi + 1) * 128], float(1.0 / w), None, ALU.mult, None, [r_("acc")], [r_("PW")])
        dma(accv[:, 0:160].rearrange("p (k n) -> p k n", k=8), wr_d[:, :].rearrange("(k p) n -> p k n", p=128), [r_("PW")], [r_("acc")])
        for k in range(8):
            tsc(WRb[:, k, :], accv[:, k * 20:(k + 1) * 20], colt[:, c_nf + k:c_nf + k + 1], None, ALU.mult, None, [r_("acc"), r_("colt")], [r_("WRb")])

        spt = XTb[0][:, 0, 0:96]
        dma(spt, sp_s[:, :], [], [r_("spt")])
        dma(pp, b1_s[:, :, :], [], [r_("Gt")])
        dma(pq, b2_s[:, :, :], [], [r_("XT")])
        P = XTb[1].rearrange("p a b -> p (a b)")[:, 0:512].rearrange("p (a b) -> p a b", a=16)
        Rp = r_("P")
        LR, LI, LS = spt[:, 0:32], spt[:, 32:64], spt[:, 64:96]
        STEP, ARG, MG, CS, SN, AR, AI, DEN, QR, QI, TA, TB, KI = [P[:, i, :] for i in range(13)]
        KII = sb("KII", [128, 32], I32)

        def exp_to(dst, src, rd):
            act(TA, src, AF.Tanh, rd, [Rp], scale=0.5)
            tsc(TB, TA, -1.0, 1.0, ALU.mult, ALU.add, [Rp], [Rp])
            recip(TB, TB, [Rp], [Rp])
            tsc(TA, TA, 1.0, None, ALU.add, None, [Rp], [Rp])
            tt("dve", dst, TA, TB, ALU.mult, [Rp], [Rp])

        def sin_to(dst, src, shift):
            tsc(TA, src, float(shift), 1.0 / TWO_PI, ALU.add, ALU.mult, [Rp], [Rp])
            cp("dve", KII[:], TA, [Rp], [Rp])
            cp("dve", TB, KII[:], [Rp], [Rp])
            tt("dve", TA, TA, TB, ALU.subtract, [Rp], [Rp])
            act(dst, TA, AF.Sin, [Rp], [Rp], scale=TWO_PI)
        exp_to(STEP, LS, [r_("spt")])
        tt("dve", ARG, LI, STEP, ALU.mult, [Rp, r_("spt")], [Rp])
        tt("dve", MG, LR, STEP, ALU.mult, [Rp, r_("spt")], [Rp])
        cp("dve", P[:, 13, :], MG, [Rp], [Rp])
        exp_to(MG, MG, [Rp])
        cp("dve", MAG[:], MG, [Rp], [r_("MAG")])
        sin_to(SN, ARG, 0.0)
        sin_to(CS, ARG, math.pi / 2)
        tt("dve", AR, MG, CS, ALU.mult, [Rp], [Rp])
        tt("dve", AI, MG, SN, ALU.mult, [Rp], [Rp])
        tt("dve", DEN, LR, LR, ALU.mult, [Rp], [Rp])
        tt("dve", TA, LI, LI, ALU.mult, [Rp], [Rp])
        tt("dve", DEN, DEN, TA, ALU.add, [Rp], [Rp])
        recip(DEN, DEN, [Rp], [Rp])
        tsc(TA, AR, -1.0, None, ALU.add, None, [Rp], [Rp])
        tt("dve", QR, TA, LR, ALU.mult, [Rp], [Rp])
        tt("dve", TB, AI, LI, ALU.mult, [Rp], [Rp])
        tt("dve", QR, QR, TB, ALU.add, [Rp], [Rp])
        tt("dve", QR, QR, DEN, ALU.mult, [Rp], [Rp])
        tt("dve", QI, AI, LR, ALU.mult, [Rp], [Rp])
        tt("dve", TB, TA, LI, ALU.mult, [Rp], [Rp])
        tt("dve", QI, QI, TB, ALU.subtract, [Rp], [Rp])
        tt("dve", QI, QI, DEN, ALU.mult, [Rp], [Rp])
        tsc(QI, QI, SGN, -1.0, ALU.mult, ALU.mult, [Rp, r_("cstt")], [Rp])
        tt("dve", pp, pp, QR.unsqueeze(2).to_broadcast([128, 32, 16]), ALU.mult, [Rp, r_("Gt")], [r_("Gt")])
        tt("dve", pq, pq, QI.unsqueeze(2).to_broadcast([128, 32, 16]), ALU.mult, [Rp, r_("XT")], [r_("XT")])
        tt("dve", pp, pp, pq, ALU.add, [r_("XT")], [r_("Gt")])
        S.op("pool", lambda e: e.memset(pz, 0.0), wr=[r_("acc")])
        for gl in range(8):
            cp("dve", pz[:, gl::8, gl * 16:(gl + 1) * 16], pp[:, gl::8, :], [r_("Gt")], [r_("acc")])
        for g in range(32):
            cp("dve", pzb[:], pz[:, g, :], [r_("acc")], [r_("pzb")])
            tr(PB0[:, 0:128], pzb[:], [r_("pzb"), r_("ident")], [r_("PB0")])
            cp("dve", LBa[:, g, :], PB0[:, 0:128], [r_("PB0")], [r_("LBa")])
        cp("dve", LBb[:, :, 0:64], LBa[:, :, 64:128], [r_("LBa")], [r_("LBb")])
        cp("dve", LBb[:, :, 64:128], LBa[:, :, 0:64], [r_("LBa")], [r_("LBb")])
        dma(pq, c1_s[:, :, :], [r_("Gt")], [r_("XT")])
        tsc(pq, pq, SGN, None, ALU.mult, None, [r_("XT"), r_("cstt")], [r_("XT")])
        S.op("pool", lambda e: e.memset(pz, 0.0), rd=[], wr=[r_("acc")])
        for gl in range(8):
            cp("dve", pz[:, gl::8, gl * 16:(gl + 1) * 16], pq[:, gl::8, :], [r_("XT")], [r_("acc")])
        cp("dve", LC[:].rearrange("p a b -> p (a b)"), pz.rearrange("p a b -> p (a b)"), [r_("acc")], [r_("LC")])
        for t4 in range(4):
            tsc(Dg[:, t4, :], ident[:], colt[:, c_d + t4:c_d + t4 + 1], None, ALU.mult, None, [r_("ident"), r_("colt")], [r_("Dg")])
        SCR = [T1, T2]
        for g in range(32):
            sc = SCR[g % 2]
            rsc = r_("scr%d" % (g % 2))
            tsc(sc[:, 0, :], JIDX, P[:, 1, g:g + 1], 1.0 / TWO_PI, ALU.mult, ALU.mult, [Rp, r_("cstt")], [rsc])
            for ti_, (tab, shift) in enumerate(((SINM, 0.0), (COS, 0.25))):
                rs2 = r_("scr%d_%d" % (g % 2, ti_))
                a, b_, c = 1 + 3 * ti_, 2 + 3 * ti_, 3 + 3 * ti_
                tsc(sc[:, a, :], sc[:, 0, :], float(shift), None, ALU.add, None, [rsc], [rs2])
                cp("dve", sc[:, b_, :].bitcast(I32), sc[:, a, :], [rs2], [rs2])
                cp("dve", sc[:, c, :], sc[:, b_, :].bitcast(I32), [rs2], [rs2])
                tt("dve", sc[:, a, :], sc[:, a, :], sc[:, c, :], ALU.subtract, [rs2], [rs2])
                act(tab[:, g, :], sc[:, a, :], AF.Sin, [rs2], [r_("tab")], scale=TWO_PI)
        tsc(SINM[:].rearrange("p a b -> p (a b)"), SINM[:].rearrange("p a b -> p (a b)"), SGN, None, ALU.mult, None, [r_("tab"), r_("cstt")], [r_("tab")])
        _save = _off[0]
        _off[0] = yT_off
        BBR = carve([128, 32, 16])
        BBIs = carve([128, 32, 16])
        VTa = carve([128, 32, 128], BF16)
        VTb = carve([128, 32, 128], BF16)
        assert _off[0] <= M1_off
        _off[0] = _save
        ztok = [M1[1].rearrange("p a b -> p (a b)").bitcast(BF16)[:, 0:1024][:, i * 512:(i + 1) * 512] for i in range(2)]
        Ytmp = [M2[1].rearrange("p a b -> p (a b)")[:, i * 512:(i + 1) * 512] for i in range(2)]
        CARb = XA[1].rearrange("p a b -> p (a b)").bitcast(F32)[:, 0:32]
        Ssum = XA[1].rearrange("p a b -> p (a b)").bitcast(F32)[:, 32:64]
        Ctmp = XA[1].rearrange("p a b -> p (a b)").bitcast(F32)[:, 64:128]
        A128 = XA[1].rearrange("p a b -> p (a b)").bitcast(F32)[:, 128:192]
        bA = XT[1].rearrange("p a b -> p (a b)")[:, 0:512].rearrange("p (a b) -> p a b", a=32)
        bB = XT[1].rearrange("p a b -> p (a b)")[:, 512:1024].rearrange("p (a b) -> p a b", a=32)
        dma(bA, b2_s[:, :, :], [], [r_("bA")])
        dma(bB, b1_s[:, :, :], [], [r_("bB")])
        tt("dve", bA, bA, QR.unsqueeze(2).to_broadcast([128, 32, 16]), ALU.mult, [Rp, r_("bA")], [r_("bA")])
        tt("dve", bB, bB, QI.unsqueeze(2).to_broadcast([128, 32, 16]), ALU.mult, [Rp, r_("bB")], [r_("bB")])
        tt("dve", bA, bA, bB, ALU.subtract, [r_("bB")], [r_("bA")])
        cp("dve", BBR[0:64], pp[0:64], [r_("Gt")], [r_("BBR")])
        cp("dve", BBR[64:128], bA[64:128], [r_("bA")], [r_("BBR")])
        tsc(BBIs[0:64].rearrange("p a b -> p (a b)"), bA[0:64].rearrange("p a b -> p (a b)"), -1.0, None, ALU.mult, None, [r_("bA")], [r_("BBI")])
        cp("dve", BBIs[64:128], pp[64:128], [r_("Gt")], [r_("BBI")])
        S.barrier()
        SHC = sm[:, 70:71]
        tsc(SHC, SGN, 0.125, 0.125, ALU.mult, ALU.add, [r_("cstt")], [r_("shc")])
        JREV = cstt[:, 258:386]
        pzbs = [XA[0][:, 0, :], XA[0][:, 1, :]]
        for g in range(32):
            sc = SCR[g % 2]
            rsc = r_("vscr%d" % (g % 2))
            rpz = r_("pzbs%d" % (g % 2))
            tsc(sc[:, 0, :], JREV, P[:, 1, g:g + 1], 1.0 / TWO_PI, ALU.mult, ALU.mult, [Rp, r_("cstt"), r_("tab")], [rsc])
            tsc(sc[:, 1, :], sc[:, 0, :], SHC, None, ALU.add, None, [rsc, r_("shc")], [rsc])
            cp("dve", sc[:, 2, :].bitcast(I32), sc[:, 1, :], [rsc], [rsc])
            cp("dve", sc[:, 3, :], sc[:, 2, :].bitcast(I32), [rsc], [rsc])
            tt("dve", sc[:, 1, :], sc[:, 1, :], sc[:, 3, :], ALU.subtract, [rsc], [rsc])
            act(sc[:, 4, :], sc[:, 1, :], AF.Sin, [rsc], [r_("vsb%d" % (g % 2))], scale=TWO_PI)
            act(sc[:, 5, :], JREV, AF.Exp, [r_("cstt"), Rp, rsc], [r_("vsc%d" % (g % 2))], scale=P[:, 13, g:g + 1])
            tt("dve", pzbs[g % 2], sc[:, 4, :], sc[:, 5, :], ALU.mult, [r_("vsb%d" % (g % 2)), r_("vsc%d" % (g % 2))], [rpz])
            tr(PB0[:, (g % 2) * 128:(g % 2 + 1) * 128], pzbs[g % 2], [rpz, r_("ident")], [r_("PB0")])
            cp("dve", VTa[:, g, :], PB0[:, (g % 2) * 128:(g % 2 + 1) * 128], [r_("PB0")], [r_("VTa")])
        cp("dve", VTb[:, :, 0:64], VTa[:, :, 64:128], [r_("VTa")], [r_("VTb")])
        cp("dve", VTb[:, :, 64:128], VTa[:, :, 0:64], [r_("VTa")], [r_("VTb")])
        act(Ctmp[:, 0:32], P[:, 13, :], AF.Exp, [Rp], [r_("ctmp")], scale=128.0)
        tt("dve", A128[:, 0:32], Ctmp[:, 0:32], COS[:, :, 127], ALU.mult, [r_("ctmp"), r_("tab")], [r_("A128")])
        tt("dve", A128[:, 32:64], Ctmp[:, 0:32], SINM[:, :, 127], ALU.mult, [r_("ctmp"), r_("tab")], [r_("A128")])
        tsc(A128[:, 32:64], A128[:, 32:64], -1.0, None, ALU.mult, None, [r_("A128")], [r_("A128")])
        S.op("pool", lambda e: e.memset(CARb, 0.0), wr=[r_("CARb")])
        S.barrier()
        LCs = XTb[0].rearrange("p a b -> p (a b)").bitcast(BF16)[:, 0:2048]
        LCs2 = XTb[1].rearrange("p a b -> p (a b)").bitcast(BF16)[:, 0:2048]
        dma(pq, c2_s[:, :, :], [], [r_("XT")])
        tsc(pq, pq, SGN, -1.0, ALU.mult, ALU.mult, [r_("XT"), r_("cstt")], [r_("XT")])
        S.op("pool", lambda e: e.memset(pz, 0.0), rd=[], wr=[r_("acc")])
        for gl in range(8):
            cp("dve", pz[:, gl::8, gl * 16:(gl + 1) * 16], pq[:, gl::8, :], [r_("XT")], [r_("acc")])
        pzf = pz.rearrange("p a b -> p (a b)")
        cp("dve", LCs, pzf[:, 0:2048], [r_("acc")], [r_("LCs")])
        cp("dve", LCs2, pzf[:, 2048:4096], [r_("acc")], [r_("LCs")])
        LCsv = [LCs.rearrange("p (g c) -> p g c", g=16), LCs2.rearrange("p (g c) -> p g c", g=16)]
        S.op("pool", lambda e: e.memset(CAR[:], 0.0), wr=[r_("CAR")])
        for p_ in range(2):
            S.op("pool", lambda e, p_=p_: e.memset(zT[p_], 0.0), wr=[r_("zT%d" % p_)])
        tsc(sm[:, 64:68], colt[:, c_gb:c_gb + 4], 0.5, None, ALU.mult, None, [r_("colt")], [r_("sm2")])

        def rms(src_ap, dst_bf, srcres, dstres, col, jk):
            S.op("dve", lambda e: e.memset(smc(col), 0.0), wr=[r_("sm%d" % col)])
            act(jk, src_ap, AF.Square, srcres, [r_("junk"), r_("sm%d" % col)], accum_out=smc(col))
            act(smc(col + 1), smc(col), AF.Sqrt, [r_("sm%d" % col)], [r_("sm%d" % col)], scale=1.0 / 1024.0, bias=EPS)
            recip(smc(col + 2), smc(col + 1), [r_("sm%d" % col)], [r_("sm%d" % col)])
            act(dst_bf, src_ap, AF.Copy, srcres + [r_("sm%d" % col)], dstres, scale=smc(col + 2))

        def rms_a(src_ap, srcres, col, jk, jkres):
            S.op("dve", lambda e: e.memset(smc(col), 0.0), wr=[r_("sm%d" % col)])
            act(jk, src_ap, AF.Square, srcres, [jkres, r_("sm%d" % col)], accum_out=smc(col))
            act(smc(col + 1), smc(col), AF.Sqrt, [r_("sm%d" % col)], [r_("sm%d" % col)], scale=1.0 / 1024.0, bias=EPS)

        def rms_b(src_ap, dst_bf, srcres, dstres, col):
            recip(smc(col + 2), smc(col + 1), [r_("sm%d" % col)], [r_("sm%d" % col)])
            act(dst_bf, src_ap, AF.Copy, srcres + [r_("sm%d" % col)], dstres, scale=smc(col + 2))

        def transp8(src_bf, srcres):
            for k in range(8):
                tr(PB0[:, k * 128:(k + 1) * 128], src_bf[:, k * 128:(k + 1) * 128], srcres + [r_("ident")], [r_("PB0")])

        tcnt = [0]

        def ssm_state(tau, full, p):
            g0 = tau * 8
            q = tau % 2
            ba, bb = PBs[2 + q * 2], PBs[3 + q * 2]
            rba, rbb = r_("bank%d" % (2 + q * 2)), r_("bank%d" % (3 + q * 2))
            bav = ba[:].rearrange("p (g t) -> p g t", g=4)
            bbv = bb[:].rearrange("p (g t) -> p g t", g=4)
            rz = r_("zT%d" % p)
            rG = r_("Gt%d" % q)
            for hh in range(2):
                for gl4 in range(4):
                    g = g0 + hh * 4 + gl4
                    mm(bav[:, gl4, :], LBa[:, g, :], zT[p][:, 4 + tau, 16:144], True, True, [r_("LBa"), rz], [rba])
                    mm(bbv[:, gl4, :], LBb[:, g, :], zT[p][:, 4 + tau, 16:144], True, True, [r_("LBb"), rz], [rbb])
                sl = slice(hh * 4, hh * 4 + 4)
                gs = slice(g0 + hh * 4, g0 + hh * 4 + 4)
                rD = r_("D1%d" % hh)
                tt("dve", Gt[q][:, sl, :], bav, COS[:, gs, :], ALU.mult, [rba, r_("tab")], [rG])
                tt("dve", D1[hh][:], bbv, SINM[:, gs, :], ALU.mult, [rbb, r_("tab")], [rD])
                tt("pool", Gt[q][:, sl, :], Gt[q][:, sl, :], D1[hh][:], ALU.add, [rD], [rG])
                yield
            rX = r_("XT%d" % q)
            for gl in range(8):
                g = g0 + gl
                S.op("dve", lambda e, gl=gl, g=g, q=q: e.tensor_tensor_scan(
                    out=XT[q][:, gl, :], data0=MAG[:, g:g + 1].to_broadcast([128, 128]), data1=Gt[q][:, gl, :],
                    initial=CAR[:, g:g + 1], op0=ALU.mult, op1=ALU.add),
                    rd=[rG, r_("MAG"), r_("CAR")], wr=[rX])
            yield
            gs = slice(g0, g0 + 8)
            rXb, rXb2 = r_("XTb%d" % q), r_("XTc%d" % q)
            rM1, rM2 = r_("M1%d" % q), r_("M2%d" % q)
            if full:
                dma(XTb[q][0:64, :, :], XT[q][64:128, :, :], [rX], [rXb])
                dma(XTb[q][64:128, :, :], XT[q][0:64, :, :], [rX], [rXb2])
                tt("dve", M1[q], XT[q], COS[:, gs, :], ALU.mult, [rX, r_("tab")], [rM1])
                tt("pool", M2[q], XTb[q], SINM[:, gs, :], ALU.mult, [rXb, rXb2, r_("tab")], [rM2])
                tt("pool", XA[q], M1[q], M2[q], ALU.subtract, [rM1, rM2], [r_("XA%d" % q)])
                tt("pool", CAR[:, gs], M1[q][:, :, 127], M2[q][:, :, 127], ALU.subtract, [rM1, rM2], [r_("CAR")])
            else:
                dma(XTb[q][0:64, :, 127:128], XT[q][64:128, :, 127:128], [rX], [rXb], slow=True)
                dma(XTb[q][64:128, :, 127:128], XT[q][0:64, :, 127:128], [rX], [rXb2], slow=True)
                tt("pool", M1[q][:, :, 127], XT[q][:, :, 127], COS[:, gs, 127], ALU.mult, [rX, r_("tab")], [rM1])
                tt("pool", M2[q][:, :, 127], XTb[q][:, :, 127], SINM[:, gs, 127], ALU.mult, [rXb, rXb2, r_("tab")], [rM2])
                tt("pool", CAR[:, gs], M1[q][:, :, 127], M2[q][:, :, 127], ALU.subtract, [rM1, rM2], [r_("CAR")])
            yield

        def front(tile_idx, mlist, p):
            rz, rzo = r_("zT%d" % p), r_("zT%d" % (1 - p))
            rx, rh, rhT = r_("xt%d" % p), r_("hn%d" % p), r_("hnT%d" % p)
            dma(xt[p], xall[tile_idx * 128:(tile_idx + 1) * 128, :], [], [rx])
            rms(xt[p], hn[p], [rx], [rh], 0, junk)
            transp8(hn[p], [rh])
            act(hnT[p].rearrange("p k t -> p (k t)"), PB0[:], AF.Copy, [r_("PB0")], [rhT])
            yield
            cp("pool", zT[p][:, 0:4, 0:16], zT[1 - p][:, 0:4, 128:144], [rzo], [rz])
            for m in mlist:
                pb = PBs[0] if m < 4 else PBs[1]
                rpb = r_("bank%d" % (m // 4))
                o = pb[:, (m % 4) * 128:(m % 4 + 1) * 128]
                for k in range(8):
                    mm(o, WI[:, k, m * 128:(m + 1) * 128], hnT[p][:, k, :], k == 0, k == 7, [r_("WI"), rhT], [rpb])
            if 0 in mlist:
                act(zT[p][:, 0:4, 16:144], PBs[0][:].rearrange("p (m t) -> p m t", m=4), AF.Copy, [r_("bank0")], [rz])
            if 4 in mlist:
                act(zT[p][:, 4:8, 16:144], PBs[1][:].rearrange("p (m t) -> p m t", m=4), AF.Copy, [r_("bank1")], [rz])
            yield "F"

        PQ = PBs[6]

        def mixer(tile_idx, lt, first, p):
            rz = r_("zT%d" % p)
            ryT, ryg, rygf = r_("yT%d" % p), r_("yg%d" % p), r_("ygf%d" % p)
            yield from front(tile_idx, list(range(8)), p)
            for gi, w in enumerate(WINS):
                o = PQ[:, gi * 128:(gi + 1) * 128]
                for l in range(w):
                    mm(o, PW[:, 2 * gi + (1 if l > 0 else 0), :], zT[p][:, gi, 16 - l:144 - l], l == 0, (l == w - 1) and not first, [r_("PW"), rz], [r_("bank6")])
                if first:
                    S.op("dve", lambda e, gi=gi: e.tensor_tensor_scan(
                        out=cf[:, 0:16], data0=cstt[:, 257:258].to_broadcast([128, 16]), data1=zT[p][:, gi, 16:32],
                        initial=0.0, op0=ALU.mult, op1=ALU.add), rd=[rz, r_("cstt")], wr=[r_("cf")])
                    tt("dve", cf[:, 16:32], cf[:, 0:16], RB[:, 20 + gi * 16:36 + gi * 16], ALU.mult, [r_("cf"), r_("RB")], [r_("cf2")])
                    cp("dve", pzb[:, 0:16], cf[:, 16:32], [r_("cf2")], [r_("pzb")])
                    mm(o[:, 0:16], PW[:, 2 * gi + 1, :], pzb[:, 0:16], False, True, [r_("PW"), r_("pzb")], [r_("bank6")])
            for gi in range(4):
                act(yT[p][:, gi, :], PQ[:, gi * 128:(gi + 1) * 128], AF.Copy, [r_("bank6")], [ryT], scale=colt[:, c_ps_ + gi:c_ps_ + gi + 1])
            yield
            for tau in range(4):
                q = tau % 2
                yield from ssm_state(tau, True, p)
                o = PBs[0][:, tau * 128:(tau + 1) * 128]
                ry = r_("bank0")
                for gl in range(8):
                    mm(o, LC[:, tau * 8 + gl, :], XA[q][:, gl, :], gl == 0, False, [r_("LC"), r_("XA%d" % q)], [ry])
                mm(o, Dg[:, tau, :], zT[p][:, 4 + tau, 16:144], False, True, [r_("Dg"), rz], [ry])
                rg1, rg2 = r_("g1%d" % q), r_("g2%d" % q)
                act(g1[q], o, AF.Square, [ry], [rg1])
                tsc(g1[q], g1[q], 0.044715, 1.0, ALU.mult, ALU.add, [rg1], [rg1])
                tt("dve", g1[q], g1[q], o, ALU.mult, [rg1, ry], [rg1])
                act(g2[q], g1[q], AF.Tanh, [rg1], [rg2], scale=0.7978845608028654)
                tsc(g2[q], g2[q], 0.5, 0.5, ALU.mult, ALU.add, [rg2], [rg2])
                tt("dve", ygf[p][:, tau, :], g2[q], o, ALU.mult, [rg2, ry], [rygf])
                cp("pool", yg[p][:, tau, :], ygf[p][:, tau, :], [rygf], [ryg])
                yield
            for m in range(4):
                q = m % 2
                rg1 = r_("g1%d" % q)
                o = PBs[1][:, m * 128:(m + 1) * 128]
                for k in range(4):
                    mm(o, GW[:, k, m * 128:(m + 1) * 128], yg[p][:, k, :], k == 0, k == 3, [r_("GW"), ryg], [r_("bank1")])
                act(g1[q], o, AF.Tanh, [r_("bank1"), r_("sm2")], [rg1], scale=0.5, bias=sm[:, 64 + m:65 + m])
                tsc(g1[q], g1[q], 0.5, 0.5, ALU.mult, ALU.add, [rg1], [rg1])
                tt("pool", yT[p][:, 4 + m, :], g1[q], ygf[p][:, m, :], ALU.mult, [rg1, rygf], [ryT])
            yield
            for half in range(2):
                pb = PBs[half]
                rpb = r_("bank%d" % half)
                for k in range(8):
                    mm(pb[:], yT[p][:, k, :], WO[:, k, half * 512:(half + 1) * 512], k == 0, k == 7, [ryT, r_("WO")], [rpb])
                tt("dve", acc[:, lt, half * 512:(half + 1) * 512], pb[:], xt[p][:, half * 512:(half + 1) * 512], ALU.add, [rpb, r_("xt%d" % p)], [r_("acc%d" % lt)])
            yield
            rh = r_("hn%d" % p)
            rms(acc[:, lt, :], hn[p], [r_("acc%d" % lt)], [rh], 4, junk)
            transp8(hn[p], [rh])
            act(hn2T[:, :, lt * 128:(lt + 1) * 128], PB0[:].rearrange("p (k t) -> p k t", k=8), AF.Copy, [r_("PB0")], [r_("hn2T")])
            yield
            o = PQ[:, 0:20]
            for k in range(8):
                mm(o, hn2T[:, k, lt * 128:(lt + 1) * 128], WRb[:, k, :], k == 0, k == 7, [r_("hn2T"), r_("WRb")], [r_("bank6")])
            L_ = sm[:, 100:120]
            rS = r_("smr")
            tt("dve", L_, o, RB[:, 0:20], ALU.add, [r_("bank6"), r_("RB")], [rS])
            cL, fL = sm[:, 100:104], sm[:, 104:120]
            M, GM, CM, TH, NUM, SS, PG = smc(120), smc(121, 4), smc(125, 4), smc(129, 4), smc(133, 4), smc(137), smc(138)
            red(M, cL, ALU.max, [rS], [rS])
            tsc(GM, cL, M, None, ALU.is_equal, None, [rS], [rS])
            tsc(CM, cL, M, None, ALU.subtract, None, [rS], [rS])
            act(TH, CM, AF.Tanh, [rS], [rS], scale=0.5)
            tsc(NUM, TH, 1.0, None, ALU.add, None, [rS], [rS])
            tsc(TH, TH, -1.0, 1.0, ALU.mult, ALU.add, [rS], [rS])
            recip(TH, TH, [rS], [rS])
            tt("dve", NUM, NUM, TH, ALU.mult, [rS], [rS])
            red(SS, NUM, ALU.add, [rS], [rS])
            recip(PG, SS, [rS], [rS])
            FT = sm[:, 140:156]
            tt("dve", FT.rearrange("p (g j) -> p g j", g=4), fL.rearrange("p (g j) -> p g j", g=4),
               GM.unsqueeze(2).to_broadcast([128, 4, 4]), ALU.mult, [rS], [rS])
            FS = smc(156, 4)
            red(FS, FT.rearrange("p (g j) -> p j g", g=4), ALU.add, [rS], [rS])
            M1_, K1, F2, M2_, K2, DD, W1, W2, WJ = smc(160), smc(161, 4), smc(165, 4), smc(169), smc(170, 4), smc(174), smc(175), smc(176), smc(177, 4)
            red(M1_, FS, ALU.max, [rS], [rS])
            tsc(K1, FS, M1_, None, ALU.is_equal, None, [rS], [rS])
            stt(F2, K1, -1e30, FS, ALU.mult, ALU.add, [rS], [rS])
            red(M2_, F2, ALU.max, [rS], [rS])
            tsc(K2, F2, M2_, None, ALU.is_equal, None, [rS], [rS])
            tt("dve", DD, M2_, M1_, ALU.subtract, [rS], [rS])
            act(DD, DD, AF.Tanh, [rS], [rS], scale=0.5)
            tsc(W1, DD, -0.5, 0.5, ALU.mult, ALU.add, [rS], [rS])
            tsc(W2, DD, 0.5, 0.5, ALU.mult, ALU.add, [rS], [rS])
            tsc(WJ, K1, W1, None, ALU.mult, None, [rS], [rS])
            stt(WJ, K2, W2, WJ, ALU.mult, ALU.add, [rS], [rS])
            tsc(WJ, WJ, PG, None, ALU.mult, None, [rS], [rS])
            tt("dve", gates[:, lt, :].rearrange("p (g j) -> p g j", g=4), GM.unsqueeze(2).to_broadcast([128, 4, 4]),
               WJ.unsqueeze(1).to_broadcast([128, 4, 4]), ALU.mult, [rS], [r_("gates")])
            yield

        def router(lt, o, rq0):
            L_ = sm[:, 100:120]
            rS = r_("smr")
            tt("dve", L_, o, RB[:, 0:20], ALU.add, [rq0, r_("RB")], [rS])
            cL, fL = sm[:, 100:104], sm[:, 104:120]
            M, GM, CM, TH, NUM, SS, PG = smc(120), smc(121, 4), smc(125, 4), smc(129, 4), smc(133, 4), smc(137), smc(138)
            red(M, cL, ALU.max, [rS], [rS])
            tsc(GM, cL, M, None, ALU.is_equal, None, [rS], [rS])
            tsc(CM, cL, M, None, ALU.subtract, None, [rS], [rS])
            act(TH, CM, AF.Tanh, [rS], [rS], scale=0.5)
            tsc(NUM, TH, 1.0, None, ALU.add, None, [rS], [rS])
            tsc(TH, TH, -1.0, 1.0, ALU.mult, ALU.add, [rS], [rS])
            recip(TH, TH, [rS], [rS])
            tt("dve", NUM, NUM, TH, ALU.mult, [rS], [rS])
            red(SS, NUM, ALU.add, [rS], [rS])
            recip(PG, SS, [rS], [rS])
            FT = sm[:, 140:156]
            tt("dve", FT.rearrange("p (g j) -> p g j", g=4), fL.rearrange("p (g j) -> p g j", g=4),
               GM.unsqueeze(2).to_broadcast([128, 4, 4]), ALU.mult, [rS], [rS])
            FS = smc(156, 4)
            red(FS, FT.rearrange("p (g j) -> p j g", g=4), ALU.add, [rS], [rS])
            M1_, K1, F2, M2_, K2, DD, W1, W2, WJ = smc(160), smc(161, 4), smc(165, 4), smc(169), smc(170, 4), smc(174), smc(175), smc(176), smc(177, 4)
            red(M1_, FS, ALU.max, [rS], [rS])
            tsc(K1, FS, M1_, None, ALU.is_equal, None, [rS], [rS])
            stt(F2, K1, -1e30, FS, ALU.mult, ALU.add, [rS], [rS])
            red(M2_, F2, ALU.max, [rS], [rS])
            tsc(K2, F2, M2_, None, ALU.is_equal, None, [rS], [rS])
            tt("dve", DD, M2_, M1_, ALU.subtract, [rS], [rS])
            act(DD, DD, AF.Tanh, [rS], [rS], scale=0.5)
            tsc(W1, DD, -0.5, 0.5, ALU.mult, ALU.add, [rS], [rS])
            tsc(W2, DD, 0.5, 0.5, ALU.mult, ALU.add, [rS], [rS])
            tsc(WJ, K1, W1, None, ALU.mult, None, [rS], [rS])
            stt(WJ, K2, W2, WJ, ALU.mult, ALU.add, [rS], [rS])
            tsc(WJ, WJ, PG, None, ALU.mult, None, [rS], [rS])
            tt("dve", gates[:, lt, :].rearrange("p (g j) -> p g j", g=4), GM.unsqueeze(2).to_broadcast([128, 4, 4]),
               WJ.unsqueeze(1).to_broadcast([128, 4, 4]), ALU.mult, [rS], [r_("gates")])

        RRs = [M2[0], M2[1]]

        def build_RR(tau, q):
            gs = slice(tau * 8, tau * 8 + 8)
            act(RRs[q], MAG[:, gs].unsqueeze(2).to_broadcast([128, 8, 128]), AF.Copy, [r_("MAG")], [r_("RR%d" % q)])
            S.op("pool", lambda e: e.memset(RRs[q][:, :, 0:1], 0.0), rd=[], wr=[r_("RR%d" % q)])

        def run_gen(g):
            for _ in g:
                pass

        def g_front(tile_idx, lt, p):
            rz, rzo = r_("zT%d" % p), r_("zT%d" % (1 - p))
            rx, rh, rhT = r_("xt%d" % p), r_("hn%d" % p), r_("hnT%d" % p)
            dma(xt[p], xall[tile_idx * 128:(tile_idx + 1) * 128, :], [], [rx])
            dma(acc[:, lt, :], xall[tile_idx * 128:(tile_idx + 1) * 128, :], [], [r_("acc%d" % lt)])
            rms_a(xt[p], [rx], 0, hn[p], rh)
            yield
            rms_b(xt[p], hn[p], [rx], [rh], 0)
            yield
            transp8(hn[p], [rh])
            act(hnT[p].rearrange("p k t -> p (k t)"), PB0[:], AF.Copy, [r_("PB0")], [rhT])
            yield
            cp("pool", zT[p][:, 0:4, 0:16], zT[1 - p][:, 0:4, 128:144], [rzo], [rz])
            for m in range(8):
                pb = PBs[0] if m < 4 else PBs[1]
                rpb = r_("bank%d" % (m // 4))
                o = pb[:, (m % 4) * 128:(m % 4 + 1) * 128]
                for k in range(8):
                    mm(o, WI[:, k, m * 128:(m + 1) * 128], hnT[p][:, k, :], k == 0, k == 7, [r_("WI"), rhT], [rpb])
                if m == 3:
                    act(zT[p][:, 0:4, 16:144], PBs[0][:].rearrange("p (m t) -> p m t", m=4), AF.Copy, [r_("bank0")], [rz])
            act(zT[p][:, 4:8, 16:144], PBs[1][:].rearrange("p (m t) -> p m t", m=4), AF.Copy, [r_("bank1")], [rz])
            yield

        def st_pool(p, first):
            rz, ryT = r_("zT%d" % p), r_("yT%d" % p)
            rq = r_("bank6")
            for gi, w in enumerate(WINS):
                o = PQ[:, gi * 128:(gi + 1) * 128]
                for l in range(w):
                    mm(o, PW[:, 2 * gi + (1 if l > 0 else 0), :], zT[p][:, gi, 16 - l:144 - l], l == 0, (l == w - 1) and not first, [r_("PW"), rz], [rq])
                if first:
                    S.op("dve", lambda e, gi=gi: e.tensor_tensor_scan(
                        out=cf[:, 0:16], data0=cstt[:, 257:258].to_broadcast([128, 16]), data1=zT[p][:, gi, 16:32],
                        initial=0.0, op0=ALU.mult, op1=ALU.add), rd=[rz, r_("cstt")], wr=[r_("cf")])
                    tt("dve", cf[:, 16:32], cf[:, 0:16], RB[:, 20 + gi * 16:36 + gi * 16], ALU.mult, [r_("cf"), r_("RB")], [r_("cf2")])
                    cp("dve", pzb[:, 0:16], cf[:, 16:32], [r_("cf2")], [r_("pzb")])
                    mm(o[:, 0:16], PW[:, 2 * gi + 1, :], pzb[:, 0:16], False, True, [r_("PW"), r_("pzb")], [rq])
            for gi in range(4):
                act(yT[p][:, gi, :], PQ[:, gi * 128:(gi + 1) * 128], AF.Copy, [rq], [ryT], scale=colt[:, c_ps_ + gi:c_ps_ + gi + 1])

        def st_Dp(p, tau):
            g0 = tau * 8
            rz = r_("zT%d" % p)
            for hh in range(2):
                ba, bb = PBs[2 + hh * 2], PBs[3 + hh * 2]
                rba, rbb = r_("bank%d" % (2 + hh * 2)), r_("bank%d" % (3 + hh * 2))
                bav = ba[:].rearrange("p (g t) -> p g t", g=4)
                bbv = bb[:].rearrange("p (g t) -> p g t", g=4)
                for gl4 in range(4):
                    g = g0 + hh * 4 + gl4
                    mm(bav[:, gl4, :], LBa[:, g, :], zT[p][:, 4 + tau, 16:144], True, True, [r_("LBa"), rz], [rba])
                    mm(bbv[:, gl4, :], LBb[:, g, :], zT[p][:, 4 + tau, 16:144], True, True, [r_("LBb"), rz], [rbb])

        def st_Dd(tau, q):
            g0 = tau * 8
            rG, rM1 = r_("Gt%d" % q), r_("M1t")
            for hh in range(2):
                ba, bb = PBs[2 + hh * 2], PBs[3 + hh * 2]
                rba, rbb = r_("bank%d" % (2 + hh * 2)), r_("bank%d" % (3 + hh * 2))
                bav = ba[:].rearrange("p (g t) -> p g t", g=4)
                bbv = bb[:].rearrange("p (g t) -> p g t", g=4)
                sl = slice(hh * 4, hh * 4 + 4)
                gs = slice(g0 + hh * 4, g0 + hh * 4 + 4)
                tt("dve", Gt[q][:, sl, :], bav, COS[:, gs, :], ALU.mult, [rba, r_("tab")], [rG])
                tt("dve", M1[0][:, sl, :], bbv, SINM[:, gs, :], ALU.mult, [rbb, r_("tab")], [rM1])
                tt("dve", Gt[q][:, sl, :], Gt[q][:, sl, :], M1[0][:, sl, :], ALU.add, [rM1], [rG])

        def st_S(tau, q):
            g0 = tau * 8
            gs = slice(g0, g0 + 8)
            rG, rX = r_("Gt%d" % q), r_("XT%d" % q)
            c8 = sm[:, 80:88]
            tt("dve", c8, MAG[:, gs], CAR[:, gs], ALU.mult, [r_("MAG"), r_("CAR")], [r_("c8")])
            tt("dve", Gt[q][:, :, 0], Gt[q][:, :, 0], c8, ALU.add, [r_("c8")], [rG])
            S.op("dve", lambda e, q=q: e.tensor_tensor_scan(
                out=XT[q].rearrange("p a b -> p (a b)"), data0=RRs[q].rearrange("p a b -> p (a b)"),
                data1=Gt[q].rearrange("p a b -> p (a b)"), initial=0.0, op0=ALU.mult, op1=ALU.add),
                rd=[rG, r_("RR%d" % q)], wr=[rX])
            xs = sm[:, 16 + 8 * q:24 + 8 * q]
            dma(xs[0:64, :], XT[q][64:128, :, 127], [rX], [r_("xs%d" % q)], slow=True)
            dma(xs[64:128, :], XT[q][0:64, :, 127], [rX], [r_("xsb%d" % q)], slow=True)
            build_RR((tau + 2) % 4, q)

        M1b = [XA[0], XA[1]]
        M2b = [M1[1].rearrange("p a b -> p (a b)").bitcast(BF16)[:, i * 1024:(i + 1) * 1024].rearrange("p (a b) -> p a b", a=8) for i in range(2)]

        def st_M(tau, q):
            g0 = tau * 8
            gs = slice(g0, g0 + 8)
            rX = r_("XT%d" % q)
            tt("dve", M1b[q], XT[q], COS[:, gs, :], ALU.mult, [rX, r_("tab")], [r_("XA%d" % q)])
            tt("dve", M2b[q], XT[q], SINM[:, gs, :], ALU.mult, [rX, r_("tab")], [r_("M2b%d" % q)])
            xs = sm[:, 16 + 8 * q:24 + 8 * q]
            t1c = sm[:, 32 + 8 * q:40 + 8 * q]
            tt("dve", t1c, XT[q][:, :, 127], COS[:, gs, 127], ALU.mult, [rX, r_("tab")], [r_("t1c%d" % q)])
            tt("dve", xs, xs, SINM[:, gs, 127], ALU.mult, [r_("xs%d" % q), r_("xsb%d" % q), r_("tab")], [r_("xs%d" % q), r_("xsb%d" % q)])
            tt("dve", CAR[:, gs], t1c, xs, ALU.subtract, [r_("t1c%d" % q), r_("xs%d" % q), r_("xsb%d" % q)], [r_("CAR")])

        def st_Cp(p, tau, q):
            rz = r_("zT%d" % p)
            o = PQ[:, tau * 128:(tau + 1) * 128]
            ry = r_("bank6")
            for gl in range(8):
                g = tau * 8 + gl
                mm(o, LC[:, g, :], M1b[q][:, gl, :], gl == 0, False, [r_("LC"), r_("XA%d" % q)], [ry])
                mm(o, LCsv[g // 16][:, g % 16, :], M2b[q][:, gl, :], False, False, [r_("LCs"), r_("M2b%d" % q)], [ry])
            mm(o, Dg[:, tau, :], zT[p][:, 4 + tau, 16:144], False, True, [r_("Dg"), rz], [ry])
            act(g1[q], o, AF.Square, [ry], [r_("g1%d" % q)], scale=0.21145921592590347)

        def st_Ca(p, tau, q):
            o = PQ[:, tau * 128:(tau + 1) * 128]
            ry = r_("bank6")
            rg1, rg2 = r_("g1%d" % q), r_("g2%d" % q)
            stt(g1[q], g1[q], 1.0, o, ALU.add, ALU.mult, [rg1, ry], [rg1])
            act(g2[q], g1[q], AF.Tanh, [rg1], [rg2], scale=0.7978845608028654)

        def st_Cb(p, tau, q):
            ryg, rygf = r_("yg%d" % p), r_("ygf%d" % p)
            o = PQ[:, tau * 128:(tau + 1) * 128]
            ry = r_("bank6")
            rg2 = r_("g2%d" % q)
            stt(ygf[p][:, tau, :], g2[q], 1.0, o, ALU.add, ALU.mult, [rg2, ry], [rygf])
            act(yg[p][:, tau, :], ygf[p][:, tau, :], AF.Copy, [rygf], [ryg])

        def g_tail(lt, p):
            ryT, ryg, rygf = r_("yT%d" % p), r_("yg%d" % p), r_("ygf%d" % p)
            jf = junk.bitcast(F32)
            glt = [jf[:, i * 128:(i + 1) * 128] for i in range(4)]
            rgl = [r_("glt%d" % i) for i in range(4)]
            rbk = [r_("bank1"), r_("bank0")]
            for m in range(4):
                pbm = PBs[1] if m % 2 == 0 else PBs[0]
                rb = rbk[m % 2]
                o = pbm[:, (m // 2) * 128:(m // 2 + 1) * 128]
                for k in range(4):
                    mm(o, GW[:, k, m * 128:(m + 1) * 128], yg[p][:, k, :], k == 0, k == 3, [r_("GW"), ryg], [rb])
                act(glt[m], o, AF.Tanh, [rb, r_("sm2")], [rgl[m]], scale=0.5, bias=sm[:, 64 + m:65 + m])
            yield
            for m in range(4):
                stt(yT[p][:, 4 + m, :], glt[m], 1.0, ygf[p][:, m, :], ALU.add, ALU.mult, [rgl[m], rygf], [ryT])
            for half in range(2):
                pb = PBs[half]
                rqs = [r_("bank%d" % half)]
                for k in range(8):
                    mm(pb[:], yT[p][:, k, :], WO[:, k, half * 512:(half + 1) * 512], k == 0, k == 7, [ryT, r_("WO")], rqs)
            yield
            for half in range(2):
                pb = PBs[half]
                rqs = [r_("bank%d" % half)]
                tt("dve", acc[:, lt, half * 512:(half + 1) * 512], pb[:], acc[:, lt, half * 512:(half + 1) * 512], ALU.add, rqs + [r_("acc%d" % lt)], [r_("acc%d" % lt)])
            rh = r_("hn2b%d" % p)
            rms_a(acc[:, lt, :], [r_("acc%d" % lt)], 4, hn2b[p], rh)
            yield
            rms_b(acc[:, lt, :], hn2b[p], [r_("acc%d" % lt)], [rh], 4)
            yield
            transp8(hn2b[p], [rh])
            act(hn2T[:, :, lt * 128:(lt + 1) * 128], PB0[:].rearrange("p (k t) -> p k t", k=8), AF.Copy, [r_("PB0")], [r_("hn2T")])
            yield
            o = PQ[:, 0:20]
            rq0 = r_("bank6")
            for k in range(8):
                mm(o, hn2T[:, k, lt * 128:(lt + 1) * 128], WRb[:, k, :], k == 0, k == 7, [r_("hn2T"), r_("WRb")], [rq0])
            router(lt, o, rq0)
            yield

        def mixer_sb(sbi):
            nun = SBT * 4
            par = lambda lt: (sbi * SBT + lt) % 2
            bg = []

            def advance():
                for g in list(bg):
                    try:
                        next(g)
                    except StopIteration:
                        bg.remove(g)
            run_gen(g_front(NT_PRE + sbi * SBT, 0, par(0)))
            build_RR(0, 0)
            build_RR(1, 1)
            st_Dp(par(0), 0)
            if SBT > 1:
                bg.append(g_front(NT_PRE + sbi * SBT + 1, 1, par(1)))
            k = 0
            while k < nun + 4 or bg:
                advance()
                if 0 <= k - 4 < nun:
                    lt3, tau3 = divmod(k - 4, 4)
                    st_Ca(par(lt3), tau3, (k - 4) % 2)
                if 0 <= k - 3 < nun:
                    st_M((k - 3) % 4, (k - 3) % 2)
                if 0 <= k - 4 < nun:
                    st_Cb(par(lt3), tau3, (k - 4) % 2)
                    if tau3 == 3:
                        bg.append(g_tail(lt3, par(lt3)))
                if k < nun:
                    lt, tau = divmod(k, 4)
                    if tau == 2:
                        st_pool(par(lt), sbi == 0 and lt == 0)
                    st_Dd(tau, k % 2)
                    if tau == 3 and 1 <= lt + 1 and lt + 2 < SBT:
                        bg.append(g_front(NT_PRE + sbi * SBT + lt + 2, lt + 2, par(lt + 2)))
                if 0 <= k - 1 < nun:
                    st_S((k - 1) % 4, (k - 1) % 2)
                if 0 <= k - 3 < nun:
                    ltp, taup = divmod(k - 3, 4)
                    st_Cp(par(ltp), taup, (k - 3) % 2)
                if k + 1 < nun:
                    ltn, taun = divmod(k + 1, 4)
                    st_Dp(par(ltn), taun)
                k += 1

        def pipeline(gens):
            gens = list(gens)
            active = []
            while gens or active:
                if gens and len(active) < 2 and (not active or active[-1][1][0]):
                    active.append((gens.pop(0), [False]))
                for it in list(active):
                    g, st = it
                    try:
                        v = next(g)
                        if v == "F":
                            st[0] = True
                    except StopIteration:
                        active.remove(it)

        wgb = nc.dram_tensor("wgb", [16, 128, 2048], BF16).ap()
        wub = nc.dram_tensor("wub", [16, 128, 2048], BF16).ap()
        wdb = nc.dram_tensor("wdb", [16, 128, 2048], BF16).ap()
        accf = acc[:].rearrange("p a b -> p (a b)")
        hn2f = hn2T[:].rearrange("p a b -> p (a b)")
        cast_units = []
        for e in range(16):
            cast_units += [(0, e), (1, e), (2, e)]
        ucnt = [0]

        def cast_unit():
            if not cast_units:
                return
            kind, e = cast_units.pop(0)
            sl = ucnt[0] % 2
            ucnt[0] += 1
            stg = accf[:, sl * 2048:(sl + 1) * 2048]
            tmp = hn2f[:, sl * 2048:(sl + 1) * 2048]
            rs_, rt_ = r_("stg%d" % sl), r_("tmpb%d" % sl)
            if kind == 2:
                dma(stg.rearrange("p (k n) -> p k n", k=2), wd_d[e, :, :].rearrange("(k p) n -> p k n", p=128), [], [rs_], q="pool")
                cp("pool", tmp, stg, [rs_], [rt_])
                dma(wdb[e, :, :], tmp, [rt_], [r_("wscr")], q="pool")
            else:
                src = wg_d if kind == 0 else wu_d
                dst = wgb if kind == 0 else wub
                dma(stg.rearrange("p (k n) -> p k n", k=8), src[e, :, :].rearrange("(k p) n -> p k n", p=128), [], [rs_], q="pool")
                tt("pool", tmp.rearrange("p (k n) -> p k n", k=8), stg.rearrange("p (k n) -> p k n", k=8),
                   colt[:, c_nf:c_nf + 8].unsqueeze(2).to_broadcast([128, 8, 256]), ALU.mult, [rs_, r_("colt")], [rt_])
                dma(dst[e, :, :], tmp, [rt_], [r_("wscr")], q="pool")

        def moe(sbi):
            dma(NF, rows[0:1, 0:1024].partition_broadcast(128), [], [r_("NF")])
            nblk = SBT // 4
            tok = slice(0, SBT * 128)

            def gu(e):
                sl = e % 2
                rwg, rwu, rwd = r_("WG%d" % sl), r_("WU%d" % sl), r_("WD%d" % sl)
                dma(WGs[sl].rearrange("p k n -> p (k n)"), wgb[e, :, :], [r_("wscr")], [rwg])
                dma(WUs[sl].rearrange("p k n -> p (k n)"), wub[e, :, :], [r_("wscr")], [rwu])
                dma(WDs[sl].rearrange("p k n -> p (k n)"), wdb[e, :, :], [r_("wscr")], [rwd])
                for ft in range(2):
                    gp, up = PBs[2 + ft], PBs[4 + ft]
                    rg, ru = r_("bank%d" % (2 + ft)), r_("bank%d" % (4 + ft))
                    rsg, rhT_ = r_("sgT%d%d" % (sl, ft)), r_("hT%d%d" % (sl, ft))
                    for k in range(8):
                        mm(gp[:], WGs[sl][:, k, ft * 128:(ft + 1) * 128], hn2T[:, k, tok], k == 0, k == 7, [rwg, r_("hn2T")], [rg])
                    for k in range(8):
                        mm(up[:], WUs[sl][:, k, ft * 128:(ft + 1) * 128], hn2T[:, k, tok], k == 0, k == 7, [rwu, r_("hn2T")], [ru])
                    act(sgT[sl][:, ft, :], gp[:], AF.Silu, [rg], [rsg])
                    tt("dve", hT[sl][:, ft, :], up[:], sgT[sl][:, ft, :], ALU.mult, [ru, rsg], [rhT_])

            def down(e):
                sl = e % 2
                rwd = r_("WD%d" % sl)
                for lt in range(SBT):
                    for half in range(2):
                        bi = [0, 1, 6][(lt * 2 + half) % 3]
                        pb = PBs[bi]
                        rpb = r_("bank%d" % bi)
                        for ft in range(2):
                            mm(pb[:], hT[sl][:, ft, lt * 128:(lt + 1) * 128], WDs[sl][:, ft, half * 512:(half + 1) * 512], ft == 0, ft == 1,
                               [r_("hT%d%d" % (sl, ft)), rwd], [rpb])
                        stt(acc[:, lt, half * 512:(half + 1) * 512], pb[:], gates[:, lt, e:e + 1], acc[:, lt, half * 512:(half + 1) * 512],
                            ALU.mult, ALU.add, [rpb, r_("gates"), r_("acc%d" % lt)], [r_("acc%d" % lt)])
            for e in range(17):
                if e < 16:
                    gu(e)
                if e >= 1:
                    down(e - 1)
            for lt in range(SBT):
                tix = sbi * SBT + lt
                o2 = lt % 2
                ro = r_("outt%d" % o2)
                S.op("dve", lambda e: e.memset(smc(8), 0.0), wr=[r_("sm8")])
                act(junk2, acc[:, lt, :], AF.Square, [r_("acc%d" % lt)], [r_("junk2"), r_("sm8")], accum_out=smc(8))
                act(smc(9), smc(8), AF.Sqrt, [r_("sm8")], [r_("sm8")], scale=1.0 / 1024.0, bias=EPS)
                recip(smc(10), smc(9), [r_("sm8")], [r_("sm8")])
                stt(outt[o2], acc[:, lt, :], smc(10), NF, ALU.mult, ALU.mult, [r_("acc%d" % lt), r_("sm8"), r_("NF")], [ro])
                dma(out[tix * 128:(tix + 1) * 128, :], outt[o2], [ro], [r_("out")])

        S.barrier()
        def pre_tile(t, p):
            last = t == NT_PRE - 1
            yield from front(t, [0, 1, 2, 3] if last else [], p)
            rhT = r_("hnT%d" % p)
            for k in range(8):
                mm(PBs[1][:], hnT[p][:, k, :], WI[:, k, 512:1024], k == 0, k == 7, [r_("WI"), rhT], [r_("bank1")])
            act(ztok[p], PBs[1][:], AF.Copy, [r_("bank1")], [r_("ztok%d" % p)])
            yield
            ya, yb = PBs[2 + 2 * p], PBs[3 + 2 * p]
            rya, ryb = r_("bank%d" % (2 + 2 * p)), r_("bank%d" % (3 + 2 * p))
            for g in range(32):
                mm(ya[:, g * 16:(g + 1) * 16], VTa[:, g, :], ztok[p][:, g * 16:(g + 1) * 16], True, True, [r_("VTa"), r_("ztok%d" % p)], [rya])
            for g in range(32):
                mm(yb[:, g * 16:(g + 1) * 16], VTb[:, g, :], ztok[p][:, g * 16:(g + 1) * 16], True, True, [r_("VTb"), r_("ztok%d" % p)], [ryb])
            yield
            tt("dve", Ytmp[0], ya[:], BBR.rearrange("p a b -> p (a b)"), ALU.mult, [rya, r_("BBR")], [r_("Ytmp0")])
            tt("dve", Ytmp[1], yb[:], BBIs.rearrange("p a b -> p (a b)"), ALU.mult, [ryb, r_("BBI")], [r_("Ytmp1")])
            tt("dve", Ytmp[0], Ytmp[0], Ytmp[1], ALU.add, [r_("Ytmp1")], [r_("Ytmp0")])
            red(Ssum, Ytmp[0].rearrange("p (a b) -> p a b", a=32), ALU.add, [r_("Ytmp0")], [r_("Ssum")])
            tt("dve", Ctmp[:, 0:32], A128[:, 0:32], CAR[:], ALU.mult, [r_("A128"), r_("CAR")], [r_("ctmp")])
            tt("dve", Ctmp[:, 32:64], A128[:, 32:64], CARb, ALU.mult, [r_("A128"), r_("CARb"), r_("CARb2")], [r_("ctmp2")])
            tt("dve", Ctmp[:, 0:32], Ctmp[:, 0:32], Ctmp[:, 32:64], ALU.add, [r_("ctmp2")], [r_("ctmp")])
            tt("dve", CAR[:], Ctmp[:, 0:32], Ssum, ALU.add, [r_("ctmp"), r_("Ssum")], [r_("CAR")])
            dma(CARb[0:64, :], CAR[64:128, :], [r_("CAR")], [r_("CARb")])
            dma(CARb[64:128, :], CAR[0:64, :], [r_("CAR")], [r_("CARb2")])
            cast_unit()
            if t % 2 == 1:
                cast_unit()
            yield
        pipeline([pre_tile(t, t % 2) for t in range(NT_PRE)])
        while cast_units:
            cast_unit()
        S.barrier()
        for sbi in range(NT_MAIN // SBT):
            mixer_sb(sbi)
            S.barrier()
            moe(sbi)
            S.barrier()
        S.final_wait("sp", [r_("out")])

        print('SBUF remaining', nc.sbuf_bytes_remaining, 'mixer_end', mixer_end, 'moe_end', _off[0])
        block = es.enter_context(nc.Block())

        @block.sync
        def _(e):
            S.replay("sp", e)

        @block.tensor
        def _(e):
            S.replay("pe", e)

        @block.scalar
        def _(e):
            S.replay("act", e)

        @block.vector
        def _(e):
            S.replay("dve", e)

        @block.gpsimd
        def _(e):
            S.replay("pool", e)
    return nc


def _col(v, k):
    return np.ascontiguousarray(np.asarray(v, np.float32).reshape(k, 128).T)


def kernel(x, norm_mix, w_in, pool_w, pool_scale, ssm_a_re, ssm_a_im, ssm_log_step,
           ssm_b_re, ssm_b_im, ssm_c_re, ssm_c_im, ssm_d, glu_w, glu_b, w_out, norm_ffn,
           router_coarse_w, router_coarse_b, router_fine_w, router_fine_b,
           exp_w_gate, exp_w_up, exp_w_down, norm_final):
    f = np.float32
    x = np.asarray(x, f)
    cols = np.zeros((128, 64), f)
    cols[:, 0:8] = _col(norm_mix[0], 8)
    cols[:, 8:16] = _col(norm_ffn[0], 8)
    cols[:, 16:20] = _col(pool_scale[0], 4)
    cols[:, 20:24] = _col(ssm_d[0], 4)
    cols[:, 24:28] = _col(glu_b[0], 4)
    are = np.asarray(ssm_a_re[0], f)
    aim = np.asarray(ssm_a_im[0], f)
    ls = np.asarray(ssm_log_step[0], f)
    sp_s = np.zeros((128, 96), f)
    sp_s[:, 0:32] = np.concatenate([are.T, are.T], 0)
    sp_s[:, 32:64] = np.concatenate([aim.T, aim.T], 0)
    sp_s[:, 64:96] = np.broadcast_to(ls[None, :], (128, 32))
    br = np.asarray(ssm_b_re[0], f).transpose(1, 0, 2)
    bi = np.asarray(ssm_b_im[0], f).transpose(1, 0, 2)
    b1 = np.ascontiguousarray(np.concatenate([br, bi], 0))
    b2 = np.ascontiguousarray(np.concatenate([bi, br], 0))
    cr = np.asarray(ssm_c_re[0], f).transpose(2, 0, 1)
    ci = np.asarray(ssm_c_im[0], f).transpose(2, 0, 1)
    c1 = np.ascontiguousarray(np.concatenate([cr, ci], 0))
    c2 = np.ascontiguousarray(np.concatenate([ci, cr], 0))
    cst = np.zeros((128, 386), f)
    cst[:, 258:386] = np.arange(127, -1, -1, dtype=f)[None, :]
    cst[:, 0:128] = np.eye(128, dtype=f)
    cst[:, 128:256] = np.arange(1, 129, dtype=f)[None, :]
    cst[:, 256] = np.where(np.arange(128) < 64, 1.0, -1.0)
    cst[:, 257] = 1.0
    wr = np.ascontiguousarray(np.concatenate([np.asarray(router_coarse_w[0], f), np.asarray(router_fine_w[0], f)], 1))
    rb = np.concatenate([np.asarray(router_coarse_b[0], f), np.asarray(router_fine_b[0], f)])
    in_maps = []
    for c in range(8):
        b, half = c // 2, c % 2
        main = x[b, half * 4096:(half + 1) * 4096]
        pre = x[b, 0:4096] if half == 1 else np.zeros((4096, 1024), f)
        fix = np.zeros((4, 16), f)
        if half == 0:
            for gi, w in enumerate(WINS):
                for t in range(w - 1):
                    fix[gi, t] = w / (t + 1.0) - 1.0
        rows = np.concatenate([np.asarray(norm_final, f), rb, fix.reshape(-1)])[None, :].astype(f)
        in_maps.append({
            "xall": np.ascontiguousarray(np.concatenate([pre, main], 0)),
            "w_in": np.asarray(w_in[0], f), "w_out": np.asarray(w_out[0], f), "glu_w": np.asarray(glu_w[0], f),
            "pool_w": np.asarray(pool_w[0], f), "cols": cols, "rows": rows, "sp_s": sp_s,
            "b1_s": b1, "b2_s": b2, "c1_s": c1, "c2_s": c2, "cst": cst, "wr": wr,
            "wg": np.asarray(exp_w_gate[0], f), "wu": np.asarray(exp_w_up[0], f), "wd": np.asarray(exp_w_down[0], f),
        })
    nc = build()
    res = run_bass_kernel_spmd(nc, in_maps, core_ids=list(range(8)))
    outs = [np.asarray(r["out"], f) for r in res.results]
    full = np.zeros((4, 8192, 1024), f)
    for c in range(8):
        b, half = c // 2, c % 2
        full[b, half * 4096:(half + 1) * 4096] = outs[c]
    return full
```

```python
import math
import numpy as np
from contextlib import ExitStack
import concourse.bass as bass
import concourse.mybir as mybir
from concourse.bass_utils import run_bass_kernel_spmd

F32 = mybir.dt.float32
BF16 = mybir.dt.bfloat16
I32 = mybir.dt.int32
AF = mybir.ActivationFunctionType
ALU = mybir.AluOpType
AX = mybir.AxisListType

NT_PRE = 32
NT_MAIN = 32
SBT = 4
EPS = 1e-6
WINS = (2, 4, 8, 16)
TWO_PI = 2.0 * math.pi


class R:
    def __init__(self, name):
        self.name = name
        self.w = None
        self.rd = {}


class Sched:
    ENG = ("pe", "act", "dve", "pool", "sp")

    def __init__(self, nc, es):
        self.nc = nc
        self.sem = {e: es.enter_context(nc.semaphore("s_" + e)) for e in self.ENG}
        self.cnt = {e: 0 for e in self.ENG}
        self.ops = {e: [] for e in self.ENG}
        self.seen = {e: {} for e in self.ENG}
        self.dma_pool = [es.enter_context(nc.semaphore("d%d" % i)) for i in range(48)]
        self.dma_cnt = {}
        self.dma_of = {}

    def dsem(self, res):
        if res.name not in self.dma_of:
            s = self.dma_pool[len(self.dma_of)]
            self.dma_of[res.name] = s
            self.dma_cnt[s.name] = 0
        return self.dma_of[res.name]

    def op(self, eng, fn, rd=(), wr=(), dma=None):
        need = {}

        def add(tok):
            if tok is None:
                return
            s, v = tok
            if need.get(s.name, (None, -1))[1] < v:
                need[s.name] = (s, v)
        for r in rd:
            add(r.w)
        for r in wr:
            add(r.w)
            for t in r.rd.values():
                add(t)
        waits = []
        for name, (s, v) in need.items():
            if eng == "pe" and s is self.sem["pe"]:
                continue
            if self.seen[eng].get(name, -1) >= v:
                continue
            self.seen[eng][name] = v
            waits.append((s, v))
        if dma is not None:
            s = self.dsem(dma)
            self.dma_cnt[s.name] += 16
            tok = (s, self.dma_cnt[s.name])
            inc = (s, 16)
        else:
            self.cnt[eng] += 1
            tok = (self.sem[eng], self.cnt[eng])
            inc = (self.sem[eng], 1)
        for r in rd:
            old = r.rd.get(tok[0].name)
            if old is None or old[1] < tok[1]:
                r.rd[tok[0].name] = tok
        for r in wr:
            r.w = tok
            r.rd = {}
        self.ops[eng].append((waits, fn, inc))
        return tok

    def final_wait(self, eng, ress):
        waits = []
        for r in ress:
            if r.w is not None:
                waits.append(r.w)
        self.ops[eng].append((waits, None, None))

    def barrier(self):
        toks = [(self.sem[e], self.cnt[e]) for e in self.ENG if self.cnt[e] > 0]
        for name, sm_ in self.dma_of.items():
            toks.append((sm_, self.dma_cnt[sm_.name]))
        for eng in self.ENG:
            waits = []
            for s_, v in toks:
                if s_ is self.sem[eng]:
                    continue
                if self.seen[eng].get(s_.name, -1) >= v:
                    continue
                self.seen[eng][s_.name] = v
                waits.append((s_, v))
            if waits:
                self.ops[eng].append((waits, None, None))

    def replay(self, eng, e):
        for waits, fn, inc in self.ops[eng]:
            for s, v in waits:
                e.wait_ge(s, v)
            if fn is not None:
                fn(e).then_inc(inc[0], inc[1])


def build(debug=False):
    nc = bass.Bass("TRN2", target_bir_lowering=False)

    def din(name, shape, dt=F32):
        return nc.dram_tensor(name, list(shape), dt, kind="ExternalInput").ap()
    xall = din("xall", [(NT_PRE + NT_MAIN) * 128, 1024])
    w_in = din("w_in", [1024, 1024])
    w_out = din("w_out", [1024, 1024])
    glu_w = din("glu_w", [512, 512])
    pool_w = din("pool_w", [4, 128, 128])
    cols = din("cols", [128, 64])
    rows = din("rows", [1, 1024 + 20 + 64])
    sp_s = din("sp_s", [128, 96])
    b1_s = din("b1_s", [128, 32, 16])
    b2_s = din("b2_s", [128, 32, 16])
    c1_s = din("c1_s", [128, 32, 16])
    c2_s = din("c2_s", [128, 32, 16])
    cst = din("cst", [128, 386])
    wr_d = din("wr", [1024, 20])
    wg_d = din("wg", [16, 1024, 256])
    wu_d = din("wu", [16, 1024, 256])
    wd_d = din("wd", [16, 256, 1024])
    out = nc.dram_tensor("out", [NT_MAIN * 128, 1024], F32, kind="ExternalOutput").ap()

    es = ExitStack()
    with es:
        S = Sched(nc, es)

        def sb(name, shape, dt=F32):
            return es.enter_context(nc.sbuf_tensor(name, list(shape), dt))

        def ps(name, shape, dt=F32):
            return es.enter_context(nc.psum_tensor(name, list(shape), dt))

        ident = sb("ident", [128, 128], BF16)
        cstt = sb("cstt", [128, 386])
        colt = sb("colt", [128, 64])
        RB = sb("RB", [128, 84])
        WI = sb("WI", [128, 8, 1024], BF16)
        WO = sb("WO", [128, 8, 1024], BF16)
        GW = sb("GW", [128, 4, 512], BF16)
        PW = sb("PW", [128, 8, 128], BF16)
        WRb = sb("WRb", [128, 8, 20], BF16)
        LBa = sb("LBa", [128, 32, 128], BF16)
        LBb = sb("LBb", [128, 32, 128], BF16)
        LC = sb("LC", [128, 32, 128], BF16)
        Dg = sb("Dg", [128, 4, 128], BF16)
        COS = sb("COS", [128, 32, 128])
        SINM = sb("SINM", [128, 32, 128])
        MAG = sb("MAG", [128, 32])
        CAR = sb("CAR", [128, 32])
        acc = sb("acc", [128, SBT, 1024])
        hn2T = sb("hn2T", [128, 8, SBT * 128], BF16)
        gates = sb("gates", [128, SBT, 16])
        cf = sb("cf", [128, 64])
        sm = sb("sm", [128, 256])
        pzb = sb("pzb", [128, 128], BF16)
        ARENA_W = 21504
        arena = sb("arena", [128, ARENA_W])
        _off = [0]

        def carve(shape, dt=F32):
            n = 1
            for d in shape[1:]:
                n *= d
            nb = n * (4 if dt == F32 else 2)
            nb = (nb + 63) // 64 * 64
            o = _off[0]
            _off[0] += nb
            assert _off[0] <= ARENA_W * 4, ("arena overflow", _off[0])
            v = arena[:, o // 4:(o + nb) // 4]
            if dt != F32:
                v = v.bitcast(dt)
            v = v[:, 0:n]
            if len(shape) == 3:
                v = v.rearrange("p (a b) -> p a b", a=shape[1])
            elif len(shape) == 4:
                v = v.rearrange("p (a b c) -> p a b c", a=shape[1], b=shape[2])
            return v
        xt = [carve([128, 1024]) for _ in range(2)]
        hn = [carve([128, 1024], BF16) for _ in range(2)]
        hnT = [carve([128, 8, 128], BF16) for _ in range(2)]
        yT_off = _off[0]
        yT = [carve([128, 8, 128], BF16) for _ in range(2)]
        yg = [carve([128, 4, 128], BF16) for _ in range(2)]
        ygf = [carve([128, 4, 128]) for _ in range(2)]
        g1 = [carve([128, 128]) for _ in range(2)]
        g2 = [carve([128, 128]) for _ in range(2)]
        Gt = [carve([128, 8, 128]) for _ in range(2)]
        hn2b = [carve([128, 1024], BF16) for _ in range(2)]
        D1 = None
        M1_off = _off[0]
        M1 = [carve([128, 8, 128]) for _ in range(2)]
        M2 = [carve([128, 8, 128]) for _ in range(2)]
        XT = [carve([128, 8, 128]) for _ in range(2)]
        XTb = [carve([128, 8, 128]) for _ in range(2)]
        XA = [carve([128, 8, 128], BF16) for _ in range(2)]
        junk = carve([128, 1024], BF16)
        assert _off[0] >= 48 * 1024
        zT = [carve([128, 8, 144], BF16) for _ in range(2)]
        mixer_end = _off[0]
        _off[0] = 0
        WGs = [carve([128, 8, 256], BF16) for _ in range(2)]
        WUs = [carve([128, 8, 256], BF16) for _ in range(2)]
        WDs = [carve([128, 2, 1024], BF16) for _ in range(2)]
        sgT = [carve([128, 2, 512], BF16) for _ in range(2)]
        hT = [carve([128, 2, 512], BF16) for _ in range(2)]
        outt = [carve([128, 1024]) for _ in range(2)]
        NF = carve([128, 1024])
        junk2 = carve([128, 1024], BF16)
        assert _off[0] <= 48 * 1024
        pp = Gt[0].rearrange("p a b -> p (a b)")[:, 0:512].rearrange("p (a b) -> p a b", a=32)
        pq = XT[0].rearrange("p a b -> p (a b)")[:, 0:512].rearrange("p (a b) -> p a b", a=32)
        pz = acc[:].rearrange("p a b -> p (a b)").rearrange("p (a b) -> p a b", a=32)
        T1 = M1[0]
        T2 = M2[0]

        PB0 = ps("PB0", [128, 1024], BF16)
        PBs = [ps("PB%d" % i, [128, 512]) for i in range(1, 8)]
        PB0F = PB0[:].bitcast(F32)

        res = {}

        def rs(name):
            if name not in res:
                res[name] = R(name)
            return res[name]

        def dma(out_ap, in_ap, rd, wr, slow=False, q="sp"):
            if slow:
                S.op(q, lambda e: e.dma_start(out=out_ap, in_=in_ap, allow_slow_non_contiguous=True), rd=rd, wr=wr, dma=wr[0])
            else:
                S.op(q, lambda e: e.dma_start(out=out_ap, in_=in_ap), rd=rd, wr=wr, dma=wr[0])

        def act(out_ap, in_ap, func, rd, wr, **kw):
            S.op("act", lambda e: e.activation(out=out_ap, in_=in_ap, func=func, **kw), rd=rd, wr=wr)

        def tt(eng, out_ap, a, b, op, rd, wr):
            S.op(eng, lambda e: e.tensor_tensor(out=out_ap, in0=a, in1=b, op=op), rd=rd, wr=wr)

        def tsc(out_ap, a, s1, s2, op0, op1, rd, wr):
            if s2 is None:
                S.op("dve", lambda e: e.tensor_scalar(out=out_ap, in0=a, scalar1=s1, scalar2=None, op0=op0), rd=rd, wr=wr)
            else:
                S.op("dve", lambda e: e.tensor_scalar(out=out_ap, in0=a, scalar1=s1, scalar2=s2, op0=op0, op1=op1), rd=rd, wr=wr)

        def stt(out_ap, a, s, b, op0, op1, rd, wr):
            S.op("dve", lambda e: e.scalar_tensor_tensor(out=out_ap, in0=a, scalar=s, in1=b, op0=op0, op1=op1), rd=rd, wr=wr)

        def cp(eng, out_ap, in_ap, rd, wr):
            S.op(eng, lambda e: e.tensor_copy(out=out_ap, in_=in_ap), rd=rd, wr=wr)

        def mm(out_ap, lhsT, rhs, start, stop, rd, wr):
            S.op("pe", lambda e: e.matmul(out_ap, lhsT=lhsT, rhs=rhs, start=start, stop=stop), rd=rd, wr=wr)

        def tr(out_ap, in_ap, rd, wr):
            S.op("pe", lambda e: e.transpose(out=out_ap, in_=in_ap, identity=ident[:]), rd=rd, wr=wr)

        def red(out_ap, in_ap, op, rd, wr):
            S.op("dve", lambda e: e.tensor_reduce(out=out_ap, in_=in_ap, axis=AX.X, op=op), rd=rd, wr=wr)

        def recip(out_ap, in_ap, rd, wr):
            S.op("dve", lambda e: e.reciprocal(out=out_ap, in_=in_ap), rd=rd, wr=wr)

        def smc(i, n=1):
            return sm[:, i:i + n]

        r_ = rs
        dma(cstt[:], cst[:, :], [], [r_("cstt")])
        dma(colt[:], cols[:, :], [], [r_("colt")])
        dma(RB[:], rows[0:1, 1024:1108].partition_broadcast(128), [], [r_("RB")])
        cp("dve", ident[:], cstt[:, 0:128], [r_("cstt")], [r_("ident")])
        JIDX = cstt[:, 128:256]
        SGN = cstt[:, 256:257]
        c_nm, c_nf, c_ps_, c_d, c_gb = 0, 8, 16, 20, 24

        accv = acc[:].rearrange("p a b -> p (a b)")
        for half in range(2):
            dma(accv[:, 0:4096].rearrange("p (k n) -> p k n", k=4),
                w_in[half * 512:(half + 1) * 512, :].rearrange("(k p) n -> p k n", p=128), [], [r_("acc")])
            for k in range(4):
                kk = half * 4 + k
                tsc(WI[:, kk, :], accv[:, k * 1024:(k + 1) * 1024], colt[:, c_nm + kk:c_nm + kk + 1], None, ALU.mult, None,
                    [r_("acc"), r_("colt")], [r_("WI")])
        for half in range(2):
            dma(accv[:, 0:4096].rearrange("p (k n) -> p k n", k=4),
                w_out[half * 512:(half + 1) * 512, :].rearrange("(k p) n -> p k n", p=128), [r_("WI")], [r_("acc")])
            for k in range(4):
                kk = half * 4 + k
                tsc(WO[:, kk, :], accv[:, k * 1024:(k + 1) * 1024], 1.0 if kk < 4 else 0.25, None, ALU.mult, None, [r_("acc")], [r_("WO")])
        dma(accv[:, 0:2048].rearrange("p (k n) -> p k n", k=4), glu_w[:, :].rearrange("(k p) n -> p k n", p=128), [r_("WO")], [r_("acc")])
        tsc(GW[:].rearrange("p k n -> p (k n)"), accv[:, 0:2048], 0.5, None, ALU.mult, None, [r_("acc")], [r_("GW")])
        dma(accv[:, 0:512].rearrange("p (g n) -> p g n", g=4), pool_w[:, :, :].rearrange("g p n -> p g n"), [r_("GW")], [r_("acc")])
        for gi, w in enumerate(WINS):
            tsc(PW[:, 2 * gi, :], accv[:, gi * 128:(gi + 1) * 128], float(1.0 / w - 1.0), None, ALU.mult, None, [r_("acc")], [r_("PW")])
            tsc(PW[:, 2 * gi + 1, :], accv[:, gi * 128:(gi + 1) * 128], float(1.0 / w), None, ALU.mult, None, [r_("acc")], [r_("PW")])
        dma(accv[:, 0:160].rearrange("p (k n) -> p k n", k=8), wr_d[:, :].rearrange("(k p) n -> p k n", p=128), [r_("PW")], [r_("acc")])
        for k in range(8):
            tsc(WRb[:, k, :], accv[:, k * 20:(k + 1) * 20], colt[:, c_nf + k:c_nf + k + 1], None, ALU.mult, None, [r_("acc"), r_("colt")], [r_("WRb")])

        spt = XTb[0][:, 0, 0:96]
        dma(spt, sp_s[:, :], [], [r_("spt")])
        dma(pp, b1_s[:, :, :], [], [r_("Gt")])
        dma(pq, b2_s[:, :, :], [], [r_("XT")])
        P = XTb[1].rearrange("p a b -> p (a b)")[:, 0:512].rearrange("p (a b) -> p a b", a=16)
        Rp = r_("P")
        LR, LI, LS = spt[:, 0:32], spt[:, 32:64], spt[:, 64:96]
        STEP, ARG, MG, CS, SN, AR, AI, DEN, QR, QI, TA, TB, KI = [P[:, i, :] for i in range(13)]
        KII = sb("KII", [128, 32], I32)

        def exp_to(dst, src, rd):
            act(TA, src, AF.Tanh, rd, [Rp], scale=0.5)
            tsc(TB, TA, -1.0, 1.0, ALU.mult, ALU.add, [Rp], [Rp])
            recip(TB, TB, [Rp], [Rp])
            tsc(TA, TA, 1.0, None, ALU.add, None, [Rp], [Rp])
            tt("dve", dst, TA, TB, ALU.mult, [Rp], [Rp])

        def sin_to(dst, src, shift):
            tsc(TA, src, float(shift), 1.0 / TWO_PI, ALU.add, ALU.mult, [Rp], [Rp])
            cp("dve", KII[:], TA, [Rp], [Rp])
            cp("dve", TB, KII[:], [Rp], [Rp])
            tt("dve", TA, TA, TB, ALU.subtract, [Rp], [Rp])
            act(dst, TA, AF.Sin, [Rp], [Rp], scale=TWO_PI)
        exp_to(STEP, LS, [r_("spt")])
        tt("dve", ARG, LI, STEP, ALU.mult, [Rp, r_("spt")], [Rp])
        tt("dve", MG, LR, STEP, ALU.mult, [Rp, r_("spt")], [Rp])
        cp("dve", P[:, 13, :], MG, [Rp], [Rp])
        exp_to(MG, MG, [Rp])
        cp("dve", MAG[:], MG, [Rp], [r_("MAG")])
        sin_to(SN, ARG, 0.0)
        sin_to(CS, ARG, math.pi / 2)
        tt("dve", AR, MG, CS, ALU.mult, [Rp], [Rp])
        tt("dve", AI, MG, SN, ALU.mult, [Rp], [Rp])
        tt("dve", DEN, LR, LR, ALU.mult, [Rp], [Rp])
        tt("dve", TA, LI, LI, ALU.mult, [Rp], [Rp])
        tt("dve", DEN, DEN, TA, ALU.add, [Rp], [Rp])
        recip(DEN, DEN, [Rp], [Rp])
        tsc(TA, AR, -1.0, None, ALU.add, None, [Rp], [Rp])
        tt("dve", QR, TA, LR, ALU.mult, [Rp], [Rp])
        tt("dve", TB, AI, LI, ALU.mult, [Rp], [Rp])
        tt("dve", QR, QR, TB, ALU.add, [Rp], [Rp])
        tt("dve", QR, QR, DEN, ALU.mult, [Rp], [Rp])
        tt("dve", QI, AI, LR, ALU.mult, [Rp], [Rp])
        tt("dve", TB, TA, LI, ALU.mult, [Rp], [Rp])
        tt("dve", QI, QI, TB, ALU.subtract, [Rp], [Rp])
        tt("dve", QI, QI, DEN, ALU.mult, [Rp], [Rp])
        tsc(QI, QI, SGN, -1.0, ALU.mult, ALU.mult, [Rp, r_("cstt")], [Rp])
        tt("dve", pp, pp, QR.unsqueeze(2).to_broadcast([128, 32, 16]), ALU.mult, [Rp, r_("Gt")], [r_("Gt")])
        tt("dve", pq, pq, QI.unsqueeze(2).to_broadcast([128, 32, 16]), ALU.mult, [Rp, r_("XT")], [r_("XT")])
        tt("dve", pp, pp, pq, ALU.add, [r_("XT")], [r_("Gt")])
        S.op("pool", lambda e: e.memset(pz, 0.0), wr=[r_("acc")])
        for gl in range(8):
            cp("dve", pz[:, gl::8, gl * 16:(gl + 1) * 16], pp[:, gl::8, :], [r_("Gt")], [r_("acc")])
        for g in range(32):
            cp("dve", pzb[:], pz[:, g, :], [r_("acc")], [r_("pzb")])
            tr(PB0[:, 0:128], pzb[:], [r_("pzb"), r_("ident")], [r_("PB0")])
            cp("dve", LBa[:, g, :], PB0[:, 0:128], [r_("PB0")], [r_("LBa")])
        cp("dve", LBb[:, :, 0:64], LBa[:, :, 64:128], [r_("LBa")], [r_("LBb")])
        cp("dve", LBb[:, :, 64:128], LBa[:, :, 0:64], [r_("LBa")], [r_("LBb")])
        dma(pq, c1_s[:, :, :], [r_("Gt")], [r_("XT")])
        tsc(pq, pq, SGN, None, ALU.mult, None, [r_("XT"), r_("cstt")], [r_("XT")])
        S.op("pool", lambda e: e.memset(pz, 0.0), rd=[], wr=[r_("acc")])
        for gl in range(8):
            cp("dve", pz[:, gl::8, gl * 16:(gl + 1) * 16], pq[:, gl::8, :], [r_("XT")], [r_("acc")])
        cp("dve", LC[:].rearrange("p a b -> p (a b)"), pz.rearrange("p a b -> p (a b)"), [r_("acc")], [r_("LC")])
        for t4 in range(4):
            tsc(Dg[:, t4, :], ident[:], colt[:, c_d + t4:c_d + t4 + 1], None, ALU.mult, None, [r_("ident"), r_("colt")], [r_("Dg")])
        SCR = [T1, T2]
        for g in range(32):
            sc = SCR[g % 2]
            rsc = r_("scr%d" % (g % 2))
            tsc(sc[:, 0, :], JIDX, P[:, 1, g:g + 1], 1.0 / TWO_PI, ALU.mult, ALU.mult, [Rp, r_("cstt")], [rsc])
            for ti_, (tab, shift) in enumerate(((SINM, 0.0), (COS, 0.25))):
                rs2 = r_("scr%d_%d" % (g % 2, ti_))
                a, b_, c = 1 + 3 * ti_, 2 + 3 * ti_, 3 + 3 * ti_
                tsc(sc[:, a, :], sc[:, 0, :], float(shift), None, ALU.add, None, [rsc], [rs2])
                cp("dve", sc[:, b_, :].bitcast(I32), sc[:, a, :], [rs2], [rs2])
                cp("dve", sc[:, c, :], sc[:, b_, :].bitcast(I32), [rs2], [rs2])
                tt("dve", sc[:, a, :], sc[:, a, :], sc[:, c, :], ALU.subtract, [rs2], [rs2])
                act(tab[:, g, :], sc[:, a, :], AF.Sin, [rs2], [r_("tab")], scale=TWO_PI)
        tsc(SINM[:].rearrange("p a b -> p (a b)"), SINM[:].rearrange("p a b -> p (a b)"), SGN, None, ALU.mult, None, [r_("tab"), r_("cstt")], [r_("tab")])
        _save = _off[0]
        _off[0] = yT_off
        BBR = carve([128, 32, 16])
        BBIs = carve([128, 32, 16])
        VTa = carve([128, 32, 128], BF16)
        VTb = carve([128, 32, 128], BF16)
        assert _off[0] <= M1_off
        _off[0] = _save
        ztok = [M1[1].rearrange("p a b -> p (a b)").bitcast(BF16)[:, 0:1024][:, i * 512:(i + 1) * 512] for i in range(2)]
        Ytmp = [M2[1].rearrange("p a b -> p (a b)")[:, i * 512:(i + 1) * 512] for i in range(2)]
        CARb = XA[1].rearrange("p a b -> p (a b)").bitcast(F32)[:, 0:32]
        Ssum = XA[1].rearrange("p a b -> p (a b)").bitcast(F32)[:, 32:64]
        Ctmp = XA[1].rearrange("p a b -> p (a b)").bitcast(F32)[:, 64:128]
        A128 = XA[1].rearrange("p a b -> p (a b)").bitcast(F32)[:, 128:192]
        bA = XT[1].rearrange("p a b -> p (a b)")[:, 0:512].rearrange("p (a b) -> p a b", a=32)
        bB = XT[1].rearrange("p a b -> p (a b)")[:, 512:1024].rearrange("p (a b) -> p a b", a=32)
        dma(bA, b2_s[:, :, :], [], [r_("bA")])
        dma(bB, b1_s[:, :, :], [], [r_("bB")])
        tt("dve", bA, bA, QR.unsqueeze(2).to_broadcast([128, 32, 16]), ALU.mult, [Rp, r_("bA")], [r_("bA")])
        tt("dve", bB, bB, QI.unsqueeze(2).to_broadcast([128, 32, 16]), ALU.mult, [Rp, r_("bB")], [r_("bB")])
        tt("dve", bA, bA, bB, ALU.subtract, [r_("bB")], [r_("bA")])
        cp("dve", BBR[0:64], pp[0:64], [r_("Gt")], [r_("BBR")])
        cp("dve", BBR[64:128], bA[64:128], [r_("bA")], [r_("BBR")])
        tsc(BBIs[0:64].rearrange("p a b -> p (a b)"), bA[0:64].rearrange("p a b -> p (a b)"), -1.0, None, ALU.mult, None, [r_("bA")], [r_("BBI")])
        cp("dve", BBIs[64:128], pp[64:128], [r_("Gt")], [r_("BBI")])
        S.barrier()
        SHC = sm[:, 70:71]
        tsc(SHC, SGN, 0.125, 0.125, ALU.mult, ALU.add, [r_("cstt")], [r_("shc")])
        JREV = cstt[:, 258:386]
        pzbs = [XA[0][:, 0, :], XA[0][:, 1, :]]
        for g in range(32):
            sc = SCR[g % 2]
            rsc = r_("vscr%d" % (g % 2))
            rpz = r_("pzbs%d" % (g % 2))
            tsc(sc[:, 0, :], JREV, P[:, 1, g:g + 1], 1.0 / TWO_PI, ALU.mult, ALU.mult, [Rp, r_("cstt"), r_("tab")], [rsc])
            tsc(sc[:, 1, :], sc[:, 0, :], SHC, None, ALU.add, None, [rsc, r_("shc")], [rsc])
            cp("dve", sc[:, 2, :].bitcast(I32), sc[:, 1, :], [rsc], [rsc])
            cp("dve", sc[:, 3, :], sc[:, 2, :].bitcast(I32), [rsc], [rsc])
            tt("dve", sc[:, 1, :], sc[:, 1, :], sc[:, 3, :], ALU.subtract, [rsc], [rsc])
            act(sc[:, 4, :], sc[:, 1, :], AF.Sin, [rsc], [r_("vsb%d" % (g % 2))], scale=TWO_PI)
            act(sc[:, 5, :], JREV, AF.Exp, [r_("cstt"), Rp, rsc], [r_("vsc%d" % (g % 2))], scale=P[:, 13, g:g + 1])
            tt("dve", pzbs[g % 2], sc[:, 4, :], sc[:, 5, :], ALU.mult, [r_("vsb%d" % (g % 2)), r_("vsc%d" % (g % 2))], [rpz])
            tr(PB0[:, (g % 2) * 128:(g % 2 + 1) * 128], pzbs[g % 2], [rpz, r_("ident")], [r_("PB0")])
            cp("dve", VTa[:, g, :], PB0[:, (g % 2) * 128:(g % 2 + 1) * 128], [r_("PB0")], [r_("VTa")])
        cp("dve", VTb[:, :, 0:64], VTa[:, :, 64:128], [r_("VTa")], [r_("VTb")])
        cp("dve", VTb[:, :, 64:128], VTa[:, :, 0:64], [r_("VTa")], [r_("VTb")])
        act(Ctmp[:, 0:32], P[:, 13, :], AF.Exp, [Rp], [r_("ctmp")], scale=128.0)
        tt("dve", A128[:, 0:32], Ctmp[:, 0:32], COS[:, :, 127], ALU.mult, [r_("ctmp"), r_("tab")], [r_("A128")])
        tt("dve", A128[:, 32:64], Ctmp[:, 0:32], SINM[:, :, 127], ALU.mult, [r_("ctmp"), r_("tab")], [r_("A128")])
        tsc(A128[:, 32:64], A128[:, 32:64], -1.0, None, ALU.mult, None, [r_("A128")], [r_("A128")])
        S.op("pool", lambda e: e.memset(CARb, 0.0), wr=[r_("CARb")])
        S.barrier()
        LCs = XTb[0].rearrange("p a b -> p (a b)").bitcast(BF16)[:, 0:2048]
        LCs2 = XTb[1].rearrange("p a b -> p (a b)").bitcast(BF16)[:, 0:2048]
        dma(pq, c2_s[:, :, :], [], [r_("XT")])
        tsc(pq, pq, SGN, -1.0, ALU.mult, ALU.mult, [r_("XT"), r_("cstt")], [r_("XT")])
        S.op("pool", lambda e: e.memset(pz, 0.0), rd=[], wr=[r_("acc")])
        for gl in range(8):
            cp("dve", pz[:, gl::8, gl * 16:(gl + 1) * 16], pq[:, gl::8, :], [r_("XT")], [r_("acc")])
        pzf = pz.rearrange("p a b -> p (a b)")
        cp("dve", LCs, pzf[:, 0:2048], [r_("acc")], [r_("LCs")])
        cp("dve", LCs2, pzf[:, 2048:4096], [r_("acc")], [r_("LCs")])
        LCsv = [LCs.rearrange("p (g c) -> p g c", g=16), LCs2.rearrange("p (g c) -> p g c", g=16)]
        S.op("pool", lambda e: e.memset(CAR[:], 0.0), wr=[r_("CAR")])
        for p_ in range(2):
            S.op("pool", lambda e, p_=p_: e.memset(zT[p_], 0.0), wr=[r_("zT%d" % p_)])
        tsc(sm[:, 64:68], colt[:, c_gb:c_gb + 4], 0.5, None, ALU.mult, None, [r_("colt")], [r_("sm2")])

        def rms(src_ap, dst_bf, srcres, dstres, col, jk):
            S.op("dve", lambda e: e.memset(smc(col), 0.0), wr=[r_("sm%d" % col)])
            act(jk, src_ap, AF.Square, srcres, [r_("junk"), r_("sm%d" % col)], accum_out=smc(col))
            act(smc(col + 1), smc(col), AF.Sqrt, [r_("sm%d" % col)], [r_("sm%d" % col)], scale=1.0 / 1024.0, bias=EPS)
            recip(smc(col + 2), smc(col + 1), [r_("sm%d" % col)], [r_("sm%d" % col)])
            act(dst_bf, src_ap, AF.Copy, srcres + [r_("sm%d" % col)], dstres, scale=smc(col + 2))

        def rms_a(src_ap, srcres, col, jk, jkres):
            S.op("dve", lambda e: e.memset(smc(col), 0.0), wr=[r_("sm%d" % col)])
            act(jk, src_ap, AF.Square, srcres, [jkres, r_("sm%d" % col)], accum_out=smc(col))
            act(smc(col + 1), smc(col), AF.Sqrt, [r_("sm%d" % col)], [r_("sm%d" % col)], scale=1.0 / 1024.0, bias=EPS)

        def rms_b(src_ap, dst_bf, srcres, dstres, col):
            recip(smc(col + 2), smc(col + 1), [r_("sm%d" % col)], [r_("sm%d" % col)])
            act(dst_bf, src_ap, AF.Copy, srcres + [r_("sm%d" % col)], dstres, scale=smc(col + 2))

        def transp8(src_bf, srcres):
            for k in range(8):
                tr(PB0[:, k * 128:(k + 1) * 128], src_bf[:, k * 128:(k + 1) * 128], srcres + [r_("ident")], [r_("PB0")])

        tcnt = [0]

        def ssm_state(tau, full, p):
            g0 = tau * 8
            q = tau % 2
            ba, bb = PBs[2 + q * 2], PBs[3 + q * 2]
            rba, rbb = r_("bank%d" % (2 + q * 2)), r_("bank%d" % (3 + q * 2))
            bav = ba[:].rearrange("p (g t) -> p g t", g=4)
            bbv = bb[:].rearrange("p (g t) -> p g t", g=4)
            rz = r_("zT%d" % p)
            rG = r_("Gt%d" % q)
            for hh in range(2):
                for gl4 in range(4):
                    g = g0 + hh * 4 + gl4
                    mm(bav[:, gl4, :], LBa[:, g, :], zT[p][:, 4 + tau, 16:144], True, True, [r_("LBa"), rz], [rba])
                    mm(bbv[:, gl4, :], LBb[:, g, :], zT[p][:, 4 + tau, 16:144], True, True, [r_("LBb"), rz], [rbb])
                sl = slice(hh * 4, hh * 4 + 4)
                gs = slice(g0 + hh * 4, g0 + hh * 4 + 4)
                rD = r_("D1%d" % hh)
                tt("dve", Gt[q][:, sl, :], bav, COS[:, gs, :], ALU.mult, [rba, r_("tab")], [rG])
                tt("dve", D1[hh][:], bbv, SINM[:, gs, :], ALU.mult, [rbb, r_("tab")], [rD])
                tt("pool", Gt[q][:, sl, :], Gt[q][:, sl, :], D1[hh][:], ALU.add, [rD], [rG])
                yield
            rX = r_("XT%d" % q)
            for gl in range(8):
                g = g0 + gl
                S.op("dve", lambda e, gl=gl, g=g, q=q: e.tensor_tensor_scan(
                    out=XT[q][:, gl, :], data0=MAG[:, g:g + 1].to_broadcast([128, 128]), data1=Gt[q][:, gl, :],
                    initial=CAR[:, g:g + 1], op0=ALU.mult, op1=ALU.add),
                    rd=[rG, r_("MAG"), r_("CAR")], wr=[rX])
            yield
            gs = slice(g0, g0 + 8)
            rXb, rXb2 = r_("XTb%d" % q), r_("XTc%d" % q)
            rM1, rM2 = r_("M1%d" % q), r_("M2%d" % q)
            if full:
                dma(XTb[q][0:64, :, :], XT[q][64:128, :, :], [rX], [rXb])
                dma(XTb[q][64:128, :, :], XT[q][0:64, :, :], [rX], [rXb2])
                tt("dve", M1[q], XT[q], COS[:, gs, :], ALU.mult, [rX, r_("tab")], [rM1])
                tt("pool", M2[q], XTb[q], SINM[:, gs, :], ALU.mult, [rXb, rXb2, r_("tab")], [rM2])
                tt("pool", XA[q], M1[q], M2[q], ALU.subtract, [rM1, rM2], [r_("XA%d" % q)])
                tt("pool", CAR[:, gs], M1[q][:, :, 127], M2[q][:, :, 127], ALU.subtract, [rM1, rM2], [r_("CAR")])
            else:
                dma(XTb[q][0:64, :, 127:128], XT[q][64:128, :, 127:128], [rX], [rXb], slow=True)
                dma(XTb[q][64:128, :, 127:128], XT[q][0:64, :, 127:128], [rX], [rXb2], slow=True)
                tt("pool", M1[q][:, :, 127], XT[q][:, :, 127], COS[:, gs, 127], ALU.mult, [rX, r_("tab")], [rM1])
                tt("pool", M2[q][:, :, 127], XTb[q][:, :, 127], SINM[:, gs, 127], ALU.mult, [rXb, rXb2, r_("tab")], [rM2])
                tt("pool", CAR[:, gs], M1[q][:, :, 127], M2[q][:, :, 127], ALU.subtract, [rM1, rM2], [r_("CAR")])
            yield

        def front(tile_idx, mlist, p):
            rz, rzo = r_("zT%d" % p), r_("zT%d" % (1 - p))
            rx, rh, rhT = r_("xt%d" % p), r_("hn%d" % p), r_("hnT%d" % p)
            dma(xt[p], xall[tile_idx * 128:(tile_idx + 1) * 128, :], [], [rx])
            rms(xt[p], hn[p], [rx], [rh], 0, junk)
            transp8(hn[p], [rh])
            act(hnT[p].rearrange("p k t -> p (k t)"), PB0[:], AF.Copy, [r_("PB0")], [rhT])
            yield
            cp("pool", zT[p][:, 0:4, 0:16], zT[1 - p][:, 0:4, 128:144], [rzo], [rz])
            for m in mlist:
                pb = PBs[0] if m < 4 else PBs[1]
                rpb = r_("bank%d" % (m // 4))
                o = pb[:, (m % 4) * 128:(m % 4 + 1) * 128]
                for k in range(8):
                    mm(o, WI[:, k, m * 128:(m + 1) * 128], hnT[p][:, k, :], k == 0, k == 7, [r_("WI"), rhT], [rpb])
            if 0 in mlist:
                act(zT[p][:, 0:4, 16:144], PBs[0][:].rearrange("p (m t) -> p m t", m=4), AF.Copy, [r_("bank0")], [rz])
            if 4 in mlist:
                act(zT[p][:, 4:8, 16:144], PBs[1][:].rearrange("p (m t) -> p m t", m=4), AF.Copy, [r_("bank1")], [rz])
            yield "F"

        PQ = PBs[6]

        def mixer(tile_idx, lt, first, p):
            rz = r_("zT%d" % p)
            ryT, ryg, rygf = r_("yT%d" % p), r_("yg%d" % p), r_("ygf%d" % p)
            yield from front(tile_idx, list(range(8)), p)
            for gi, w in enumerate(WINS):
                o = PQ[:, gi * 128:(gi + 1) * 128]
                for l in range(w):
                    mm(o, PW[:, 2 * gi + (1 if l > 0 else 0), :], zT[p][:, gi, 16 - l:144 - l], l == 0, (l == w - 1) and not first, [r_("PW"), rz], [r_("bank6")])
                if first:
                    S.op("dve", lambda e, gi=gi: e.tensor_tensor_scan(
                        out=cf[:, 0:16], data0=cstt[:, 257:258].to_broadcast([128, 16]), data1=zT[p][:, gi, 16:32],
                        initial=0.0, op0=ALU.mult, op1=ALU.add), rd=[rz, r_("cstt")], wr=[r_("cf")])
                    tt("dve", cf[:, 16:32], cf[:, 0:16], RB[:, 20 + gi * 16:36 + gi * 16], ALU.mult, [r_("cf"), r_("RB")], [r_("cf2")])
                    cp("dve", pzb[:, 0:16], cf[:, 16:32], [r_("cf2")], [r_("pzb")])
                    mm(o[:, 0:16], PW[:, 2 * gi + 1, :], pzb[:, 0:16], False, True, [r_("PW"), r_("pzb")], [r_("bank6")])
            for gi in range(4):
                act(yT[p][:, gi, :], PQ[:, gi * 128:(gi + 1) * 128], AF.Copy, [r_("bank6")], [ryT], scale=colt[:, c_ps_ + gi:c_ps_ + gi + 1])
            yield
            for tau in range(4):
                q = tau % 2
                yield from ssm_state(tau, True, p)
                o = PBs[0][:, tau * 128:(tau + 1) * 128]
                ry = r_("bank0")
                for gl in range(8):
                    mm(o, LC[:, tau * 8 + gl, :], XA[q][:, gl, :], gl == 0, False, [r_("LC"), r_("XA%d" % q)], [ry])
                mm(o, Dg[:, tau, :], zT[p][:, 4 + tau, 16:144], False, True, [r_("Dg"), rz], [ry])
                rg1, rg2 = r_("g1%d" % q), r_("g2%d" % q)
                act(g1[q], o, AF.Square, [ry], [rg1])
                tsc(g1[q], g1[q], 0.044715, 1.0, ALU.mult, ALU.add, [rg1], [rg1])
                tt("dve", g1[q], g1[q], o, ALU.mult, [rg1, ry], [rg1])
                act(g2[q], g1[q], AF.Tanh, [rg1], [rg2], scale=0.7978845608028654)
                tsc(g2[q], g2[q], 0.5, 0.5, ALU.mult, ALU.add, [rg2], [rg2])
                tt("dve", ygf[p][:, tau, :], g2[q], o, ALU.mult, [rg2, ry], [rygf])
                cp("pool", yg[p][:, tau, :], ygf[p][:, tau, :], [rygf], [ryg])
                yield
            for m in range(4):
                q = m % 2
                rg1 = r_("g1%d" % q)
                o = PBs[1][:, m * 128:(m + 1) * 128]
                for k in range(4):
                    mm(o, GW[:, k, m * 128:(m + 1) * 128], yg[p][:, k, :], k == 0, k == 3, [r_("GW"), ryg], [r_("bank1")])
                act(g1[q], o, AF.Tanh, [r_("bank1"), r_("sm2")], [rg1], scale=0.5, bias=sm[:, 64 + m:65 + m])
                tsc(g1[q], g1[q], 0.5, 0.5, ALU.mult, ALU.add, [rg1], [rg1])
                tt("pool", yT[p][:, 4 + m, :], g1[q], ygf[p][:, m, :], ALU.mult, [rg1, rygf], [ryT])
            yield
            for half in range(2):
                pb = PBs[half]
                rpb = r_("bank%d" % half)
                for k in range(8):
                    mm(pb[:], yT[p][:, k, :], WO[:, k, half * 512:(half + 1) * 512], k == 0, k == 7, [ryT, r_("WO")], [rpb])
                tt("dve", acc[:, lt, half * 512:(half + 1) * 512], pb[:], xt[p][:, half * 512:(half + 1) * 512], ALU.add, [rpb, r_("xt%d" % p)], [r_("acc%d" % lt)])
            yield
            rh = r_("hn%d" % p)
            rms(acc[:, lt, :], hn[p], [r_("acc%d" % lt)], [rh], 4, junk)
            transp8(hn[p], [rh])
            act(hn2T[:, :, lt * 128:(lt + 1) * 128], PB0[:].rearrange("p (k t) -> p k t", k=8), AF.Copy, [r_("PB0")], [r_("hn2T")])
            yield
            o = PQ[:, 0:20]
            for k in range(8):
                mm(o, hn2T[:, k, lt * 128:(lt + 1) * 128], WRb[:, k, :], k == 0, k == 7, [r_("hn2T"), r_("WRb")], [r_("bank6")])
            L_ = sm[:, 100:120]
            rS = r_("smr")
            tt("dve", L_, o, RB[:, 0:20], ALU.add, [r_("bank6"), r_("RB")], [rS])
            cL, fL = sm[:, 100:104], sm[:, 104:120]
            M, GM, CM, TH, NUM, SS, PG = smc(120), smc(121, 4), smc(125, 4), smc(129, 4), smc(133, 4), smc(137), smc(138)
            red(M, cL, ALU.max, [rS], [rS])
            tsc(GM, cL, M, None, ALU.is_equal, None, [rS], [rS])
            tsc(CM, cL, M, None, ALU.subtract, None, [rS], [rS])
            act(TH, CM, AF.Tanh, [rS], [rS], scale=0.5)
            tsc(NUM, TH, 1.0, None, ALU.add, None, [rS], [rS])
            tsc(TH, TH, -1.0, 1.0, ALU.mult, ALU.add, [rS], [rS])
            recip(TH, TH, [rS], [rS])
            tt("dve", NUM, NUM, TH, ALU.mult, [rS], [rS])
            red(SS, NUM, ALU.add, [rS], [rS])
            recip(PG, SS, [rS], [rS])
            FT = sm[:, 140:156]
            tt("dve", FT.rearrange("p (g j) -> p g j", g=4), fL.rearrange("p (g j) -> p g j", g=4),
               GM.unsqueeze(2).to_broadcast([128, 4, 4]), ALU.mult, [rS], [rS])
            FS = smc(156, 4)
            red(FS, FT.rearrange("p (g j) -> p j g", g=4), ALU.add, [rS], [rS])
            M1_, K1, F2, M2_, K2, DD, W1, W2, WJ = smc(160), smc(161, 4), smc(165, 4), smc(169), smc(170, 4), smc(174), smc(175), smc(176), smc(177, 4)
            red(M1_, FS, ALU.max, [rS], [rS])
            tsc(K1, FS, M1_, None, ALU.is_equal, None, [rS], [rS])
            stt(F2, K1, -1e30, FS, ALU.mult, ALU.add, [rS], [rS])
            red(M2_, F2, ALU.max, [rS], [rS])
            tsc(K2, F2, M2_, None, ALU.is_equal, None, [rS], [rS])
            tt("dve", DD, M2_, M1_, ALU.subtract, [rS], [rS])
            act(DD, DD, AF.Tanh, [rS], [rS], scale=0.5)
            tsc(W1, DD, -0.5, 0.5, ALU.mult, ALU.add, [rS], [rS])
            tsc(W2, DD, 0.5, 0.5, ALU.mult, ALU.add, [rS], [rS])
            tsc(WJ, K1, W1, None, ALU.mult, None, [rS], [rS])
            stt(WJ, K2, W2, WJ, ALU.mult, ALU.add, [rS], [rS])
            tsc(WJ, WJ, PG, None, ALU.mult, None, [rS], [rS])
            tt("dve", gates[:, lt, :].rearrange("p (g j) -> p g j", g=4), GM.unsqueeze(2).to_broadcast([128, 4, 4]),
               WJ.unsqueeze(1).to_broadcast([128, 4, 4]), ALU.mult, [rS], [r_("gates")])
            yield

        def router(lt, o, rq0):
            L_ = sm[:, 100:120]
            rS = r_("smr")
            tt("dve", L_, o, RB[:, 0:20], ALU.add, [rq0, r_("RB")], [rS])
            cL, fL = sm[:, 100:104], sm[:, 104:120]
            M, GM, CM, TH, NUM, SS, PG = smc(120), smc(121, 4), smc(125, 4), smc(129, 4), smc(133, 4), smc(137), smc(138)
            red(M, cL, ALU.max, [rS], [rS])
            tsc(GM, cL, M, None, ALU.is_equal, None, [rS], [rS])
            tsc(CM, cL, M, None, ALU.subtract, None, [rS], [rS])
            act(TH, CM, AF.Tanh, [rS], [rS], scale=0.5)
            tsc(NUM, TH, 1.0, None, ALU.add, None, [rS], [rS])
            tsc(TH, TH, -1.0, 1.0, ALU.mult, ALU.add, [rS], [rS])
            recip(TH, TH, [rS], [rS])
            tt("dve", NUM, NUM, TH, ALU.mult, [rS], [rS])
            red(SS, NUM, ALU.add, [rS], [rS])
            recip(PG, SS, [rS], [rS])
            FT = sm[:, 140:156]
            tt("dve", FT.rearrange("p (g j) -> p g j", g=4), fL.rearrange("p (g j) -> p g j", g=4),
               GM.unsqueeze(2).to_broadcast([128, 4, 4]), ALU.mult, [rS], [rS])
            FS = smc(156, 4)
            red(FS, FT.rearrange("p (g j) -> p j g", g=4), ALU.add, [rS], [rS])
            M1_, K1, F2, M2_, K2, DD, W1, W2, WJ = smc(160), smc(161, 4), smc(165, 4), smc(169), smc(170, 4), smc(174), smc(175), smc(176), smc(177, 4)
            red(M1_, FS, ALU.max, [rS], [rS])
            tsc(K1, FS, M1_, None, ALU.is_equal, None, [rS], [rS])
            stt(F2, K1, -1e30, FS, ALU.mult, ALU.add, [rS], [rS])
            red(M2_, F2, ALU.max, [rS], [rS])
            tsc(K2, F2, M2_, None, ALU.is_equal, None, [rS], [rS])
            tt("dve", DD, M2_, M1_, ALU.subtract, [rS], [rS])
            act(DD, DD, AF.Tanh, [rS], [rS], scale=0.5)
            tsc(W1, DD, -0.5, 0.5, ALU.mult, ALU.add, [rS], [rS])
            tsc(W2, DD, 0.5, 0.5, ALU.mult, ALU.add, [rS], [rS])
            tsc(WJ, K1, W1, None, ALU.mult, None, [rS], [rS])
            stt(WJ, K2, W2, WJ, ALU.mult, ALU.add, [rS], [rS])
            tsc(WJ, WJ, PG, None, ALU.mult, None, [rS], [rS])
            tt("dve", gates[:, lt, :].rearrange("p (g j) -> p g j", g=4), GM.unsqueeze(2).to_broadcast([128, 4, 4]),
               WJ.unsqueeze(1).to_broadcast([128, 4, 4]), ALU.mult, [rS], [r_("gates")])

        RRs = [M2[0], M2[1]]

        def build_RR(tau, q):
            gs = slice(tau * 8, tau * 8 + 8)
            act(RRs[q], MAG[:, gs].unsqueeze(2).to_broadcast([128, 8, 128]), AF.Copy, [r_("MAG")], [r_("RR%d" % q)])
            S.op("pool", lambda e: e.memset(RRs[q][:, :, 0:1], 0.0), rd=[], wr=[r_("RR%d" % q)])

        def run_gen(g):
            for _ in g:
                pass

        def g_front(tile_idx, lt, p):
            rz, rzo = r_("zT%d" % p), r_("zT%d" % (1 - p))
            rx, rh, rhT = r_("xt%d" % p), r_("hn%d" % p), r_("hnT%d" % p)
            dma(xt[p], xall[tile_idx * 128:(tile_idx + 1) * 128, :], [], [rx])
            dma(acc[:, lt, :], xall[tile_idx * 128:(tile_idx + 1) * 128, :], [], [r_("acc%d" % lt)])
            rms_a(xt[p], [rx], 0, hn[p], rh)
            yield
            rms_b(xt[p], hn[p], [rx], [rh], 0)
            yield
            transp8(hn[p], [rh])
            act(hnT[p].rearrange("p k t -> p (k t)"), PB0[:], AF.Copy, [r_("PB0")], [rhT])
            yield
            cp("pool", zT[p][:, 0:4, 0:16], zT[1 - p][:, 0:4, 128:144], [rzo], [rz])
            for m in range(8):
                pb = PBs[0] if m < 4 else PBs[1]
                rpb = r_("bank%d" % (m // 4))
                o = pb[:, (m % 4) * 128:(m % 4 + 1) * 128]
                for k in range(8):
                    mm(o, WI[:, k, m * 128:(m + 1) * 128], hnT[p][:, k, :], k == 0, k == 7, [r_("WI"), rhT], [rpb])
                if m == 3:
                    act(zT[p][:, 0:4, 16:144], PBs[0][:].rearrange("p (m t) -> p m t", m=4), AF.Copy, [r_("bank0")], [rz])
            act(zT[p][:, 4:8, 16:144], PBs[1][:].rearrange("p (m t) -> p m t", m=4), AF.Copy, [r_("bank1")], [rz])
            yield

        def st_pool(p, first):
            rz, ryT = r_("zT%d" % p), r_("yT%d" % p)
            rq = r_("bank6")
            for gi, w in enumerate(WINS):
                o = PQ[:, gi * 128:(gi + 1) * 128]
                for l in range(w):
                    mm(o, PW[:, 2 * gi + (1 if l > 0 else 0), :], zT[p][:, gi, 16 - l:144 - l], l == 0, (l == w - 1) and not first, [r_("PW"), rz], [rq])
                if first:
                    S.op("dve", lambda e, gi=gi: e.tensor_tensor_scan(
                        out=cf[:, 0:16], data0=cstt[:, 257:258].to_broadcast([128, 16]), data1=zT[p][:, gi, 16:32],
                        initial=0.0, op0=ALU.mult, op1=ALU.add), rd=[rz, r_("cstt")], wr=[r_("cf")])
                    tt("dve", cf[:, 16:32], cf[:, 0:16], RB[:, 20 + gi * 16:36 + gi * 16], ALU.mult, [r_("cf"), r_("RB")], [r_("cf2")])
                    cp("dve", pzb[:, 0:16], cf[:, 16:32], [r_("cf2")], [r_("pzb")])
                    mm(o[:, 0:16], PW[:, 2 * gi + 1, :], pzb[:, 0:16], False, True, [r_("PW"), r_("pzb")], [rq])
            for gi in range(4):
                act(yT[p][:, gi, :], PQ[:, gi * 128:(gi + 1) * 128], AF.Copy, [rq], [ryT], scale=colt[:, c_ps_ + gi:c_ps_ + gi + 1])

        def st_Dp(p, tau):
            g0 = tau * 8
            rz = r_("zT%d" % p)
            for hh in range(2):
                ba, bb = PBs[2 + hh * 2], PBs[3 + hh * 2]
                rba, rbb = r_("bank%d" % (2 + hh * 2)), r_("bank%d" % (3 + hh * 2))
                bav = ba[:].rearrange("p (g t) -> p g t", g=4)
                bbv = bb[:].rearrange("p (g t) -> p g t", g=4)
                for gl4 in range(4):
                    g = g0 + hh * 4 + gl4
                    mm(bav[:, gl4, :], LBa[:, g, :], zT[p][:, 4 + tau, 16:144], True, True, [r_("LBa"), rz], [rba])
                    mm(bbv[:, gl4, :], LBb[:, g, :], zT[p][:, 4 + tau, 16:144], True, True, [r_("LBb"), rz], [rbb])

        def st_Dd(tau, q):
            g0 = tau * 8
            rG, rM1 = r_("Gt%d" % q), r_("M1t")
            for hh in range(2):
                ba, bb = PBs[2 + hh * 2], PBs[3 + hh * 2]
                rba, rbb = r_("bank%d" % (2 + hh * 2)), r_("bank%d" % (3 + hh * 2))
                bav = ba[:].rearrange("p (g t) -> p g t", g=4)
                bbv = bb[:].rearrange("p (g t) -> p g t", g=4)
                sl = slice(hh * 4, hh * 4 + 4)
                gs = slice(g0 + hh * 4, g0 + hh * 4 + 4)
                tt("dve", Gt[q][:, sl, :], bav, COS[:, gs, :], ALU.mult, [rba, r_("tab")], [rG])
                tt("dve", M1[0][:, sl, :], bbv, SINM[:, gs, :], ALU.mult, [rbb, r_("tab")], [rM1])
                tt("dve", Gt[q][:, sl, :], Gt[q][:, sl, :], M1[0][:, sl, :], ALU.add, [rM1], [rG])

        def st_S(tau, q):
            g0 = tau * 8
            gs = slice(g0, g0 + 8)
            rG, rX = r_("Gt%d" % q), r_("XT%d" % q)
            c8 = sm[:, 80:88]
            tt("dve", c8, MAG[:, gs], CAR[:, gs], ALU.mult, [r_("MAG"), r_("CAR")], [r_("c8")])
            tt("dve", Gt[q][:, :, 0], Gt[q][:, :, 0], c8, ALU.add, [r_("c8")], [rG])
            S.op("dve", lambda e, q=q: e.tensor_tensor_scan(
                out=XT[q].rearrange("p a b -> p (a b)"), data0=RRs[q].rearrange("p a b -> p (a b)"),
                data1=Gt[q].rearrange("p a b -> p (a b)"), initial=0.0, op0=ALU.mult, op1=ALU.add),
                rd=[rG, r_("RR%d" % q)], wr=[rX])
            xs = sm[:, 16 + 8 * q:24 + 8 * q]
            dma(xs[0:64, :], XT[q][64:128, :, 127], [rX], [r_("xs%d" % q)], slow=True)
            dma(xs[64:128, :], XT[q][0:64, :, 127], [rX], [r_("xsb%d" % q)], slow=True)
            build_RR((tau + 2) % 4, q)

        M1b = [XA[0], XA[1]]
        M2b = [M1[1].rearrange("p a b -> p (a b)").bitcast(BF16)[:, i * 1024:(i + 1) * 1024].rearrange("p (a b) -> p a b", a=8) for i in range(2)]

        def st_M(tau, q):
            g0 = tau * 8
            gs = slice(g0, g0 + 8)
            rX = r_("XT%d" % q)
            tt("dve", M1b[q], XT[q], COS[:, gs, :], ALU.mult, [rX, r_("tab")], [r_("XA%d" % q)])
            tt("dve", M2b[q], XT[q], SINM[:, gs, :], ALU.mult, [rX, r_("tab")], [r_("M2b%d" % q)])
            xs = sm[:, 16 + 8 * q:24 + 8 * q]
            t1c = sm[:, 32 + 8 * q:40 + 8 * q]
            tt("dve", t1c, XT[q][:, :, 127], COS[:, gs, 127], ALU.mult, [rX, r_("tab")], [r_("t1c%d" % q)])
            tt("dve", xs, xs, SINM[:, gs, 127], ALU.mult, [r_("xs%d" % q), r_("xsb%d" % q), r_("tab")], [r_("xs%d" % q), r_("xsb%d" % q)])
            tt("dve", CAR[:, gs], t1c, xs, ALU.subtract, [r_("t1c%d" % q), r_("xs%d" % q), r_("xsb%d" % q)], [r_("CAR")])

        def st_Cp(p, tau, q):
            rz = r_("zT%d" % p)
            o = PQ[:, tau * 128:(tau + 1) * 128]
            ry = r_("bank6")
            for gl in range(8):
                g = tau * 8 + gl
                mm(o, LC[:, g, :], M1b[q][:, gl, :], gl == 0, False, [r_("LC"), r_("XA%d" % q)], [ry])
                mm(o, LCsv[g // 16][:, g % 16, :], M2b[q][:, gl, :], False, False, [r_("LCs"), r_("M2b%d" % q)], [ry])
            mm(o, Dg[:, tau, :], zT[p][:, 4 + tau, 16:144], False, True, [r_("Dg"), rz], [ry])
            act(g1[q], o, AF.Square, [ry], [r_("g1%d" % q)], scale=0.21145921592590347)

        def st_Ca(p, tau, q):
            o = PQ[:, tau * 128:(tau + 1) * 128]
            ry = r_("bank6")
            rg1, rg2 = r_("g1%d" % q), r_("g2%d" % q)
            stt(g1[q], g1[q], 1.0, o, ALU.add, ALU.mult, [rg1, ry], [rg1])
            act(g2[q], g1[q], AF.Tanh, [rg1], [rg2], scale=0.7978845608028654)

        def st_Cb(p, tau, q):
            ryg, rygf = r_("yg%d" % p), r_("ygf%d" % p)
            o = PQ[:, tau * 128:(tau + 1) * 128]
            ry = r_("bank6")
            rg2 = r_("g2%d" % q)
            stt(ygf[p][:, tau, :], g2[q], 1.0, o, ALU.add, ALU.mult, [rg2, ry], [rygf])
            act(yg[p][:, tau, :], ygf[p][:, tau, :], AF.Copy, [rygf], [ryg])

        def g_tail(lt, p):
            ryT, ryg, rygf = r_("yT%d" % p), r_("yg%d" % p), r_("ygf%d" % p)
            jf = junk.bitcast(F32)
            glt = [jf[:, i * 128:(i + 1) * 128] for i in range(4)]
            rgl = [r_("glt%d" % i) for i in range(4)]
            rbk = [r_("bank1"), r_("bank0")]
            for m in range(4):
                pbm = PBs[1] if m % 2 == 0 else PBs[0]
                rb = rbk[m % 2]
                o = pbm[:, (m // 2) * 128:(m // 2 + 1) * 128]
                for k in range(4):
                    mm(o, GW[:, k, m * 128:(m + 1) * 128], yg[p][:, k, :], k == 0, k == 3, [r_("GW"), ryg], [rb])
                act(glt[m], o, AF.Tanh, [rb, r_("sm2")], [rgl[m]], scale=0.5, bias=sm[:, 64 + m:65 + m])
            yield
            for m in range(4):
                stt(yT[p][:, 4 + m, :], glt[m], 1.0, ygf[p][:, m, :], ALU.add, ALU.mult, [rgl[m], rygf], [ryT])
            for half in range(2):
                pb = PBs[half]
                rqs = [r_("bank%d" % half)]
                for k in range(8):
                    mm(pb[:], yT[p][:, k, :], WO[:, k, half * 512:(half + 1) * 512], k == 0, k == 7, [ryT, r_("WO")], rqs)
            yield
            for half in range(2):
                pb = PBs[half]
                rqs = [r_("bank%d" % half)]
                tt("dve", acc[:, lt, half * 512:(half + 1) * 512], pb[:], acc[:, lt, half * 512:(half + 1) * 512], ALU.add, rqs + [r_("acc%d" % lt)], [r_("acc%d" % lt)])
            rh = r_("hn2b%d" % p)
            rms_a(acc[:, lt, :], [r_("acc%d" % lt)], 4, hn2b[p], rh)
            yield
            rms_b(acc[:, lt, :], hn2b[p], [r_("acc%d" % lt)], [rh], 4)
            yield
            transp8(hn2b[p], [rh])
            act(hn2T[:, :, lt * 128:(lt + 1) * 128], PB0[:].rearrange("p (k t) -> p k t", k=8), AF.Copy, [r_("PB0")], [r_("hn2T")])
            yield
            o = PQ[:, 0:20]
            rq0 = r_("bank6")
            for k in range(8):
                mm(o, hn2T[:, k, lt * 128:(lt + 1) * 128], WRb[:, k, :], k == 0, k == 7, [r_("hn2T"), r_("WRb")], [rq0])
            router(lt, o, rq0)
            yield

        def mixer_sb(sbi):
            nun = SBT * 4
            par = lambda lt: (sbi * SBT + lt) % 2
            bg = []

            def advance():
                for g in list(bg):
                    try:
                        next(g)
                    except StopIteration:
                        bg.remove(g)
            run_gen(g_front(NT_PRE + sbi * SBT, 0, par(0)))
            build_RR(0, 0)
            build_RR(1, 1)
            st_Dp(par(0), 0)
            if SBT > 1:
                bg.append(g_front(NT_PRE + sbi * SBT + 1, 1, par(1)))
            k = 0
            while k < nun + 4 or bg:
                advance()
                if 0 <= k - 4 < nun:
                    lt3, tau3 = divmod(k - 4, 4)
                    st_Ca(par(lt3), tau3, (k - 4) % 2)
                if 0 <= k - 3 < nun:
                    st_M((k - 3) % 4, (k - 3) % 2)
                if 0 <= k - 4 < nun:
                    st_Cb(par(lt3), tau3, (k - 4) % 2)
                    if tau3 == 3:
                        bg.append(g_tail(lt3, par(lt3)))
                if k < nun:
                    lt, tau = divmod(k, 4)
                    if tau == 2:
                        st_pool(par(lt), sbi == 0 and lt == 0)
                    st_Dd(tau, k % 2)
                    if tau == 3 and 1 <= lt + 1 and lt + 2 < SBT:
                        bg.append(g_front(NT_PRE + sbi * SBT + lt + 2, lt + 2, par(lt + 2)))
                if 0 <= k - 1 < nun:
                    st_S((k - 1) % 4, (k - 1) % 2)
                if 0 <= k - 3 < nun:
                    ltp, taup = divmod(k - 3, 4)
                    st_Cp(par(ltp), taup, (k - 3) % 2)
                if k + 1 < nun:
                    ltn, taun = divmod(k + 1, 4)
                    st_Dp(par(ltn), taun)
                k += 1

        def pipeline(gens):
            gens = list(gens)
            active = []
            while gens or active:
                if gens and len(active) < 2 and (not active or active[-1][1][0]):
                    active.append((gens.pop(0), [False]))
                for it in list(active):
                    g, st = it
                    try:
                        v = next(g)
                        if v == "F":
                            st[0] = True
                    except StopIteration:
                        active.remove(it)

        wgb = nc.dram_tensor("wgb", [16, 128, 2048], BF16).ap()
        wub = nc.dram_tensor("wub", [16, 128, 2048], BF16).ap()
        wdb = nc.dram_tensor("wdb", [16, 128, 2048], BF16).ap()
        accf = acc[:].rearrange("p a b -> p (a b)")
        hn2f = hn2T[:].rearrange("p a b -> p (a b)")
        cast_units = []
        for e in range(16):
            cast_units += [(0, e), (1, e), (2, e)]
        ucnt = [0]

        def cast_unit():
            if not cast_units:
                return
            kind, e = cast_units.pop(0)
            sl = ucnt[0] % 2
            ucnt[0] += 1
            stg = accf[:, sl * 2048:(sl + 1) * 2048]
            tmp = hn2f[:, sl * 2048:(sl + 1) * 2048]
            rs_, rt_ = r_("stg%d" % sl), r_("tmpb%d" % sl)
            if kind == 2:
                dma(stg.rearrange("p (k n) -> p k n", k=2), wd_d[e, :, :].rearrange("(k p) n -> p k n", p=128), [], [rs_], q="pool")
                cp("pool", tmp, stg, [rs_], [rt_])
                dma(wdb[e, :, :], tmp, [rt_], [r_("wscr")], q="pool")
            else:
                src = wg_d if kind == 0 else wu_d
                dst = wgb if kind == 0 else wub
                dma(stg.rearrange("p (k n) -> p k n", k=8), src[e, :, :].rearrange("(k p) n -> p k n", p=128), [], [rs_], q="pool")
                tt("pool", tmp.rearrange("p (k n) -> p k n", k=8), stg.rearrange("p (k n) -> p k n", k=8),
                   colt[:, c_nf:c_nf + 8].unsqueeze(2).to_broadcast([128, 8, 256]), ALU.mult, [rs_, r_("colt")], [rt_])
                dma(dst[e, :, :], tmp, [rt_], [r_("wscr")], q="pool")

        def moe(sbi):
            dma(NF, rows[0:1, 0:1024].partition_broadcast(128), [], [r_("NF")])
            nblk = SBT // 4
            tok = slice(0, SBT * 128)

            def gu(e):
                sl = e % 2
                rwg, rwu, rwd = r_("WG%d" % sl), r_("WU%d" % sl), r_("WD%d" % sl)
                dma(WGs[sl].rearrange("p k n -> p (k n)"), wgb[e, :, :], [r_("wscr")], [rwg])
                dma(WUs[sl].rearrange("p k n -> p (k n)"), wub[e, :, :], [r_("wscr")], [rwu])
                dma(WDs[sl].rearrange("p k n -> p (k n)"), wdb[e, :, :], [r_("wscr")], [rwd])
                for ft in range(2):
                    gp, up = PBs[2 + ft], PBs[4 + ft]
                    rg, ru = r_("bank%d" % (2 + ft)), r_("bank%d" % (4 + ft))
                    rsg, rhT_ = r_("sgT%d%d" % (sl, ft)), r_("hT%d%d" % (sl, ft))
                    for k in range(8):
                        mm(gp[:], WGs[sl][:, k, ft * 128:(ft + 1) * 128], hn2T[:, k, tok], k == 0, k == 7, [rwg, r_("hn2T")], [rg])
                    for k in range(8):
                        mm(up[:], WUs[sl][:, k, ft * 128:(ft + 1) * 128], hn2T[:, k, tok], k == 0, k == 7, [rwu, r_("hn2T")], [ru])
                    act(sgT[sl][:, ft, :], gp[:], AF.Silu, [rg], [rsg])
                    tt("dve", hT[sl][:, ft, :], up[:], sgT[sl][:, ft, :], ALU.mult, [ru, rsg], [rhT_])

            def down(e):
                sl = e % 2
                rwd = r_("WD%d" % sl)
                for lt in range(SBT):
                    for half in range(2):
                        bi = [0, 1, 6, 7][(lt * 2 + half) % 4]
                        pb = PBs[bi] if bi < 7 else PB0F
                        rpb = r_("bank%d" % bi) if bi < 7 else r_("PB0")
                        for ft in range(2):
                            mm(pb[:] if bi < 7 else pb, hT[sl][:, ft, lt * 128:(lt + 1) * 128], WDs[sl][:, ft, half * 512:(half + 1) * 512], ft == 0, ft == 1,
                               [r_("hT%d%d" % (sl, ft)), rwd], [rpb])
                        stt(acc[:, lt, half * 512:(half + 1) * 512], pb[:] if bi < 7 else pb, gates[:, lt, e:e + 1], acc[:, lt, half * 512:(half + 1) * 512],
                            ALU.mult, ALU.add, [rpb, r_("gates"), r_("acc%d" % lt)], [r_("acc%d" % lt)])
            for e in range(17):
                if e < 16:
                    gu(e)
                if e >= 1:
                    down(e - 1)
            for lt in range(SBT):
                tix = sbi * SBT + lt
                o2 = lt % 2
                ro = r_("outt%d" % o2)
                S.op("dve", lambda e: e.memset(smc(8), 0.0), wr=[r_("sm8")])
                act(junk2, acc[:, lt, :], AF.Square, [r_("acc%d" % lt)], [r_("junk2"), r_("sm8")], accum_out=smc(8))
                act(smc(9), smc(8), AF.Sqrt, [r_("sm8")], [r_("sm8")], scale=1.0 / 1024.0, bias=EPS)
                recip(smc(10), smc(9), [r_("sm8")], [r_("sm8")])
                stt(outt[o2], acc[:, lt, :], smc(10), NF, ALU.mult, ALU.mult, [r_("acc%d" % lt), r_("sm8"), r_("NF")], [ro])
                dma(out[tix * 128:(tix + 1) * 128, :], outt[o2], [ro], [r_("out")])

        S.barrier()
        def pre_tile(t, p):
            last = t == NT_PRE - 1
            yield from front(t, [0, 1, 2, 3] if last else [], p)
            rhT = r_("hnT%d" % p)
            for k in range(8):
                mm(PBs[1][:], hnT[p][:, k, :], WI[:, k, 512:1024], k == 0, k == 7, [r_("WI"), rhT], [r_("bank1")])
            act(ztok[p], PBs[1][:], AF.Copy, [r_("bank1")], [r_("ztok%d" % p)])
            yield
            ya, yb = PBs[2 + 2 * p], PBs[3 + 2 * p]
            rya, ryb = r_("bank%d" % (2 + 2 * p)), r_("bank%d" % (3 + 2 * p))
            for g in range(32):
                mm(ya[:, g * 16:(g + 1) * 16], VTa[:, g, :], ztok[p][:, g * 16:(g + 1) * 16], True, True, [r_("VTa"), r_("ztok%d" % p)], [rya])
            for g in range(32):
                mm(yb[:, g * 16:(g + 1) * 16], VTb[:, g, :], ztok[p][:, g * 16:(g + 1) * 16], True, True, [r_("VTb"), r_("ztok%d" % p)], [ryb])
            yield
            tt("dve", Ytmp[0], ya[:], BBR.rearrange("p a b -> p (a b)"), ALU.mult, [rya, r_("BBR")], [r_("Ytmp0")])
            tt("dve", Ytmp[1], yb[:], BBIs.rearrange("p a b -> p (a b)"), ALU.mult, [ryb, r_("BBI")], [r_("Ytmp1")])
            tt("dve", Ytmp[0], Ytmp[0], Ytmp[1], ALU.add, [r_("Ytmp1")], [r_("Ytmp0")])
            red(Ssum, Ytmp[0].rearrange("p (a b) -> p a b", a=32), ALU.add, [r_("Ytmp0")], [r_("Ssum")])
            tt("dve", Ctmp[:, 0:32], A128[:, 0:32], CAR[:], ALU.mult, [r_("A128"), r_("CAR")], [r_("ctmp")])
            tt("dve", Ctmp[:, 32:64], A128[:, 32:64], CARb, ALU.mult, [r_("A128"), r_("CARb"), r_("CARb2")], [r_("ctmp2")])
            tt("dve", Ctmp[:, 0:32], Ctmp[:, 0:32], Ctmp[:, 32:64], ALU.add, [r_("ctmp2")], [r_("ctmp")])
            tt("dve", CAR[:], Ctmp[:, 0:32], Ssum, ALU.add, [r_("ctmp"), r_("Ssum")], [r_("CAR")])
            dma(CARb[0:64, :], CAR[64:128, :], [r_("CAR")], [r_("CARb")])
            dma(CARb[64:128, :], CAR[0:64, :], [r_("CAR")], [r_("CARb2")])
            cast_unit()
            if t % 2 == 1:
                cast_unit()
            yield
        pipeline([pre_tile(t, t % 2) for t in range(NT_PRE)])
        while cast_units:
            cast_unit()
        S.barrier()
        for sbi in range(NT_MAIN // SBT):
            mixer_sb(sbi)
            S.barrier()
            moe(sbi)
            S.barrier()
        S.final_wait("sp", [r_("out")])

        print('SBUF remaining', nc.sbuf_bytes_remaining, 'mixer_end', mixer_end, 'moe_end', _off[0])
        block = es.enter_context(nc.Block())

        @block.sync
        def _(e):
            S.replay("sp", e)

        @block.tensor
        def _(e):
            S.replay("pe", e)

        @block.scalar
        def _(e):
            S.replay("act", e)

        @block.vector
        def _(e):
            S.replay("dve", e)

        @block.gpsimd
        def _(e):
            S.replay("pool", e)
    return nc


def _col(v, k):
    return np.ascontiguousarray(np.asarray(v, np.float32).reshape(k, 128).T)


def kernel(x, norm_mix, w_in, pool_w, pool_scale, ssm_a_re, ssm_a_im, ssm_log_step,
           ssm_b_re, ssm_b_im, ssm_c_re, ssm_c_im, ssm_d, glu_w, glu_b, w_out, norm_ffn,
           router_coarse_w, router_coarse_b, router_fine_w, router_fine_b,
           exp_w_gate, exp_w_up, exp_w_down, norm_final):
    f = np.float32
    x = np.asarray(x, f)
    cols = np.zeros((128, 64), f)
    cols[:, 0:8] = _col(norm_mix[0], 8)
    cols[:, 8:16] = _col(norm_ffn[0], 8)
    cols[:, 16:20] = _col(pool_scale[0], 4)
    cols[:, 20:24] = _col(ssm_d[0], 4)
    cols[:, 24:28] = _col(glu_b[0], 4)
    are = np.asarray(ssm_a_re[0], f)
    aim = np.asarray(ssm_a_im[0], f)
    ls = np.asarray(ssm_log_step[0], f)
    sp_s = np.zeros((128, 96), f)
    sp_s[:, 0:32] = np.concatenate([are.T, are.T], 0)
    sp_s[:, 32:64] = np.concatenate([aim.T, aim.T], 0)
    sp_s[:, 64:96] = np.broadcast_to(ls[None, :], (128, 32))
    br = np.asarray(ssm_b_re[0], f).transpose(1, 0, 2)
    bi = np.asarray(ssm_b_im[0], f).transpose(1, 0, 2)
    b1 = np.ascontiguousarray(np.concatenate([br, bi], 0))
    b2 = np.ascontiguousarray(np.concatenate([bi, br], 0))
    cr = np.asarray(ssm_c_re[0], f).transpose(2, 0, 1)
    ci = np.asarray(ssm_c_im[0], f).transpose(2, 0, 1)
    c1 = np.ascontiguousarray(np.concatenate([cr, ci], 0))
    c2 = np.ascontiguousarray(np.concatenate([ci, cr], 0))
    cst = np.zeros((128, 386), f)
    cst[:, 258:386] = np.arange(127, -1, -1, dtype=f)[None, :]
    cst[:, 0:128] = np.eye(128, dtype=f)
    cst[:, 128:256] = np.arange(1, 129, dtype=f)[None, :]
    cst[:, 256] = np.where(np.arange(128) < 64, 1.0, -1.0)
    cst[:, 257] = 1.0
    wr = np.ascontiguousarray(np.concatenate([np.asarray(router_coarse_w[0], f), np.asarray(router_fine_w[0], f)], 1))
    rb = np.concatenate([np.asarray(router_coarse_b[0], f), np.asarray(router_fine_b[0], f)])
    in_maps = []
    for c in range(8):
        b, half = c // 2, c % 2
        main = x[b, half * 4096:(half + 1) * 4096]
        pre = x[b, 0:4096] if half == 1 else np.zeros((4096, 1024), f)
        fix = np.zeros((4, 16), f)
        if half == 0:
            for gi, w in enumerate(WINS):
                for t in range(w - 1):
                    fix[gi, t] = w / (t + 1.0) - 1.0
        rows = np.concatenate([np.asarray(norm_final, f), rb, fix.reshape(-1)])[None, :].astype(f)
        in_maps.append({
            "xall": np.ascontiguousarray(np.concatenate([pre, main], 0)),
            "w_in": np.asarray(w_in[0], f), "w_out": np.asarray(w_out[0], f), "glu_w": np.asarray(glu_w[0], f),
            "pool_w": np.asarray(pool_w[0], f), "cols": cols, "rows": rows, "sp_s": sp_s,
            "b1_s": b1, "b2_s": b2, "c1_s": c1, "c2_s": c2, "cst": cst, "wr": wr,
            "wg": np.asarray(exp_w_gate[0], f), "wu": np.asarray(exp_w_up[0], f), "wd": np.asarray(exp_w_down[0], f),
        })
    nc = build()
    res = run_bass_kernel_spmd(nc, in_maps, core_ids=list(range(8)))
    outs = [np.asarray(r["out"], f) for r in res.results]
    full = np.zeros((4, 8192, 1024), f)
    for c in range(8):
        b, half = c // 2, c % 2
        full[b, half * 4096:(half + 1) * 4096] = outs[c]
    return full
```
